# Optimizing a Trainium2 kernel written in Bass

```python
import math
import jax
import jax.numpy as jnp
from jax import lax
import numpy as np

D_MODEL = 1024
BATCH = 16
SEQ = 2048
DEPTH = 2

GRID_W = 64
CTX_LEN = 256
NORM_EPS = 1e-6
CHUNK = 64
Q_BLOCK = 128
ROPE_THETA = 10000.0

ML_HEADS = 4
ML_DK = 128
ML_DV = 128
DF_HEADS = 4
DF_HD = 64
DF_DV = 2 * DF_HD
GL_HEADS = 4
GL_DK = 64
GL_DV = 128
GL_RANK = 16
GL_TAU = 16.0
N_BRANCH = 3
BRANCH_W = 512
N_GROUPS = 4
EXPERTS_PER_GROUP = 8
N_EXPERTS = N_GROUPS * EXPERTS_PER_GROUP
TOP_K = 2
D_EXPERT = 512

IN_SIZES = (
    ML_HEADS * ML_DK, ML_HEADS * ML_DK, ML_HEADS * ML_DV, ML_HEADS * ML_DV, 4 * ML_HEADS,
    DF_HEADS * 2 * DF_HD, DF_HEADS * 2 * DF_HD, DF_HEADS * DF_DV,
    GL_HEADS * GL_DK, GL_HEADS * GL_DK, GL_HEADS * GL_DV, GL_HEADS * GL_DV, 2 * GL_RANK,
    N_BRANCH * D_MODEL,
)
IN_WIDTH = sum(IN_SIZES)

kernel_name = "hybrid_mlstm_diffattn_gla_hmoe_dit"

F32 = jnp.float32


def rms_norm(x, g):
    xf = x.astype(F32)
    y = xf * lax.rsqrt(jnp.mean(xf * xf, axis=-1, keepdims=True) + NORM_EPS)
    return (y * g.astype(F32)).astype(x.dtype)


def to_heads(t, h):
    b, n, w = t.shape
    return t.reshape(b, n, h, w // h).transpose(0, 2, 1, 3)


def merge_heads(t):
    b, h, n, d = t.shape
    return t.transpose(0, 2, 1, 3).reshape(b, n, h * d)


def split_proj(p):
    idx = [int(i) for i in np.cumsum(IN_SIZES)[:-1]]
    return jnp.split(p, idx, axis=-1)


def flip_t(t):
    return jnp.flip(t, axis=2)


def to_chunks(t):
    n = t.shape[2]
    t = t.reshape(t.shape[:2] + (n // CHUNK, CHUNK) + t.shape[3:])
    return jnp.moveaxis(t, 2, 0)


def from_chunks(t):
    t = jnp.moveaxis(t, 0, 2)
    return t.reshape(t.shape[:2] + (t.shape[2] * t.shape[3],) + t.shape[4:])


def axial_rope_tables(rows):
    r = jnp.repeat(jnp.arange(rows), GRID_W).astype(F32)
    col = jnp.tile(jnp.arange(GRID_W), rows).astype(F32)
    n_freq = DF_HD // 4
    inv = ROPE_THETA ** (-jnp.arange(n_freq, dtype=F32) / n_freq)
    ang_r = r[:, None] * inv
    ang_c = col[:, None] * inv
    return jnp.cos(ang_r), jnp.sin(ang_r), jnp.cos(ang_c), jnp.sin(ang_c)


def rotate_half(x, cos, sin):
    x1, x2 = jnp.split(x, 2, axis=-1)
    return jnp.concatenate([x1 * cos - x2 * sin, x2 * cos + x1 * sin], axis=-1)


def apply_axial_rope(x, tabs):
    cr, sr, cc, sc = tabs
    xf = x.astype(F32)
    half = DF_HD // 2
    y = jnp.concatenate([rotate_half(xf[..., :half], cr, sr), rotate_half(xf[..., half:], cc, sc)], axis=-1)
    return y.astype(x.dtype)


def mlstm_scan(q, k, v, ig, fg, state):
    causal = jnp.tril(jnp.ones((CHUNK, CHUNK), dtype=bool))
    logf = jax.nn.log_sigmoid(fg)
    xs = (to_chunks(q), to_chunks(k), to_chunks(v), to_chunks(ig), to_chunks(logf))

    def step(carry, inp):
        C, nv, m = carry
        qc, kc, vc, ic, lfc = inp
        b = jnp.cumsum(lfc, axis=-1)
        dmat = b[..., :, None] - b[..., None, :] + ic[..., None, :]
        dmat = jnp.where(causal, dmat, -jnp.inf)
        inter = b + m[..., None]
        mj = jnp.maximum(jnp.max(dmat, axis=-1), inter)
        w = jnp.exp(dmat - mj[..., None])
        w_inter = jnp.exp(inter - mj)
        s = jnp.einsum('bhjd,bhid->bhji', qc, kc).astype(F32) * w
        num = jnp.einsum('bhji,bhiv->bhjv', s, vc) + w_inter[..., None] * jnp.einsum('bhvd,bhjd->bhjv', C, qc)
        den = jnp.sum(s, axis=-1) + w_inter * jnp.einsum('bhd,bhjd->bhj', nv, qc)
        h = num / jnp.maximum(jnp.abs(den), jnp.exp(-mj))[..., None]
        b_last = b[..., -1]
        gi = b_last[..., None] - b + ic
        m_new = jnp.maximum(b_last + m, jnp.max(gi, axis=-1))
        wk = jnp.exp(gi - m_new[..., None])
        decay = jnp.exp(b_last + m - m_new)
        C_new = decay[..., None, None] * C + jnp.einsum('bhi,bhiv,bhid->bhvd', wk, vc, kc)
        n_new = decay[..., None] * nv + jnp.einsum('bhi,bhid->bhd', wk, kc)
        return (C_new, n_new, m_new), h

    final, hs = lax.scan(step, state, xs)
    return from_chunks(hs), final


def gla_scan(q, k, v, loga, S):
    causal = jnp.tril(jnp.ones((CHUNK, CHUNK), dtype=bool))
    xs = (to_chunks(q), to_chunks(k), to_chunks(v), to_chunks(loga))

    def step(S, inp):
        qc, kc, vc, lac = inp
        bc = jnp.cumsum(lac, axis=-2)
        diff = bc[..., :, None, :] - bc[..., None, :, :]
        decay = jnp.exp(jnp.where(causal[:, :, None], diff, -jnp.inf))
        A = jnp.einsum('bhjid,bhjd,bhid->bhji', decay, qc.astype(F32), kc.astype(F32))
        o = jnp.einsum('bhji,bhiv->bhjv', A, vc) + jnp.einsum('bhjd,bhdv->bhjv', qc * jnp.exp(bc), S)
        b_last = bc[..., -1, :]
        S_new = jnp.exp(b_last)[..., None] * S + jnp.einsum('bhid,bhiv->bhdv', kc * jnp.exp(b_last[..., None, :] - bc), vc)
        return S_new, o

    final, os_ = lax.scan(step, S, xs)
    return from_chunks(os_), final


def prefix_bidir(scan_fn, init, ctx_fwd, lat_fwd, ctx_bwd, lat_bwd):
    hc_f, st = scan_fn(*ctx_fwd, init)
    hl_f, _ = scan_fn(*lat_fwd, st)
    hc_b, st = scan_fn(*[flip_t(a) for a in ctx_bwd], init)
    hl_b, _ = scan_fn(*[flip_t(a) for a in lat_bwd], st)
    return hc_f + flip_t(hc_b), hl_f + flip_t(hl_b)


def mlstm_branch(pc, pl, gate_b, norm_g, need_ctx):
    def prep(p):
        q, k, v, o, g = p
        b_, n_ = g.shape[:2]
        q = to_heads(q, ML_HEADS)
        k = to_heads(k, ML_HEADS) * (ML_DK ** -0.5)
        v = to_heads(v, ML_HEADS)
        pre = g.astype(F32).reshape(b_, n_, 4, ML_HEADS) + gate_b.astype(F32).reshape(4, ML_HEADS)
        pre = jnp.transpose(pre, (2, 0, 3, 1))
        return (q, k, v, pre[0], pre[1]), (q, k, v, pre[2], pre[3]), o

    c_f, c_b, o_c = prep(pc)
    l_f, l_b, o_l = prep(pl)
    b_ = pl[0].shape[0]
    init = (jnp.zeros((b_, ML_HEADS, ML_DV, ML_DK), F32), jnp.zeros((b_, ML_HEADS, ML_DK), F32),
            jnp.zeros((b_, ML_HEADS), F32))
    h_c, h_l = prefix_bidir(mlstm_scan, init, c_f, l_f, c_b, l_b)

    def finish(h, o):
        return merge_heads(rms_norm(h, norm_g)).astype(o.dtype) * jax.nn.sigmoid(o)

    return (finish(h_c, o_c) if need_ctx else None), finish(h_l, o_l)


def gla_branch(pc, pl, w_alpha, b_alpha, norm_g, need_ctx):
    def prep(p):
        q, k, v, g, a = p
        q = to_heads(q, GL_HEADS) * (GL_DK ** -0.5)
        k = to_heads(k, GL_HEADS)
        v = to_heads(v, GL_HEADS)
        a_f, a_b = jnp.split(a, 2, axis=-1)
        la_f = jax.nn.log_sigmoid((a_f @ w_alpha[0]).astype(F32) + b_alpha[0].astype(F32)) / GL_TAU
        la_b = jax.nn.log_sigmoid((a_b @ w_alpha[1]).astype(F32) + b_alpha[1].astype(F32)) / GL_TAU
        return (q, k, v, to_heads(la_f, GL_HEADS)), (q, k, v, to_heads(la_b, GL_HEADS)), g

    c_f, c_b, g_c = prep(pc)
    l_f, l_b, g_l = prep(pl)
    b_ = pl[0].shape[0]
    init = jnp.zeros((b_, GL_HEADS, GL_DK, GL_DV), F32)
    o_c, o_l = prefix_bidir(gla_scan, init, c_f, l_f, c_b, l_b)

    def finish(o, g):
        return merge_heads(rms_norm(o, norm_g)).astype(g.dtype) * jax.nn.silu(g)

    return (finish(o_c, g_c) if need_ctx else None), finish(o_l, g_l)


def diff_heads(t):
    b_, n_ = t.shape[:2]
    t = t.reshape(b_, n_, DF_HEADS, 2, DF_HD).transpose(3, 0, 2, 1, 4)
    return t[0], t[1]


def diff_attend(q1, q2, k1, k2, v, lam):
    scale = DF_HD ** -0.5
    a1 = jax.nn.softmax(jnp.einsum('bhqd,bhkd->bhqk', q1, k1).astype(F32) * scale, axis=-1)
    a2 = jax.nn.softmax(jnp.einsum('bhqd,bhkd->bhqk', q2, k2).astype(F32) * scale, axis=-1)
    a = a1 - lam * a2
    return jnp.einsum('bhqk,bhkd->bhqd', a.astype(v.dtype), v)


def diff_attn_branch(pc, pl, lam_p, norm_g, lam_init, rope, need_ctx):
    lp = lam_p.astype(F32)
    lam = jnp.exp(jnp.sum(lp[0] * lp[1])) - jnp.exp(jnp.sum(lp[2] * lp[3])) + lam_init
    q1c, q2c = diff_heads(pc[0])
    k1c, k2c = diff_heads(pc[1])
    vc = to_heads(pc[2], DF_HEADS)
    q1l, q2l = [apply_axial_rope(t, rope) for t in diff_heads(pl[0])]
    k1l, k2l = [apply_axial_rope(t, rope) for t in diff_heads(pl[1])]
    vl = to_heads(pl[2], DF_HEADS)
    k1 = jnp.concatenate([k1c, k1l], axis=2)
    k2 = jnp.concatenate([k2c, k2l], axis=2)
    v = jnp.concatenate([vc, vl], axis=2)
    b_, h_, n_, _ = q1l.shape
    nb = n_ // Q_BLOCK

    def blocks(t):
        return jnp.moveaxis(t.reshape(b_, h_, nb, Q_BLOCK, DF_HD), 2, 0)

    o_l = lax.map(lambda qs: diff_attend(qs[0], qs[1], k1, k2, v, lam), (blocks(q1l), blocks(q2l)))
    o_l = jnp.moveaxis(o_l, 0, 2).reshape(b_, h_, n_, DF_DV)

    def finish(o):
        return merge_heads(rms_norm(o, norm_g) * (1.0 - lam_init))

    o_c = finish(diff_attend(q1c, q2c, k1c, k2c, vc, lam)) if need_ctx else None
    return o_c, finish(o_l)


def gated_merge(gate_pre, branches, w_branch, w_out):
    b_, n_ = gate_pre.shape[:2]
    gates = jax.nn.sigmoid(gate_pre.reshape(b_, n_, N_BRANCH, D_MODEL))
    y = gates[:, :, 0] * (branches[0] @ w_branch[0])
    for br in range(1, N_BRANCH):
        y = y + gates[:, :, br] * (branches[br] @ w_branch[br])
    return y @ w_out


def token_mixers(hc, hl, w_in, ml_gate_b, ml_norm_g, df_lambda, df_norm_g, gl_w_alpha, gl_b_alpha,
                 gl_norm_g, w_branch, w_out, lam_init, rope, need_ctx):
    pc = split_proj(hc @ w_in)
    pl = split_proj(hl @ w_in)
    ml_c, ml_l = mlstm_branch(pc[0:5], pl[0:5], ml_gate_b, ml_norm_g, need_ctx)
    df_c, df_l = diff_attn_branch(pc[5:8], pl[5:8], df_lambda, df_norm_g, lam_init, rope, need_ctx)
    gl_c, gl_l = gla_branch(pc[8:13], pl[8:13], gl_w_alpha, gl_b_alpha, gl_norm_g, need_ctx)
    out_l = gated_merge(pl[13], (ml_l, df_l, gl_l), w_branch, w_out)
    out_c = gated_merge(pc[13], (ml_c, df_c, gl_c), w_branch, w_out) if need_ctx else None
    return out_c, out_l


def hier_moe(h, wg, bg, we, be, w_gate, w_up, w_down):
    t = h.shape[0]
    pg = jax.nn.softmax((h @ wg).astype(F32) + bg.astype(F32), axis=-1)
    pg_top, g_idx = lax.top_k(pg, 1)
    le = ((h @ we).astype(F32) + be.astype(F32)).reshape(t, N_GROUPS, EXPERTS_PER_GROUP)
    le_g = jnp.take_along_axis(le, g_idx[:, :, None], axis=1)[:, 0]
    pe = jax.nn.softmax(le_g, axis=-1)
    pe_top, e_idx = lax.top_k(pe, TOP_K)
    w = pg_top * pe_top / jnp.sum(pe_top, axis=-1, keepdims=True)
    eid = g_idx * EXPERTS_PER_GROUP + e_idx
    comb = jnp.sum(jax.nn.one_hot(eid, N_EXPERTS, dtype=F32) * w[..., None], axis=1)
    y = jnp.zeros(h.shape, F32)
    for e in range(N_EXPERTS):
        hid = jax.nn.silu(h @ w_gate[e]) * (h @ w_up[e])
        y = y + comb[:, e:e + 1] * (hid @ w_down[e])
    return y.astype(h.dtype)


def setup_inputs(seed: int = 0) -> dict:
    key = jax.random.key(seed)
    ks = jax.random.split(key, 32)
    D = D_MODEL

    def nrm(k, shape, s):
        return jax.random.normal(k, shape, F32) * s

    ig_b = nrm(ks[9], (DEPTH, 2, ML_HEADS), 0.1)
    fg_b = jnp.linspace(3.0, 6.0, ML_HEADS, dtype=F32) + nrm(ks[10], (DEPTH, 2, ML_HEADS), 0.1)
    ml_gate_b = jnp.stack([ig_b[:, 0], fg_b[:, 0], ig_b[:, 1], fg_b[:, 1]], axis=1).reshape(DEPTH, 4 * ML_HEADS)
    return {
        "x": nrm(ks[0], (BATCH, SEQ, D), 1.0),
        "c": nrm(ks[1], (BATCH, D), 1.0),
        "ctx": nrm(ks[2], (BATCH, CTX_LEN, D), 1.0),
        "c_ctx": nrm(ks[3], (D,), 1.0),
        "w_mod": nrm(ks[4], (DEPTH, D, 6 * D), 0.5 * D ** -0.5),
        "b_mod": nrm(ks[5], (DEPTH, 6 * D), 0.02),
        "norm_mix_g": 1.0 + nrm(ks[6], (DEPTH, D), 0.02),
        "norm_ffn_g": 1.0 + nrm(ks[7], (DEPTH, D), 0.02),
        "w_in": nrm(ks[8], (DEPTH, D, IN_WIDTH), D ** -0.5),
        "ml_gate_b": ml_gate_b,
        "ml_norm_g": 1.0 + nrm(ks[11], (DEPTH, ML_DV), 0.02),
        "df_lambda": nrm(ks[12], (DEPTH, 4, DF_HD), 0.1),
        "df_norm_g": 1.0 + nrm(ks[13], (DEPTH, DF_DV), 0.02),
        "gl_w_alpha": nrm(ks[14], (DEPTH, 2, GL_RANK, GL_HEADS * GL_DK), GL_RANK ** -0.5),
        "gl_b_alpha": 2.0 + nrm(ks[15], (DEPTH, 2, GL_HEADS * GL_DK), 0.1),
        "gl_norm_g": 1.0 + nrm(ks[16], (DEPTH, GL_DV), 0.02),
        "w_branch": nrm(ks[17], (DEPTH, N_BRANCH, BRANCH_W, D), BRANCH_W ** -0.5),
        "w_out": nrm(ks[18], (DEPTH, D, D), D ** -0.5),
        "router_group_w": nrm(ks[19], (DEPTH, D, N_GROUPS), D ** -0.5),
        "router_group_b": nrm(ks[20], (DEPTH, N_GROUPS), 0.01),
        "router_expert_w": nrm(ks[21], (DEPTH, D, N_EXPERTS), D ** -0.5),
        "router_expert_b": nrm(ks[22], (DEPTH, N_EXPERTS), 0.01),
        "moe_w_gate": nrm(ks[23], (DEPTH, N_EXPERTS, D, D_EXPERT), D ** -0.5),
        "moe_w_up": nrm(ks[24], (DEPTH, N_EXPERTS, D, D_EXPERT), D ** -0.5),
        "moe_w_down": nrm(ks[25], (DEPTH, N_EXPERTS, D_EXPERT, D), D_EXPERT ** -0.5),
        "final_norm_g": 1.0 + nrm(ks[26], (D,), 0.02),
    }


def reference(x, c, ctx, c_ctx, w_mod, b_mod, norm_mix_g, norm_ffn_g, w_in, ml_gate_b, ml_norm_g,
              df_lambda, df_norm_g, gl_w_alpha, gl_b_alpha, gl_norm_g, w_branch, w_out,
              router_group_w, router_group_b, router_expert_w, router_expert_b,
              moe_w_gate, moe_w_up, moe_w_down, final_norm_g):
    n = x.shape[1]
    rows = n // GRID_W
    rope = axial_rope_tables(rows)
    for li in range(DEPTH):
        need_ctx = li < DEPTH - 1
        lam_init = 0.8 - 0.6 * math.exp(-0.3 * li)
        mod = jax.nn.silu(c) @ w_mod[li] + b_mod[li]
        mod_c = jax.nn.silu(c_ctx) @ w_mod[li] + b_mod[li]
        sh1, sc1, g1, sh2, sc2, g2 = jnp.split(mod[:, None, :], 6, axis=-1)
        sh1c, sc1c, g1c, sh2c, sc2c, g2c = jnp.split(mod_c, 6, axis=-1)

        hl = rms_norm(x, norm_mix_g[li]) * (1.0 + sc1) + sh1
        hc = rms_norm(ctx, norm_mix_g[li]) * (1.0 + sc1c) + sh1c
        out_c, out_l = token_mixers(hc, hl, w_in[li], ml_gate_b[li], ml_norm_g[li], df_lambda[li],
                                    df_norm_g[li], gl_w_alpha[li], gl_b_alpha[li], gl_norm_g[li],
                                    w_branch[li], w_out[li], lam_init, rope, need_ctx)
        x = x + g1 * out_l
        hl = rms_norm(x, norm_ffn_g[li]) * (1.0 + sc2) + sh2
        moe_w = (router_group_w[li], router_group_b[li], router_expert_w[li], router_expert_b[li],
                 moe_w_gate[li], moe_w_up[li], moe_w_down[li])
        if need_ctx:
            ctx = ctx + g1c * out_c
            hc = rms_norm(ctx, norm_ffn_g[li]) * (1.0 + sc2c) + sh2c
            n_ctx_tok = hc.shape[0] * hc.shape[1]
            tokens = jnp.concatenate([hc.reshape(-1, D_MODEL), hl.reshape(-1, D_MODEL)], axis=0)
            y = hier_moe(tokens, *moe_w)
            ctx = ctx + g2c * y[:n_ctx_tok].reshape(hc.shape)
            x = x + g2 * y[n_ctx_tok:].reshape(hl.shape)
        else:
            x = x + g2 * hier_moe(hl.reshape(-1, D_MODEL), *moe_w).reshape(hl.shape)
    return rms_norm(x, final_norm_g)
```

```python
import contextlib
import math
import numpy as np
import concourse.bass as bass
import concourse.mybir as mybir
from concourse.bass_utils import run_bass_kernel_spmd

F32 = mybir.dt.float32
BF16 = mybir.dt.bfloat16
AF = mybir.ActivationFunctionType
ALU = mybir.AluOpType
AX = mybir.AxisListType

COMPUTE = ("pe", "act", "dve", "pool")


class _Op:
    __slots__ = ("eng", "fn", "deps", "marked", "val", "dma", "dsem", "dval", "qwait", "emitted", "seq")

    def __init__(self, eng, fn):
        self.eng = eng
        self.fn = fn
        self.deps = []
        self.marked = False
        self.val = None
        self.dma = False
        self.dsem = None
        self.dval = None
        self.qwait = None
        self.emitted = False


class MK:
    def __init__(self, nc, n_dma_sems=8):
        self.nc = nc
        self.stacks = [contextlib.ExitStack()]
        self.e = {"pe": nc.tensor, "act": nc.scalar, "dve": nc.vector, "pool": nc.gpsimd, "sp": nc.sync}
        self.ops = {k: [] for k in self.e}
        self.pstep = {}
        self.notrack = set()
        self.psum_names = set()
        self.recs = {}
        self.n_dma_sems = n_dma_sems
        self.dma_rr = {k: 0 for k in self.e}
        self.dma_last = {}
        self.sem = {k: nc.alloc_semaphore("s_" + k) for k in COMPUTE}
        self.dsems = {}
        for k in ("sp", "pool", "act"):
            for s in range(n_dma_sems):
                self.dsems[(k, s)] = nc.alloc_semaphore("d_%s_%d" % (k, s))
        self.cnt = {k: 0 for k in self.e}
        self.dcnt = {}
        self.seen = {k: {} for k in self.e}
        self.last_op = {k: None for k in self.e}
        self.n_inst = 0
        self.trace = {k: [] for k in self.e}

    def sbuf(self, name, shape, dtype):
        self.uid = getattr(self, "uid", 0) + 1
        name = "%s_%d" % (name, self.uid)
        t = self.stacks[-1].enter_context(self.nc.sbuf_tensor(name, list(shape), dtype))
        self.pstep[name] = int(np.prod(shape[1:]))
        self.recs.pop(name, None)
        return t

    def psum(self, name, shape, dtype=F32):
        t = self.stacks[-1].enter_context(self.nc.psum_tensor(name, list(shape), dtype))
        self.pstep[name] = int(np.prod(shape[1:]))
        self.psum_names.add(name)
        return t

    def dram(self, name, shape, dtype, kind="Internal", rowlen=None):
        t = self.nc.dram_tensor(name, list(shape), dtype, kind=kind)
        self.pstep[name] = int(rowlen if rowlen is not None else shape[-1])
        if kind == "ExternalInput":
            self.notrack.add(name)
        return t.ap()

    @contextlib.contextmanager
    def scope(self):
        self.stacks.append(contextlib.ExitStack())
        try:
            yield
            self.flush()
            self.barrier()
            self.flush()
        finally:
            self.stacks.pop().close()

    def _box(self, ap):
        name = ap.name
        ps = self.pstep[name]
        off = int(ap.offset)
        p0, f0 = divmod(off, ps)
        plo = phi = p0
        flo = fhi = f0
        for (step, cnt) in ap.ap:
            ext = step * (cnt - 1)
            if step != 0 and abs(step) >= ps and step % ps == 0:
                e = ext // ps
                if e < 0:
                    plo += e
                else:
                    phi += e
            else:
                if ext < 0:
                    flo += ext
                else:
                    fhi += ext
        return name, plo, phi, flo, fhi

    def _access(self, op, ap, is_write):
        if ap.name in self.notrack:
            return
        name, plo, phi, flo, fhi = self._box(ap)
        excl = name in self.psum_names
        if excl:
            flo, fhi = 0, self.pstep[name] - 1
            plo, phi = (plo // 32) * 32, (phi // 32) * 32 + 31
        lst = self.recs.get(name, [])
        keep = []
        for r in lst:
            ov = not (r[1] < plo or phi < r[0] or r[3] < flo or fhi < r[2])
            o = r[4]
            if ov and o is not op:
                same = (o.eng == op.eng) and not o.dma and not op.dma
                if same:
                    need = (op.eng != "pe") and r[5] and not is_write
                else:
                    need = excl or is_write or r[5]
                if need:
                    op.deps.append(o)
            contained = (plo <= r[0] and r[1] <= phi and flo <= r[2] and r[3] <= fhi)
            if contained and o is not op:
                same = (o.eng == op.eng) and not o.dma and not op.dma
                if excl or is_write or ((not r[5]) and same):
                    if not (excl and same and r[5] and not is_write and False):
                        continue
            keep.append(r)
        keep.append([plo, phi, flo, fhi, op, is_write])
        self.recs[name] = keep

    def _record(self, eng, fn, reads, writes, dma=False):
        op = _Op(eng, fn)
        op.dma = dma
        for ap in reads:
            if ap is not None and hasattr(ap, "ap"):
                self._access(op, ap, False)
        for ap in writes:
            if ap is not None and hasattr(ap, "ap"):
                self._access(op, ap, True)
        self.gseq = getattr(self, "gseq", 0) + 1
        op.seq = self.gseq
        if op.deps:
            best = {}
            keep = []
            for d_ in op.deps:
                if d_.dma:
                    keep.append(d_)
                else:
                    b_ = best.get(d_.eng)
                    if b_ is None or d_.seq > b_.seq:
                        best[d_.eng] = d_
            op.deps = keep + list(best.values())
        if dma:
            slot = self.dma_rr[eng]
            self.dma_rr[eng] = (slot + 1) % self.n_dma_sems
            op.dsem = (eng, slot)
            op.qwait = self.dma_last.get((eng, slot))
            self.dma_last[(eng, slot)] = op
            self.dcnt[op.dsem] = self.dcnt.get(op.dsem, 0) + 16
            op.dval = self.dcnt[op.dsem]
        self.ops[eng].append(op)
        self.last_op[eng] = op
        return op

    def mm(self, out, lhsT, rhs, start=True, stop=True):
        return self._record("pe", lambda: self.nc.tensor.matmul(out, lhsT, rhs, start=start, stop=stop),
                            [lhsT, rhs], [out])

    def tr(self, out, in_, ident):
        return self._record("pe", lambda: self.nc.tensor.transpose(out, in_, ident), [in_, ident], [out])

    def act(self, out, in_, func, bias=None, scale=None, accum_out=None):
        kw = {}
        if bias is not None:
            kw["bias"] = bias
        if scale is not None:
            kw["scale"] = scale
        if accum_out is not None:
            kw["accum_out"] = accum_out
        return self._record("act", lambda: self.nc.scalar.activation(out, in_, func, **kw),
                            [in_, bias, scale], [out, accum_out])

    def tt(self, eng, out, in0, in1, op):
        return self._record(eng, lambda: self.e[eng].tensor_tensor(out, in0, in1, op), [in0, in1], [out])

    def ts(self, eng, out, in0, s1, s2=None, op0=ALU.mult, op1=None):
        kw = {}
        if op1 is not None:
            kw["op1"] = op1
        return self._record(eng, lambda: self.e[eng].tensor_scalar(out, in0, s1, s2, op0, **kw),
                            [in0, s1, s2], [out])

    def stt(self, eng, out, in0, scalar, in1, op0, op1):
        return self._record(eng, lambda: self.e[eng].scalar_tensor_tensor(out, in0, scalar, in1, op0, op1),
                            [in0, scalar, in1], [out])

    def copy(self, eng, out, in_):
        if eng == "act":
            return self._record("act", lambda: self.nc.scalar.copy(out, in_), [in_], [out])
        return self._record(eng, lambda: self.e[eng].tensor_copy(out, in_), [in_], [out])

    def memset(self, eng, ap, val):
        return self._record(eng, lambda: self.e[eng].memset(ap, val), [], [ap])

    def reduce(self, eng, out, in_, op, axis=AX.X):
        return self._record(eng, lambda: self.e[eng].tensor_reduce(out, in_, axis, op), [in_], [out])

    def scan(self, eng, out, d0, d1, init, op0, op1):
        return self._record(eng, lambda: self.e[eng].tensor_tensor_scan(out, d0, d1, init, op0, op1),
                            [d0, d1, init], [out])

    def recip(self, out, in_):
        return self._record("dve", lambda: self.nc.vector.reciprocal(out, in_), [in_], [out])

    def max8(self, out, in_):
        return self._record("dve", lambda: self.nc.vector.max(out, in_), [in_], [out])

    def dma(self, out, in_, q="sp", **kw):
        return self._record(q, lambda: self.e[q].dma_start(out=out, in_=in_, **kw), [in_], [out], dma=True)

    def barrier(self):
        lasts = [self.last_op[k] for k in COMPUTE if self.last_op[k] is not None]
        dlast = list(self.dma_last.values())
        for k in self.e:
            op = _Op(k, None)
            op.deps = [o for o in lasts if o.eng != k] + dlast
            self.ops[k].append(op)

    def flush(self):
        for k in self.ops:
            for op in self.ops[k]:
                for d in op.deps:
                    d.marked = True
        for k in self.ops:
            lst = self.ops[k]
            if not lst:
                continue
            if k in COMPUTE:
                real = [o for o in lst if o.fn is not None and not o.dma]
                if real:
                    real[-1].marked = True
                c = self.cnt[k]
                for op in lst:
                    if op.fn is not None and not op.dma and op.marked:
                        c += 1
                        op.val = c
                nxt = None
                for op in reversed(lst):
                    if op.fn is None or op.dma:
                        continue
                    if op.marked:
                        nxt = op.val
                    else:
                        op.val = nxt
        for k in self.ops:
            eng = self.e[k]
            seen = self.seen[k]
            for op in self.ops[k]:
                waits = {}
                for d in op.deps:
                    if d.dma:
                        key = ("d",) + d.dsem
                        s, v = self.dsems[d.dsem], d.dval
                    else:
                        if d.fn is None:
                            continue
                        key = ("c", d.eng)
                        s, v = self.sem[d.eng], d.val
                    if seen.get(key, 0) >= v:
                        continue
                    if key not in waits or waits[key][1] < v:
                        waits[key] = (s, v)
                if op.dma and op.qwait is not None:
                    key = ("d",) + op.dsem
                    v = op.qwait.dval
                    if seen.get(key, 0) < v and (key not in waits or waits[key][1] < v):
                        waits[key] = (self.dsems[op.dsem], v)
                for key, (s, v) in waits.items():
                    eng.wait_ge(s, v)
                    seen[key] = v
                    self.trace[k].append(("w", key, v))
                if op.fn is None:
                    continue
                ins = op.fn()
                self.n_inst += 1
                op.fn = True
                if op.dma:
                    ins.then_inc(self.dsems[op.dsem], 16)
                    self.trace[k].append(("i", ("d",) + op.dsem, 16))
                elif op.marked:
                    ins.then_inc(self.sem[k], 1)
                    self.cnt[k] = op.val
                    self.trace[k].append(("i", ("c", k), 1))
                else:
                    self.trace[k].append(("n", None, 0))
            self.ops[k] = []

    def simulate(self):
        sem = {}
        pos = {k: 0 for k in self.trace}
        progress = True
        while progress:
            progress = False
            for k, tr in self.trace.items():
                while pos[k] < len(tr):
                    typ, key, v = tr[pos[k]]
                    if typ == "w":
                        if sem.get(key, 0) < v:
                            break
                    elif typ == "i":
                        sem[key] = sem.get(key, 0) + v
                    pos[k] += 1
                    progress = True
        stuck = {k: (pos[k], len(tr), tr[pos[k]] if pos[k] < len(tr) else None) for k, tr in self.trace.items()}
        return all(pos[k] == len(tr) for k, tr in self.trace.items()), stuck, sem

    def finish(self):
        self.flush()
        for (k, slot), op in self.dma_last.items():
            self.e[k].wait_ge(self.dsems[(k, slot)], op.dval)
        self.stacks[0].close()


D = 1024
NTOK = 4608
NG = 9
SEQ = 2048
CTX = 256
NS = 2304
IN_W = 8240
EPS = 1e-6
C_MLQ, C_MLK, C_MLV, C_MLO, C_MLG = 0, 512, 1024, 1536, 2048
C_DFQ, C_DFK, C_DFV = 2064, 2576, 3088
C_GLQ, C_GLK, C_GLV, C_GLG, C_GLA = 3600, 3856, 4112, 4624, 5136
C_GATE = 5168
T_MLQ, T_MLK, T_DFQ, T_DFK, T_GLQ, T_GLK, T_GATE = 0, 512, 1024, 1536, 2048, 2304, 2560
T_ROWS = 2560 + 3072
K_MLK, K_MLV, K_MLO, K_DFV, K_GLK, K_GLV, K_GLG = 0, 512, 1024, 1536, 2048, 2304, 2816
K_COLS = 3328


def seq_ranges(s):
    return 256 * s, 512 + 2048 * s


def build(n_layers=2, stop_after=None, dbg=False, do_moe=True, skip=()):
    nc = bass.Bass("TRN2", target_bir_lowering=False)
    mk = MK(nc)
    IN = {}

    def din(name, shape, dtype=F32):
        IN[name] = mk.dram(name, shape, dtype, kind="ExternalInput")
        return IN[name]

    x2 = din("x2", [2, SEQ, D])
    ctx2 = din("ctx2", [2, CTX, D])
    cT = din("cT", [128, 8, 3])
    w_mod = din("w_mod", [2, D, 6 * D])
    b_modT = din("b_modT", [2, 128, 48])
    gmixT = din("gmixT", [2, 128, 8])
    gffnT = din("gffnT", [2, 128, 8])
    w_in = din("w_in", [2, D, IN_W])
    ml_gb = din("ml_gb", [2, 4, 4])
    ml_ng = din("ml_ng", [2, 128])
    df_lam = din("df_lam", [2, 4, 64])
    df_ng = din("df_ng", [2, 128])
    gl_wa = din("gl_wa", [2, 2, 16, 256])
    gl_baT = din("gl_baT", [2, 2, 128, 2])
    gl_ng = din("gl_ng", [2, 128])
    w_branch = din("w_branch", [2, 3, 512, D])
    w_out = din("w_out", [2, D, D])
    router_w = din("router_w", [2, D, 36])
    router_b = din("router_b", [2, 36])
    if do_moe:
        moe_wg = din("moe_wg", [2, 32, D, 512])
        moe_wu = din("moe_wu", [2, 32, D, 512])
        moe_wd = din("moe_wd", [2, 32, 512, D])
    fin_gT = din("fin_gT", [128, 8])
    consts = din("consts", [128, 128 + 128 + 128 + 2048 + 2048])

    okind = "ExternalOutput"
    y_out = mk.dram("y_out", [2, SEQ, D], F32, kind=okind)
    skind = "ExternalOutput" if dbg else "Internal"
    xT = mk.dram("xT", [D, NTOK], F32, kind=skind)
    PT = mk.dram("PT", [T_ROWS, NTOK], BF16, kind=skind)
    PK = mk.dram("PK", [NTOK, K_COLS], BF16, kind=skind)
    GT = mk.dram("GT", [48, NTOK], F32, kind=skind)
    BR = mk.dram("BR", [1536, NTOK], BF16, kind=skind)

    ident = mk.sbuf("ident", [128, 128], F32)
    identb = mk.sbuf("identb", [128, 128], BF16)
    onesb = mk.sbuf("onesb", [128, 128], BF16)
    onesf = mk.sbuf("onesf", [128, 128], F32)
    maskF = mk.sbuf("maskF", [128, 128], F32)
    maskB = mk.sbuf("maskB", [128, 128], F32)
    modv = mk.sbuf("modv", [128, 2, 6, 8, 3], F32)
    PS = [mk.psum("psb%d" % i, [128, 512], F32) for i in range(7)]
    PSB = mk.psum("psbf", [128, 1024], BF16)

    mk.dma(ident[:], consts[:, 0:128])
    mk.dma(maskF[:], consts[:, 128:256])
    mk.dma(maskB[:], consts[:, 256:384])
    mk.copy("dve", identb[:], ident[:])
    mk.memset("pool", onesb[:], 1.0)
    mk.memset("pool", onesf[:], 1.0)

    rr = {"ev": 0}

    def evac_eng():
        rr["ev"] ^= 1
        return "act" if rr["ev"] else "dve"

    def phase_load_x():
        with mk.scope():
            xin = [mk.sbuf("xin%d" % i, [128, D], F32) for i in range(2)]
            xst = [mk.sbuf("xst%d" % i, [128, 8, 512], F32) for i in range(2)]
            for g in range(NG):
                st = xst[g % 2]
                for t4 in range(4):
                    tt = g * 4 + t4
                    if tt < 4:
                        src = ctx2[tt // 2, (tt % 2) * 128:(tt % 2) * 128 + 128, :]
                    else:
                        u = tt - 4
                        src = x2[u // 16, (u % 16) * 128:(u % 16) * 128 + 128, :]
                    xi = xin[tt % 2]
                    mk.dma(xi[:], src)
                    for half in range(2):
                        pb = PS[(tt * 2 + half) % 4]
                        for k4 in range(4):
                            kc = half * 4 + k4
                            mk.tr(pb[:, k4 * 128:(k4 + 1) * 128], xi[:, kc * 128:(kc + 1) * 128], ident[:])
                        mk.copy(evac_eng(), st[:, half * 4:(half + 1) * 4, t4 * 128:(t4 + 1) * 128],
                                pb[:].rearrange("p (k t) -> p k t", k=4))
                mk.dma(xT[:, g * 512:(g + 1) * 512].rearrange("(kc p) t -> p kc t", p=128), st[:], q="act")

    def phase_mod():
        with mk.scope():
            cs = mk.sbuf("cs", [128, 8, 3], F32)
            csg = mk.sbuf("csg", [128, 8, 3], F32)
            wm = [mk.sbuf("wm%d" % i, [128, 8, 512], F32) for i in range(2)]
            modT = mk.sbuf("modT", [128, 48, 3], F32)
            bm = mk.sbuf("bm", [128, 48], F32)
            gm = mk.sbuf("gm", [128, 8], F32)
            gf = mk.sbuf("gf", [128, 8], F32)
            mk.dma(cs[:], cT[:, :, :])
            mk.act(csg[:], cs[:], AF.Sigmoid)
            mk.tt("dve", cs[:], cs[:], csg[:], ALU.mult)
            for li in range(n_layers):
                mk.dma(bm[:], b_modT[li])
                mk.dma(gm[:], gmixT[li])
                mk.dma(gf[:], gffnT[li])
                for blk in range(12):
                    w = wm[blk % 2]
                    mk.dma(w[:], w_in_view(w_mod[li], blk * 512, 512), q=("sp" if blk % 2 else "act"))
                    for c4 in range(4):
                        fc = blk * 4 + c4
                        pb = PS[fc % 4]
                        for kc in range(8):
                            mk.mm(pb[:, 0:3], w[:, kc, c4 * 128:(c4 + 1) * 128], cs[:, kc, :],
                                  start=(kc == 0), stop=(kc == 7))
                        mk.ts("dve", modT[:, fc, :], pb[:, 0:3], bm[:, fc:fc + 1], None, op0=ALU.add)
                mv = modv[:, li]
                for j in range(3):
                    mk.ts("dve", mv[:, 0, :, j], modT[:, 8:16, j], 1.0, None, op0=ALU.add)
                    mk.tt("dve", mv[:, 0, :, j], mv[:, 0, :, j], gm[:], ALU.mult)
                    mk.copy("dve", mv[:, 1, :, j], modT[:, 0:8, j])
                    mk.copy("dve", mv[:, 2, :, j], modT[:, 16:24, j])
                    mk.ts("dve", mv[:, 3, :, j], modT[:, 32:40, j], 1.0, None, op0=ALU.add)
                    mk.tt("dve", mv[:, 3, :, j], mv[:, 3, :, j], gf[:], ALU.mult)
                    mk.copy("dve", mv[:, 4, :, j], modT[:, 24:32, j])
                    mk.copy("dve", mv[:, 5, :, j], modT[:, 40:48, j])

    def w_in_view(w2d, c0, n):
        return w2d[:, c0:c0 + n].rearrange("(kc p) n -> p kc n", p=128)

    def gset(g):
        return 0 if g == 0 else (1 if g <= 4 else 2)

    def norm_groups(li, which_gs, hT, scratch, h32_cb=None):
        xs_b, sq_b, rs_b = scratch
        for g in range(NG):
            j = gset(g)
            xs = xs_b[g % 2]
            sq = sq_b[g % 2]
            rs = rs_b[g % 2]
            mk.dma(xs[:], xT[:, g * 512:(g + 1) * 512].rearrange("(kc p) t -> p kc t", p=128),
                   q=("sp" if g % 2 else "act"))
            mk.tt("pool", sq[:], xs[:], xs[:], ALU.mult)
            pb = PS[4 + g % 2]
            for kc in range(8):
                mk.mm(pb[:], onesb[:], sq[:, kc, :], start=(kc == 0), stop=(kc == 7))
            mk.act(rs[:], pb[:], AF.Sqrt, bias=EPS, scale=1.0 / D)
            mk.recip(rs[:], rs[:])
            for kc in range(8):
                mk.stt("dve", xs[:, kc, :], xs[:, kc, :], modv[:, li, which_gs, kc, j:j + 1], rs[:],
                       ALU.mult, ALU.mult)
                mk.act(hT[:, kc, g * 512:(g + 1) * 512], xs[:, kc, :], AF.Identity,
                       bias=modv[:, li, which_gs + 1, kc, j:j + 1], scale=1.0)
                if h32_cb is not None:
                    mk.ts("pool", xs[:, kc, :], xs[:, kc, :], modv[:, li, which_gs + 1, kc, j:j + 1], None,
                          op0=ALU.add)
            if h32_cb is not None:
                h32_cb(g, xs)

    def phase_proj(li, hT):
        cosT = mk.sbuf("cosT", [128, SEQ], F32)
        sinT = mk.sbuf("sinT", [128, SEQ], F32)
        mk.dma(cosT[:], consts[:, 384:384 + SEQ])
        mk.dma(sinT[:], consts[:, 384 + SEQ:384 + 2 * SEQ])
        wb = [mk.sbuf("wblk%d" % i, [128, 8, 512], BF16) for i in range(2)]
        stT = [mk.sbuf("stT%d" % i, [128, NTOK], BF16) for i in range(2)]
        stK = [mk.sbuf("stK%d" % i, [128, 4, 512], BF16) for i in range(2)]
        stG = mk.sbuf("stG", [16, NTOK], F32)
        wrot = mk.sbuf("wrot", [128, 8, 512], BF16)
        stG2 = mk.sbuf("stG2", [32, NTOK], F32)
        t1 = [mk.sbuf("rp1_%d" % i, [128, 512], F32) for i in range(2)]
        t2 = [mk.sbuf("rp2_%d" % i, [128, 512], F32) for i in range(2)]
        cnt = {"blk": 0, "ps": 0, "st": 0}

        def load_blk(c0, n):
            w = wb[cnt["blk"] % 2]
            cnt["blk"] += 1
            mk.dma(w[:, :, 0:n], w_in_view(w_in[li], c0, n), q="pool")
            return w

        def nextps():
            cnt["ps"] += 1
            return PS[cnt["ps"] % 4]

        tblocks = [
            (C_MLQ, 512, T_MLQ, "copy", 1.0),
            (C_MLK, 512, T_MLK, "copy", 128.0 ** -0.5),
            (C_DFQ, 512, T_DFQ, "rope", 1.0),
            (C_DFK, 512, T_DFK, "rope", 1.0),
            (C_GLQ, 256, T_GLQ, "copy", 0.125),
            (C_GLK, 256, T_GLK, "copy", 1.0),
        ] + [(C_GATE + i * 512, 512, T_GATE + i * 512, "sigmoid", 1.0) for i in range(6)]
        for (c0, n, r0, mode, scale) in tblocks:
            w = load_blk(c0, n)
            if mode == "rope":
                wv = w[:].rearrange("p k (b h e) -> p (k b) h e", h=2, e=16)
                rv = wrot[:].rearrange("p k (b h e) -> p (k b) h e", h=2, e=16)
                mk.copy("pool", rv[:, :, 0, :], wv[:, :, 1, :])
                mk.copy("pool", rv[:, :, 1, :], wv[:, :, 0, :])
            for cc in range(n // 128):
                st = stT[cnt["st"] % 2]
                cnt["st"] += 1
                for g in range(NG):
                    pb = nextps()
                    for kc in range(8):
                        mk.mm(pb[:], w[:, kc, cc * 128:(cc + 1) * 128], hT[:, kc, g * 512:(g + 1) * 512],
                              start=(kc == 0), stop=(kc == 7))
                    dst = st[:, g * 512:(g + 1) * 512]
                    if mode == "sigmoid":
                        mk.act(dst, pb[:], AF.Sigmoid)
                    elif mode == "rope" and g >= 1:
                        pr = nextps()
                        for kc in range(8):
                            mk.mm(pr[:], wrot[:, kc, cc * 128:(cc + 1) * 128], hT[:, kc, g * 512:(g + 1) * 512],
                                  start=(kc == 0), stop=(kc == 7))
                        tok0 = ((g - 1) % 4) * 512
                        a = t1[g % 2]
                        b = t2[g % 2]
                        mk.tt("dve", a[:], pb[:], cosT[:, tok0:tok0 + 512], ALU.mult)
                        mk.tt("dve", b[:], pr[:], sinT[:, tok0:tok0 + 512], ALU.mult)
                        mk.tt("pool", dst, a[:], b[:], ALU.add)
                    else:
                        e = evac_eng()
                        if e == "act":
                            mk.act(dst, pb[:], AF.Copy, scale=scale)
                        else:
                            mk.ts("dve", dst, pb[:], scale, None, op0=ALU.mult)
                mk.dma(PT[r0 + cc * 128:r0 + (cc + 1) * 128, :], st[:], q="sp")
        for (c0, n, r0) in [(C_MLG, 16, 0), (C_GLA, 32, 16)]:
            w = load_blk(c0, n)
            for g in range(NG):
                pb = nextps()
                for kc in range(8):
                    mk.mm(pb[0:n, :], w[:, kc, 0:n], hT[:, kc, g * 512:(g + 1) * 512],
                          start=(kc == 0), stop=(kc == 7))
                if r0 == 0:
                    mk.copy("dve", stG[0:16, g * 512:(g + 1) * 512], pb[0:16, :])
                else:
                    mk.copy("dve", stG2[:, g * 512:(g + 1) * 512], pb[0:32, :])
        mk.dma(GT[0:16, :], stG[0:16, :], q="sp")
        mk.dma(GT[16:48, :], stG2[:, :], q="sp")
        kblocks = [
            (C_MLK, 512, K_MLK, "copy", 128.0 ** -0.5),
            (C_MLV, 512, K_MLV, "copy", 1.0),
            (C_MLO, 512, K_MLO, "sigmoid", 1.0),
            (C_DFV, 512, K_DFV, "copy", 1.0),
            (C_GLK, 256, K_GLK, "copy", 1.0),
            (C_GLV, 512, K_GLV, "copy", 1.0),
            (C_GLG, 512, K_GLG, "silu", 1.0),
        ]
        sg = [mk.sbuf("sgk%d" % i, [128, 512], F32) for i in range(2)]
        for (c0, n, k0, mode, scale) in kblocks:
            w = load_blk(c0, n)
            for t4 in range(NTOK // 512):
                st = stK[cnt["st"] % 2]
                cnt["st"] += 1
                for q4 in range(4):
                    tt_ = t4 * 4 + q4
                    pb = nextps()
                    for kc in range(8):
                        mk.mm(pb[:, 0:n], hT[:, kc, tt_ * 128:(tt_ + 1) * 128], w[:, kc, 0:n],
                              start=(kc == 0), stop=(kc == 7))
                    dst = st[:, q4, 0:n]
                    if mode == "sigmoid":
                        mk.act(dst, pb[:, 0:n], AF.Sigmoid)
                    elif mode == "silu":
                        s_ = sg[tt_ % 2]
                        mk.act(s_[:, 0:n], pb[:, 0:n], AF.Sigmoid)
                        mk.tt("dve", dst, pb[:, 0:n], s_[:, 0:n], ALU.mult)
                    else:
                        e = evac_eng()
                        if e == "act":
                            mk.act(dst, pb[:, 0:n], AF.Copy, scale=scale)
                        else:
                            mk.ts("dve", dst, pb[:, 0:n], scale, None, op0=ALU.mult)
                mk.dma(PK[t4 * 512:(t4 + 1) * 512, k0:k0 + n].rearrange("(a p) c -> p a c", p=128),
                       st[:, :, 0:n], q="sp")

    def load_seq_T(dst, src_rows, s, q="sp"):
        c0, l0 = seq_ranges(s)
        mk.dma(dst[:, 0:CTX], src_rows[:, c0:c0 + CTX], q=q)
        mk.dma(dst[:, CTX:NS], src_rows[:, l0:l0 + SEQ], q=q)

    def store_seq_T(dst_rows, src, s, q="sp"):
        c0, l0 = seq_ranges(s)
        mk.dma(dst_rows[:, c0:c0 + CTX], src[:, 0:CTX], q=q)
        mk.dma(dst_rows[:, l0:l0 + SEQ], src[:, CTX:NS], q=q)

    def load_seq_K(dst3, k0, ncol, s, q="sp"):
        c0, l0 = seq_ranges(s)
        mk.dma(dst3[:, 0:2], PK[c0:c0 + CTX, k0:k0 + ncol].rearrange("(a p) c -> p a c", p=128), q=q)
        mk.dma(dst3[:, 2:18], PK[l0:l0 + SEQ, k0:k0 + ncol].rearrange("(a p) c -> p a c", p=128), q=q)

    def bcast_row(dst, src_row):
        mk.dma(dst, src_row.to_broadcast([128, src_row.shape[-1]]))

    def tok_to_BR(src, r0, s, stg):
        for h in range(4):
            st = stg[h % 2]
            for t4 in range(5):
                n = min(4, 18 - t4 * 4)
                pb = PS[t4 % 4]
                for j in range(n):
                    t = t4 * 4 + j
                    mk.mm(pb[:, j * 128:(j + 1) * 128], src[:, t, h * 128:(h + 1) * 128], identb[:])
                mk.copy(evac_eng(), st[:, t4 * 512:t4 * 512 + n * 128], pb[:, 0:n * 128])
            store_seq_T(BR[r0 + h * 128:r0 + (h + 1) * 128, :], st, s, q="act")

    def phase_attn(li):
        lam_init = 0.8 - 0.6 * math.exp(-0.3 * li)
        with mk.scope():
            lam_t = mk.sbuf("lam_t", [1, 4, 64], F32)
            lam_p = mk.sbuf("lam_p", [1, 2, 64], F32)
            lam_s = mk.sbuf("lam_s", [1, 4], F32)
            nlam = mk.sbuf("nlam", [128, 1], F32)
            gv = mk.sbuf("dfgv", [128, 128], F32)
            mk.dma(lam_t[:], df_lam[li:li + 1, :, :])
            mk.tt("dve", lam_p[:, 0, :], lam_t[:, 0, :], lam_t[:, 1, :], ALU.mult)
            mk.tt("dve", lam_p[:, 1, :], lam_t[:, 2, :], lam_t[:, 3, :], ALU.mult)
            mk.reduce("dve", lam_s[:, 0:2], lam_p[:], ALU.add)
            mk.act(lam_s[:, 0:2], lam_s[:, 0:2], AF.Exp)
            mk.tt("dve", lam_s[:, 2:3], lam_s[:, 0:1], lam_s[:, 1:2], ALU.subtract)
            mk.ts("dve", lam_s[:, 2:3], lam_s[:, 2:3], lam_init, -1.0, op0=ALU.add, op1=ALU.mult)
            mk.mm(PS[6][:, 0:1], onesf[0:1, :], lam_s[0:1, 2:3])
            mk.copy("dve", nlam[:], PS[6][:, 0:1])
            bcast_row(gv[:], df_ng[li:li + 1, :])
            mk.ts("dve", gv[:], gv[:], 1.0 - lam_init, None, op0=ALU.mult)
            qT = mk.sbuf("aqT", [128, 4, NS], BF16)
            kT = mk.sbuf("akT", [128, 4, NS], BF16)
            sq = mk.sbuf("asq", [128, 4, NS], BF16)
            vA = mk.sbuf("avA", [128, 18, 4, 129], BF16)
            DF = mk.sbuf("aDF", [128, 18, 512], BF16)
            pt = [mk.sbuf("apt%d" % i, [128, 512], BF16) for i in range(4)]
            mxs = mk.sbuf("amx", [1, 64], F32)
            negM = mk.sbuf("anegM", [128, 1], F32)
            o1 = [mk.sbuf("ao1_%d" % i, [128, 128], F32) for i in range(2)]
            o2 = [mk.sbuf("ao2_%d" % i, [128, 128], F32) for i in range(2)]
            rc = [mk.sbuf("arc%d" % i, [128, 4], F32) for i in range(2)]
            stg = [mk.sbuf("astg%d" % i, [128, NS], BF16) for i in range(2)]
            mk.memset("pool", vA[:], 1.0)
            for s in range(2):
                for h in range(4):
                    load_seq_T(qT[:, h, :], PT[T_DFQ + h * 128:T_DFQ + (h + 1) * 128, :], s, q="sp")
                    load_seq_T(kT[:, h, :], PT[T_DFK + h * 128:T_DFK + (h + 1) * 128, :], s, q="act")
                c0, l0 = seq_ranges(s)
                for h in range(4):
                    mk.dma(vA[:, 0:2, h, 0:128],
                           PK[c0:c0 + CTX, K_DFV + h * 128:K_DFV + (h + 1) * 128].rearrange("(a p) c -> p a c", p=128))
                    mk.dma(vA[:, 2:18, h, 0:128],
                           PK[l0:l0 + SEQ, K_DFV + h * 128:K_DFV + (h + 1) * 128].rearrange("(a p) c -> p a c", p=128))
                mk.memset("dve", mxs[:], 0.0)
                for which, src in ((0, qT), (1, kT)):
                    mk.tt("pool", sq[:], src[:], src[:], ALU.mult)
                    idx = 0
                    for h in range(4):
                        for cst in range(0, NS, 512):
                            n = min(512, NS - cst)
                            pb = PS[3 + idx % 4]
                            mk.mm(pb[0:1, 0:n], onesb[:, 0:1], sq[:, h, cst:cst + n])
                            mk.reduce("dve", mxs[:, which * 32 + idx:which * 32 + idx + 1], pb[0:1, 0:n], ALU.max)
                            idx += 1
                mk.reduce("dve", mxs[:, 60:61], mxs[:, 0:32], ALU.max)
                mk.reduce("dve", mxs[:, 61:62], mxs[:, 32:60], ALU.max)
                mk.tt("dve", mxs[:, 62:63], mxs[:, 60:61], mxs[:, 61:62], ALU.mult)
                mk.act(mxs[:, 63:64], mxs[:, 62:63], AF.Sqrt, scale=1.0 / 64.0)
                mk.ts("dve", mxs[:, 63:64], mxs[:, 63:64], -1.0, None, op0=ALU.mult)
                mk.mm(PS[6][:, 0:1], onesf[0:1, :], mxs[0:1, 63:64])
                mk.copy("dve", negM[:], PS[6][:, 0:1])
                import os as _os
                if _os.environ.get('ATTN_STOP') == '1':
                    return
                qgroups = [(0, CTX, 0, 2)] + [(CTX + i * 512, 512, 0, 18) for i in range(4)]
                ci = 0
                for (q0, nq, tk0, tk1) in qgroups:
                    nsub = nq // 128
                    for h in range(4):
                        O = [[PS[(a * 4 + sb) // 3][:, ((a * 4 + sb) % 3) * 160:((a * 4 + sb) % 3) * 160 + 129]
                              for sb in range(4)] for a in range(2)]
                        started = set()
                        for tk in range(tk0, tk1):
                            for a in range(2):
                                pb = PS[3 + ci % 4]
                                p_ = pt[ci % 4]
                                ci += 1
                                mk.mm(pb[:, 0:nq], kT[a * 64:(a + 1) * 64, h, tk * 128:(tk + 1) * 128],
                                      qT[a * 64:(a + 1) * 64, h, q0:q0 + nq])
                                mk.act(p_[:, 0:nq], pb[:, 0:nq], AF.Exp, bias=negM[:, 0:1], scale=0.125)
                                for sb in range(nsub):
                                    bank = (a * 4 + sb) // 3
                                    st_ = (tk == tk0) and (bank not in started)
                                    started.add(bank)
                                    mk.mm(O[a][sb], p_[:, sb * 128:(sb + 1) * 128], vA[:, tk, h, :],
                                          start=st_, stop=(tk == tk1 - 1))
                        for sb in range(nsub):
                            tile = (q0 + sb * 128) // 128
                            r_ = rc[sb % 2]
                            a1 = o1[sb % 2]
                            a2 = o2[sb % 2]
                            mk.recip(r_[:, 0:1], O[0][sb][:, 128:129])
                            mk.recip(r_[:, 1:2], O[1][sb][:, 128:129])
                            mk.tt("dve", r_[:, 1:2], r_[:, 1:2], nlam[:, 0:1], ALU.mult)
                            mk.ts("dve", a1[:], O[0][sb][:, 0:128], r_[:, 0:1], None, op0=ALU.mult)
                            mk.stt("dve", a1[:], O[1][sb][:, 0:128], r_[:, 1:2], a1[:], ALU.mult, ALU.add)
                            mk.act(a2[:], a1[:], AF.Square, accum_out=r_[:, 2:3])
                            mk.act(r_[:, 3:4], r_[:, 2:3], AF.Sqrt, bias=EPS, scale=1.0 / 128.0)
                            mk.recip(r_[:, 3:4], r_[:, 3:4])
                            mk.stt("dve", DF[:, tile, h * 128:(h + 1) * 128], a1[:], r_[:, 3:4], gv[:],
                                   ALU.mult, ALU.mult)
                if _os.environ.get('ATTN_STOP') == '2':
                    return
                tok_to_BR(DF, 512, s, stg)
                if _os.environ.get('ATTN_STOP') == '3':
                    return

    def phase_merge(li):
        with mk.scope():
            wbr = mk.sbuf("wbr", [128, 12, D], BF16)
            wo = mk.sbuf("wo", [128, 8, D], BF16)
            for b in range(3):
                mk.dma(wbr[:, b * 4:(b + 1) * 4, :], w_branch[li, b].rearrange("(kc p) n -> p kc n", p=128), q="pool")
            mk.dma(wo[:], w_out[li].rearrange("(kc p) n -> p kc n", p=128), q="pool")
            brT = [mk.sbuf("mbr%d" % i, [128, 12, 512], BF16) for i in range(2)]
            gt = [mk.sbuf("mgt%d" % i, [128, 24, 512], BF16) for i in range(2)]
            xs_b = [mk.sbuf("mxs%d" % i, [128, 8, 512], F32) for i in range(2)]
            yT = [mk.sbuf("myT%d" % i, [128, 8, 512], BF16) for i in range(2)]
            ya = [mk.sbuf("mya%d" % i, [128, 512], F32) for i in range(2)]
            tmp = [mk.sbuf("mtm%d" % i, [128, 512], F32) for i in range(2)]
            ci = 0
            for g in range(NG):
                j = gset(g)
                b_ = brT[g % 2]
                g_ = gt[g % 2]
                xs = xs_b[g % 2]
                y_ = yT[g % 2]
                tsl = slice(g * 512, (g + 1) * 512)
                mk.dma(b_[:], BR[:, tsl].rearrange("(c p) t -> p c t", p=128), q="sp")
                mk.dma(g_[:], PT[T_GATE:T_GATE + 3072, tsl].rearrange("(c p) t -> p c t", p=128), q="act")
                mk.dma(xs[:], xT[:, tsl].rearrange("(kc p) t -> p kc t", p=128), q="sp")
                for oc in range(8):
                    acc = ya[oc % 2]
                    for br in range(3):
                        pb = PS[ci % 4]
                        ci += 1
                        for kc in range(4):
                            mk.mm(pb[:], wbr[:, br * 4 + kc, oc * 128:(oc + 1) * 128], b_[:, br * 4 + kc, :],
                                  start=(kc == 0), stop=(kc == 3))
                        if br == 0:
                            mk.tt("dve", acc[:], pb[:], g_[:, br * 8 + oc, :], ALU.mult)
                        else:
                            t_ = tmp[br % 2]
                            mk.tt("dve", t_[:], pb[:], g_[:, br * 8 + oc, :], ALU.mult)
                            mk.tt("pool", (acc[:] if br == 1 else y_[:, oc, :]), acc[:], t_[:], ALU.add)
                for oc in range(8):
                    pb = PS[4 + oc % 3]
                    for kc in range(8):
                        mk.mm(pb[:], wo[:, kc, oc * 128:(oc + 1) * 128], y_[:, kc, :], start=(kc == 0), stop=(kc == 7))
                    mk.stt("dve", xs[:, oc, :], pb[:], modv[:, li, 2, oc, j:j + 1], xs[:, oc, :], ALU.mult, ALU.add)
                mk.dma(xT[:, tsl].rearrange("(kc p) t -> p kc t", p=128), xs[:], q="act")

    def phase_moe(li):
        HALF = NTOK // 2
        with mk.scope():
            h2T = mk.sbuf("h2T", [128, 8, NTOK], BF16)
            comb = mk.sbuf("comb", [128, 36, 32], F32)
            with mk.scope():
                scratch = ([mk.sbuf("nxs%d" % i, [128, 8, 512], F32) for i in range(2)],
                           [mk.sbuf("nsq%d" % i, [128, 8, 512], BF16) for i in range(2)],
                           [mk.sbuf("nrs%d" % i, [128, 512], F32) for i in range(2)])
                wr = mk.sbuf("wr", [128, 8, 36], F32)
                rb = mk.sbuf("rb", [128, 36], F32)
                L = mk.sbuf("rL", [128, 36], F32)
                sm = mk.sbuf("rsm", [128, 16], F32)
                mg = mk.sbuf("rmg", [128, 4], F32)
                eg = mk.sbuf("reg", [128, 4], F32)
                ls = mk.sbuf("rls", [128, 8], F32)
                t8 = mk.sbuf("rt8", [128, 8], F32)
                s1 = mk.sbuf("rs1", [128, 8], F32)
                s2 = mk.sbuf("rs2", [128, 8], F32)
                mk.dma(wr[:], router_w[li].rearrange("(kc p) n -> p kc n", p=128))
                bcast_row(rb[:], router_b[li:li + 1, :])

                def route(g, xs):
                    for t4 in range(4):
                        tile = g * 4 + t4
                        pb = PS[t4 % 4]
                        for kc in range(8):
                            mk.mm(pb[:, 0:36], xs[:, kc, t4 * 128:(t4 + 1) * 128], wr[:, kc, :],
                                  start=(kc == 0), stop=(kc == 7))
                        mk.tt("dve", L[:], pb[:, 0:36], rb[:], ALU.add)
                        mk.reduce("dve", sm[:, 0:1], L[:, 0:4], ALU.max)
                        mk.ts("dve", mg[:], L[:, 0:4], sm[:, 0:1], None, op0=ALU.is_ge)
                        mk.ts("dve", sm[:, 1:2], sm[:, 0:1], -1.0, None, op0=ALU.mult)
                        mk.act(eg[:], L[:, 0:4], AF.Exp, bias=sm[:, 1:2], scale=1.0, accum_out=sm[:, 2:3])
                        mk.recip(sm[:, 3:4], sm[:, 2:3])
                        mk.ts("dve", ls[:], L[:, 4:12], mg[:, 0:1], None, op0=ALU.mult)
                        for gi in range(1, 4):
                            mk.stt("dve", ls[:], L[:, 4 + 8 * gi:12 + 8 * gi], mg[:, gi:gi + 1], ls[:],
                                   ALU.mult, ALU.add)
                        mk.max8(t8[:], ls[:])
                        mk.tt("dve", sm[:, 4:5], t8[:, 1:2], t8[:, 0:1], ALU.subtract)
                        mk.act(sm[:, 5:6], sm[:, 4:5], AF.Exp)
                        mk.ts("dve", sm[:, 6:7], sm[:, 5:6], 1.0, None, op0=ALU.add)
                        mk.recip(sm[:, 6:7], sm[:, 6:7])
                        mk.tt("dve", sm[:, 7:8], sm[:, 6:7], sm[:, 3:4], ALU.mult)
                        mk.tt("dve", sm[:, 8:9], sm[:, 7:8], sm[:, 5:6], ALU.mult)
                        mk.tt("dve", sm[:, 9:10], sm[:, 7:8], sm[:, 8:9], ALU.subtract)
                        mk.ts("dve", s1[:], ls[:], t8[:, 0:1], sm[:, 9:10], op0=ALU.is_ge, op1=ALU.mult)
                        mk.ts("dve", s2[:], ls[:], t8[:, 1:2], sm[:, 8:9], op0=ALU.is_ge, op1=ALU.mult)
                        mk.tt("dve", s1[:], s1[:], s2[:], ALU.add)
                        for gi in range(4):
                            mk.ts("dve", comb[:, tile, gi * 8:(gi + 1) * 8], s1[:], mg[:, gi:gi + 1], None,
                                  op0=ALU.mult)

                norm_groups(li, 3, h2T, scratch, h32_cb=route)
            if dbg:
                comb_dbg = mk.dram("comb_dbg%d" % li, [128, 36 * 32], F32, kind="ExternalOutput")
                mk.dma(comb_dbg[:, :], comb[:].rearrange("p a b -> p (a b)"))
            wg_b = [mk.sbuf("ewg%d" % i, [128, 8, 512], BF16) for i in range(2)]
            wu_b = [mk.sbuf("ewu%d" % i, [128, 8, 512], BF16) for i in range(2)]
            wd_b = [mk.sbuf("ewd%d" % i, [128, 4, D], BF16) for i in range(2)]
            hid_b = [mk.sbuf("ehid%d" % i, [128, 4, 512], BF16) for i in range(2)]
            sl_b = [mk.sbuf("esl%d" % i, [128, 512], F32) for i in range(2)]
            NPART = 3
            PT_TILES = 36 // NPART
            yacc = mk.sbuf("yacc", [128, PT_TILES, D], F32)
            xs_b = [mk.sbuf("exs%d" % i, [128, 8, 128], F32) for i in range(2)]
            ci = 0
            for hf in range(NPART):
                mk.memset("pool", yacc[:], 0.0)
                grp = [(hf * PT_TILES * 128 + i * 512, 512) for i in range(PT_TILES // 4)]
                for e in range(32):
                    wg, wu, wd = wg_b[e % 2], wu_b[e % 2], wd_b[e % 2]
                    mk.dma(wg[:], moe_wg[li, e].rearrange("(kc p) n -> p kc n", p=128), q="pool")
                    mk.dma(wu[:], moe_wu[li, e].rearrange("(kc p) n -> p kc n", p=128), q="pool")
                    mk.dma(wd[:], moe_wd[li, e].rearrange("(kc p) n -> p kc n", p=128), q="pool")
                    for (t0, nt) in grp:
                        hid = hid_b[ci % 2]
                        for fc in range(4):
                            pg = PS[ci % 3]
                            pu = PS[3 + ci % 2]
                            sl = sl_b[ci % 2]
                            ci += 1
                            for kc in range(8):
                                mk.mm(pg[:, 0:nt], wg[:, kc, fc * 128:(fc + 1) * 128], h2T[:, kc, t0:t0 + nt],
                                      start=(kc == 0), stop=(kc == 7))
                            for kc in range(8):
                                mk.mm(pu[:, 0:nt], wu[:, kc, fc * 128:(fc + 1) * 128], h2T[:, kc, t0:t0 + nt],
                                      start=(kc == 0), stop=(kc == 7))
                            mk.act(sl[:, 0:nt], pg[:, 0:nt], AF.Silu)
                            mk.tt("dve", hid[:, fc, 0:nt], pu[:, 0:nt], sl[:, 0:nt], ALU.mult)
                        for sb in range(nt // 128):
                            tile = (t0 + sb * 128) // 128
                            lt = tile - hf * PT_TILES
                            for hh in range(2):
                                py = PS[5 + (sb * 2 + hh) % 2]
                                for fc in range(4):
                                    mk.mm(py[:], hid[:, fc, sb * 128:(sb + 1) * 128], wd[:, fc, hh * 512:(hh + 1) * 512],
                                          start=(fc == 0), stop=(fc == 3))
                                mk.stt("dve", yacc[:, lt, hh * 512:(hh + 1) * 512], py[:], comb[:, tile, e:e + 1],
                                       yacc[:, lt, hh * 512:(hh + 1) * 512], ALU.mult, ALU.add)
                for lt in range(PT_TILES):
                    tile = hf * PT_TILES + lt
                    j = gset(tile // 4)
                    xs = xs_b[lt % 2]
                    tsl = slice(tile * 128, (tile + 1) * 128)
                    mk.dma(xs[:], xT[:, tsl].rearrange("(kc p) t -> p kc t", p=128), q="sp")
                    for half in range(2):
                        pb = PS[(lt * 2 + half) % 4]
                        for k4 in range(4):
                            kc = half * 4 + k4
                            mk.tr(pb[:, k4 * 128:(k4 + 1) * 128], yacc[:, lt, kc * 128:(kc + 1) * 128], ident[:])
                        for k4 in range(4):
                            kc = half * 4 + k4
                            mk.stt("dve", xs[:, kc, :], pb[:, k4 * 128:(k4 + 1) * 128], modv[:, li, 5, kc, j:j + 1],
                                   xs[:, kc, :], ALU.mult, ALU.add)
                    mk.dma(xT[:, tsl].rearrange("(kc p) t -> p kc t", p=128), xs[:], q="act")

    def phase_final():
        with mk.scope():
            fg = mk.sbuf("fg", [128, 8], F32)
            mk.dma(fg[:], fin_gT[:, :])
            xs_b = [mk.sbuf("fxs%d" % i, [128, 8, 512], F32) for i in range(2)]
            sq_b = [mk.sbuf("fsq%d" % i, [128, 8, 512], BF16) for i in range(2)]
            rs_b = [mk.sbuf("frs%d" % i, [128, 512], F32) for i in range(2)]
            ot = [mk.sbuf("fot%d" % i, [128, D], F32) for i in range(2)]
            for g in range(1, NG):
                xs, sq, rs = xs_b[g % 2], sq_b[g % 2], rs_b[g % 2]
                mk.dma(xs[:], xT[:, g * 512:(g + 1) * 512].rearrange("(kc p) t -> p kc t", p=128),
                       q=("sp" if g % 2 else "act"))
                mk.tt("pool", sq[:], xs[:], xs[:], ALU.mult)
                pb = PS[4 + g % 2]
                for kc in range(8):
                    mk.mm(pb[:], onesb[:], sq[:, kc, :], start=(kc == 0), stop=(kc == 7))
                mk.act(rs[:], pb[:], AF.Sqrt, bias=EPS, scale=1.0 / D)
                mk.recip(rs[:], rs[:])
                for kc in range(8):
                    mk.stt("dve", xs[:, kc, :], xs[:, kc, :], fg[:, kc:kc + 1], rs[:], ALU.mult, ALU.mult)
                for t4 in range(4):
                    u = (g - 1) * 4 + t4
                    o_ = ot[u % 2]
                    for half in range(2):
                        pq = PS[(u * 2 + half) % 4]
                        for k4 in range(4):
                            kc = half * 4 + k4
                            mk.tr(pq[:, k4 * 128:(k4 + 1) * 128], xs[:, kc, t4 * 128:(t4 + 1) * 128], ident[:])
                        mk.copy(evac_eng(), o_[:, half * 512:(half + 1) * 512], pq[:])
                    mk.dma(y_out[u // 16, (u % 16) * 128:(u % 16) * 128 + 128, :], o_[:], q="sp")

    def nat_chunk(d, cs):
        if d == 0:
            return cs
        return (3 - cs) if cs < 4 else (35 - (cs - 4))

    def rev_copy(eng, dst, src):
        mk.copy(eng, dst[:, 0:CTX], src[:, 0:CTX][:, ::-1])
        mk.copy(eng, dst[:, CTX:NS], src[:, CTX:NS][:, ::-1])

    def phase_mlstm(li):
        with mk.scope():
            HS = mk.sbuf("mHS", [128, 18, 512], F32)
            gb = mk.sbuf("mgb", [4, 4], F32)
            ngb = mk.sbuf("mngb", [4, 4], F32)
            gvn = mk.sbuf("mgvn", [128, 128], F32)
            ones4 = mk.sbuf("mones4", [4, NS], F32)
            mk.dma(gb[:], ml_gb[li])
            mk.ts("dve", ngb[:], gb[:], -1.0, None, op0=ALU.mult)
            bcast_row(gvn[:], ml_ng[li:li + 1, :])
            mk.memset("pool", ones4[:], 1.0)
            for s in range(2):
                with mk.scope():
                    qT = mk.sbuf("mqT", [128, 4, NS], BF16)
                    kT = mk.sbuf("mkT", [128, 4, NS], BF16)
                    kK = mk.sbuf("mkK", [128, 18, 512], BF16)
                    vA = mk.sbuf("mvA", [128, 18, 4, 129], BF16)
                    uV = mk.sbuf("muV", [128, 18, 4, 129], BF16)
                    R = [mk.sbuf("mR%d" % i, [4, NS], F32) for i in range(5)]
                    COL = mk.sbuf("mCOL", [128, 3, 18, 4], F32)
                    DECr = mk.sbuf("mDECr", [4, 36, 4], F32)
                    DECb = mk.sbuf("mDECb", [128, 36, 4], F32)
                    CT = mk.sbuf("mCT", [128, 4, 129], F32)
                    tmpC = mk.sbuf("mtmpC", [128, 4, 129], F32)
                    CTb = [mk.sbuf("mCTb%d" % i, [128, 4, 129], BF16) for i in range(2)]
                    smb = [mk.sbuf("msm%d" % i, [128, 4, 128], BF16) for i in range(2)]
                    t4b = [mk.sbuf("mt4%d" % i, [128, 4], F32) for i in range(2)]
                    tmo = [mk.sbuf("mtmo%d" % i, [128, 4, 128], F32) for i in range(2)]
                    c0, l0 = seq_ranges(s)
                    for h in range(4):
                        load_seq_T(qT[:, h, :], PT[T_MLQ + h * 128:T_MLQ + (h + 1) * 128, :], s, q="sp")
                        load_seq_T(kT[:, h, :], PT[T_MLK + h * 128:T_MLK + (h + 1) * 128, :], s, q="act")
                    load_seq_K(kK, K_MLK, 512, s)
                    mk.memset("pool", vA[:], 1.0)
                    for h in range(4):
                        mk.dma(vA[:, 0:2, h, 0:128],
                               PK[c0:c0 + CTX, K_MLV + h * 128:K_MLV + (h + 1) * 128].rearrange("(a p) c -> p a c", p=128))
                        mk.dma(vA[:, 2:18, h, 0:128],
                               PK[l0:l0 + SEQ, K_MLV + h * 128:K_MLV + (h + 1) * 128].rearrange("(a p) c -> p a c", p=128))
                    for d in range(2):
                        irow = GT[d * 8:d * 8 + 4, :]
                        frow = GT[d * 8 + 4:d * 8 + 8, :]
                        if d == 0:
                            load_seq_T(R[1], irow, s)
                            load_seq_T(R[0], frow, s)
                        else:
                            load_seq_T(R[3], irow, s)
                            load_seq_T(R[4], frow, s)
                            rev_copy("dve", R[1], R[3])
                            rev_copy("pool", R[0], R[4])
                        R0, R1, R2, R3, R4 = R
                        v3 = lambda t: t[:].rearrange("p (c j) -> p c j", j=64)
                        mk.act(R0[:], R0[:], AF.Exp, bias=ngb[:, 2 * d + 1:2 * d + 2], scale=-1.0)
                        mk.act(R0[:], R0[:], AF.Ln, bias=1.0, scale=1.0)
                        mk.ts("dve", R0[:], R0[:], -1.0, None, op0=ALU.mult)
                        mk.scan("dve", R2[:], ones4[:], R0[:], 0.0, ALU.mult, ALU.add)
                        mk.stt("dve", R1[:], R1[:], gb[:, 2 * d:2 * d + 1], R2[:], ALU.add, ALU.subtract)
                        mk.scan("dve", R0[:], ones4[:], R1[:], 0.0, ALU.mult, ALU.max)
                        mprev = v3(R0)[:, 0:35, 63:64].to_broadcast([4, 35, 64])
                        mk.copy("dve", R3[:, 0:64], R1[:, 0:64])
                        mk.tt("dve", v3(R3)[:, 1:36, :], v3(R1)[:, 1:36, :], mprev, ALU.subtract)
                        mk.act(R3[:], R3[:], AF.Exp)
                        mk.ts("dve", R4[:, 0:64], R0[:, 0:64], -1.0, None, op0=ALU.mult)
                        mk.tt("dve", v3(R4)[:, 1:36, :], mprev, v3(R0)[:, 1:36, :], ALU.subtract)
                        mk.act(R4[:], R4[:], AF.Exp)
                        mk.tt("dve", R2[:], R2[:], R0[:], ALU.add)
                        mk.act(R2[:], R2[:], AF.Exp, scale=-1.0)
                        mk.tt("dve", DECr[:], v3(R4)[:, :, 63:64].to_broadcast([4, 36, 4]),
                              ident[0:4, 0:4].unsqueeze(1).to_broadcast([4, 36, 4]), ALU.mult)
                        mk.mm(PS[6][:, 0:144], onesf[0:4, :], DECr[:].rearrange("p c h -> p (c h)"))
                        mk.copy("dve", DECb[:].rearrange("p c h -> p (c h)"), PS[6][:, 0:144])
                        if d == 0:
                            NAT = [R3, R4, R2]
                        else:
                            rev_copy("dve", R0, R3)
                            rev_copy("pool", R1, R4)
                            rev_copy("dve", R3, R2)
                            NAT = [R0, R1, R3]
                        for qi, arr in enumerate(NAT):
                            for t in range(18):
                                o_ = (qi * 18 + t) * 4
                                mk.tr(PS[5][:, o_:o_ + 4], arr[:, t * 128:(t + 1) * 128], ident[0:4, 0:4])
                        mk.copy("dve", COL[:].rearrange("p a t h -> p (a t h)"), PS[5][:, 0:216])
                        mk.tt("pool", uV[:], vA[:], COL[:, 0].unsqueeze(3).to_broadcast([128, 18, 4, 129]), ALU.mult)
                        mk.memset("dve", CT[:], 0.0)
                        mk.memset("pool", CTb[0][:], 0.0)
                        mask = maskF if d == 0 else maskB
                        cur_tile = -1
                        for cs in range(36):
                            c = nat_chunk(d, cs)
                            tile, par = c // 2, c % 2
                            rows = slice(par * 64, par * 64 + 64)
                            ctb_cur, ctb_nxt = CTb[cs % 2], CTb[(cs + 1) % 2]
                            if tile != cur_tile:
                                cur_tile = tile
                                sm = smb[tile % 2]
                                for h in range(4):
                                    mk.mm(PS[4][:, h * 128:(h + 1) * 128], kT[:, h, tile * 128:(tile + 1) * 128],
                                          qT[:, h, tile * 128:(tile + 1) * 128])
                                mk.tt("dve", sm[:], PS[4][:].rearrange("p (h j) -> p h j", h=4),
                                      mask[:].unsqueeze(1).to_broadcast([128, 4, 128]), ALU.mult)
                            for h in range(4):
                                o_ = PS[h // 2][rows, (h % 2) * 256:(h % 2) * 256 + 129]
                                mk.mm(o_, sm[:, h, par * 64:par * 64 + 64], uV[:, tile, h, :], start=True, stop=False)
                                mk.mm(o_, qT[:, h, c * 64:(c + 1) * 64], ctb_cur[:, h, :], start=False, stop=True)
                            for h in range(4):
                                mk.mm(PS[2 + h // 2][:, (h % 2) * 256:(h % 2) * 256 + 129],
                                      kK[rows, tile, h * 128:(h + 1) * 128], uV[rows, tile, h, :])
                            for b in range(2):
                                mk.tt("dve", tmpC[:, 2 * b:2 * b + 2, :],
                                      PS[2 + b][:, 0:512].rearrange("p (h v) -> p h v", h=2)[:, :, 0:129], CT[:, 2 * b:2 * b + 2, :], ALU.add)
                            mk.tt("pool", CT[:], tmpC[:], DECb[:, cs, :].unsqueeze(2).to_broadcast([128, 4, 129]), ALU.mult)
                            mk.copy("act", ctb_nxt[:], CT[:])
                            t4 = t4b[cs % 2]
                            for b in range(2):
                                mk.tt("dve", t4[rows, 2 * b:2 * b + 2], PS[b][rows, 128:512:256],
                                      COL[rows, 1, tile, 2 * b:2 * b + 2], ALU.mult)
                            mk.stt("dve", t4[rows, :], t4[rows, :], -1.0, t4[rows, :], ALU.mult, ALU.max)
                            mk.tt("dve", t4[rows, :], t4[rows, :], COL[rows, 2, tile, :], ALU.max)
                            mk.recip(t4[rows, :], t4[rows, :])
                            mk.tt("dve", t4[rows, :], t4[rows, :], COL[rows, 1, tile, :], ALU.mult)
                            for b in range(2):
                                src = PS[b][rows, 0:512].rearrange("p (h v) -> p h v", h=2)[:, :, 0:128]
                                sc = t4[rows, 2 * b:2 * b + 2].unsqueeze(2).to_broadcast([64, 2, 128])
                                hs = HS[rows, tile, 2 * b * 128:(2 * b + 2) * 128].rearrange("p (h v) -> p h v", h=2)
                                if d == 0:
                                    mk.tt("dve", hs, src, sc, ALU.mult)
                                else:
                                    to = tmo[cs % 2][rows, 2 * b:2 * b + 2, :]
                                    mk.tt("dve", to, src, sc, ALU.mult)
                                    mk.tt("pool", hs, hs, to, ALU.add)
                with mk.scope():
                    sgo = mk.sbuf("msgo", [128, 18, 512], BF16)
                    sqh = mk.sbuf("msqh", [128, 18, 512], F32)
                    ML = mk.sbuf("mML", [128, 18, 512], BF16)
                    ss = mk.sbuf("mss", [128, 72], F32)
                    stg = [mk.sbuf("mstg%d" % i, [128, NS], BF16) for i in range(2)]
                    load_seq_K(sgo, K_MLO, 512, s)
                    finish_branch(HS, sgo, gvn, sqh, ss, ML)
                    tok_to_BR(ML, 0, s, stg)

    def finish_branch(HS, gate, gvn, sqh, ss, OUT):
        h3 = lambda t: t[:].rearrange("p t (h v) -> p (t h) v", v=128)
        mk.tt("pool", sqh[:], HS[:], HS[:], ALU.mult)
        mk.reduce("dve", ss[:], h3(sqh), ALU.add)
        mk.act(ss[:], ss[:], AF.Sqrt, bias=EPS, scale=1.0 / 128.0)
        mk.recip(ss[:], ss[:])
        mk.tt("dve", h3(sqh), h3(HS), ss[:].unsqueeze(2).to_broadcast([128, 72, 128]), ALU.mult)
        mk.tt("pool", h3(sqh), h3(sqh), gvn[:].unsqueeze(1).to_broadcast([128, 72, 128]), ALU.mult)
        mk.tt("dve", OUT[:], sqh[:], gate[:], ALU.mult)

    def phase_gla(li):
        with mk.scope():
            OS = mk.sbuf("gOS", [128, 18, 512], F32)
            gvn = mk.sbuf("ggvn", [128, 128], F32)
            wa = mk.sbuf("gwa", [16, 2, 256], F32)
            nba = mk.sbuf("gnba", [128, 2, 2], F32)
            RST = mk.sbuf("gRST", [128, NS], F32)
            bcast_row(gvn[:], gl_ng[li:li + 1, :])
            mk.dma(wa[:], gl_wa[li].rearrange("d r n -> r d n"))
            mk.dma(nba[:], gl_baT[li].rearrange("d p h -> p d h"))
            mk.ts("dve", nba[:], nba[:], -1.0, None, op0=ALU.mult)
            mk.memset("pool", RST[:], 1.0)
            mk.memset("pool", RST[:].rearrange("p (c j) -> p c j", j=64)[:, :, 0:1], 0.0)
            for s in range(2):
                with mk.scope():
                    qT = mk.sbuf("gqT", [128, 2, NS], BF16)
                    kT = mk.sbuf("gkT", [128, 2, NS], BF16)
                    vK = mk.sbuf("gvK", [128, 18, 512], BF16)
                    aT = mk.sbuf("gaT", [16, 2, NS], F32)
                    LA = mk.sbuf("gLA", [128, 2, NS], F32)
                    Pc = mk.sbuf("gPc", [128, 2, NS], F32)
                    qg = mk.sbuf("gqg", [128, 2, NS], BF16)
                    kg = mk.sbuf("gkg", [128, 2, NS], BF16)
                    kg2 = mk.sbuf("gkg2", [128, 2, NS], BF16)
                    kgK = mk.sbuf("gkgK", [128, 18, 256], BF16)
                    Tc = mk.sbuf("gTc", [128, 2, 36], F32)
                    eb = mk.sbuf("geb", [128, 2, 36], F32)
                    S = mk.sbuf("gS", [128, 2, 128], F32)
                    Sb = [mk.sbuf("gSb%d" % i, [128, 4, 128], BF16) for i in range(2)]
                    amb = [mk.sbuf("gam%d" % i, [128, 4, 128], BF16) for i in range(2)]
                    for hh in range(2):
                        load_seq_T(qT[:, hh, :], PT[T_GLQ + hh * 128:T_GLQ + (hh + 1) * 128, :], s, q="sp")
                        load_seq_T(kT[:, hh, :], PT[T_GLK + hh * 128:T_GLK + (hh + 1) * 128, :], s, q="act")
                    load_seq_K(vK, K_GLV, 512, s)
                    for d in range(2):
                        load_seq_T(aT[:, d, :], GT[16 + d * 16:32 + d * 16, :], s)
                    v4 = lambda t: t[:].rearrange("p h (c j) -> p h c j", j=64)
                    for d in range(2):
                        ci = 0
                        for hh in range(2):
                            for t0 in range(0, NS, 512):
                                n = min(512, NS - t0)
                                pb = PS[5 + ci % 2]
                                ci += 1
                                mk.mm(pb[:, 0:n], wa[:, d, hh * 128:(hh + 1) * 128], aT[:, d, t0:t0 + n])
                                mk.act(LA[:, hh, t0:t0 + n], pb[:, 0:n], AF.Exp, bias=nba[:, d, hh:hh + 1], scale=-1.0)
                        mk.act(LA[:], LA[:], AF.Ln, bias=1.0, scale=1.0)
                        mk.ts("dve", LA[:], LA[:], -1.0 / 16.0, None, op0=ALU.mult)
                        for hh in range(2):
                            mk.scan("dve", Pc[:, hh, :], RST[:], LA[:, hh, :], 0.0, ALU.mult, ALU.add)
                        mk.copy("dve", Tc[:], v4(Pc)[:, :, :, 63])
                        mk.act(eb[:], Tc[:], AF.Exp)
                        if d == 1:
                            mk.tt("dve", v4(Pc), Tc[:].unsqueeze(3).to_broadcast([128, 2, 36, 64]), v4(Pc), ALU.subtract)
                            mk.tt("pool", Pc[:], Pc[:], LA[:], ALU.add)
                        mk.act(LA[:], Pc[:], AF.Exp)
                        mk.tt("dve", qg[:], qT[:], LA[:], ALU.mult)
                        mk.act(LA[:], Pc[:], AF.Exp, scale=-1.0)
                        mk.tt("dve", kg[:], kT[:], LA[:], ALU.mult)
                        mk.tt("pool", v4(kg2), v4(kg), eb[:].unsqueeze(3).to_broadcast([128, 2, 36, 64]), ALU.mult)
                        import os as _osg
                        _gs = _osg.environ.get("GLA_STOP", "")
                        if _gs == "1":
                            return
                        for hh in range(2):
                            for t in range(18):
                                pv = PS[5 + t % 2][:, (t % 4) * 128:(t % 4) * 128 + 128]
                                mk.mm(pv, kg2[:, hh, t * 128:(t + 1) * 128], identb[:])
                                mk.copy(evac_eng(), kgK[:, t, hh * 128:(hh + 1) * 128], pv)
                        if _gs == "2":
                            return
                        mk.memset("dve", S[:], 0.0)
                        mk.memset("pool", Sb[0][:], 0.0)
                        mk.memset("pool", Sb[1][:], 0.0)
                        mask = maskF if d == 0 else maskB
                        cur_tile = -1
                        for cs in range(36):
                            c = nat_chunk(d, cs)
                            tile, par = c // 2, c % 2
                            rows = slice(par * 64, par * 64 + 64)
                            sb_cur, sb_nxt = Sb[cs % 2], Sb[(cs + 1) % 2]
                            if tile != cur_tile:
                                cur_tile = tile
                                am = amb[tile % 2]
                                for h in range(4):
                                    hr = slice((h % 2) * 64, (h % 2) * 64 + 64)
                                    mk.mm(PS[4 + h % 2][:, (h // 2) * 128:(h // 2 + 1) * 128],
                                          kg[hr, h // 2, tile * 128:(tile + 1) * 128],
                                          qg[hr, h // 2, tile * 128:(tile + 1) * 128])
                                for wh in range(2):
                                    mk.tt("dve", am[:, wh::2, :], PS[4 + wh][:, 0:256].rearrange("p (h j) -> p h j", h=2),
                                          mask[:].unsqueeze(1).to_broadcast([128, 2, 128]), ALU.mult)
                            _lv = int(_gs) if _gs else 9
                            if _lv >= 4:
                                for h in range(4):
                                    o_ = PS[h // 2][rows, (h % 2) * 128:(h % 2) * 128 + 128]
                                    mk.mm(o_, am[:, h, par * 64:par * 64 + 64], vK[:, tile, h * 128:(h + 1) * 128],
                                          start=True, stop=False)
                                    mk.mm(o_, qg[:, h // 2, c * 64:(c + 1) * 64], sb_cur[:, h, :], start=False, stop=True)
                            if _lv >= 5:
                                for h in range(4):
                                    hh, wh = h // 2, h % 2
                                    mk.mm(PS[2 + hh][:, wh * 128:(wh + 1) * 128], kgK[rows, tile, hh * 128:(hh + 1) * 128],
                                          vK[rows, tile, h * 128:(h + 1) * 128])
                            if _lv >= 6:
                                for h in range(4):
                                    hh, wh = h // 2, h % 2
                                    hr = slice(wh * 64, wh * 64 + 64)
                                    mk.stt("dve", S[hr, hh, :], S[hr, hh, :], eb[hr, hh, c:c + 1],
                                           PS[2 + hh][hr, wh * 128:(wh + 1) * 128], ALU.mult, ALU.add)
                            if _lv >= 7:
                                for wh in range(2):
                                    hr = slice(wh * 64, wh * 64 + 64)
                                    mk.copy("act", sb_nxt[hr, wh::2, :], S[hr, :, :])
                            if _lv >= 4:
                                for b in range(2):
                                    dst = OS[rows, tile, b * 256:(b + 1) * 256]
                                    if d == 0:
                                        mk.copy("dve", dst, PS[b][rows, 0:256])
                                    else:
                                        mk.tt("dve", dst, dst, PS[b][rows, 0:256], ALU.add)
                with mk.scope():
                    sg = mk.sbuf("gsg", [128, 18, 512], BF16)
                    sqh = mk.sbuf("gsqh", [128, 18, 512], F32)
                    GLo = mk.sbuf("gGLo", [128, 18, 512], BF16)
                    ss = mk.sbuf("gss", [128, 72], F32)
                    stg = [mk.sbuf("gstg%d" % i, [128, NS], BF16) for i in range(2)]
                    load_seq_K(sg, K_GLG, 512, s)
                    finish_branch(OS, sg, gvn, sqh, ss, GLo)
                    tok_to_BR(GLo, 1024, s, stg)

    phase_load_x()
    phase_mod()
    order = ["norm1", "proj", "mlstm", "attn", "gla", "merge", "moe"]
    lim = order.index(stop_after) if stop_after else len(order)
    for li in range(n_layers):
        with mk.scope():
            hT = mk.sbuf("hT", [128, 8, NTOK], BF16)
            with mk.scope():
                scratch = ([mk.sbuf("nxs%d" % i, [128, 8, 512], F32) for i in range(2)],
                           [mk.sbuf("nsq%d" % i, [128, 8, 512], BF16) for i in range(2)],
                           [mk.sbuf("nrs%d" % i, [128, 512], F32) for i in range(2)])
                norm_groups(li, 0, hT, scratch)
            if dbg:
                hT_dbg = mk.dram("hT_dbg%d" % li, [D, NTOK], BF16, kind="ExternalOutput")
                mk.dma(hT_dbg[:, :].rearrange("(kc p) t -> p kc t", p=128), hT[:])
            if lim >= 1:
                with mk.scope():
                    phase_proj(li, hT)
        if lim >= 2 and "mlstm" not in skip:
            phase_mlstm(li)
        if lim >= 3 and "attn" not in skip:
            phase_attn(li)
        if lim >= 4 and "gla" not in skip:
            phase_gla(li)
        if lim >= 5:
            phase_merge(li)
        if lim >= 6 and do_moe:
            phase_moe(li)
    if lim >= 6:
        phase_final()
    if dbg:
        modv_dbg = mk.dram("modv_dbg", [128, 2 * 6 * 8 * 3], F32, kind="ExternalOutput")
        mk.dma(modv_dbg[:, :], modv[:].rearrange("p a b c d -> p (a b c d)"))
    mk.finish()
    return nc, mk


def make_consts():
    c = np.zeros((128, 384 + 2 * SEQ), np.float32)
    c[:, 0:128] = np.eye(128, dtype=np.float32)
    i = np.arange(128)[:, None]
    j = np.arange(128)[None, :]
    same = (i // 64) == (j // 64)
    c[:, 128:256] = (same & (i <= j)).astype(np.float32)
    c[:, 256:384] = (same & (i >= j)).astype(np.float32)
    t = np.arange(SEQ)
    rowp = (t // 64).astype(np.float32)
    colp = (t % 64).astype(np.float32)
    inv = (10000.0 ** (-np.arange(16, dtype=np.float32) / 16)).astype(np.float32)
    for d in range(128):
        dd = d % 64
        axis = dd // 32
        f = dd % 16
        half = (dd % 32) // 16
        ang = (rowp if axis == 0 else colp) * inv[f]
        c[d, 384:384 + SEQ] = np.cos(ang)
        c[d, 384 + SEQ:384 + 2 * SEQ] = np.sin(ang) * (-1.0 if half == 0 else 1.0)
    return c


def pcol(v, nch):
    return np.ascontiguousarray(np.asarray(v, np.float32).reshape(nch, 128).T)


def prep_shared(inp):
    sh = {}
    f = lambda a: np.ascontiguousarray(np.asarray(a, np.float32))
    sh["w_mod"] = f(inp["w_mod"])
    sh["b_modT"] = np.stack([pcol(inp["b_mod"][li], 48) for li in range(2)])
    sh["gmixT"] = np.stack([pcol(inp["norm_mix_g"][li], 8) for li in range(2)])
    sh["gffnT"] = np.stack([pcol(inp["norm_ffn_g"][li], 8) for li in range(2)])
    sh["w_in"] = f(inp["w_in"])
    sh["ml_gb"] = f(np.asarray(inp["ml_gate_b"]).reshape(2, 4, 4).transpose(0, 2, 1))
    sh["ml_ng"] = f(inp["ml_norm_g"])
    sh["df_lam"] = f(inp["df_lambda"])
    sh["df_ng"] = f(inp["df_norm_g"])
    sh["gl_wa"] = f(inp["gl_w_alpha"])
    sh["gl_baT"] = f(np.asarray(inp["gl_b_alpha"]).reshape(2, 2, 2, 128).transpose(0, 1, 3, 2))
    sh["gl_ng"] = f(inp["gl_norm_g"])
    sh["w_branch"] = f(inp["w_branch"])
    sh["w_out"] = f(inp["w_out"])
    sh["router_w"] = f(np.concatenate([inp["router_group_w"], inp["router_expert_w"]], axis=-1))
    sh["router_b"] = f(np.concatenate([inp["router_group_b"], inp["router_expert_b"]], axis=-1))
    sh["moe_wg"] = f(inp["moe_w_gate"])
    sh["moe_wu"] = f(inp["moe_w_up"])
    sh["moe_wd"] = f(inp["moe_w_down"])
    sh["fin_gT"] = pcol(inp["final_norm_g"], 8)
    sh["consts"] = make_consts()
    return sh


def prep_core(inp, core):
    b0 = 2 * core
    m = {}
    m["x2"] = np.ascontiguousarray(np.asarray(inp["x"][b0:b0 + 2], np.float32))
    m["ctx2"] = np.ascontiguousarray(np.asarray(inp["ctx"][b0:b0 + 2], np.float32))
    cs = np.stack([np.asarray(inp["c_ctx"], np.float32), np.asarray(inp["c"][b0], np.float32),
                   np.asarray(inp["c"][b0 + 1], np.float32)], axis=-1)
    m["cT"] = np.ascontiguousarray(cs.reshape(8, 128, 3).transpose(1, 0, 2))
    return m


_CACHE = {}
CORES_PER_LAUNCH = 1


def kernel(**inputs):
    if "nc" not in _CACHE:
        _CACHE["nc"] = build()[0]
    nc = _CACHE["nc"]
    sh = prep_shared(inputs)
    in_maps = []
    for core in range(8):
        m = dict(sh)
        m.update(prep_core(inputs, core))
        in_maps.append(m)
    outs = []
    for g0 in range(0, 8, CORES_PER_LAUNCH):
        res = run_bass_kernel_spmd(nc, in_maps[g0:g0 + CORES_PER_LAUNCH], core_ids=list(range(CORES_PER_LAUNCH)))
        outs.extend(np.asarray(r["y_out"], np.float32) for r in res.results)
    return np.concatenate(outs, axis=0)
```

```python
import contextlib
import math
import numpy as np
import concourse.bass as bass
import concourse.mybir as mybir
from concourse.bass_utils import run_bass_kernel_spmd

F32 = mybir.dt.float32
BF16 = mybir.dt.bfloat16
AF = mybir.ActivationFunctionType
ALU = mybir.AluOpType
AX = mybir.AxisListType

COMPUTE = ("pe", "act", "dve", "pool")


class _Op:
    __slots__ = ("eng", "fn", "deps", "marked", "val", "dma", "dsem", "dval", "qwait", "emitted", "seq")

    def __init__(self, eng, fn):
        self.eng = eng
        self.fn = fn
        self.deps = []
        self.marked = False
        self.val = None
        self.dma = False
        self.dsem = None
        self.dval = None
        self.qwait = None
        self.emitted = False


class MK:
    def __init__(self, nc, n_dma_sems=8):
        self.nc = nc
        self.stacks = [contextlib.ExitStack()]
        self.e = {"pe": nc.tensor, "act": nc.scalar, "dve": nc.vector, "pool": nc.gpsimd, "sp": nc.sync}
        self.ops = {k: [] for k in self.e}
        self.pstep = {}
        self.notrack = set()
        self.psum_names = set()
        self.recs = {}
        self.n_dma_sems = n_dma_sems
        self.dma_rr = {k: 0 for k in self.e}
        self.dma_last = {}
        self.sem = {k: nc.alloc_semaphore("s_" + k) for k in COMPUTE}
        self.dsems = {}
        for k in ("sp", "pool", "act"):
            for s in range(n_dma_sems):
                self.dsems[(k, s)] = nc.alloc_semaphore("d_%s_%d" % (k, s))
        self.cnt = {k: 0 for k in self.e}
        self.dcnt = {}
        self.seen = {k: {} for k in self.e}
        self.last_op = {k: None for k in self.e}
        self.n_inst = 0
        self.trace = {k: [] for k in self.e}

    def sbuf(self, name, shape, dtype):
        self.uid = getattr(self, "uid", 0) + 1
        name = "%s_%d" % (name, self.uid)
        t = self.stacks[-1].enter_context(self.nc.sbuf_tensor(name, list(shape), dtype))
        self.pstep[name] = int(np.prod(shape[1:]))
        self.recs.pop(name, None)
        return t

    def psum(self, name, shape, dtype=F32):
        t = self.stacks[-1].enter_context(self.nc.psum_tensor(name, list(shape), dtype))
        self.pstep[name] = int(np.prod(shape[1:]))
        self.psum_names.add(name)
        return t

    def dram(self, name, shape, dtype, kind="Internal", rowlen=None):
        t = self.nc.dram_tensor(name, list(shape), dtype, kind=kind)
        self.pstep[name] = int(rowlen if rowlen is not None else shape[-1])
        if kind == "ExternalInput":
            self.notrack.add(name)
        return t.ap()

    @contextlib.contextmanager
    def scope(self):
        self.stacks.append(contextlib.ExitStack())
        try:
            yield
            self.flush()
            self.barrier()
            self.flush()
        finally:
            self.stacks.pop().close()

    def _box(self, ap):
        name = ap.name
        ps = self.pstep[name]
        off = int(ap.offset)
        p0, f0 = divmod(off, ps)
        plo = phi = p0
        flo = fhi = f0
        for (step, cnt) in ap.ap:
            ext = step * (cnt - 1)
            if step != 0 and abs(step) >= ps and step % ps == 0:
                e = ext // ps
                if e < 0:
                    plo += e
                else:
                    phi += e
            else:
                if ext < 0:
                    flo += ext
                else:
                    fhi += ext
        return name, plo, phi, flo, fhi

    def _access(self, op, ap, is_write):
        if ap.name in self.notrack:
            return
        name, plo, phi, flo, fhi = self._box(ap)
        excl = name in self.psum_names
        if excl:
            flo, fhi = 0, self.pstep[name] - 1
            plo, phi = (plo // 32) * 32, (phi // 32) * 32 + 31
        lst = self.recs.get(name, [])
        keep = []
        for r in lst:
            ov = not (r[1] < plo or phi < r[0] or r[3] < flo or fhi < r[2])
            o = r[4]
            if ov and o is not op:
                same = (o.eng == op.eng) and not o.dma and not op.dma
                if same:
                    need = (op.eng != "pe") and r[5] and not is_write
                else:
                    need = excl or is_write or r[5]
                if need:
                    op.deps.append(o)
            contained = (plo <= r[0] and r[1] <= phi and flo <= r[2] and r[3] <= fhi)
            if contained and o is not op:
                same = (o.eng == op.eng) and not o.dma and not op.dma
                if excl or is_write or ((not r[5]) and same):
                    if not (excl and same and r[5] and not is_write and False):
                        continue
            keep.append(r)
        keep.append([plo, phi, flo, fhi, op, is_write])
        self.recs[name] = keep

    def _record(self, eng, fn, reads, writes, dma=False):
        op = _Op(eng, fn)
        op.dma = dma
        for ap in reads:
            if ap is not None and hasattr(ap, "ap"):
                self._access(op, ap, False)
        for ap in writes:
            if ap is not None and hasattr(ap, "ap"):
                self._access(op, ap, True)
        self.gseq = getattr(self, "gseq", 0) + 1
        op.seq = self.gseq
        if op.deps:
            best = {}
            keep = []
            for d_ in op.deps:
                if d_.dma:
                    keep.append(d_)
                else:
                    b_ = best.get(d_.eng)
                    if b_ is None or d_.seq > b_.seq:
                        best[d_.eng] = d_
            op.deps = keep + list(best.values())
        if dma:
            slot = self.dma_rr[eng]
            self.dma_rr[eng] = (slot + 1) % (2 if eng == "pool" else self.n_dma_sems)
            op.dsem = (eng, slot)
            op.qwait = self.dma_last.get((eng, slot))
            self.dma_last[(eng, slot)] = op
            self.dcnt[op.dsem] = self.dcnt.get(op.dsem, 0) + 16
            op.dval = self.dcnt[op.dsem]
        self.ops[eng].append(op)
        self.last_op[eng] = op
        return op

    def mm(self, out, lhsT, rhs, start=True, stop=True):
        return self._record("pe", lambda: self.nc.tensor.matmul(out, lhsT, rhs, start=start, stop=stop),
                            [lhsT, rhs], [out])

    def tr(self, out, in_, ident):
        return self._record("pe", lambda: self.nc.tensor.transpose(out, in_, ident), [in_, ident], [out])

    def act(self, out, in_, func, bias=None, scale=None, accum_out=None):
        kw = {}
        if bias is not None:
            kw["bias"] = bias
        if scale is not None:
            kw["scale"] = scale
        if accum_out is not None:
            kw["accum_out"] = accum_out
        return self._record("act", lambda: self.nc.scalar.activation(out, in_, func, **kw),
                            [in_, bias, scale], [out, accum_out])

    def tt(self, eng, out, in0, in1, op):
        return self._record(eng, lambda: self.e[eng].tensor_tensor(out, in0, in1, op), [in0, in1], [out])

    def ts(self, eng, out, in0, s1, s2=None, op0=ALU.mult, op1=None):
        kw = {}
        if op1 is not None:
            kw["op1"] = op1
        return self._record(eng, lambda: self.e[eng].tensor_scalar(out, in0, s1, s2, op0, **kw),
                            [in0, s1, s2], [out])

    def stt(self, eng, out, in0, scalar, in1, op0, op1):
        return self._record(eng, lambda: self.e[eng].scalar_tensor_tensor(out, in0, scalar, in1, op0, op1),
                            [in0, scalar, in1], [out])

    def copy(self, eng, out, in_):
        if eng == "act":
            return self._record("act", lambda: self.nc.scalar.copy(out, in_), [in_], [out])
        return self._record(eng, lambda: self.e[eng].tensor_copy(out, in_), [in_], [out])

    def memset(self, eng, ap, val):
        return self._record(eng, lambda: self.e[eng].memset(ap, val), [], [ap])

    def reduce(self, eng, out, in_, op, axis=AX.X):
        return self._record(eng, lambda: self.e[eng].tensor_reduce(out, in_, axis, op), [in_], [out])

    def scan(self, eng, out, d0, d1, init, op0, op1):
        return self._record(eng, lambda: self.e[eng].tensor_tensor_scan(out, d0, d1, init, op0, op1),
                            [d0, d1, init], [out])

    def recip(self, out, in_):
        return self._record("dve", lambda: self.nc.vector.reciprocal(out, in_), [in_], [out])

    def max8(self, out, in_):
        return self._record("dve", lambda: self.nc.vector.max(out, in_), [in_], [out])

    def dma(self, out, in_, q="sp", **kw):
        return self._record(q, lambda: self.e[q].dma_start(out=out, in_=in_, **kw), [in_], [out], dma=True)

    def barrier(self):
        lasts = [self.last_op[k] for k in COMPUTE if self.last_op[k] is not None]
        dlast = list(self.dma_last.values())
        for k in self.e:
            op = _Op(k, None)
            op.deps = [o for o in lasts if o.eng != k] + dlast
            self.ops[k].append(op)

    def flush(self):
        for k in self.ops:
            for op in self.ops[k]:
                for d in op.deps:
                    d.marked = True
        for k in self.ops:
            lst = self.ops[k]
            if not lst:
                continue
            if k in COMPUTE:
                real = [o for o in lst if o.fn is not None and not o.dma]
                if real:
                    real[-1].marked = True
                c = self.cnt[k]
                for op in lst:
                    if op.fn is not None and not op.dma and op.marked:
                        c += 1
                        op.val = c
                nxt = None
                for op in reversed(lst):
                    if op.fn is None or op.dma:
                        continue
                    if op.marked:
                        nxt = op.val
                    else:
                        op.val = nxt
        for k in self.ops:
            eng = self.e[k]
            seen = self.seen[k]
            for op in self.ops[k]:
                waits = {}
                for d in op.deps:
                    if d.dma:
                        key = ("d",) + d.dsem
                        s, v = self.dsems[d.dsem], d.dval
                    else:
                        if d.fn is None:
                            continue
                        key = ("c", d.eng)
                        s, v = self.sem[d.eng], d.val
                    if seen.get(key, 0) >= v:
                        continue
                    if key not in waits or waits[key][1] < v:
                        waits[key] = (s, v)
                if op.dma and op.qwait is not None:
                    key = ("d",) + op.dsem
                    v = op.qwait.dval
                    if seen.get(key, 0) < v and (key not in waits or waits[key][1] < v):
                        waits[key] = (self.dsems[op.dsem], v)
                for key, (s, v) in waits.items():
                    eng.wait_ge(s, v)
                    seen[key] = v
                    self.trace[k].append(("w", key, v))
                if op.fn is None:
                    continue
                ins = op.fn()
                self.n_inst += 1
                op.fn = True
                if op.dma:
                    ins.then_inc(self.dsems[op.dsem], 16)
                    self.trace[k].append(("i", ("d",) + op.dsem, 16))
                elif op.marked:
                    ins.then_inc(self.sem[k], 1)
                    self.cnt[k] = op.val
                    self.trace[k].append(("i", ("c", k), 1))
                else:
                    self.trace[k].append(("n", None, 0))
            self.ops[k] = []

    def simulate(self):
        sem = {}
        pos = {k: 0 for k in self.trace}
        progress = True
        while progress:
            progress = False
            for k, tr in self.trace.items():
                while pos[k] < len(tr):
                    typ, key, v = tr[pos[k]]
                    if typ == "w":
                        if sem.get(key, 0) < v:
                            break
                    elif typ == "i":
                        sem[key] = sem.get(key, 0) + v
                    pos[k] += 1
                    progress = True
        stuck = {k: (pos[k], len(tr), tr[pos[k]] if pos[k] < len(tr) else None) for k, tr in self.trace.items()}
        return all(pos[k] == len(tr) for k, tr in self.trace.items()), stuck, sem

    def finish(self):
        self.flush()
        for (k, slot), op in self.dma_last.items():
            self.e[k].wait_ge(self.dsems[(k, slot)], op.dval)
        self.stacks[0].close()


D = 1024
NTOK = 4608
NG = 9
SEQ = 2048
CTX = 256
NS = 2304
IN_W = 8240
EPS = 1e-6
C_MLQ, C_MLK, C_MLV, C_MLO, C_MLG = 0, 512, 1024, 1536, 2048
C_DFQ, C_DFK, C_DFV = 2064, 2576, 3088
C_GLQ, C_GLK, C_GLV, C_GLG, C_GLA = 3600, 3856, 4112, 4624, 5136
C_GATE = 5168
T_MLQ, T_MLK, T_DFQ, T_DFK, T_GLQ, T_GLK, T_GATE = 0, 512, 1024, 1536, 2048, 2304, 2560
T_ROWS = 2560 + 3072
K_MLK, K_MLV, K_MLO, K_DFV, K_GLK, K_GLV, K_GLG = 0, 512, 1024, 1536, 2048, 2304, 2816
K_COLS = 3328


def seq_ranges(s):
    return 256 * s, 512 + 2048 * s


def build(n_layers=2, stop_after=None, dbg=False, do_moe=True, skip=()):
    nc = bass.Bass("TRN2", target_bir_lowering=False)
    mk = MK(nc)
    IN = {}

    def din(name, shape, dtype=F32):
        IN[name] = mk.dram(name, shape, dtype, kind="ExternalInput")
        return IN[name]

    x2 = din("x2", [2, SEQ, D])
    ctx2 = din("ctx2", [2, CTX, D])
    cT = din("cT", [128, 8, 3])
    w_mod = din("w_mod", [2, D, 6 * D])
    b_modT = din("b_modT", [2, 128, 48])
    gmixT = din("gmixT", [2, 128, 8])
    gffnT = din("gffnT", [2, 128, 8])
    w_in = din("w_in", [2, D, IN_W])
    ml_gb = din("ml_gb", [2, 4, 4])
    ml_ng = din("ml_ng", [2, 128])
    df_lam = din("df_lam", [2, 4, 64])
    df_ng = din("df_ng", [2, 128])
    gl_wa = din("gl_wa", [2, 2, 16, 256])
    gl_baT = din("gl_baT", [2, 2, 128, 2])
    gl_ng = din("gl_ng", [2, 128])
    w_branch = din("w_branch", [2, 3, 512, D])
    w_out = din("w_out", [2, D, D])
    router_w = din("router_w", [2, D, 36])
    router_b = din("router_b", [2, 36])
    if do_moe:
        moe_wg = din("moe_wg", [2, 32, D, 512])
        moe_wu = din("moe_wu", [2, 32, D, 512])
        moe_wd = din("moe_wd", [2, 32, 512, D])
    fin_gT = din("fin_gT", [128, 8])
    consts = din("consts", [128, 128 + 128 + 128 + 2048 + 2048])

    okind = "ExternalOutput"
    y_out = mk.dram("y_out", [2, SEQ, D], F32, kind=okind)
    skind = "ExternalOutput" if dbg else "Internal"
    xT = mk.dram("xT", [D, NTOK], F32, kind=skind)
    PT = mk.dram("PT", [T_ROWS, NTOK], BF16, kind=skind)
    PK = mk.dram("PK", [NTOK, K_COLS], BF16, kind=skind)
    GT = mk.dram("GT", [48, NTOK], F32, kind=skind)
    BR = mk.dram("BR", [1536, NTOK], BF16, kind=skind)

    ident = mk.sbuf("ident", [128, 128], F32)
    identb = mk.sbuf("identb", [128, 128], BF16)
    onesb = mk.sbuf("onesb", [128, 128], BF16)
    onesf = mk.sbuf("onesf", [128, 128], F32)
    maskF = mk.sbuf("maskF", [128, 128], F32)
    maskB = mk.sbuf("maskB", [128, 128], F32)
    modv = mk.sbuf("modv", [128, 2, 6, 8, 3], F32)
    PS = [mk.psum("psb%d" % i, [128, 512], F32) for i in range(7)]
    PSB = mk.psum("psbf", [128, 1024], BF16)

    mk.dma(ident[:], consts[:, 0:128])
    mk.dma(maskF[:], consts[:, 128:256])
    mk.dma(maskB[:], consts[:, 256:384])
    mk.copy("dve", identb[:], ident[:])
    mk.memset("pool", onesb[:], 1.0)
    mk.memset("pool", onesf[:], 1.0)

    rr = {"ev": 0}

    def evac_eng():
        rr["ev"] ^= 1
        return "act" if rr["ev"] else "dve"

    def phase_load_x():
        with mk.scope():
            xin = [mk.sbuf("xin%d" % i, [128, D], F32) for i in range(2)]
            xst = [mk.sbuf("xst%d" % i, [128, 8, 512], F32) for i in range(2)]
            for g in range(NG):
                st = xst[g % 2]
                for t4 in range(4):
                    tt = g * 4 + t4
                    if tt < 4:
                        src = ctx2[tt // 2, (tt % 2) * 128:(tt % 2) * 128 + 128, :]
                    else:
                        u = tt - 4
                        src = x2[u // 16, (u % 16) * 128:(u % 16) * 128 + 128, :]
                    xi = xin[tt % 2]
                    mk.dma(xi[:], src)
                    for half in range(2):
                        pb = PS[(tt * 2 + half) % 4]
                        for k4 in range(4):
                            kc = half * 4 + k4
                            mk.tr(pb[:, k4 * 128:(k4 + 1) * 128], xi[:, kc * 128:(kc + 1) * 128], ident[:])
                        mk.copy(evac_eng(), st[:, half * 4:(half + 1) * 4, t4 * 128:(t4 + 1) * 128],
                                pb[:].rearrange("p (k t) -> p k t", k=4))
                mk.dma(xT[:, g * 512:(g + 1) * 512].rearrange("(kc p) t -> p kc t", p=128), st[:], q="act")

    def phase_mod():
        with mk.scope():
            cs = mk.sbuf("cs", [128, 8, 3], F32)
            csg = mk.sbuf("csg", [128, 8, 3], F32)
            wm = [mk.sbuf("wm%d" % i, [128, 8, 512], F32) for i in range(2)]
            modT = mk.sbuf("modT", [128, 48, 3], F32)
            bm = mk.sbuf("bm", [128, 48], F32)
            gm = mk.sbuf("gm", [128, 8], F32)
            gf = mk.sbuf("gf", [128, 8], F32)
            mk.dma(cs[:], cT[:, :, :])
            mk.act(csg[:], cs[:], AF.Sigmoid)
            mk.tt("dve", cs[:], cs[:], csg[:], ALU.mult)
            for li in range(n_layers):
                mk.dma(bm[:], b_modT[li])
                mk.dma(gm[:], gmixT[li])
                mk.dma(gf[:], gffnT[li])
                for blk in range(12):
                    w = wm[blk % 2]
                    mk.dma(w[:], w_in_view(w_mod[li], blk * 512, 512), q=("sp" if blk % 2 else "act"))
                    for c4 in range(4):
                        fc = blk * 4 + c4
                        pb = PS[fc % 4]
                        for kc in range(8):
                            mk.mm(pb[:, 0:3], w[:, kc, c4 * 128:(c4 + 1) * 128], cs[:, kc, :],
                                  start=(kc == 0), stop=(kc == 7))
                        mk.ts("dve", modT[:, fc, :], pb[:, 0:3], bm[:, fc:fc + 1], None, op0=ALU.add)
                mv = modv[:, li]
                for j in range(3):
                    mk.ts("dve", mv[:, 0, :, j], modT[:, 8:16, j], 1.0, None, op0=ALU.add)
                    mk.tt("dve", mv[:, 0, :, j], mv[:, 0, :, j], gm[:], ALU.mult)
                    mk.copy("dve", mv[:, 1, :, j], modT[:, 0:8, j])
                    mk.copy("dve", mv[:, 2, :, j], modT[:, 16:24, j])
                    mk.ts("dve", mv[:, 3, :, j], modT[:, 32:40, j], 1.0, None, op0=ALU.add)
                    mk.tt("dve", mv[:, 3, :, j], mv[:, 3, :, j], gf[:], ALU.mult)
                    mk.copy("dve", mv[:, 4, :, j], modT[:, 24:32, j])
                    mk.copy("dve", mv[:, 5, :, j], modT[:, 40:48, j])

    def w_in_view(w2d, c0, n):
        return w2d[:, c0:c0 + n].rearrange("(kc p) n -> p kc n", p=128)

    def gset(g):
        return 0 if g == 0 else (1 if g <= 4 else 2)

    def norm_groups(li, which_gs, hT, scratch, h32_cb=None):
        xs_b, sq_b, rs_b = scratch
        for g in range(NG):
            j = gset(g)
            xs = xs_b[g % 2]
            sq = sq_b[g % 2]
            rs = rs_b[g % 2]
            mk.dma(xs[:], xT[:, g * 512:(g + 1) * 512].rearrange("(kc p) t -> p kc t", p=128),
                   q=("sp" if g % 2 else "act"))
            mk.tt("pool", sq[:], xs[:], xs[:], ALU.mult)
            pb = PS[4 + g % 2]
            for kc in range(8):
                mk.mm(pb[:], onesb[:], sq[:, kc, :], start=(kc == 0), stop=(kc == 7))
            mk.act(rs[:], pb[:], AF.Sqrt, bias=EPS, scale=1.0 / D)
            mk.recip(rs[:], rs[:])
            for kc in range(8):
                mk.stt("dve", xs[:, kc, :], xs[:, kc, :], modv[:, li, which_gs, kc, j:j + 1], rs[:],
                       ALU.mult, ALU.mult)
                mk.act(hT[:, kc, g * 512:(g + 1) * 512], xs[:, kc, :], AF.Identity,
                       bias=modv[:, li, which_gs + 1, kc, j:j + 1], scale=1.0)
                if h32_cb is not None:
                    mk.ts("pool", xs[:, kc, :], xs[:, kc, :], modv[:, li, which_gs + 1, kc, j:j + 1], None,
                          op0=ALU.add)
            if h32_cb is not None:
                h32_cb(g, xs)

    def phase_proj(li, hT):
        cosT = mk.sbuf("cosT", [128, SEQ], F32)
        sinT = mk.sbuf("sinT", [128, SEQ], F32)
        mk.dma(cosT[:], consts[:, 384:384 + SEQ])
        mk.dma(sinT[:], consts[:, 384 + SEQ:384 + 2 * SEQ])
        wb = [mk.sbuf("wblk%d" % i, [128, 8, 512], BF16) for i in range(2)]
        stT = [mk.sbuf("stT%d" % i, [128, NTOK], BF16) for i in range(2)]
        stK = [mk.sbuf("stK%d" % i, [128, 4, 512], BF16) for i in range(2)]
        stG = mk.sbuf("stG", [16, NTOK], F32)
        wrot = mk.sbuf("wrot", [128, 8, 512], BF16)
        stG2 = mk.sbuf("stG2", [32, NTOK], F32)
        t1 = [mk.sbuf("rp1_%d" % i, [128, 512], F32) for i in range(2)]
        t2 = [mk.sbuf("rp2_%d" % i, [128, 512], F32) for i in range(2)]
        cnt = {"blk": 0, "ps": 0, "st": 0}

        def load_blk(c0, n):
            w = wb[cnt["blk"] % 2]
            cnt["blk"] += 1
            mk.dma(w[:, :, 0:n], w_in_view(w_in[li], c0, n), q="pool")
            return w

        def nextps():
            cnt["ps"] += 1
            return PS[cnt["ps"] % 4]

        tblocks = [
            (C_MLQ, 512, T_MLQ, "copy", 1.0),
            (C_MLK, 512, T_MLK, "copy", 128.0 ** -0.5),
            (C_DFQ, 512, T_DFQ, "rope", 1.0),
            (C_DFK, 512, T_DFK, "rope", 1.0),
            (C_GLQ, 256, T_GLQ, "copy", 0.125),
            (C_GLK, 256, T_GLK, "copy", 1.0),
        ] + [(C_GATE + i * 512, 512, T_GATE + i * 512, "sigmoid", 1.0) for i in range(6)]
        for (c0, n, r0, mode, scale) in tblocks:
            w = load_blk(c0, n)
            if mode == "rope":
                wv = w[:].rearrange("p k (b h e) -> p (k b) h e", h=2, e=16)
                rv = wrot[:].rearrange("p k (b h e) -> p (k b) h e", h=2, e=16)
                mk.copy("pool", rv[:, :, 0, :], wv[:, :, 1, :])
                mk.copy("pool", rv[:, :, 1, :], wv[:, :, 0, :])
            for cc in range(n // 128):
                st = stT[cnt["st"] % 2]
                cnt["st"] += 1
                for g in range(NG):
                    pb = nextps()
                    for kc in range(8):
                        mk.mm(pb[:], w[:, kc, cc * 128:(cc + 1) * 128], hT[:, kc, g * 512:(g + 1) * 512],
                              start=(kc == 0), stop=(kc == 7))
                    dst = st[:, g * 512:(g + 1) * 512]
                    if mode == "sigmoid":
                        mk.act(dst, pb[:], AF.Sigmoid)
                    elif mode == "rope" and g >= 1:
                        pr = nextps()
                        for kc in range(8):
                            mk.mm(pr[:], wrot[:, kc, cc * 128:(cc + 1) * 128], hT[:, kc, g * 512:(g + 1) * 512],
                                  start=(kc == 0), stop=(kc == 7))
                        tok0 = ((g - 1) % 4) * 512
                        a = t1[g % 2]
                        b = t2[g % 2]
                        mk.tt("dve", a[:], pb[:], cosT[:, tok0:tok0 + 512], ALU.mult)
                        mk.tt("dve", b[:], pr[:], sinT[:, tok0:tok0 + 512], ALU.mult)
                        mk.tt("pool", dst, a[:], b[:], ALU.add)
                    else:
                        e = evac_eng()
                        if e == "act":
                            mk.act(dst, pb[:], AF.Copy, scale=scale)
                        else:
                            mk.ts("dve", dst, pb[:], scale, None, op0=ALU.mult)
                mk.dma(PT[r0 + cc * 128:r0 + (cc + 1) * 128, :], st[:], q="sp")
        for (c0, n, r0) in [(C_MLG, 16, 0), (C_GLA, 32, 16)]:
            w = load_blk(c0, n)
            for g in range(NG):
                pb = nextps()
                for kc in range(8):
                    mk.mm(pb[0:n, :], w[:, kc, 0:n], hT[:, kc, g * 512:(g + 1) * 512],
                          start=(kc == 0), stop=(kc == 7))
                if r0 == 0:
                    mk.copy("dve", stG[0:16, g * 512:(g + 1) * 512], pb[0:16, :])
                else:
                    mk.copy("dve", stG2[:, g * 512:(g + 1) * 512], pb[0:32, :])
        mk.dma(GT[0:16, :], stG[0:16, :], q="sp")
        mk.dma(GT[16:48, :], stG2[:, :], q="sp")
        kblocks = [
            (C_MLK, 512, K_MLK, "copy", 128.0 ** -0.5),
            (C_MLV, 512, K_MLV, "copy", 1.0),
            (C_MLO, 512, K_MLO, "sigmoid", 1.0),
            (C_DFV, 512, K_DFV, "copy", 1.0),
            (C_GLK, 256, K_GLK, "copy", 1.0),
            (C_GLV, 512, K_GLV, "copy", 1.0),
            (C_GLG, 512, K_GLG, "silu", 1.0),
        ]
        sg = [mk.sbuf("sgk%d" % i, [128, 512], F32) for i in range(2)]
        for (c0, n, k0, mode, scale) in kblocks:
            w = load_blk(c0, n)
            for t4 in range(NTOK // 512):
                st = stK[cnt["st"] % 2]
                cnt["st"] += 1
                for q4 in range(4):
                    tt_ = t4 * 4 + q4
                    pb = nextps()
                    for kc in range(8):
                        mk.mm(pb[:, 0:n], hT[:, kc, tt_ * 128:(tt_ + 1) * 128], w[:, kc, 0:n],
                              start=(kc == 0), stop=(kc == 7))
                    dst = st[:, q4, 0:n]
                    if mode == "sigmoid":
                        mk.act(dst, pb[:, 0:n], AF.Sigmoid)
                    elif mode == "silu":
                        s_ = sg[tt_ % 2]
                        mk.act(s_[:, 0:n], pb[:, 0:n], AF.Sigmoid)
                        mk.tt("dve", dst, pb[:, 0:n], s_[:, 0:n], ALU.mult)
                    else:
                        e = evac_eng()
                        if e == "act":
                            mk.act(dst, pb[:, 0:n], AF.Copy, scale=scale)
                        else:
                            mk.ts("dve", dst, pb[:, 0:n], scale, None, op0=ALU.mult)
                mk.dma(PK[t4 * 512:(t4 + 1) * 512, k0:k0 + n].rearrange("(a p) c -> p a c", p=128),
                       st[:, :, 0:n], q="sp")

    def load_seq_T(dst, src_rows, s, q="sp"):
        c0, l0 = seq_ranges(s)
        mk.dma(dst[:, 0:CTX], src_rows[:, c0:c0 + CTX], q=q)
        mk.dma(dst[:, CTX:NS], src_rows[:, l0:l0 + SEQ], q=q)

    def store_seq_T(dst_rows, src, s, q="sp"):
        c0, l0 = seq_ranges(s)
        mk.dma(dst_rows[:, c0:c0 + CTX], src[:, 0:CTX], q=q)
        mk.dma(dst_rows[:, l0:l0 + SEQ], src[:, CTX:NS], q=q)

    def load_seq_K(dst3, k0, ncol, s, q="sp"):
        c0, l0 = seq_ranges(s)
        mk.dma(dst3[:, 0:2], PK[c0:c0 + CTX, k0:k0 + ncol].rearrange("(a p) c -> p a c", p=128), q=q)
        mk.dma(dst3[:, 2:18], PK[l0:l0 + SEQ, k0:k0 + ncol].rearrange("(a p) c -> p a c", p=128), q=q)

    def bcast_row(dst, src_row):
        mk.dma(dst, src_row.to_broadcast([128, src_row.shape[-1]]))

    def tok_to_BR(src, r0, s, stg):
        for h in range(4):
            st = stg[h % 2]
            for t4 in range(5):
                n = min(4, 18 - t4 * 4)
                pb = PS[t4 % 4]
                for j in range(n):
                    t = t4 * 4 + j
                    mk.mm(pb[:, j * 128:(j + 1) * 128], src[:, t, h * 128:(h + 1) * 128], identb[:])
                mk.copy(evac_eng(), st[:, t4 * 512:t4 * 512 + n * 128], pb[:, 0:n * 128])
            store_seq_T(BR[r0 + h * 128:r0 + (h + 1) * 128, :], st, s, q="act")

    def phase_attn(li):
        lam_init = 0.8 - 0.6 * math.exp(-0.3 * li)
        with mk.scope():
            lam_t = mk.sbuf("lam_t", [1, 4, 64], F32)
            lam_p = mk.sbuf("lam_p", [1, 2, 64], F32)
            lam_s = mk.sbuf("lam_s", [1, 4], F32)
            nlam = mk.sbuf("nlam", [128, 1], F32)
            gv = mk.sbuf("dfgv", [128, 128], F32)
            mk.dma(lam_t[:], df_lam[li:li + 1, :, :])
            mk.tt("dve", lam_p[:, 0, :], lam_t[:, 0, :], lam_t[:, 1, :], ALU.mult)
            mk.tt("dve", lam_p[:, 1, :], lam_t[:, 2, :], lam_t[:, 3, :], ALU.mult)
            mk.reduce("dve", lam_s[:, 0:2], lam_p[:], ALU.add)
            mk.act(lam_s[:, 0:2], lam_s[:, 0:2], AF.Exp)
            mk.tt("dve", lam_s[:, 2:3], lam_s[:, 0:1], lam_s[:, 1:2], ALU.subtract)
            mk.ts("dve", lam_s[:, 2:3], lam_s[:, 2:3], lam_init, -1.0, op0=ALU.add, op1=ALU.mult)
            mk.mm(PS[6][:, 0:1], onesf[0:1, :], lam_s[0:1, 2:3])
            mk.copy("dve", nlam[:], PS[6][:, 0:1])
            bcast_row(gv[:], df_ng[li:li + 1, :])
            mk.ts("dve", gv[:], gv[:], 1.0 - lam_init, None, op0=ALU.mult)
            qT = mk.sbuf("aqT", [128, 4, NS], BF16)
            kT = mk.sbuf("akT", [128, 4, NS], BF16)
            sq = mk.sbuf("asq", [128, 4, NS], BF16)
            vA = mk.sbuf("avA", [128, 18, 4, 129], BF16)
            DF = mk.sbuf("aDF", [128, 18, 512], BF16)
            pt = [mk.sbuf("apt%d" % i, [128, 512], BF16) for i in range(4)]
            mxs = mk.sbuf("amx", [1, 64], F32)
            negM = mk.sbuf("anegM", [128, 1], F32)
            o1 = [mk.sbuf("ao1_%d" % i, [128, 128], F32) for i in range(2)]
            o2 = [mk.sbuf("ao2_%d" % i, [128, 128], F32) for i in range(2)]
            rc = [mk.sbuf("arc%d" % i, [128, 4], F32) for i in range(2)]
            stg = [mk.sbuf("astg%d" % i, [128, NS], BF16) for i in range(2)]
            mk.memset("pool", vA[:], 1.0)
            for s in range(2):
                for h in range(4):
                    load_seq_T(qT[:, h, :], PT[T_DFQ + h * 128:T_DFQ + (h + 1) * 128, :], s, q="sp")
                    load_seq_T(kT[:, h, :], PT[T_DFK + h * 128:T_DFK + (h + 1) * 128, :], s, q="act")
                c0, l0 = seq_ranges(s)
                for h in range(4):
                    mk.dma(vA[:, 0:2, h, 0:128],
                           PK[c0:c0 + CTX, K_DFV + h * 128:K_DFV + (h + 1) * 128].rearrange("(a p) c -> p a c", p=128))
                    mk.dma(vA[:, 2:18, h, 0:128],
                           PK[l0:l0 + SEQ, K_DFV + h * 128:K_DFV + (h + 1) * 128].rearrange("(a p) c -> p a c", p=128))
                mk.memset("dve", mxs[:], 0.0)
                for which, src in ((0, qT), (1, kT)):
                    mk.tt("pool", sq[:], src[:], src[:], ALU.mult)
                    idx = 0
                    for h in range(4):
                        for cst in range(0, NS, 512):
                            n = min(512, NS - cst)
                            pb = PS[3 + idx % 4]
                            mk.mm(pb[0:1, 0:n], onesb[:, 0:1], sq[:, h, cst:cst + n])
                            mk.reduce("dve", mxs[:, which * 32 + idx:which * 32 + idx + 1], pb[0:1, 0:n], ALU.max)
                            idx += 1
                mk.reduce("dve", mxs[:, 60:61], mxs[:, 0:32], ALU.max)
                mk.reduce("dve", mxs[:, 61:62], mxs[:, 32:60], ALU.max)
                mk.tt("dve", mxs[:, 62:63], mxs[:, 60:61], mxs[:, 61:62], ALU.mult)
                mk.act(mxs[:, 63:64], mxs[:, 62:63], AF.Sqrt, scale=1.0 / 64.0)
                mk.ts("dve", mxs[:, 63:64], mxs[:, 63:64], -1.0, None, op0=ALU.mult)
                mk.mm(PS[6][:, 0:1], onesf[0:1, :], mxs[0:1, 63:64])
                mk.copy("dve", negM[:], PS[6][:, 0:1])
                import os as _os
                if _os.environ.get('ATTN_STOP') == '1':
                    return
                qgroups = [(0, CTX, 0, 2)] + [(CTX + i * 512, 512, 0, 18) for i in range(4)]
                ci = 0
                for (q0, nq, tk0, tk1) in qgroups:
                    nsub = nq // 128
                    for h in range(4):
                        O = [[PS[(a * 4 + sb) // 3][:, ((a * 4 + sb) % 3) * 160:((a * 4 + sb) % 3) * 160 + 129]
                              for sb in range(4)] for a in range(2)]
                        started = set()
                        for tk in range(tk0, tk1):
                            for a in range(2):
                                pb = PS[3 + ci % 4]
                                p_ = pt[ci % 4]
                                ci += 1
                                mk.mm(pb[:, 0:nq], kT[a * 64:(a + 1) * 64, h, tk * 128:(tk + 1) * 128],
                                      qT[a * 64:(a + 1) * 64, h, q0:q0 + nq])
                                mk.act(p_[:, 0:nq], pb[:, 0:nq], AF.Exp, bias=negM[:, 0:1], scale=0.125)
                                for sb in range(nsub):
                                    bank = (a * 4 + sb) // 3
                                    st_ = (tk == tk0) and (bank not in started)
                                    started.add(bank)
                                    mk.mm(O[a][sb], p_[:, sb * 128:(sb + 1) * 128], vA[:, tk, h, :],
                                          start=st_, stop=(tk == tk1 - 1))
                        for sb in range(nsub):
                            tile = (q0 + sb * 128) // 128
                            r_ = rc[sb % 2]
                            a1 = o1[sb % 2]
                            a2 = o2[sb % 2]
                            mk.recip(r_[:, 0:1], O[0][sb][:, 128:129])
                            mk.recip(r_[:, 1:2], O[1][sb][:, 128:129])
                            mk.tt("dve", r_[:, 1:2], r_[:, 1:2], nlam[:, 0:1], ALU.mult)
                            mk.ts("dve", a1[:], O[0][sb][:, 0:128], r_[:, 0:1], None, op0=ALU.mult)
                            mk.stt("dve", a1[:], O[1][sb][:, 0:128], r_[:, 1:2], a1[:], ALU.mult, ALU.add)
                            mk.act(a2[:], a1[:], AF.Square, accum_out=r_[:, 2:3])
                            mk.act(r_[:, 3:4], r_[:, 2:3], AF.Sqrt, bias=EPS, scale=1.0 / 128.0)
                            mk.recip(r_[:, 3:4], r_[:, 3:4])
                            mk.stt("dve", DF[:, tile, h * 128:(h + 1) * 128], a1[:], r_[:, 3:4], gv[:],
                                   ALU.mult, ALU.mult)
                if _os.environ.get('ATTN_STOP') == '2':
                    return
                tok_to_BR(DF, 512, s, stg)
                if _os.environ.get('ATTN_STOP') == '3':
                    return

    def phase_merge(li):
        with mk.scope():
            wbr = mk.sbuf("wbr", [128, 12, D], BF16)
            wo = mk.sbuf("wo", [128, 8, D], BF16)
            for b in range(3):
                mk.dma(wbr[:, b * 4:(b + 1) * 4, :], w_branch[li, b].rearrange("(kc p) n -> p kc n", p=128), q="pool")
            mk.dma(wo[:], w_out[li].rearrange("(kc p) n -> p kc n", p=128), q="pool")
            brT = [mk.sbuf("mbr%d" % i, [128, 12, 512], BF16) for i in range(2)]
            gt = [mk.sbuf("mgt%d" % i, [128, 24, 512], BF16) for i in range(2)]
            xs_b = [mk.sbuf("mxs%d" % i, [128, 8, 512], F32) for i in range(2)]
            yT = [mk.sbuf("myT%d" % i, [128, 8, 512], BF16) for i in range(2)]
            ya = [mk.sbuf("mya%d" % i, [128, 512], F32) for i in range(2)]
            tmp = [mk.sbuf("mtm%d" % i, [128, 512], F32) for i in range(2)]
            ci = 0
            for g in range(NG):
                j = gset(g)
                b_ = brT[g % 2]
                g_ = gt[g % 2]
                xs = xs_b[g % 2]
                y_ = yT[g % 2]
                tsl = slice(g * 512, (g + 1) * 512)
                mk.dma(b_[:], BR[:, tsl].rearrange("(c p) t -> p c t", p=128), q="sp")
                mk.dma(g_[:], PT[T_GATE:T_GATE + 3072, tsl].rearrange("(c p) t -> p c t", p=128), q="act")
                mk.dma(xs[:], xT[:, tsl].rearrange("(kc p) t -> p kc t", p=128), q="sp")
                for oc in range(8):
                    acc = ya[oc % 2]
                    for br in range(3):
                        pb = PS[ci % 4]
                        ci += 1
                        for kc in range(4):
                            mk.mm(pb[:], wbr[:, br * 4 + kc, oc * 128:(oc + 1) * 128], b_[:, br * 4 + kc, :],
                                  start=(kc == 0), stop=(kc == 3))
                        if br == 0:
                            mk.tt("dve", acc[:], pb[:], g_[:, br * 8 + oc, :], ALU.mult)
                        else:
                            t_ = tmp[br % 2]
                            mk.tt("dve", t_[:], pb[:], g_[:, br * 8 + oc, :], ALU.mult)
                            mk.tt("pool", (acc[:] if br == 1 else y_[:, oc, :]), acc[:], t_[:], ALU.add)
                for oc in range(8):
                    pb = PS[4 + oc % 3]
                    for kc in range(8):
                        mk.mm(pb[:], wo[:, kc, oc * 128:(oc + 1) * 128], y_[:, kc, :], start=(kc == 0), stop=(kc == 7))
                    mk.stt("dve", xs[:, oc, :], pb[:], modv[:, li, 2, oc, j:j + 1], xs[:, oc, :], ALU.mult, ALU.add)
                mk.dma(xT[:, tsl].rearrange("(kc p) t -> p kc t", p=128), xs[:], q="act")

    def phase_moe(li):
        HALF = NTOK // 2
        with mk.scope():
            h2T = mk.sbuf("h2T", [128, 8, NTOK], BF16)
            comb = mk.sbuf("comb", [128, 36, 32], F32)
            with mk.scope():
                scratch = ([mk.sbuf("nxs%d" % i, [128, 8, 512], F32) for i in range(2)],
                           [mk.sbuf("nsq%d" % i, [128, 8, 512], BF16) for i in range(2)],
                           [mk.sbuf("nrs%d" % i, [128, 512], F32) for i in range(2)])
                wr = mk.sbuf("wr", [128, 8, 36], F32)
                rb = mk.sbuf("rb", [128, 36], F32)
                L = mk.sbuf("rL", [128, 36], F32)
                sm = mk.sbuf("rsm", [128, 16], F32)
                mg = mk.sbuf("rmg", [128, 4], F32)
                eg = mk.sbuf("reg", [128, 4], F32)
                ls = mk.sbuf("rls", [128, 8], F32)
                t8 = mk.sbuf("rt8", [128, 8], F32)
                s1 = mk.sbuf("rs1", [128, 8], F32)
                s2 = mk.sbuf("rs2", [128, 8], F32)
                mk.dma(wr[:], router_w[li].rearrange("(kc p) n -> p kc n", p=128))
                bcast_row(rb[:], router_b[li:li + 1, :])

                def route(g, xs):
                    for t4 in range(4):
                        tile = g * 4 + t4
                        pb = PS[t4 % 4]
                        for kc in range(8):
                            mk.mm(pb[:, 0:36], xs[:, kc, t4 * 128:(t4 + 1) * 128], wr[:, kc, :],
                                  start=(kc == 0), stop=(kc == 7))
                        mk.tt("dve", L[:], pb[:, 0:36], rb[:], ALU.add)
                        mk.reduce("dve", sm[:, 0:1], L[:, 0:4], ALU.max)
                        mk.ts("dve", mg[:], L[:, 0:4], sm[:, 0:1], None, op0=ALU.is_ge)
                        mk.ts("dve", sm[:, 1:2], sm[:, 0:1], -1.0, None, op0=ALU.mult)
                        mk.act(eg[:], L[:, 0:4], AF.Exp, bias=sm[:, 1:2], scale=1.0, accum_out=sm[:, 2:3])
                        mk.recip(sm[:, 3:4], sm[:, 2:3])
                        mk.ts("dve", ls[:], L[:, 4:12], mg[:, 0:1], None, op0=ALU.mult)
                        for gi in range(1, 4):
                            mk.stt("dve", ls[:], L[:, 4 + 8 * gi:12 + 8 * gi], mg[:, gi:gi + 1], ls[:],
                                   ALU.mult, ALU.add)
                        mk.max8(t8[:], ls[:])
                        mk.tt("dve", sm[:, 4:5], t8[:, 1:2], t8[:, 0:1], ALU.subtract)
                        mk.act(sm[:, 5:6], sm[:, 4:5], AF.Exp)
                        mk.ts("dve", sm[:, 6:7], sm[:, 5:6], 1.0, None, op0=ALU.add)
                        mk.recip(sm[:, 6:7], sm[:, 6:7])
                        mk.tt("dve", sm[:, 7:8], sm[:, 6:7], sm[:, 3:4], ALU.mult)
                        mk.tt("dve", sm[:, 8:9], sm[:, 7:8], sm[:, 5:6], ALU.mult)
                        mk.tt("dve", sm[:, 9:10], sm[:, 7:8], sm[:, 8:9], ALU.subtract)
                        mk.ts("dve", s1[:], ls[:], t8[:, 0:1], sm[:, 9:10], op0=ALU.is_ge, op1=ALU.mult)
                        mk.ts("dve", s2[:], ls[:], t8[:, 1:2], sm[:, 8:9], op0=ALU.is_ge, op1=ALU.mult)
                        mk.tt("dve", s1[:], s1[:], s2[:], ALU.add)
                        for gi in range(4):
                            mk.ts("dve", comb[:, tile, gi * 8:(gi + 1) * 8], s1[:], mg[:, gi:gi + 1], None,
                                  op0=ALU.mult)

                norm_groups(li, 3, h2T, scratch, h32_cb=route)
            if dbg:
                comb_dbg = mk.dram("comb_dbg%d" % li, [128, 36 * 32], F32, kind="ExternalOutput")
                mk.dma(comb_dbg[:, :], comb[:].rearrange("p a b -> p (a b)"))
            wg_b = [mk.sbuf("ewg%d" % i, [128, 8, 512], BF16) for i in range(2)]
            wu_b = [mk.sbuf("ewu%d" % i, [128, 8, 512], BF16) for i in range(2)]
            wd_b = [mk.sbuf("ewd%d" % i, [128, 4, D], BF16) for i in range(2)]
            hid_b = [mk.sbuf("ehid%d" % i, [128, 4, 512], BF16) for i in range(2)]
            sl_b = [mk.sbuf("esl%d" % i, [128, 512], F32) for i in range(2)]
            NPART = 3
            PT_TILES = 36 // NPART
            yacc = mk.sbuf("yacc", [128, PT_TILES, D], F32)
            xs_b = [mk.sbuf("exs%d" % i, [128, 8, 128], F32) for i in range(2)]
            ci = 0
            for hf in range(NPART):
                mk.memset("pool", yacc[:], 0.0)
                grp = [(hf * PT_TILES * 128 + i * 512, 512) for i in range(PT_TILES // 4)]
                for e in range(32):
                    wg, wu, wd = wg_b[e % 2], wu_b[e % 2], wd_b[e % 2]
                    mk.dma(wg[:], moe_wg[li, e].rearrange("(kc p) n -> p kc n", p=128), q="pool")
                    mk.dma(wu[:], moe_wu[li, e].rearrange("(kc p) n -> p kc n", p=128), q="pool")
                    mk.dma(wd[:], moe_wd[li, e].rearrange("(kc p) n -> p kc n", p=128), q="pool")
                    for (t0, nt) in grp:
                        hid = hid_b[ci % 2]
                        for fc in range(4):
                            pg = PS[ci % 3]
                            pu = PS[3 + ci % 2]
                            sl = sl_b[ci % 2]
                            ci += 1
                            for kc in range(8):
                                mk.mm(pg[:, 0:nt], wg[:, kc, fc * 128:(fc + 1) * 128], h2T[:, kc, t0:t0 + nt],
                                      start=(kc == 0), stop=(kc == 7))
                            for kc in range(8):
                                mk.mm(pu[:, 0:nt], wu[:, kc, fc * 128:(fc + 1) * 128], h2T[:, kc, t0:t0 + nt],
                                      start=(kc == 0), stop=(kc == 7))
                            mk.act(sl[:, 0:nt], pg[:, 0:nt], AF.Silu)
                            mk.tt("dve", hid[:, fc, 0:nt], pu[:, 0:nt], sl[:, 0:nt], ALU.mult)
                        for sb in range(nt // 128):
                            tile = (t0 + sb * 128) // 128
                            lt = tile - hf * PT_TILES
                            for hh in range(2):
                                py = PS[5 + (sb * 2 + hh) % 2]
                                for fc in range(4):
                                    mk.mm(py[:], hid[:, fc, sb * 128:(sb + 1) * 128], wd[:, fc, hh * 512:(hh + 1) * 512],
                                          start=(fc == 0), stop=(fc == 3))
                                mk.stt("dve", yacc[:, lt, hh * 512:(hh + 1) * 512], py[:], comb[:, tile, e:e + 1],
                                       yacc[:, lt, hh * 512:(hh + 1) * 512], ALU.mult, ALU.add)
                for lt in range(PT_TILES):
                    tile = hf * PT_TILES + lt
                    j = gset(tile // 4)
                    xs = xs_b[lt % 2]
                    tsl = slice(tile * 128, (tile + 1) * 128)
                    mk.dma(xs[:], xT[:, tsl].rearrange("(kc p) t -> p kc t", p=128), q="sp")
                    for half in range(2):
                        pb = PS[(lt * 2 + half) % 4]
                        for k4 in range(4):
                            kc = half * 4 + k4
                            mk.tr(pb[:, k4 * 128:(k4 + 1) * 128], yacc[:, lt, kc * 128:(kc + 1) * 128], ident[:])
                        for k4 in range(4):
                            kc = half * 4 + k4
                            mk.stt("dve", xs[:, kc, :], pb[:, k4 * 128:(k4 + 1) * 128], modv[:, li, 5, kc, j:j + 1],
                                   xs[:, kc, :], ALU.mult, ALU.add)
                    mk.dma(xT[:, tsl].rearrange("(kc p) t -> p kc t", p=128), xs[:], q="act")

    def phase_final():
        with mk.scope():
            fg = mk.sbuf("fg", [128, 8], F32)
            mk.dma(fg[:], fin_gT[:, :])
            xs_b = [mk.sbuf("fxs%d" % i, [128, 8, 512], F32) for i in range(2)]
            sq_b = [mk.sbuf("fsq%d" % i, [128, 8, 512], BF16) for i in range(2)]
            rs_b = [mk.sbuf("frs%d" % i, [128, 512], F32) for i in range(2)]
            ot = [mk.sbuf("fot%d" % i, [128, D], F32) for i in range(2)]
            for g in range(1, NG):
                xs, sq, rs = xs_b[g % 2], sq_b[g % 2], rs_b[g % 2]
                mk.dma(xs[:], xT[:, g * 512:(g + 1) * 512].rearrange("(kc p) t -> p kc t", p=128),
                       q=("sp" if g % 2 else "act"))
                mk.tt("pool", sq[:], xs[:], xs[:], ALU.mult)
                pb = PS[4 + g % 2]
                for kc in range(8):
                    mk.mm(pb[:], onesb[:], sq[:, kc, :], start=(kc == 0), stop=(kc == 7))
                mk.act(rs[:], pb[:], AF.Sqrt, bias=EPS, scale=1.0 / D)
                mk.recip(rs[:], rs[:])
                for kc in range(8):
                    mk.stt("dve", xs[:, kc, :], xs[:, kc, :], fg[:, kc:kc + 1], rs[:], ALU.mult, ALU.mult)
                for t4 in range(4):
                    u = (g - 1) * 4 + t4
                    o_ = ot[u % 2]
                    for half in range(2):
                        pq = PS[(u * 2 + half) % 4]
                        for k4 in range(4):
                            kc = half * 4 + k4
                            mk.tr(pq[:, k4 * 128:(k4 + 1) * 128], xs[:, kc, t4 * 128:(t4 + 1) * 128], ident[:])
                        mk.copy(evac_eng(), o_[:, half * 512:(half + 1) * 512], pq[:])
                    mk.dma(y_out[u // 16, (u % 16) * 128:(u % 16) * 128 + 128, :], o_[:], q="sp")

    def nat_chunk(d, cs):
        if d == 0:
            return cs
        return (3 - cs) if cs < 4 else (35 - (cs - 4))

    def rev_copy(eng, dst, src):
        mk.copy(eng, dst[:, 0:CTX], src[:, 0:CTX][:, ::-1])
        mk.copy(eng, dst[:, CTX:NS], src[:, CTX:NS][:, ::-1])

    def phase_mlstm(li):
        with mk.scope():
            HS = mk.sbuf("mHS", [128, 18, 512], F32)
            gb = mk.sbuf("mgb", [4, 4], F32)
            ngb = mk.sbuf("mngb", [4, 4], F32)
            gvn = mk.sbuf("mgvn", [128, 128], F32)
            ones4 = mk.sbuf("mones4", [4, NS], F32)
            mk.dma(gb[:], ml_gb[li])
            mk.ts("dve", ngb[:], gb[:], -1.0, None, op0=ALU.mult)
            bcast_row(gvn[:], ml_ng[li:li + 1, :])
            mk.memset("pool", ones4[:], 1.0)
            for s in range(2):
                with mk.scope():
                    qT = mk.sbuf("mqT", [128, 4, NS], BF16)
                    kT = mk.sbuf("mkT", [128, 4, NS], BF16)
                    kK = mk.sbuf("mkK", [128, 18, 512], BF16)
                    vA = mk.sbuf("mvA", [128, 18, 4, 129], BF16)
                    uV = mk.sbuf("muV", [128, 18, 4, 129], BF16)
                    R = [mk.sbuf("mR%d" % i, [4, NS], F32) for i in range(5)]
                    COL = mk.sbuf("mCOL", [128, 3, 18, 4], F32)
                    DECr = mk.sbuf("mDECr", [4, 36, 4], F32)
                    DECb = mk.sbuf("mDECb", [128, 36, 4], F32)
                    CT = mk.sbuf("mCT", [128, 4, 129], F32)
                    tmpC = mk.sbuf("mtmpC", [128, 4, 129], F32)
                    CTb = [mk.sbuf("mCTb%d" % i, [128, 4, 129], BF16) for i in range(2)]
                    smb = [mk.sbuf("msm%d" % i, [128, 4, 128], BF16) for i in range(2)]
                    t4b = [mk.sbuf("mt4%d" % i, [128, 4], F32) for i in range(2)]
                    tmo = [mk.sbuf("mtmo%d" % i, [128, 4, 128], F32) for i in range(2)]
                    c0, l0 = seq_ranges(s)
                    for h in range(4):
                        load_seq_T(qT[:, h, :], PT[T_MLQ + h * 128:T_MLQ + (h + 1) * 128, :], s, q="sp")
                        load_seq_T(kT[:, h, :], PT[T_MLK + h * 128:T_MLK + (h + 1) * 128, :], s, q="act")
                    load_seq_K(kK, K_MLK, 512, s)
                    mk.memset("pool", vA[:], 1.0)
                    for h in range(4):
                        mk.dma(vA[:, 0:2, h, 0:128],
                               PK[c0:c0 + CTX, K_MLV + h * 128:K_MLV + (h + 1) * 128].rearrange("(a p) c -> p a c", p=128))
                        mk.dma(vA[:, 2:18, h, 0:128],
                               PK[l0:l0 + SEQ, K_MLV + h * 128:K_MLV + (h + 1) * 128].rearrange("(a p) c -> p a c", p=128))
                    for d in range(2):
                        irow = GT[d * 8:d * 8 + 4, :]
                        frow = GT[d * 8 + 4:d * 8 + 8, :]
                        if d == 0:
                            load_seq_T(R[1], irow, s)
                            load_seq_T(R[0], frow, s)
                        else:
                            load_seq_T(R[3], irow, s)
                            load_seq_T(R[4], frow, s)
                            rev_copy("dve", R[1], R[3])
                            rev_copy("pool", R[0], R[4])
                        R0, R1, R2, R3, R4 = R
                        v3 = lambda t: t[:].rearrange("p (c j) -> p c j", j=64)
                        mk.act(R0[:], R0[:], AF.Exp, bias=ngb[:, 2 * d + 1:2 * d + 2], scale=-1.0)
                        mk.act(R0[:], R0[:], AF.Ln, bias=1.0, scale=1.0)
                        mk.ts("dve", R0[:], R0[:], -1.0, None, op0=ALU.mult)
                        mk.scan("dve", R2[:], ones4[:], R0[:], 0.0, ALU.mult, ALU.add)
                        mk.stt("dve", R1[:], R1[:], gb[:, 2 * d:2 * d + 1], R2[:], ALU.add, ALU.subtract)
                        mk.scan("dve", R0[:], ones4[:], R1[:], 0.0, ALU.mult, ALU.max)
                        mprev = v3(R0)[:, 0:35, 63:64].to_broadcast([4, 35, 64])
                        mk.copy("dve", R3[:, 0:64], R1[:, 0:64])
                        mk.tt("dve", v3(R3)[:, 1:36, :], v3(R1)[:, 1:36, :], mprev, ALU.subtract)
                        mk.act(R3[:], R3[:], AF.Exp)
                        mk.ts("dve", R4[:, 0:64], R0[:, 0:64], -1.0, None, op0=ALU.mult)
                        mk.tt("dve", v3(R4)[:, 1:36, :], mprev, v3(R0)[:, 1:36, :], ALU.subtract)
                        mk.act(R4[:], R4[:], AF.Exp)
                        mk.tt("dve", R2[:], R2[:], R0[:], ALU.add)
                        mk.act(R2[:], R2[:], AF.Exp, scale=-1.0)
                        mk.tt("dve", DECr[:], v3(R4)[:, :, 63:64].to_broadcast([4, 36, 4]),
                              ident[0:4, 0:4].unsqueeze(1).to_broadcast([4, 36, 4]), ALU.mult)
                        mk.mm(PS[6][:, 0:144], onesf[0:4, :], DECr[:].rearrange("p c h -> p (c h)"))
                        mk.copy("dve", DECb[:].rearrange("p c h -> p (c h)"), PS[6][:, 0:144])
                        if d == 0:
                            NAT = [R3, R4, R2]
                        else:
                            rev_copy("dve", R0, R3)
                            rev_copy("pool", R1, R4)
                            rev_copy("dve", R3, R2)
                            NAT = [R0, R1, R3]
                        for qi, arr in enumerate(NAT):
                            for t in range(18):
                                o_ = (qi * 18 + t) * 4
                                mk.tr(PS[5][:, o_:o_ + 4], arr[:, t * 128:(t + 1) * 128], ident[0:4, 0:4])
                        mk.copy("dve", COL[:].rearrange("p a t h -> p (a t h)"), PS[5][:, 0:216])
                        mk.tt("pool", uV[:], vA[:], COL[:, 0].unsqueeze(3).to_broadcast([128, 18, 4, 129]), ALU.mult)
                        mk.memset("dve", CT[:], 0.0)
                        mk.memset("pool", CTb[0][:], 0.0)
                        mask = maskF if d == 0 else maskB
                        cur_tile = -1
                        for cs in range(36):
                            c = nat_chunk(d, cs)
                            tile, par = c // 2, c % 2
                            rows = slice(par * 64, par * 64 + 64)
                            ctb_cur, ctb_nxt = CTb[cs % 2], CTb[(cs + 1) % 2]
                            if tile != cur_tile:
                                cur_tile = tile
                                sm = smb[tile % 2]
                                for h in range(4):
                                    mk.mm(PS[4][:, h * 128:(h + 1) * 128], kT[:, h, tile * 128:(tile + 1) * 128],
                                          qT[:, h, tile * 128:(tile + 1) * 128])
                                mk.tt("dve", sm[:], PS[4][:].rearrange("p (h j) -> p h j", h=4),
                                      mask[:].unsqueeze(1).to_broadcast([128, 4, 128]), ALU.mult)
                            for h in range(4):
                                o_ = PS[h // 2][rows, (h % 2) * 256:(h % 2) * 256 + 129]
                                mk.mm(o_, sm[:, h, par * 64:par * 64 + 64], uV[:, tile, h, :], start=True, stop=False)
                                mk.mm(o_, qT[:, h, c * 64:(c + 1) * 64], ctb_cur[:, h, :], start=False, stop=True)
                            for h in range(4):
                                mk.mm(PS[2 + h // 2][:, (h % 2) * 256:(h % 2) * 256 + 129],
                                      kK[rows, tile, h * 128:(h + 1) * 128], uV[rows, tile, h, :])
                            for b in range(2):
                                mk.tt("dve", tmpC[:, 2 * b:2 * b + 2, :],
                                      PS[2 + b][:, 0:512].rearrange("p (h v) -> p h v", h=2)[:, :, 0:129], CT[:, 2 * b:2 * b + 2, :], ALU.add)
                            mk.tt("pool", CT[:], tmpC[:], DECb[:, cs, :].unsqueeze(2).to_broadcast([128, 4, 129]), ALU.mult)
                            mk.copy("act", ctb_nxt[:], CT[:])
                            t4 = t4b[cs % 2]
                            for b in range(2):
                                mk.tt("dve", t4[rows, 2 * b:2 * b + 2], PS[b][rows, 128:512:256],
                                      COL[rows, 1, tile, 2 * b:2 * b + 2], ALU.mult)
                            mk.stt("dve", t4[rows, :], t4[rows, :], -1.0, t4[rows, :], ALU.mult, ALU.max)
                            mk.tt("dve", t4[rows, :], t4[rows, :], COL[rows, 2, tile, :], ALU.max)
                            mk.recip(t4[rows, :], t4[rows, :])
                            mk.tt("dve", t4[rows, :], t4[rows, :], COL[rows, 1, tile, :], ALU.mult)
                            for b in range(2):
                                src = PS[b][rows, 0:512].rearrange("p (h v) -> p h v", h=2)[:, :, 0:128]
                                sc = t4[rows, 2 * b:2 * b + 2].unsqueeze(2).to_broadcast([64, 2, 128])
                                hs = HS[rows, tile, 2 * b * 128:(2 * b + 2) * 128].rearrange("p (h v) -> p h v", h=2)
                                if d == 0:
                                    mk.tt("dve", hs, src, sc, ALU.mult)
                                else:
                                    to = tmo[cs % 2][rows, 2 * b:2 * b + 2, :]
                                    mk.tt("dve", to, src, sc, ALU.mult)
                                    mk.tt("pool", hs, hs, to, ALU.add)
                with mk.scope():
                    sgo = mk.sbuf("msgo", [128, 18, 512], BF16)
                    sqh = mk.sbuf("msqh", [128, 18, 512], F32)
                    ML = mk.sbuf("mML", [128, 18, 512], BF16)
                    ss = mk.sbuf("mss", [128, 72], F32)
                    stg = [mk.sbuf("mstg%d" % i, [128, NS], BF16) for i in range(2)]
                    load_seq_K(sgo, K_MLO, 512, s)
                    finish_branch(HS, sgo, gvn, sqh, ss, ML)
                    tok_to_BR(ML, 0, s, stg)

    def finish_branch(HS, gate, gvn, sqh, ss, OUT):
        h3 = lambda t: t[:].rearrange("p t (h v) -> p (t h) v", v=128)
        mk.tt("pool", sqh[:], HS[:], HS[:], ALU.mult)
        mk.reduce("dve", ss[:], h3(sqh), ALU.add)
        mk.act(ss[:], ss[:], AF.Sqrt, bias=EPS, scale=1.0 / 128.0)
        mk.recip(ss[:], ss[:])
        mk.tt("dve", h3(sqh), h3(HS), ss[:].unsqueeze(2).to_broadcast([128, 72, 128]), ALU.mult)
        mk.tt("pool", h3(sqh), h3(sqh), gvn[:].unsqueeze(1).to_broadcast([128, 72, 128]), ALU.mult)
        mk.tt("dve", OUT[:], sqh[:], gate[:], ALU.mult)

    def phase_gla(li):
        with mk.scope():
            OS = mk.sbuf("gOS", [128, 18, 512], F32)
            gvn = mk.sbuf("ggvn", [128, 128], F32)
            wa = mk.sbuf("gwa", [16, 2, 256], F32)
            nba = mk.sbuf("gnba", [128, 2, 2], F32)
            RST = mk.sbuf("gRST", [128, NS], F32)
            bcast_row(gvn[:], gl_ng[li:li + 1, :])
            mk.dma(wa[:], gl_wa[li].rearrange("d r n -> r d n"))
            mk.dma(nba[:], gl_baT[li].rearrange("d p h -> p d h"))
            mk.ts("dve", nba[:], nba[:], -1.0, None, op0=ALU.mult)
            mk.memset("pool", RST[:], 1.0)
            mk.memset("pool", RST[:].rearrange("p (c j) -> p c j", j=64)[:, :, 0:1], 0.0)
            for s in range(2):
                with mk.scope():
                    qT = mk.sbuf("gqT", [128, 2, NS], BF16)
                    kT = mk.sbuf("gkT", [128, 2, NS], BF16)
                    vK = mk.sbuf("gvK", [128, 18, 512], BF16)
                    aT = mk.sbuf("gaT", [16, 2, NS], F32)
                    LA = mk.sbuf("gLA", [128, 2, NS], F32)
                    Pc = mk.sbuf("gPc", [128, 2, NS], F32)
                    qg = mk.sbuf("gqg", [128, 2, NS], BF16)
                    kg = mk.sbuf("gkg", [128, 2, NS], BF16)
                    kg2 = mk.sbuf("gkg2", [128, 2, NS], BF16)
                    kgK = mk.sbuf("gkgK", [128, 18, 256], BF16)
                    Tc = mk.sbuf("gTc", [128, 2, 36], F32)
                    eb = mk.sbuf("geb", [128, 2, 36], F32)
                    S = mk.sbuf("gS", [128, 2, 128], F32)
                    Sb = [mk.sbuf("gSb%d" % i, [128, 4, 128], BF16) for i in range(2)]
                    amb = [mk.sbuf("gam%d" % i, [128, 4, 128], BF16) for i in range(2)]
                    for hh in range(2):
                        load_seq_T(qT[:, hh, :], PT[T_GLQ + hh * 128:T_GLQ + (hh + 1) * 128, :], s, q="sp")
                        load_seq_T(kT[:, hh, :], PT[T_GLK + hh * 128:T_GLK + (hh + 1) * 128, :], s, q="act")
                    load_seq_K(vK, K_GLV, 512, s)
                    for d in range(2):
                        load_seq_T(aT[:, d, :], GT[16 + d * 16:32 + d * 16, :], s)
                    v4 = lambda t: t[:].rearrange("p h (c j) -> p h c j", j=64)
                    for d in range(2):
                        ci = 0
                        for hh in range(2):
                            for t0 in range(0, NS, 512):
                                n = min(512, NS - t0)
                                pb = PS[5 + ci % 2]
                                ci += 1
                                mk.mm(pb[:, 0:n], wa[:, d, hh * 128:(hh + 1) * 128], aT[:, d, t0:t0 + n])
                                mk.act(LA[:, hh, t0:t0 + n], pb[:, 0:n], AF.Exp, bias=nba[:, d, hh:hh + 1], scale=-1.0)
                        mk.act(LA[:], LA[:], AF.Ln, bias=1.0, scale=1.0)
                        mk.ts("dve", LA[:], LA[:], -1.0 / 16.0, None, op0=ALU.mult)
                        for hh in range(2):
                            mk.scan("dve", Pc[:, hh, :], RST[:], LA[:, hh, :], 0.0, ALU.mult, ALU.add)
                        mk.copy("dve", Tc[:], v4(Pc)[:, :, :, 63])
                        mk.act(eb[:], Tc[:], AF.Exp)
                        if d == 1:
                            mk.tt("dve", v4(Pc), Tc[:].unsqueeze(3).to_broadcast([128, 2, 36, 64]), v4(Pc), ALU.subtract)
                            mk.tt("pool", Pc[:], Pc[:], LA[:], ALU.add)
                        mk.act(LA[:], Pc[:], AF.Exp)
                        mk.tt("dve", qg[:], qT[:], LA[:], ALU.mult)
                        mk.act(LA[:], Pc[:], AF.Exp, scale=-1.0)
                        mk.tt("dve", kg[:], kT[:], LA[:], ALU.mult)
                        mk.tt("pool", v4(kg2), v4(kg), eb[:].unsqueeze(3).to_broadcast([128, 2, 36, 64]), ALU.mult)
                        import os as _osg
                        _gs = _osg.environ.get("GLA_STOP", "")
                        if _gs == "1":
                            return
                        for hh in range(2):
                            for t in range(18):
                                pv = PS[5 + t % 2][:, (t % 4) * 128:(t % 4) * 128 + 128]
                                mk.mm(pv, kg2[:, hh, t * 128:(t + 1) * 128], identb[:])
                                mk.copy(evac_eng(), kgK[:, t, hh * 128:(hh + 1) * 128], pv)
                        if _gs == "2":
                            return
                        mk.memset("dve", S[:], 0.0)
                        mk.memset("pool", Sb[0][:], 0.0)
                        mk.memset("pool", Sb[1][:], 0.0)
                        mask = maskF if d == 0 else maskB
                        cur_tile = -1
                        for cs in range(36):
                            c = nat_chunk(d, cs)
                            tile, par = c // 2, c % 2
                            rows = slice(par * 64, par * 64 + 64)
                            sb_cur, sb_nxt = Sb[cs % 2], Sb[(cs + 1) % 2]
                            if tile != cur_tile:
                                cur_tile = tile
                                am = amb[tile % 2]
                                for h in range(4):
                                    hr = slice((h % 2) * 64, (h % 2) * 64 + 64)
                                    mk.mm(PS[4 + h % 2][:, (h // 2) * 128:(h // 2 + 1) * 128],
                                          kg[hr, h // 2, tile * 128:(tile + 1) * 128],
                                          qg[hr, h // 2, tile * 128:(tile + 1) * 128])
                                for wh in range(2):
                                    mk.tt("dve", am[:, wh::2, :], PS[4 + wh][:, 0:256].rearrange("p (h j) -> p h j", h=2),
                                          mask[:].unsqueeze(1).to_broadcast([128, 2, 128]), ALU.mult)
                            _lv = int(_gs) if _gs else 9
                            if _lv >= 4:
                                for h in range(4):
                                    o_ = PS[h // 2][rows, (h % 2) * 128:(h % 2) * 128 + 128]
                                    mk.mm(o_, am[:, h, par * 64:par * 64 + 64], vK[:, tile, h * 128:(h + 1) * 128],
                                          start=True, stop=False)
                                    mk.mm(o_, qg[:, h // 2, c * 64:(c + 1) * 64], sb_cur[:, h, :], start=False, stop=True)
                            if _lv >= 5:
                                for h in range(4):
                                    hh, wh = h // 2, h % 2
                                    mk.mm(PS[2 + hh][:, wh * 128:(wh + 1) * 128], kgK[rows, tile, hh * 128:(hh + 1) * 128],
                                          vK[rows, tile, h * 128:(h + 1) * 128])
                            if _lv >= 6:
                                for h in range(4):
                                    hh, wh = h // 2, h % 2
                                    hr = slice(wh * 64, wh * 64 + 64)
                                    mk.stt("dve", S[hr, hh, :], S[hr, hh, :], eb[hr, hh, c:c + 1],
                                           PS[2 + hh][hr, wh * 128:(wh + 1) * 128], ALU.mult, ALU.add)
                            if _lv >= 7:
                                for wh in range(2):
                                    hr = slice(wh * 64, wh * 64 + 64)
                                    mk.copy("act", sb_nxt[hr, wh::2, :], S[hr, :, :])
                            if _lv >= 4:
                                for b in range(2):
                                    dst = OS[rows, tile, b * 256:(b + 1) * 256]
                                    if d == 0:
                                        mk.copy("dve", dst, PS[b][rows, 0:256])
                                    else:
                                        mk.tt("dve", dst, dst, PS[b][rows, 0:256], ALU.add)
                with mk.scope():
                    sg = mk.sbuf("gsg", [128, 18, 512], BF16)
                    sqh = mk.sbuf("gsqh", [128, 18, 512], F32)
                    GLo = mk.sbuf("gGLo", [128, 18, 512], BF16)
                    ss = mk.sbuf("gss", [128, 72], F32)
                    stg = [mk.sbuf("gstg%d" % i, [128, NS], BF16) for i in range(2)]
                    load_seq_K(sg, K_GLG, 512, s)
                    finish_branch(OS, sg, gvn, sqh, ss, GLo)
                    tok_to_BR(GLo, 1024, s, stg)

    phase_load_x()
    phase_mod()
    order = ["norm1", "proj", "mlstm", "attn", "gla", "merge", "moe"]
    lim = order.index(stop_after) if stop_after else len(order)
    for li in range(n_layers):
        with mk.scope():
            hT = mk.sbuf("hT", [128, 8, NTOK], BF16)
            with mk.scope():
                scratch = ([mk.sbuf("nxs%d" % i, [128, 8, 512], F32) for i in range(2)],
                           [mk.sbuf("nsq%d" % i, [128, 8, 512], BF16) for i in range(2)],
                           [mk.sbuf("nrs%d" % i, [128, 512], F32) for i in range(2)])
                norm_groups(li, 0, hT, scratch)
            if dbg:
                hT_dbg = mk.dram("hT_dbg%d" % li, [D, NTOK], BF16, kind="ExternalOutput")
                mk.dma(hT_dbg[:, :].rearrange("(kc p) t -> p kc t", p=128), hT[:])
            if lim >= 1:
                with mk.scope():
                    phase_proj(li, hT)
        if lim >= 2 and "mlstm" not in skip:
            phase_mlstm(li)
        if lim >= 3 and "attn" not in skip:
            phase_attn(li)
        if lim >= 4 and "gla" not in skip:
            phase_gla(li)
        if lim >= 5:
            phase_merge(li)
        if lim >= 6 and do_moe:
            phase_moe(li)
    if lim >= 6:
        phase_final()
    if dbg:
        modv_dbg = mk.dram("modv_dbg", [128, 2 * 6 * 8 * 3], F32, kind="ExternalOutput")
        mk.dma(modv_dbg[:, :], modv[:].rearrange("p a b c d -> p (a b c d)"))
    mk.finish()
    return nc, mk


def make_consts():
    c = np.zeros((128, 384 + 2 * SEQ), np.float32)
    c[:, 0:128] = np.eye(128, dtype=np.float32)
    i = np.arange(128)[:, None]
    j = np.arange(128)[None, :]
    same = (i // 64) == (j // 64)
    c[:, 128:256] = (same & (i <= j)).astype(np.float32)
    c[:, 256:384] = (same & (i >= j)).astype(np.float32)
    t = np.arange(SEQ)
    rowp = (t // 64).astype(np.float32)
    colp = (t % 64).astype(np.float32)
    inv = (10000.0 ** (-np.arange(16, dtype=np.float32) / 16)).astype(np.float32)
    for d in range(128):
        dd = d % 64
        axis = dd // 32
        f = dd % 16
        half = (dd % 32) // 16
        ang = (rowp if axis == 0 else colp) * inv[f]
        c[d, 384:384 + SEQ] = np.cos(ang)
        c[d, 384 + SEQ:384 + 2 * SEQ] = np.sin(ang) * (-1.0 if half == 0 else 1.0)
    return c


def pcol(v, nch):
    return np.ascontiguousarray(np.asarray(v, np.float32).reshape(nch, 128).T)


def prep_shared(inp):
    sh = {}
    f = lambda a: np.ascontiguousarray(np.asarray(a, np.float32))
    sh["w_mod"] = f(inp["w_mod"])
    sh["b_modT"] = np.stack([pcol(inp["b_mod"][li], 48) for li in range(2)])
    sh["gmixT"] = np.stack([pcol(inp["norm_mix_g"][li], 8) for li in range(2)])
    sh["gffnT"] = np.stack([pcol(inp["norm_ffn_g"][li], 8) for li in range(2)])
    sh["w_in"] = f(inp["w_in"])
    sh["ml_gb"] = f(np.asarray(inp["ml_gate_b"]).reshape(2, 4, 4).transpose(0, 2, 1))
    sh["ml_ng"] = f(inp["ml_norm_g"])
    sh["df_lam"] = f(inp["df_lambda"])
    sh["df_ng"] = f(inp["df_norm_g"])
    sh["gl_wa"] = f(inp["gl_w_alpha"])
    sh["gl_baT"] = f(np.asarray(inp["gl_b_alpha"]).reshape(2, 2, 2, 128).transpose(0, 1, 3, 2))
    sh["gl_ng"] = f(inp["gl_norm_g"])
    sh["w_branch"] = f(inp["w_branch"])
    sh["w_out"] = f(inp["w_out"])
    sh["router_w"] = f(np.concatenate([inp["router_group_w"], inp["router_expert_w"]], axis=-1))
    sh["router_b"] = f(np.concatenate([inp["router_group_b"], inp["router_expert_b"]], axis=-1))
    sh["moe_wg"] = f(inp["moe_w_gate"])
    sh["moe_wu"] = f(inp["moe_w_up"])
    sh["moe_wd"] = f(inp["moe_w_down"])
    sh["fin_gT"] = pcol(inp["final_norm_g"], 8)
    sh["consts"] = make_consts()
    return sh


def prep_core(inp, core):
    b0 = 2 * core
    m = {}
    m["x2"] = np.ascontiguousarray(np.asarray(inp["x"][b0:b0 + 2], np.float32))
    m["ctx2"] = np.ascontiguousarray(np.asarray(inp["ctx"][b0:b0 + 2], np.float32))
    cs = np.stack([np.asarray(inp["c_ctx"], np.float32), np.asarray(inp["c"][b0], np.float32),
                   np.asarray(inp["c"][b0 + 1], np.float32)], axis=-1)
    m["cT"] = np.ascontiguousarray(cs.reshape(8, 128, 3).transpose(1, 0, 2))
    return m


_CACHE = {}
CORES_PER_LAUNCH = 8


def kernel(**inputs):
    if "nc" not in _CACHE:
        _CACHE["nc"] = build()[0]
    nc = _CACHE["nc"]
    sh = prep_shared(inputs)
    in_maps = []
    for core in range(8):
        m = dict(sh)
        m.update(prep_core(inputs, core))
        in_maps.append(m)
    outs = []
    for g0 in range(0, 8, CORES_PER_LAUNCH):
        res = run_bass_kernel_spmd(nc, in_maps[g0:g0 + CORES_PER_LAUNCH], core_ids=list(range(CORES_PER_LAUNCH)))
        outs.extend(np.asarray(r["y_out"], np.float32) for r in res.results)
    return np.concatenate(outs, axis=0)
```

```python
import contextlib
import math
import numpy as np
import concourse.bass as bass
import concourse.mybir as mybir
from concourse.bass_utils import run_bass_kernel_spmd

F32 = mybir.dt.float32
BF16 = mybir.dt.bfloat16
AF = mybir.ActivationFunctionType
ALU = mybir.AluOpType
AX = mybir.AxisListType

COMPUTE = ("pe", "act", "dve", "pool")


class _Op:
    __slots__ = ("eng", "fn", "deps", "marked", "val", "dma", "dsem", "dval", "qwait", "emitted", "seq")

    def __init__(self, eng, fn):
        self.eng = eng
        self.fn = fn
        self.deps = []
        self.marked = False
        self.val = None
        self.dma = False
        self.dsem = None
        self.dval = None
        self.qwait = None
        self.emitted = False


class MK:
    def __init__(self, nc, n_dma_sems=8):
        self.nc = nc
        self.stacks = [contextlib.ExitStack()]
        self.e = {"pe": nc.tensor, "act": nc.scalar, "dve": nc.vector, "pool": nc.gpsimd, "sp": nc.sync}
        self.ops = {k: [] for k in self.e}
        self.pstep = {}
        self.notrack = set()
        self.psum_names = set()
        self.recs = {}
        self.n_dma_sems = n_dma_sems
        self.dma_rr = {k: 0 for k in self.e}
        self.dma_last = {}
        self.sem = {k: nc.alloc_semaphore("s_" + k) for k in COMPUTE}
        self.dsems = {}
        for k in ("sp", "pool", "act"):
            for s in range(n_dma_sems):
                self.dsems[(k, s)] = nc.alloc_semaphore("d_%s_%d" % (k, s))
        self.cnt = {k: 0 for k in self.e}
        self.dcnt = {}
        self.seen = {k: {} for k in self.e}
        self.last_op = {k: None for k in self.e}
        self.n_inst = 0
        self.trace = {k: [] for k in self.e}

    def sbuf(self, name, shape, dtype):
        self.uid = getattr(self, "uid", 0) + 1
        name = "%s_%d" % (name, self.uid)
        t = self.stacks[-1].enter_context(self.nc.sbuf_tensor(name, list(shape), dtype))
        self.pstep[name] = int(np.prod(shape[1:]))
        self.recs.pop(name, None)
        return t

    def psum(self, name, shape, dtype=F32):
        t = self.stacks[-1].enter_context(self.nc.psum_tensor(name, list(shape), dtype))
        self.pstep[name] = int(np.prod(shape[1:]))
        self.psum_names.add(name)
        return t

    def dram(self, name, shape, dtype, kind="Internal", rowlen=None):
        t = self.nc.dram_tensor(name, list(shape), dtype, kind=kind)
        self.pstep[name] = int(rowlen if rowlen is not None else shape[-1])
        if kind == "ExternalInput":
            self.notrack.add(name)
        return t.ap()

    @contextlib.contextmanager
    def scope(self):
        self.stacks.append(contextlib.ExitStack())
        try:
            yield
            self.flush()
            self.barrier()
            self.flush()
        finally:
            self.stacks.pop().close()

    def _box(self, ap):
        name = ap.name
        ps = self.pstep[name]
        off = int(ap.offset)
        p0, f0 = divmod(off, ps)
        plo = phi = p0
        flo = fhi = f0
        for (step, cnt) in ap.ap:
            ext = step * (cnt - 1)
            if step != 0 and abs(step) >= ps and step % ps == 0:
                e = ext // ps
                if e < 0:
                    plo += e
                else:
                    phi += e
            else:
                if ext < 0:
                    flo += ext
                else:
                    fhi += ext
        return name, plo, phi, flo, fhi

    def _access(self, op, ap, is_write):
        if ap.name in self.notrack:
            return
        name, plo, phi, flo, fhi = self._box(ap)
        excl = name in self.psum_names
        if excl:
            flo, fhi = 0, self.pstep[name] - 1
            plo, phi = (plo // 32) * 32, (phi // 32) * 32 + 31
        lst = self.recs.get(name, [])
        keep = []
        for r in lst:
            ov = not (r[1] < plo or phi < r[0] or r[3] < flo or fhi < r[2])
            o = r[4]
            if ov and o is not op:
                same = (o.eng == op.eng) and not o.dma and not op.dma
                if same:
                    need = (op.eng != "pe") and r[5] and not is_write
                else:
                    need = excl or is_write or r[5]
                if need:
                    op.deps.append(o)
            contained = (plo <= r[0] and r[1] <= phi and flo <= r[2] and r[3] <= fhi)
            if contained and o is not op:
                same = (o.eng == op.eng) and not o.dma and not op.dma
                if excl or is_write or ((not r[5]) and same):
                    if not (excl and same and r[5] and not is_write and False):
                        continue
            keep.append(r)
        keep.append([plo, phi, flo, fhi, op, is_write])
        self.recs[name] = keep

    def _record(self, eng, fn, reads, writes, dma=False):
        op = _Op(eng, fn)
        op.dma = dma
        for ap in reads:
            if ap is not None and hasattr(ap, "ap"):
                self._access(op, ap, False)
        for ap in writes:
            if ap is not None and hasattr(ap, "ap"):
                self._access(op, ap, True)
        self.gseq = getattr(self, "gseq", 0) + 1
        op.seq = self.gseq
        if op.deps:
            best = {}
            keep = []
            for d_ in op.deps:
                if d_.dma:
                    keep.append(d_)
                else:
                    b_ = best.get(d_.eng)
                    if b_ is None or d_.seq > b_.seq:
                        best[d_.eng] = d_
            op.deps = keep + list(best.values())
        if dma:
            slot = self.dma_rr[eng]
            self.dma_rr[eng] = (slot + 1) % (2 if eng == "pool" else self.n_dma_sems)
            op.dsem = (eng, slot)
            op.qwait = self.dma_last.get((eng, slot))
            self.dma_last[(eng, slot)] = op
            self.dcnt[op.dsem] = self.dcnt.get(op.dsem, 0) + 16
            op.dval = self.dcnt[op.dsem]
        self.ops[eng].append(op)
        self.last_op[eng] = op
        return op

    def mm(self, out, lhsT, rhs, start=True, stop=True):
        return self._record("pe", lambda: self.nc.tensor.matmul(out, lhsT, rhs, start=start, stop=stop),
                            [lhsT, rhs], [out])

    def tr(self, out, in_, ident):
        return self._record("pe", lambda: self.nc.tensor.transpose(out, in_, ident), [in_, ident], [out])

    def act(self, out, in_, func, bias=None, scale=None, accum_out=None):
        kw = {}
        if bias is not None:
            kw["bias"] = bias
        if scale is not None:
            kw["scale"] = scale
        if accum_out is not None:
            kw["accum_out"] = accum_out
        return self._record("act", lambda: self.nc.scalar.activation(out, in_, func, **kw),
                            [in_, bias, scale], [out, accum_out])

    def tt(self, eng, out, in0, in1, op):
        return self._record(eng, lambda: self.e[eng].tensor_tensor(out, in0, in1, op), [in0, in1], [out])

    def ts(self, eng, out, in0, s1, s2=None, op0=ALU.mult, op1=None):
        kw = {}
        if op1 is not None:
            kw["op1"] = op1
        return self._record(eng, lambda: self.e[eng].tensor_scalar(out, in0, s1, s2, op0, **kw),
                            [in0, s1, s2], [out])

    def stt(self, eng, out, in0, scalar, in1, op0, op1):
        return self._record(eng, lambda: self.e[eng].scalar_tensor_tensor(out, in0, scalar, in1, op0, op1),
                            [in0, scalar, in1], [out])

    def copy(self, eng, out, in_):
        if eng == "act":
            return self._record("act", lambda: self.nc.scalar.copy(out, in_), [in_], [out])
        return self._record(eng, lambda: self.e[eng].tensor_copy(out, in_), [in_], [out])

    def memset(self, eng, ap, val):
        return self._record(eng, lambda: self.e[eng].memset(ap, val), [], [ap])

    def reduce(self, eng, out, in_, op, axis=AX.X):
        return self._record(eng, lambda: self.e[eng].tensor_reduce(out, in_, axis, op), [in_], [out])

    def scan(self, eng, out, d0, d1, init, op0, op1):
        return self._record(eng, lambda: self.e[eng].tensor_tensor_scan(out, d0, d1, init, op0, op1),
                            [d0, d1, init], [out])

    def recip(self, out, in_):
        return self._record("dve", lambda: self.nc.vector.reciprocal(out, in_), [in_], [out])

    def max8(self, out, in_):
        return self._record("dve", lambda: self.nc.vector.max(out, in_), [in_], [out])

    def dma(self, out, in_, q="sp", **kw):
        return self._record(q, lambda: self.e[q].dma_start(out=out, in_=in_, **kw), [in_], [out], dma=True)

    def barrier(self):
        lasts = [self.last_op[k] for k in COMPUTE if self.last_op[k] is not None]
        dlast = list(self.dma_last.values())
        for k in self.e:
            op = _Op(k, None)
            op.deps = [o for o in lasts if o.eng != k] + dlast
            self.ops[k].append(op)

    def flush(self):
        for k in self.ops:
            for op in self.ops[k]:
                for d in op.deps:
                    d.marked = True
        for k in self.ops:
            lst = self.ops[k]
            if not lst:
                continue
            if k in COMPUTE:
                real = [o for o in lst if o.fn is not None and not o.dma]
                if real:
                    real[-1].marked = True
                c = self.cnt[k]
                for op in lst:
                    if op.fn is not None and not op.dma and op.marked:
                        c += 1
                        op.val = c
                nxt = None
                for op in reversed(lst):
                    if op.fn is None or op.dma:
                        continue
                    if op.marked:
                        nxt = op.val
                    else:
                        op.val = nxt
        for k in self.ops:
            eng = self.e[k]
            seen = self.seen[k]
            for op in self.ops[k]:
                waits = {}
                for d in op.deps:
                    if d.dma:
                        key = ("d",) + d.dsem
                        s, v = self.dsems[d.dsem], d.dval
                    else:
                        if d.fn is None:
                            continue
                        key = ("c", d.eng)
                        s, v = self.sem[d.eng], d.val
                    if seen.get(key, 0) >= v:
                        continue
                    if key not in waits or waits[key][1] < v:
                        waits[key] = (s, v)
                if op.dma and op.qwait is not None:
                    key = ("d",) + op.dsem
                    v = op.qwait.dval
                    if seen.get(key, 0) < v and (key not in waits or waits[key][1] < v):
                        waits[key] = (self.dsems[op.dsem], v)
                for key, (s, v) in waits.items():
                    eng.wait_ge(s, v)
                    seen[key] = v
                    self.trace[k].append(("w", key, v))
                if op.fn is None:
                    continue
                ins = op.fn()
                self.n_inst += 1
                op.fn = True
                if op.dma:
                    ins.then_inc(self.dsems[op.dsem], 16)
                    self.trace[k].append(("i", ("d",) + op.dsem, 16))
                elif op.marked:
                    ins.then_inc(self.sem[k], 1)
                    self.cnt[k] = op.val
                    self.trace[k].append(("i", ("c", k), 1))
                else:
                    self.trace[k].append(("n", None, 0))
            self.ops[k] = []

    def simulate(self):
        sem = {}
        pos = {k: 0 for k in self.trace}
        progress = True
        while progress:
            progress = False
            for k, tr in self.trace.items():
                while pos[k] < len(tr):
                    typ, key, v = tr[pos[k]]
                    if typ == "w":
                        if sem.get(key, 0) < v:
                            break
                    elif typ == "i":
                        sem[key] = sem.get(key, 0) + v
                    pos[k] += 1
                    progress = True
        stuck = {k: (pos[k], len(tr), tr[pos[k]] if pos[k] < len(tr) else None) for k, tr in self.trace.items()}
        return all(pos[k] == len(tr) for k, tr in self.trace.items()), stuck, sem

    def finish(self):
        self.flush()
        for (k, slot), op in self.dma_last.items():
            self.e[k].wait_ge(self.dsems[(k, slot)], op.dval)
        self.stacks[0].close()


D = 1024
NTOK = 4608
NG = 9
SEQ = 2048
CTX = 256
NS = 2304
IN_W = 8240
EPS = 1e-6
C_MLQ, C_MLK, C_MLV, C_MLO, C_MLG = 0, 512, 1024, 1536, 2048
C_DFQ, C_DFK, C_DFV = 2064, 2576, 3088
C_GLQ, C_GLK, C_GLV, C_GLG, C_GLA = 3600, 3856, 4112, 4624, 5136
C_GATE = 5168
T_MLQ, T_MLK, T_DFQ, T_DFK, T_GLQ, T_GLK, T_GATE = 0, 512, 1024, 1536, 2048, 2304, 2560
T_ROWS = 2560 + 3072
K_MLK, K_MLV, K_MLO, K_DFV, K_GLK, K_GLV, K_GLG = 0, 512, 1024, 1536, 2048, 2304, 2816
K_COLS = 3328


def seq_ranges(s):
    return 256 * s, 512 + 2048 * s


def build(n_layers=2, stop_after=None, dbg=False, do_moe=True, skip=()):
    nc = bass.Bass("TRN2", target_bir_lowering=False)
    mk = MK(nc)
    IN = {}

    def din(name, shape, dtype=F32):
        IN[name] = mk.dram(name, shape, dtype, kind="ExternalInput")
        return IN[name]

    x2 = din("x2", [2, SEQ, D])
    ctx2 = din("ctx2", [2, CTX, D])
    cT = din("cT", [128, 8, 3])
    w_mod = din("w_mod", [2, D, 6 * D])
    b_modT = din("b_modT", [2, 128, 48])
    gmixT = din("gmixT", [2, 128, 8])
    gffnT = din("gffnT", [2, 128, 8])
    w_in = din("w_in", [2, D, IN_W])
    ml_gb = din("ml_gb", [2, 4, 4])
    ml_ng = din("ml_ng", [2, 128])
    df_lam = din("df_lam", [2, 4, 64])
    df_ng = din("df_ng", [2, 128])
    gl_wa = din("gl_wa", [2, 2, 16, 256])
    gl_baT = din("gl_baT", [2, 2, 128, 2])
    gl_ng = din("gl_ng", [2, 128])
    w_branch = din("w_branch", [2, 3, 512, D])
    w_out = din("w_out", [2, D, D])
    router_w = din("router_w", [2, D, 36])
    router_b = din("router_b", [2, 36])
    if do_moe:
        moe_wg = din("moe_wg", [2, 32, D, 512])
        moe_wu = din("moe_wu", [2, 32, D, 512])
        moe_wd = din("moe_wd", [2, 32, 512, D])
    fin_gT = din("fin_gT", [128, 8])
    consts = din("consts", [128, 128 + 128 + 128 + 2048 + 2048])

    okind = "ExternalOutput"
    y_out = mk.dram("y_out", [2, SEQ, D], F32, kind=okind)
    skind = "ExternalOutput" if dbg else "Internal"
    xT = mk.dram("xT", [D, NTOK], F32, kind=skind)
    PT = mk.dram("PT", [T_ROWS, NTOK], BF16, kind=skind)
    PK = mk.dram("PK", [NTOK, K_COLS], BF16, kind=skind)
    GT = mk.dram("GT", [48, NTOK], F32, kind=skind)
    BR = mk.dram("BR", [1536, NTOK], BF16, kind=skind)

    ident = mk.sbuf("ident", [128, 128], F32)
    identb = mk.sbuf("identb", [128, 128], BF16)
    onesb = mk.sbuf("onesb", [128, 128], BF16)
    onesf = mk.sbuf("onesf", [128, 128], F32)
    maskF = mk.sbuf("maskF", [128, 128], F32)
    maskB = mk.sbuf("maskB", [128, 128], F32)
    modv = mk.sbuf("modv", [128, 2, 6, 8, 3], F32)
    PS = [mk.psum("psb%d" % i, [128, 512], F32) for i in range(8)]

    mk.dma(ident[:], consts[:, 0:128])
    mk.dma(maskF[:], consts[:, 128:256])
    mk.dma(maskB[:], consts[:, 256:384])
    mk.copy("dve", identb[:], ident[:])
    mk.memset("pool", onesb[:], 1.0)
    mk.memset("pool", onesf[:], 1.0)

    rr = {"ev": 0}

    def evac_eng():
        rr["ev"] ^= 1
        return "act" if rr["ev"] else "dve"

    def phase_load_x():
        with mk.scope():
            xin = [mk.sbuf("xin%d" % i, [128, D], F32) for i in range(2)]
            xst = [mk.sbuf("xst%d" % i, [128, 8, 512], F32) for i in range(2)]
            for g in range(NG):
                st = xst[g % 2]
                for t4 in range(4):
                    tt = g * 4 + t4
                    if tt < 4:
                        src = ctx2[tt // 2, (tt % 2) * 128:(tt % 2) * 128 + 128, :]
                    else:
                        u = tt - 4
                        src = x2[u // 16, (u % 16) * 128:(u % 16) * 128 + 128, :]
                    xi = xin[tt % 2]
                    mk.dma(xi[:], src)
                    for half in range(2):
                        pb = PS[(tt * 2 + half) % 4]
                        for k4 in range(4):
                            kc = half * 4 + k4
                            mk.tr(pb[:, k4 * 128:(k4 + 1) * 128], xi[:, kc * 128:(kc + 1) * 128], ident[:])
                        mk.copy(evac_eng(), st[:, half * 4:(half + 1) * 4, t4 * 128:(t4 + 1) * 128],
                                pb[:].rearrange("p (k t) -> p k t", k=4))
                mk.dma(xT[:, g * 512:(g + 1) * 512].rearrange("(kc p) t -> p kc t", p=128), st[:], q="act")

    def phase_mod():
        with mk.scope():
            cs = mk.sbuf("cs", [128, 8, 3], F32)
            csg = mk.sbuf("csg", [128, 8, 3], F32)
            wm = [mk.sbuf("wm%d" % i, [128, 8, 512], F32) for i in range(2)]
            modT = mk.sbuf("modT", [128, 48, 3], F32)
            bm = mk.sbuf("bm", [128, 48], F32)
            gm = mk.sbuf("gm", [128, 8], F32)
            gf = mk.sbuf("gf", [128, 8], F32)
            mk.dma(cs[:], cT[:, :, :])
            mk.act(csg[:], cs[:], AF.Sigmoid)
            mk.tt("dve", cs[:], cs[:], csg[:], ALU.mult)
            for li in range(n_layers):
                mk.dma(bm[:], b_modT[li])
                mk.dma(gm[:], gmixT[li])
                mk.dma(gf[:], gffnT[li])
                for blk in range(12):
                    w = wm[blk % 2]
                    mk.dma(w[:], w_in_view(w_mod[li], blk * 512, 512), q=("sp" if blk % 2 else "act"))
                    for c4 in range(4):
                        fc = blk * 4 + c4
                        pb = PS[fc % 4]
                        for kc in range(8):
                            mk.mm(pb[:, 0:3], w[:, kc, c4 * 128:(c4 + 1) * 128], cs[:, kc, :],
                                  start=(kc == 0), stop=(kc == 7))
                        mk.ts("dve", modT[:, fc, :], pb[:, 0:3], bm[:, fc:fc + 1], None, op0=ALU.add)
                mv = modv[:, li]
                for j in range(3):
                    mk.ts("dve", mv[:, 0, :, j], modT[:, 8:16, j], 1.0, None, op0=ALU.add)
                    mk.tt("dve", mv[:, 0, :, j], mv[:, 0, :, j], gm[:], ALU.mult)
                    mk.copy("dve", mv[:, 1, :, j], modT[:, 0:8, j])
                    mk.copy("dve", mv[:, 2, :, j], modT[:, 16:24, j])
                    mk.ts("dve", mv[:, 3, :, j], modT[:, 32:40, j], 1.0, None, op0=ALU.add)
                    mk.tt("dve", mv[:, 3, :, j], mv[:, 3, :, j], gf[:], ALU.mult)
                    mk.copy("dve", mv[:, 4, :, j], modT[:, 24:32, j])
                    mk.copy("dve", mv[:, 5, :, j], modT[:, 40:48, j])

    def w_in_view(w2d, c0, n):
        return w2d[:, c0:c0 + n].rearrange("(kc p) n -> p kc n", p=128)

    def gset(g):
        return 0 if g == 0 else (1 if g <= 4 else 2)

    def norm_groups(li, which_gs, hT, scratch, h32_cb=None):
        xs_b, sq_b, rs_b = scratch
        for g in range(NG):
            j = gset(g)
            xs = xs_b[g % 2]
            sq = sq_b[g % 2]
            rs = rs_b[g % 2]
            mk.dma(xs[:], xT[:, g * 512:(g + 1) * 512].rearrange("(kc p) t -> p kc t", p=128),
                   q=("sp" if g % 2 else "act"))
            mk.tt("pool", sq[:], xs[:], xs[:], ALU.mult)
            pb = PS[4 + g % 2]
            for kc in range(8):
                mk.mm(pb[:], onesb[:], sq[:, kc, :], start=(kc == 0), stop=(kc == 7))
            mk.act(rs[:], pb[:], AF.Sqrt, bias=EPS, scale=1.0 / D)
            mk.recip(rs[:], rs[:])
            for kc in range(8):
                mk.stt("dve", xs[:, kc, :], xs[:, kc, :], modv[:, li, which_gs, kc, j:j + 1], rs[:],
                       ALU.mult, ALU.mult)
                mk.act(hT[:, kc, g * 512:(g + 1) * 512], xs[:, kc, :], AF.Identity,
                       bias=modv[:, li, which_gs + 1, kc, j:j + 1], scale=1.0)
                if h32_cb is not None:
                    mk.ts("pool", xs[:, kc, :], xs[:, kc, :], modv[:, li, which_gs + 1, kc, j:j + 1], None,
                          op0=ALU.add)
            if h32_cb is not None:
                h32_cb(g, xs)

    def phase_proj(li, hT):
        cosT = mk.sbuf("cosT", [128, SEQ], F32)
        sinT = mk.sbuf("sinT", [128, SEQ], F32)
        mk.dma(cosT[:], consts[:, 384:384 + SEQ])
        mk.dma(sinT[:], consts[:, 384 + SEQ:384 + 2 * SEQ])
        wb = [mk.sbuf("wblk%d" % i, [128, 8, 512], BF16) for i in range(2)]
        stT = [mk.sbuf("stT%d" % i, [128, NTOK], BF16) for i in range(2)]
        stK = [mk.sbuf("stK%d" % i, [128, 4, 512], BF16) for i in range(2)]
        stG = mk.sbuf("stG", [16, NTOK], F32)
        wrot = mk.sbuf("wrot", [128, 8, 512], BF16)
        stG2 = mk.sbuf("stG2", [32, NTOK], F32)
        t1 = [mk.sbuf("rp1_%d" % i, [128, 512], F32) for i in range(2)]
        t2 = [mk.sbuf("rp2_%d" % i, [128, 512], F32) for i in range(2)]
        cnt = {"blk": 0, "ps": 0, "st": 0}

        def load_blk(c0, n):
            w = wb[cnt["blk"] % 2]
            cnt["blk"] += 1
            mk.dma(w[:, :, 0:n], w_in_view(w_in[li], c0, n), q="pool")
            return w

        def nextps():
            cnt["ps"] += 1
            return PS[cnt["ps"] % 4]

        tblocks = [
            (C_MLQ, 512, T_MLQ, "copy", 1.0),
            (C_MLK, 512, T_MLK, "copy", 128.0 ** -0.5),
            (C_DFQ, 512, T_DFQ, "rope", 1.0),
            (C_DFK, 512, T_DFK, "rope", 1.0),
            (C_GLQ, 256, T_GLQ, "copy", 0.125),
            (C_GLK, 256, T_GLK, "copy", 1.0),
        ] + [(C_GATE + i * 512, 512, T_GATE + i * 512, "sigmoid", 1.0) for i in range(6)]
        for (c0, n, r0, mode, scale) in tblocks:
            w = load_blk(c0, n)
            if mode == "rope":
                wv = w[:].rearrange("p k (b h e) -> p (k b) h e", h=2, e=16)
                rv = wrot[:].rearrange("p k (b h e) -> p (k b) h e", h=2, e=16)
                mk.copy("pool", rv[:, :, 0, :], wv[:, :, 1, :])
                mk.copy("pool", rv[:, :, 1, :], wv[:, :, 0, :])
            for cc in range(n // 128):
                st = stT[cnt["st"] % 2]
                cnt["st"] += 1
                for g in range(NG):
                    pb = nextps()
                    for kc in range(8):
                        mk.mm(pb[:], w[:, kc, cc * 128:(cc + 1) * 128], hT[:, kc, g * 512:(g + 1) * 512],
                              start=(kc == 0), stop=(kc == 7))
                    dst = st[:, g * 512:(g + 1) * 512]
                    if mode == "sigmoid":
                        mk.act(dst, pb[:], AF.Sigmoid)
                    elif mode == "rope" and g >= 1:
                        pr = nextps()
                        for kc in range(8):
                            mk.mm(pr[:], wrot[:, kc, cc * 128:(cc + 1) * 128], hT[:, kc, g * 512:(g + 1) * 512],
                                  start=(kc == 0), stop=(kc == 7))
                        tok0 = ((g - 1) % 4) * 512
                        a = t1[g % 2]
                        b = t2[g % 2]
                        mk.tt("dve", a[:], pb[:], cosT[:, tok0:tok0 + 512], ALU.mult)
                        mk.tt("dve", b[:], pr[:], sinT[:, tok0:tok0 + 512], ALU.mult)
                        mk.tt("pool", dst, a[:], b[:], ALU.add)
                    else:
                        e = evac_eng()
                        if e == "act":
                            mk.act(dst, pb[:], AF.Copy, scale=scale)
                        else:
                            mk.ts("dve", dst, pb[:], scale, None, op0=ALU.mult)
                mk.dma(PT[r0 + cc * 128:r0 + (cc + 1) * 128, :], st[:], q="sp")
        for (c0, n, r0) in [(C_MLG, 16, 0), (C_GLA, 32, 16)]:
            w = load_blk(c0, n)
            for g in range(NG):
                pb = nextps()
                for kc in range(8):
                    mk.mm(pb[0:n, :], w[:, kc, 0:n], hT[:, kc, g * 512:(g + 1) * 512],
                          start=(kc == 0), stop=(kc == 7))
                if r0 == 0:
                    mk.copy("dve", stG[0:16, g * 512:(g + 1) * 512], pb[0:16, :])
                else:
                    mk.copy("dve", stG2[:, g * 512:(g + 1) * 512], pb[0:32, :])
        mk.dma(GT[0:16, :], stG[0:16, :], q="sp")
        mk.dma(GT[16:48, :], stG2[:, :], q="sp")
        kblocks = [
            (C_MLK, 512, K_MLK, "copy", 128.0 ** -0.5),
            (C_MLV, 512, K_MLV, "copy", 1.0),
            (C_MLO, 512, K_MLO, "sigmoid", 1.0),
            (C_DFV, 512, K_DFV, "copy", 1.0),
            (C_GLK, 256, K_GLK, "copy", 1.0),
            (C_GLV, 512, K_GLV, "copy", 1.0),
            (C_GLG, 512, K_GLG, "silu", 1.0),
        ]
        sg = [mk.sbuf("sgk%d" % i, [128, 512], F32) for i in range(2)]
        for (c0, n, k0, mode, scale) in kblocks:
            w = load_blk(c0, n)
            for t4 in range(NTOK // 512):
                st = stK[cnt["st"] % 2]
                cnt["st"] += 1
                for q4 in range(4):
                    tt_ = t4 * 4 + q4
                    pb = nextps()
                    for kc in range(8):
                        mk.mm(pb[:, 0:n], hT[:, kc, tt_ * 128:(tt_ + 1) * 128], w[:, kc, 0:n],
                              start=(kc == 0), stop=(kc == 7))
                    dst = st[:, q4, 0:n]
                    if mode == "sigmoid":
                        mk.act(dst, pb[:, 0:n], AF.Sigmoid)
                    elif mode == "silu":
                        s_ = sg[tt_ % 2]
                        mk.act(s_[:, 0:n], pb[:, 0:n], AF.Sigmoid)
                        mk.tt("dve", dst, pb[:, 0:n], s_[:, 0:n], ALU.mult)
                    else:
                        e = evac_eng()
                        if e == "act":
                            mk.act(dst, pb[:, 0:n], AF.Copy, scale=scale)
                        else:
                            mk.ts("dve", dst, pb[:, 0:n], scale, None, op0=ALU.mult)
                mk.dma(PK[t4 * 512:(t4 + 1) * 512, k0:k0 + n].rearrange("(a p) c -> p a c", p=128),
                       st[:, :, 0:n], q="sp")

    def load_seq_T(dst, src_rows, s, q="sp"):
        c0, l0 = seq_ranges(s)
        mk.dma(dst[:, 0:CTX], src_rows[:, c0:c0 + CTX], q=q)
        mk.dma(dst[:, CTX:NS], src_rows[:, l0:l0 + SEQ], q=q)

    def store_seq_T(dst_rows, src, s, q="sp"):
        c0, l0 = seq_ranges(s)
        mk.dma(dst_rows[:, c0:c0 + CTX], src[:, 0:CTX], q=q)
        mk.dma(dst_rows[:, l0:l0 + SEQ], src[:, CTX:NS], q=q)

    def load_seq_K(dst3, k0, ncol, s, q="sp"):
        c0, l0 = seq_ranges(s)
        mk.dma(dst3[:, 0:2], PK[c0:c0 + CTX, k0:k0 + ncol].rearrange("(a p) c -> p a c", p=128), q=q)
        mk.dma(dst3[:, 2:18], PK[l0:l0 + SEQ, k0:k0 + ncol].rearrange("(a p) c -> p a c", p=128), q=q)

    def bcast_row(dst, src_row):
        mk.dma(dst, src_row.to_broadcast([128, src_row.shape[-1]]))

    def tok_to_BR(src, r0, s, stg):
        for h in range(4):
            st = stg[h % 2]
            for t4 in range(5):
                n = min(4, 18 - t4 * 4)
                pb = PS[t4 % 4]
                for j in range(n):
                    t = t4 * 4 + j
                    mk.mm(pb[:, j * 128:(j + 1) * 128], src[:, t, h * 128:(h + 1) * 128], identb[:])
                mk.copy(evac_eng(), st[:, t4 * 512:t4 * 512 + n * 128], pb[:, 0:n * 128])
            store_seq_T(BR[r0 + h * 128:r0 + (h + 1) * 128, :], st, s, q="act")

    def phase_attn(li):
        lam_init = 0.8 - 0.6 * math.exp(-0.3 * li)
        with mk.scope():
            lam_t = mk.sbuf("lam_t", [1, 4, 64], F32)
            lam_p = mk.sbuf("lam_p", [1, 2, 64], F32)
            lam_s = mk.sbuf("lam_s", [1, 4], F32)
            nlam = mk.sbuf("nlam", [128, 1], F32)
            gv = mk.sbuf("dfgv", [128, 128], F32)
            mk.dma(lam_t[:], df_lam[li:li + 1, :, :])
            mk.tt("dve", lam_p[:, 0, :], lam_t[:, 0, :], lam_t[:, 1, :], ALU.mult)
            mk.tt("dve", lam_p[:, 1, :], lam_t[:, 2, :], lam_t[:, 3, :], ALU.mult)
            mk.reduce("dve", lam_s[:, 0:2], lam_p[:], ALU.add)
            mk.act(lam_s[:, 0:2], lam_s[:, 0:2], AF.Exp)
            mk.tt("dve", lam_s[:, 2:3], lam_s[:, 0:1], lam_s[:, 1:2], ALU.subtract)
            mk.ts("dve", lam_s[:, 2:3], lam_s[:, 2:3], lam_init, -1.0, op0=ALU.add, op1=ALU.mult)
            mk.mm(PS[6][:, 0:1], onesf[0:1, :], lam_s[0:1, 2:3])
            mk.copy("dve", nlam[:], PS[6][:, 0:1])
            bcast_row(gv[:], df_ng[li:li + 1, :])
            mk.ts("dve", gv[:], gv[:], 1.0 - lam_init, None, op0=ALU.mult)
            qT = mk.sbuf("aqT", [128, 4, NS], BF16)
            kT = mk.sbuf("akT", [128, 4, NS], BF16)
            sq = mk.sbuf("asq", [128, 4, NS], BF16)
            vA = mk.sbuf("avA", [128, 18, 4, 129], BF16)
            DF = mk.sbuf("aDF", [128, 18, 512], BF16)
            pt = [mk.sbuf("apt%d" % i, [128, 512], BF16) for i in range(4)]
            mxs = mk.sbuf("amx", [1, 64], F32)
            negM = mk.sbuf("anegM", [128, 1], F32)
            o1 = [mk.sbuf("ao1_%d" % i, [128, 128], F32) for i in range(2)]
            o2 = [mk.sbuf("ao2_%d" % i, [128, 128], F32) for i in range(2)]
            rc = [mk.sbuf("arc%d" % i, [128, 4], F32) for i in range(2)]
            stg = [mk.sbuf("astg%d" % i, [128, NS], BF16) for i in range(2)]
            mk.memset("pool", vA[:], 1.0)
            for s in range(2):
                for h in range(4):
                    load_seq_T(qT[:, h, :], PT[T_DFQ + h * 128:T_DFQ + (h + 1) * 128, :], s, q="sp")
                    load_seq_T(kT[:, h, :], PT[T_DFK + h * 128:T_DFK + (h + 1) * 128, :], s, q="act")
                c0, l0 = seq_ranges(s)
                for h in range(4):
                    mk.dma(vA[:, 0:2, h, 0:128],
                           PK[c0:c0 + CTX, K_DFV + h * 128:K_DFV + (h + 1) * 128].rearrange("(a p) c -> p a c", p=128))
                    mk.dma(vA[:, 2:18, h, 0:128],
                           PK[l0:l0 + SEQ, K_DFV + h * 128:K_DFV + (h + 1) * 128].rearrange("(a p) c -> p a c", p=128))
                mk.memset("dve", mxs[:], 0.0)
                for which, src in ((0, qT), (1, kT)):
                    mk.tt("pool", sq[:], src[:], src[:], ALU.mult)
                    idx = 0
                    for h in range(4):
                        for cst in range(0, NS, 512):
                            n = min(512, NS - cst)
                            pb = PS[3 + idx % 4]
                            mk.mm(pb[0:1, 0:n], onesb[:, 0:1], sq[:, h, cst:cst + n])
                            mk.reduce("dve", mxs[:, which * 32 + idx:which * 32 + idx + 1], pb[0:1, 0:n], ALU.max)
                            idx += 1
                mk.reduce("dve", mxs[:, 60:61], mxs[:, 0:32], ALU.max)
                mk.reduce("dve", mxs[:, 61:62], mxs[:, 32:60], ALU.max)
                mk.tt("dve", mxs[:, 62:63], mxs[:, 60:61], mxs[:, 61:62], ALU.mult)
                mk.act(mxs[:, 63:64], mxs[:, 62:63], AF.Sqrt, scale=1.0 / 64.0)
                mk.ts("dve", mxs[:, 63:64], mxs[:, 63:64], -1.0, None, op0=ALU.mult)
                mk.mm(PS[6][:, 0:1], onesf[0:1, :], mxs[0:1, 63:64])
                mk.copy("dve", negM[:], PS[6][:, 0:1])
                import os as _os
                if _os.environ.get('ATTN_STOP') == '1':
                    return
                qgroups = [(0, CTX, 0, 2)] + [(CTX + i * 512, 512, 0, 18) for i in range(4)]
                ci = 0
                it = 0
                for (q0, nq, tk0, tk1) in qgroups:
                    nsub = nq // 128
                    for h in range(4):
                        ob = 3 * (it % 2)
                        it += 1
                        O = [[PS[ob + (a * 4 + sb) // 3][:, ((a * 4 + sb) % 3) * 160:((a * 4 + sb) % 3) * 160 + 129]
                              for sb in range(4)] for a in range(2)]
                        steps = [(tk, a) for tk in range(tk0, tk1) for a in range(2)]
                        started = set()
                        bufs = {}

                        def emit_S(i):
                            tk, a = steps[i]
                            pb = PS[6 + (ci + i) % 2]
                            p_ = pt[(ci + i) % 4]
                            bufs[i] = p_
                            mk.mm(pb[:, 0:nq], kT[a * 64:(a + 1) * 64, h, tk * 128:(tk + 1) * 128],
                                  qT[a * 64:(a + 1) * 64, h, q0:q0 + nq])
                            mk.act(p_[:, 0:nq], pb[:, 0:nq], AF.Exp, bias=negM[:, 0:1], scale=0.125)

                        def emit_PV(i):
                            tk, a = steps[i]
                            p_ = bufs.pop(i)
                            for sb in range(nsub):
                                bank = (a * 4 + sb) // 3
                                st_ = (tk == tk0) and (bank not in started)
                                started.add(bank)
                                mk.mm(O[a][sb], p_[:, sb * 128:(sb + 1) * 128], vA[:, tk, h, :],
                                      start=st_, stop=(tk == tk1 - 1))

                        emit_S(0)
                        for i in range(len(steps)):
                            if i + 1 < len(steps):
                                emit_S(i + 1)
                            emit_PV(i)
                        ci += len(steps)
                        for sb in range(nsub):
                            tile = (q0 + sb * 128) // 128
                            r_ = rc[sb % 2]
                            a1 = o1[sb % 2]
                            a2 = o2[sb % 2]
                            mk.recip(r_[:, 0:1], O[0][sb][:, 128:129])
                            mk.recip(r_[:, 1:2], O[1][sb][:, 128:129])
                            mk.tt("dve", r_[:, 1:2], r_[:, 1:2], nlam[:, 0:1], ALU.mult)
                            mk.ts("dve", a1[:], O[0][sb][:, 0:128], r_[:, 0:1], None, op0=ALU.mult)
                            mk.stt("dve", a1[:], O[1][sb][:, 0:128], r_[:, 1:2], a1[:], ALU.mult, ALU.add)
                            mk.act(a2[:], a1[:], AF.Square, accum_out=r_[:, 2:3])
                            mk.act(r_[:, 3:4], r_[:, 2:3], AF.Sqrt, bias=EPS, scale=1.0 / 128.0)
                            mk.recip(r_[:, 3:4], r_[:, 3:4])
                            mk.stt("dve", DF[:, tile, h * 128:(h + 1) * 128], a1[:], r_[:, 3:4], gv[:],
                                   ALU.mult, ALU.mult)
                if _os.environ.get('ATTN_STOP') == '2':
                    return
                tok_to_BR(DF, 512, s, stg)
                if _os.environ.get('ATTN_STOP') == '3':
                    return

    def phase_merge(li):
        with mk.scope():
            wbr = mk.sbuf("wbr", [128, 12, D], BF16)
            wo = mk.sbuf("wo", [128, 8, D], BF16)
            for b in range(3):
                mk.dma(wbr[:, b * 4:(b + 1) * 4, :], w_branch[li, b].rearrange("(kc p) n -> p kc n", p=128), q="pool")
            mk.dma(wo[:], w_out[li].rearrange("(kc p) n -> p kc n", p=128), q="pool")
            brT = [mk.sbuf("mbr%d" % i, [128, 12, 512], BF16) for i in range(2)]
            gt = [mk.sbuf("mgt%d" % i, [128, 24, 512], BF16) for i in range(2)]
            xs_b = [mk.sbuf("mxs%d" % i, [128, 8, 512], F32) for i in range(2)]
            yT = [mk.sbuf("myT%d" % i, [128, 8, 512], BF16) for i in range(2)]
            ya = [mk.sbuf("mya%d" % i, [128, 512], F32) for i in range(2)]
            tmp = [mk.sbuf("mtm%d" % i, [128, 512], F32) for i in range(2)]
            ci = 0
            for g in range(NG):
                j = gset(g)
                b_ = brT[g % 2]
                g_ = gt[g % 2]
                xs = xs_b[g % 2]
                y_ = yT[g % 2]
                tsl = slice(g * 512, (g + 1) * 512)
                mk.dma(b_[:], BR[:, tsl].rearrange("(c p) t -> p c t", p=128), q="sp")
                mk.dma(g_[:], PT[T_GATE:T_GATE + 3072, tsl].rearrange("(c p) t -> p c t", p=128), q="act")
                mk.dma(xs[:], xT[:, tsl].rearrange("(kc p) t -> p kc t", p=128), q="sp")
                for oc in range(8):
                    acc = ya[oc % 2]
                    for br in range(3):
                        pb = PS[ci % 4]
                        ci += 1
                        for kc in range(4):
                            mk.mm(pb[:], wbr[:, br * 4 + kc, oc * 128:(oc + 1) * 128], b_[:, br * 4 + kc, :],
                                  start=(kc == 0), stop=(kc == 3))
                        if br == 0:
                            mk.tt("dve", acc[:], pb[:], g_[:, br * 8 + oc, :], ALU.mult)
                        else:
                            t_ = tmp[br % 2]
                            mk.tt("dve", t_[:], pb[:], g_[:, br * 8 + oc, :], ALU.mult)
                            mk.tt("pool", (acc[:] if br == 1 else y_[:, oc, :]), acc[:], t_[:], ALU.add)
                for oc in range(8):
                    pb = PS[4 + oc % 3]
                    for kc in range(8):
                        mk.mm(pb[:], wo[:, kc, oc * 128:(oc + 1) * 128], y_[:, kc, :], start=(kc == 0), stop=(kc == 7))
                    mk.stt("dve", xs[:, oc, :], pb[:], modv[:, li, 2, oc, j:j + 1], xs[:, oc, :], ALU.mult, ALU.add)
                mk.dma(xT[:, tsl].rearrange("(kc p) t -> p kc t", p=128), xs[:], q="act")

    def phase_moe(li):
        HALF = NTOK // 2
        with mk.scope():
            h2T = mk.sbuf("h2T", [128, 8, NTOK], BF16)
            comb = mk.sbuf("comb", [128, 36, 32], F32)
            with mk.scope():
                scratch = ([mk.sbuf("nxs%d" % i, [128, 8, 512], F32) for i in range(2)],
                           [mk.sbuf("nsq%d" % i, [128, 8, 512], BF16) for i in range(2)],
                           [mk.sbuf("nrs%d" % i, [128, 512], F32) for i in range(2)])
                wr = mk.sbuf("wr", [128, 8, 36], F32)
                rb = mk.sbuf("rb", [128, 36], F32)
                L = mk.sbuf("rL", [128, 36], F32)
                sm = mk.sbuf("rsm", [128, 16], F32)
                mg = mk.sbuf("rmg", [128, 4], F32)
                eg = mk.sbuf("reg", [128, 4], F32)
                ls = mk.sbuf("rls", [128, 8], F32)
                t8 = mk.sbuf("rt8", [128, 8], F32)
                s1 = mk.sbuf("rs1", [128, 8], F32)
                s2 = mk.sbuf("rs2", [128, 8], F32)
                mk.dma(wr[:], router_w[li].rearrange("(kc p) n -> p kc n", p=128))
                bcast_row(rb[:], router_b[li:li + 1, :])

                def route(g, xs):
                    for t4 in range(4):
                        tile = g * 4 + t4
                        pb = PS[t4 % 4]
                        for kc in range(8):
                            mk.mm(pb[:, 0:36], xs[:, kc, t4 * 128:(t4 + 1) * 128], wr[:, kc, :],
                                  start=(kc == 0), stop=(kc == 7))
                        mk.tt("dve", L[:], pb[:, 0:36], rb[:], ALU.add)
                        mk.reduce("dve", sm[:, 0:1], L[:, 0:4], ALU.max)
                        mk.ts("dve", mg[:], L[:, 0:4], sm[:, 0:1], None, op0=ALU.is_ge)
                        mk.ts("dve", sm[:, 1:2], sm[:, 0:1], -1.0, None, op0=ALU.mult)
                        mk.act(eg[:], L[:, 0:4], AF.Exp, bias=sm[:, 1:2], scale=1.0, accum_out=sm[:, 2:3])
                        mk.recip(sm[:, 3:4], sm[:, 2:3])
                        mk.ts("dve", ls[:], L[:, 4:12], mg[:, 0:1], None, op0=ALU.mult)
                        for gi in range(1, 4):
                            mk.stt("dve", ls[:], L[:, 4 + 8 * gi:12 + 8 * gi], mg[:, gi:gi + 1], ls[:],
                                   ALU.mult, ALU.add)
                        mk.max8(t8[:], ls[:])
                        mk.tt("dve", sm[:, 4:5], t8[:, 1:2], t8[:, 0:1], ALU.subtract)
                        mk.act(sm[:, 5:6], sm[:, 4:5], AF.Exp)
                        mk.ts("dve", sm[:, 6:7], sm[:, 5:6], 1.0, None, op0=ALU.add)
                        mk.recip(sm[:, 6:7], sm[:, 6:7])
                        mk.tt("dve", sm[:, 7:8], sm[:, 6:7], sm[:, 3:4], ALU.mult)
                        mk.tt("dve", sm[:, 8:9], sm[:, 7:8], sm[:, 5:6], ALU.mult)
                        mk.tt("dve", sm[:, 9:10], sm[:, 7:8], sm[:, 8:9], ALU.subtract)
                        mk.ts("dve", s1[:], ls[:], t8[:, 0:1], sm[:, 9:10], op0=ALU.is_ge, op1=ALU.mult)
                        mk.ts("dve", s2[:], ls[:], t8[:, 1:2], sm[:, 8:9], op0=ALU.is_ge, op1=ALU.mult)
                        mk.tt("dve", s1[:], s1[:], s2[:], ALU.add)
                        for gi in range(4):
                            mk.ts("dve", comb[:, tile, gi * 8:(gi + 1) * 8], s1[:], mg[:, gi:gi + 1], None,
                                  op0=ALU.mult)

                norm_groups(li, 3, h2T, scratch, h32_cb=route)
            if dbg:
                comb_dbg = mk.dram("comb_dbg%d" % li, [128, 36 * 32], F32, kind="ExternalOutput")
                mk.dma(comb_dbg[:, :], comb[:].rearrange("p a b -> p (a b)"))
            wg_b = [mk.sbuf("ewg%d" % i, [128, 8, 512], BF16) for i in range(2)]
            wu_b = [mk.sbuf("ewu%d" % i, [128, 8, 512], BF16) for i in range(2)]
            wd_b = [mk.sbuf("ewd%d" % i, [128, 4, D], BF16) for i in range(2)]
            hid_b = [mk.sbuf("ehid%d" % i, [128, 4, 512], BF16) for i in range(2)]
            sl_b = [mk.sbuf("esl%d" % i, [128, 512], F32) for i in range(2)]
            NPART = 3
            PT_TILES = 36 // NPART
            yacc = mk.sbuf("yacc", [128, PT_TILES, D], F32)
            xs_b = [mk.sbuf("exs%d" % i, [128, 8, 128], F32) for i in range(2)]
            ci = 0
            gi = 0
            pend = [None]
            dk = [0]

            def down(hid, wd, t0, nt, e, hf):
                for sb in range(nt // 128):
                    tile = (t0 + sb * 128) // 128
                    lt = tile - hf * PT_TILES
                    for hh in range(2):
                        py = PS[5 + dk[0] % 3]
                        dk[0] += 1
                        for fc in range(4):
                            mk.mm(py[:], hid[:, fc, sb * 128:(sb + 1) * 128], wd[:, fc, hh * 512:(hh + 1) * 512],
                                  start=(fc == 0), stop=(fc == 3))
                        mk.stt("dve", yacc[:, lt, hh * 512:(hh + 1) * 512], py[:], comb[:, tile, e:e + 1],
                               yacc[:, lt, hh * 512:(hh + 1) * 512], ALU.mult, ALU.add)

            for hf in range(NPART):
                mk.memset("pool", yacc[:], 0.0)
                grp = [(hf * PT_TILES * 128 + i * 512, 512) for i in range(PT_TILES // 4)]
                for e in range(32):
                    wg, wu, wd = wg_b[e % 2], wu_b[e % 2], wd_b[e % 2]
                    mk.dma(wg[:], moe_wg[li, e].rearrange("(kc p) n -> p kc n", p=128), q="pool")
                    mk.dma(wu[:], moe_wu[li, e].rearrange("(kc p) n -> p kc n", p=128), q="pool")
                    mk.dma(wd[:], moe_wd[li, e].rearrange("(kc p) n -> p kc n", p=128), q="pool")
                    for (t0, nt) in grp:
                        hid = hid_b[gi % 2]
                        gi += 1
                        for fc in range(4):
                            pg = PS[ci % 3]
                            pu = PS[3 + ci % 2]
                            sl = sl_b[ci % 2]
                            ci += 1
                            for kc in range(8):
                                mk.mm(pg[:, 0:nt], wg[:, kc, fc * 128:(fc + 1) * 128], h2T[:, kc, t0:t0 + nt],
                                      start=(kc == 0), stop=(kc == 7))
                            for kc in range(8):
                                mk.mm(pu[:, 0:nt], wu[:, kc, fc * 128:(fc + 1) * 128], h2T[:, kc, t0:t0 + nt],
                                      start=(kc == 0), stop=(kc == 7))
                            mk.act(sl[:, 0:nt], pg[:, 0:nt], AF.Silu)
                            mk.tt("dve", hid[:, fc, 0:nt], pu[:, 0:nt], sl[:, 0:nt], ALU.mult)
                        if pend[0] is not None:
                            down(*pend[0])
                        pend[0] = (hid, wd, t0, nt, e, hf)
                if pend[0] is not None:
                    down(*pend[0])
                    pend[0] = None
                for lt in range(PT_TILES):
                    tile = hf * PT_TILES + lt
                    j = gset(tile // 4)
                    xs = xs_b[lt % 2]
                    tsl = slice(tile * 128, (tile + 1) * 128)
                    mk.dma(xs[:], xT[:, tsl].rearrange("(kc p) t -> p kc t", p=128), q="sp")
                    for half in range(2):
                        pb = PS[(lt * 2 + half) % 4]
                        for k4 in range(4):
                            kc = half * 4 + k4
                            mk.tr(pb[:, k4 * 128:(k4 + 1) * 128], yacc[:, lt, kc * 128:(kc + 1) * 128], ident[:])
                        for k4 in range(4):
                            kc = half * 4 + k4
                            mk.stt("dve", xs[:, kc, :], pb[:, k4 * 128:(k4 + 1) * 128], modv[:, li, 5, kc, j:j + 1],
                                   xs[:, kc, :], ALU.mult, ALU.add)
                    mk.dma(xT[:, tsl].rearrange("(kc p) t -> p kc t", p=128), xs[:], q="act")

    def phase_final():
        with mk.scope():
            fg = mk.sbuf("fg", [128, 8], F32)
            mk.dma(fg[:], fin_gT[:, :])
            xs_b = [mk.sbuf("fxs%d" % i, [128, 8, 512], F32) for i in range(2)]
            sq_b = [mk.sbuf("fsq%d" % i, [128, 8, 512], BF16) for i in range(2)]
            rs_b = [mk.sbuf("frs%d" % i, [128, 512], F32) for i in range(2)]
            ot = [mk.sbuf("fot%d" % i, [128, D], F32) for i in range(2)]
            for g in range(1, NG):
                xs, sq, rs = xs_b[g % 2], sq_b[g % 2], rs_b[g % 2]
                mk.dma(xs[:], xT[:, g * 512:(g + 1) * 512].rearrange("(kc p) t -> p kc t", p=128),
                       q=("sp" if g % 2 else "act"))
                mk.tt("pool", sq[:], xs[:], xs[:], ALU.mult)
                pb = PS[4 + g % 2]
                for kc in range(8):
                    mk.mm(pb[:], onesb[:], sq[:, kc, :], start=(kc == 0), stop=(kc == 7))
                mk.act(rs[:], pb[:], AF.Sqrt, bias=EPS, scale=1.0 / D)
                mk.recip(rs[:], rs[:])
                for kc in range(8):
                    mk.stt("dve", xs[:, kc, :], xs[:, kc, :], fg[:, kc:kc + 1], rs[:], ALU.mult, ALU.mult)
                for t4 in range(4):
                    u = (g - 1) * 4 + t4
                    o_ = ot[u % 2]
                    for half in range(2):
                        pq = PS[(u * 2 + half) % 4]
                        for k4 in range(4):
                            kc = half * 4 + k4
                            mk.tr(pq[:, k4 * 128:(k4 + 1) * 128], xs[:, kc, t4 * 128:(t4 + 1) * 128], ident[:])
                        mk.copy(evac_eng(), o_[:, half * 512:(half + 1) * 512], pq[:])
                    mk.dma(y_out[u // 16, (u % 16) * 128:(u % 16) * 128 + 128, :], o_[:], q="sp")

    def nat_chunk(d, cs):
        if d == 0:
            return cs
        return (3 - cs) if cs < 4 else (35 - (cs - 4))

    def rev_copy(eng, dst, src):
        mk.copy(eng, dst[:, 0:CTX], src[:, 0:CTX][:, ::-1])
        mk.copy(eng, dst[:, CTX:NS], src[:, CTX:NS][:, ::-1])

    def phase_mlstm(li):
        with mk.scope():
            HS = mk.sbuf("mHS", [128, 18, 512], F32)
            gb = mk.sbuf("mgb", [4, 4], F32)
            ngb = mk.sbuf("mngb", [4, 4], F32)
            gvn = mk.sbuf("mgvn", [128, 128], F32)
            ones4 = mk.sbuf("mones4", [4, NS], F32)
            mk.dma(gb[:], ml_gb[li])
            mk.ts("dve", ngb[:], gb[:], -1.0, None, op0=ALU.mult)
            bcast_row(gvn[:], ml_ng[li:li + 1, :])
            mk.memset("pool", ones4[:], 1.0)
            for s in range(2):
                with mk.scope():
                    qT = mk.sbuf("mqT", [128, 4, NS], BF16)
                    kT = mk.sbuf("mkT", [128, 4, NS], BF16)
                    kK = mk.sbuf("mkK", [128, 18, 512], BF16)
                    vA = mk.sbuf("mvA", [128, 18, 4, 129], BF16)
                    uV = mk.sbuf("muV", [128, 18, 4, 129], BF16)
                    R = [mk.sbuf("mR%d" % i, [4, NS], F32) for i in range(5)]
                    COL = mk.sbuf("mCOL", [128, 3, 18, 4], F32)
                    DECr = mk.sbuf("mDECr", [4, 36, 4], F32)
                    DECb = mk.sbuf("mDECb", [128, 36, 4], F32)
                    CT = mk.sbuf("mCT", [128, 4, 129], F32)
                    tmpC = mk.sbuf("mtmpC", [128, 4, 129], F32)
                    CTb = [mk.sbuf("mCTb%d" % i, [128, 4, 129], BF16) for i in range(2)]
                    smb = [mk.sbuf("msm%d" % i, [128, 4, 128], BF16) for i in range(2)]
                    t4b = [mk.sbuf("mt4%d" % i, [128, 4], F32) for i in range(2)]
                    tmo = [mk.sbuf("mtmo%d" % i, [128, 4, 128], F32) for i in range(2)]
                    c0, l0 = seq_ranges(s)
                    for h in range(4):
                        load_seq_T(qT[:, h, :], PT[T_MLQ + h * 128:T_MLQ + (h + 1) * 128, :], s, q="sp")
                        load_seq_T(kT[:, h, :], PT[T_MLK + h * 128:T_MLK + (h + 1) * 128, :], s, q="act")
                    load_seq_K(kK, K_MLK, 512, s)
                    mk.memset("pool", vA[:], 1.0)
                    for h in range(4):
                        mk.dma(vA[:, 0:2, h, 0:128],
                               PK[c0:c0 + CTX, K_MLV + h * 128:K_MLV + (h + 1) * 128].rearrange("(a p) c -> p a c", p=128))
                        mk.dma(vA[:, 2:18, h, 0:128],
                               PK[l0:l0 + SEQ, K_MLV + h * 128:K_MLV + (h + 1) * 128].rearrange("(a p) c -> p a c", p=128))
                    for d in range(2):
                        irow = GT[d * 8:d * 8 + 4, :]
                        frow = GT[d * 8 + 4:d * 8 + 8, :]
                        if d == 0:
                            load_seq_T(R[1], irow, s)
                            load_seq_T(R[0], frow, s)
                        else:
                            load_seq_T(R[3], irow, s)
                            load_seq_T(R[4], frow, s)
                            rev_copy("dve", R[1], R[3])
                            rev_copy("pool", R[0], R[4])
                        R0, R1, R2, R3, R4 = R
                        v3 = lambda t: t[:].rearrange("p (c j) -> p c j", j=64)
                        mk.act(R0[:], R0[:], AF.Exp, bias=ngb[:, 2 * d + 1:2 * d + 2], scale=-1.0)
                        mk.act(R0[:], R0[:], AF.Ln, bias=1.0, scale=1.0)
                        mk.ts("dve", R0[:], R0[:], -1.0, None, op0=ALU.mult)
                        mk.scan("dve", R2[:], ones4[:], R0[:], 0.0, ALU.mult, ALU.add)
                        mk.stt("dve", R1[:], R1[:], gb[:, 2 * d:2 * d + 1], R2[:], ALU.add, ALU.subtract)
                        mk.scan("dve", R0[:], ones4[:], R1[:], 0.0, ALU.mult, ALU.max)
                        mprev = v3(R0)[:, 0:35, 63:64].to_broadcast([4, 35, 64])
                        mk.copy("dve", R3[:, 0:64], R1[:, 0:64])
                        mk.tt("dve", v3(R3)[:, 1:36, :], v3(R1)[:, 1:36, :], mprev, ALU.subtract)
                        mk.act(R3[:], R3[:], AF.Exp)
                        mk.ts("dve", R4[:, 0:64], R0[:, 0:64], -1.0, None, op0=ALU.mult)
                        mk.tt("dve", v3(R4)[:, 1:36, :], mprev, v3(R0)[:, 1:36, :], ALU.subtract)
                        mk.act(R4[:], R4[:], AF.Exp)
                        mk.tt("dve", R2[:], R2[:], R0[:], ALU.add)
                        mk.act(R2[:], R2[:], AF.Exp, scale=-1.0)
                        mk.tt("dve", DECr[:], v3(R4)[:, :, 63:64].to_broadcast([4, 36, 4]),
                              ident[0:4, 0:4].unsqueeze(1).to_broadcast([4, 36, 4]), ALU.mult)
                        mk.mm(PS[6][:, 0:144], onesf[0:4, :], DECr[:].rearrange("p c h -> p (c h)"))
                        mk.copy("dve", DECb[:].rearrange("p c h -> p (c h)"), PS[6][:, 0:144])
                        if d == 0:
                            NAT = [R3, R4, R2]
                        else:
                            rev_copy("dve", R0, R3)
                            rev_copy("pool", R1, R4)
                            rev_copy("dve", R3, R2)
                            NAT = [R0, R1, R3]
                        for qi, arr in enumerate(NAT):
                            for t in range(18):
                                o_ = (qi * 18 + t) * 4
                                mk.tr(PS[5][:, o_:o_ + 4], arr[:, t * 128:(t + 1) * 128], ident[0:4, 0:4])
                        mk.copy("dve", COL[:].rearrange("p a t h -> p (a t h)"), PS[5][:, 0:216])
                        mk.tt("pool", uV[:], vA[:], COL[:, 0].unsqueeze(3).to_broadcast([128, 18, 4, 129]), ALU.mult)
                        mk.memset("dve", CT[:], 0.0)
                        mk.memset("pool", CTb[0][:], 0.0)
                        mask = maskF if d == 0 else maskB
                        cur_tile = -1
                        for cs in range(36):
                            c = nat_chunk(d, cs)
                            tile, par = c // 2, c % 2
                            rows = slice(par * 64, par * 64 + 64)
                            ctb_cur, ctb_nxt = CTb[cs % 2], CTb[(cs + 1) % 2]
                            if tile != cur_tile:
                                cur_tile = tile
                                sm = smb[tile % 2]
                                for h in range(4):
                                    mk.mm(PS[4][:, h * 128:(h + 1) * 128], kT[:, h, tile * 128:(tile + 1) * 128],
                                          qT[:, h, tile * 128:(tile + 1) * 128])
                                mk.tt("dve", sm[:], PS[4][:].rearrange("p (h j) -> p h j", h=4),
                                      mask[:].unsqueeze(1).to_broadcast([128, 4, 128]), ALU.mult)
                            for h in range(4):
                                o_ = PS[h // 2][rows, (h % 2) * 256:(h % 2) * 256 + 129]
                                mk.mm(o_, sm[:, h, par * 64:par * 64 + 64], uV[:, tile, h, :], start=True, stop=False)
                                mk.mm(o_, qT[:, h, c * 64:(c + 1) * 64], ctb_cur[:, h, :], start=False, stop=True)
                            for h in range(4):
                                mk.mm(PS[2 + h // 2][:, (h % 2) * 256:(h % 2) * 256 + 129],
                                      kK[rows, tile, h * 128:(h + 1) * 128], uV[rows, tile, h, :])
                            for b in range(2):
                                mk.tt("dve", tmpC[:, 2 * b:2 * b + 2, :],
                                      PS[2 + b][:, 0:512].rearrange("p (h v) -> p h v", h=2)[:, :, 0:129], CT[:, 2 * b:2 * b + 2, :], ALU.add)
                            mk.tt("pool", CT[:], tmpC[:], DECb[:, cs, :].unsqueeze(2).to_broadcast([128, 4, 129]), ALU.mult)
                            mk.copy("act", ctb_nxt[:], CT[:])
                            t4 = t4b[cs % 2]
                            for b in range(2):
                                mk.tt("dve", t4[rows, 2 * b:2 * b + 2], PS[b][rows, 128:512:256],
                                      COL[rows, 1, tile, 2 * b:2 * b + 2], ALU.mult)
                            mk.stt("dve", t4[rows, :], t4[rows, :], -1.0, t4[rows, :], ALU.mult, ALU.max)
                            mk.tt("dve", t4[rows, :], t4[rows, :], COL[rows, 2, tile, :], ALU.max)
                            mk.recip(t4[rows, :], t4[rows, :])
                            mk.tt("dve", t4[rows, :], t4[rows, :], COL[rows, 1, tile, :], ALU.mult)
                            for b in range(2):
                                src = PS[b][rows, 0:512].rearrange("p (h v) -> p h v", h=2)[:, :, 0:128]
                                sc = t4[rows, 2 * b:2 * b + 2].unsqueeze(2).to_broadcast([64, 2, 128])
                                hs = HS[rows, tile, 2 * b * 128:(2 * b + 2) * 128].rearrange("p (h v) -> p h v", h=2)
                                if d == 0:
                                    mk.tt("dve", hs, src, sc, ALU.mult)
                                else:
                                    to = tmo[cs % 2][rows, 2 * b:2 * b + 2, :]
                                    mk.tt("dve", to, src, sc, ALU.mult)
                                    mk.tt("pool", hs, hs, to, ALU.add)
                with mk.scope():
                    sgo = mk.sbuf("msgo", [128, 18, 512], BF16)
                    sqh = mk.sbuf("msqh", [128, 18, 512], F32)
                    ML = mk.sbuf("mML", [128, 18, 512], BF16)
                    ss = mk.sbuf("mss", [128, 72], F32)
                    stg = [mk.sbuf("mstg%d" % i, [128, NS], BF16) for i in range(2)]
                    load_seq_K(sgo, K_MLO, 512, s)
                    finish_branch(HS, sgo, gvn, sqh, ss, ML)
                    tok_to_BR(ML, 0, s, stg)

    def finish_branch(HS, gate, gvn, sqh, ss, OUT):
        h3 = lambda t: t[:].rearrange("p t (h v) -> p (t h) v", v=128)
        mk.tt("pool", sqh[:], HS[:], HS[:], ALU.mult)
        mk.reduce("dve", ss[:], h3(sqh), ALU.add)
        mk.act(ss[:], ss[:], AF.Sqrt, bias=EPS, scale=1.0 / 128.0)
        mk.recip(ss[:], ss[:])
        mk.tt("dve", h3(sqh), h3(HS), ss[:].unsqueeze(2).to_broadcast([128, 72, 128]), ALU.mult)
        mk.tt("pool", h3(sqh), h3(sqh), gvn[:].unsqueeze(1).to_broadcast([128, 72, 128]), ALU.mult)
        mk.tt("dve", OUT[:], sqh[:], gate[:], ALU.mult)

    def phase_gla(li):
        with mk.scope():
            OS = mk.sbuf("gOS", [128, 18, 512], F32)
            gvn = mk.sbuf("ggvn", [128, 128], F32)
            wa = mk.sbuf("gwa", [16, 2, 256], F32)
            nba = mk.sbuf("gnba", [128, 2, 2], F32)
            RST = mk.sbuf("gRST", [128, NS], F32)
            bcast_row(gvn[:], gl_ng[li:li + 1, :])
            mk.dma(wa[:], gl_wa[li].rearrange("d r n -> r d n"))
            mk.dma(nba[:], gl_baT[li].rearrange("d p h -> p d h"))
            mk.ts("dve", nba[:], nba[:], -1.0, None, op0=ALU.mult)
            mk.memset("pool", RST[:], 1.0)
            mk.memset("pool", RST[:].rearrange("p (c j) -> p c j", j=64)[:, :, 0:1], 0.0)
            for s in range(2):
                with mk.scope():
                    qT = mk.sbuf("gqT", [128, 2, NS], BF16)
                    kT = mk.sbuf("gkT", [128, 2, NS], BF16)
                    vK = mk.sbuf("gvK", [128, 18, 512], BF16)
                    aT = mk.sbuf("gaT", [16, 2, NS], F32)
                    LA = mk.sbuf("gLA", [128, 2, NS], F32)
                    Pc = mk.sbuf("gPc", [128, 2, NS], F32)
                    qg = mk.sbuf("gqg", [128, 2, NS], BF16)
                    kg = mk.sbuf("gkg", [128, 2, NS], BF16)
                    kg2 = mk.sbuf("gkg2", [128, 2, NS], BF16)
                    kgK = mk.sbuf("gkgK", [128, 18, 256], BF16)
                    Tc = mk.sbuf("gTc", [128, 2, 36], F32)
                    eb = mk.sbuf("geb", [128, 2, 36], F32)
                    S = mk.sbuf("gS", [128, 2, 128], F32)
                    Sb = [mk.sbuf("gSb%d" % i, [128, 4, 128], BF16) for i in range(2)]
                    amb = [mk.sbuf("gam%d" % i, [128, 4, 128], BF16) for i in range(2)]
                    for hh in range(2):
                        load_seq_T(qT[:, hh, :], PT[T_GLQ + hh * 128:T_GLQ + (hh + 1) * 128, :], s, q="sp")
                        load_seq_T(kT[:, hh, :], PT[T_GLK + hh * 128:T_GLK + (hh + 1) * 128, :], s, q="act")
                    load_seq_K(vK, K_GLV, 512, s)
                    for d in range(2):
                        load_seq_T(aT[:, d, :], GT[16 + d * 16:32 + d * 16, :], s)
                    v4 = lambda t: t[:].rearrange("p h (c j) -> p h c j", j=64)
                    for d in range(2):
                        ci = 0
                        for hh in range(2):
                            for t0 in range(0, NS, 512):
                                n = min(512, NS - t0)
                                pb = PS[5 + ci % 2]
                                ci += 1
                                mk.mm(pb[:, 0:n], wa[:, d, hh * 128:(hh + 1) * 128], aT[:, d, t0:t0 + n])
                                mk.act(LA[:, hh, t0:t0 + n], pb[:, 0:n], AF.Exp, bias=nba[:, d, hh:hh + 1], scale=-1.0)
                        mk.act(LA[:], LA[:], AF.Ln, bias=1.0, scale=1.0)
                        mk.ts("dve", LA[:], LA[:], -1.0 / 16.0, None, op0=ALU.mult)
                        for hh in range(2):
                            mk.scan("dve", Pc[:, hh, :], RST[:], LA[:, hh, :], 0.0, ALU.mult, ALU.add)
                        mk.copy("dve", Tc[:], v4(Pc)[:, :, :, 63])
                        mk.act(eb[:], Tc[:], AF.Exp)
                        if d == 1:
                            mk.tt("dve", v4(Pc), Tc[:].unsqueeze(3).to_broadcast([128, 2, 36, 64]), v4(Pc), ALU.subtract)
                            mk.tt("pool", Pc[:], Pc[:], LA[:], ALU.add)
                        mk.act(LA[:], Pc[:], AF.Exp)
                        mk.tt("dve", qg[:], qT[:], LA[:], ALU.mult)
                        mk.act(LA[:], Pc[:], AF.Exp, scale=-1.0)
                        mk.tt("dve", kg[:], kT[:], LA[:], ALU.mult)
                        mk.tt("pool", v4(kg2), v4(kg), eb[:].unsqueeze(3).to_broadcast([128, 2, 36, 64]), ALU.mult)
                        import os as _osg
                        _gs = _osg.environ.get("GLA_STOP", "")
                        if _gs == "1":
                            return
                        for hh in range(2):
                            for t in range(18):
                                pv = PS[5 + t % 2][:, (t % 4) * 128:(t % 4) * 128 + 128]
                                mk.mm(pv, kg2[:, hh, t * 128:(t + 1) * 128], identb[:])
                                mk.copy(evac_eng(), kgK[:, t, hh * 128:(hh + 1) * 128], pv)
                        if _gs == "2":
                            return
                        mk.memset("dve", S[:], 0.0)
                        mk.memset("pool", Sb[0][:], 0.0)
                        mk.memset("pool", Sb[1][:], 0.0)
                        mask = maskF if d == 0 else maskB
                        cur_tile = -1
                        for cs in range(36):
                            c = nat_chunk(d, cs)
                            tile, par = c // 2, c % 2
                            rows = slice(par * 64, par * 64 + 64)
                            sb_cur, sb_nxt = Sb[cs % 2], Sb[(cs + 1) % 2]
                            if tile != cur_tile:
                                cur_tile = tile
                                am = amb[tile % 2]
                                for h in range(4):
                                    hr = slice((h % 2) * 64, (h % 2) * 64 + 64)
                                    mk.mm(PS[4 + h % 2][:, (h // 2) * 128:(h // 2 + 1) * 128],
                                          kg[hr, h // 2, tile * 128:(tile + 1) * 128],
                                          qg[hr, h // 2, tile * 128:(tile + 1) * 128])
                                for wh in range(2):
                                    mk.tt("dve", am[:, wh::2, :], PS[4 + wh][:, 0:256].rearrange("p (h j) -> p h j", h=2),
                                          mask[:].unsqueeze(1).to_broadcast([128, 2, 128]), ALU.mult)
                            _lv = int(_gs) if _gs else 9
                            if _lv >= 4:
                                for h in range(4):
                                    o_ = PS[h // 2][rows, (h % 2) * 128:(h % 2) * 128 + 128]
                                    mk.mm(o_, am[:, h, par * 64:par * 64 + 64], vK[:, tile, h * 128:(h + 1) * 128],
                                          start=True, stop=False)
                                    mk.mm(o_, qg[:, h // 2, c * 64:(c + 1) * 64], sb_cur[:, h, :], start=False, stop=True)
                            if _lv >= 5:
                                for h in range(4):
                                    hh, wh = h // 2, h % 2
                                    mk.mm(PS[2 + hh][:, wh * 128:(wh + 1) * 128], kgK[rows, tile, hh * 128:(hh + 1) * 128],
                                          vK[rows, tile, h * 128:(h + 1) * 128])
                            if _lv >= 6:
                                for h in range(4):
                                    hh, wh = h // 2, h % 2
                                    hr = slice(wh * 64, wh * 64 + 64)
                                    mk.stt("dve", S[hr, hh, :], S[hr, hh, :], eb[hr, hh, c:c + 1],
                                           PS[2 + hh][hr, wh * 128:(wh + 1) * 128], ALU.mult, ALU.add)
                            if _lv >= 7:
                                for wh in range(2):
                                    hr = slice(wh * 64, wh * 64 + 64)
                                    mk.copy("act", sb_nxt[hr, wh::2, :], S[hr, :, :])
                            if _lv >= 4:
                                for b in range(2):
                                    dst = OS[rows, tile, b * 256:(b + 1) * 256]
                                    if d == 0:
                                        mk.copy("dve", dst, PS[b][rows, 0:256])
                                    else:
                                        mk.tt("dve", dst, dst, PS[b][rows, 0:256], ALU.add)
                with mk.scope():
                    sg = mk.sbuf("gsg", [128, 18, 512], BF16)
                    sqh = mk.sbuf("gsqh", [128, 18, 512], F32)
                    GLo = mk.sbuf("gGLo", [128, 18, 512], BF16)
                    ss = mk.sbuf("gss", [128, 72], F32)
                    stg = [mk.sbuf("gstg%d" % i, [128, NS], BF16) for i in range(2)]
                    load_seq_K(sg, K_GLG, 512, s)
                    finish_branch(OS, sg, gvn, sqh, ss, GLo)
                    tok_to_BR(GLo, 1024, s, stg)

    phase_load_x()
    phase_mod()
    order = ["norm1", "proj", "mlstm", "attn", "gla", "merge", "moe"]
    lim = order.index(stop_after) if stop_after else len(order)
    for li in range(n_layers):
        with mk.scope():
            hT = mk.sbuf("hT", [128, 8, NTOK], BF16)
            with mk.scope():
                scratch = ([mk.sbuf("nxs%d" % i, [128, 8, 512], F32) for i in range(2)],
                           [mk.sbuf("nsq%d" % i, [128, 8, 512], BF16) for i in range(2)],
                           [mk.sbuf("nrs%d" % i, [128, 512], F32) for i in range(2)])
                norm_groups(li, 0, hT, scratch)
            if dbg:
                hT_dbg = mk.dram("hT_dbg%d" % li, [D, NTOK], BF16, kind="ExternalOutput")
                mk.dma(hT_dbg[:, :].rearrange("(kc p) t -> p kc t", p=128), hT[:])
            if lim >= 1:
                with mk.scope():
                    phase_proj(li, hT)
        if lim >= 2 and "mlstm" not in skip:
            phase_mlstm(li)
        if lim >= 3 and "attn" not in skip:
            phase_attn(li)
        if lim >= 4 and "gla" not in skip:
            phase_gla(li)
        if lim >= 5:
            phase_merge(li)
        if lim >= 6 and do_moe:
            phase_moe(li)
    if lim >= 6:
        phase_final()
    if dbg:
        modv_dbg = mk.dram("modv_dbg", [128, 2 * 6 * 8 * 3], F32, kind="ExternalOutput")
        mk.dma(modv_dbg[:, :], modv[:].rearrange("p a b c d -> p (a b c d)"))
    mk.finish()
    return nc, mk


def make_consts():
    c = np.zeros((128, 384 + 2 * SEQ), np.float32)
    c[:, 0:128] = np.eye(128, dtype=np.float32)
    i = np.arange(128)[:, None]
    j = np.arange(128)[None, :]
    same = (i // 64) == (j // 64)
    c[:, 128:256] = (same & (i <= j)).astype(np.float32)
    c[:, 256:384] = (same & (i >= j)).astype(np.float32)
    t = np.arange(SEQ)
    rowp = (t // 64).astype(np.float32)
    colp = (t % 64).astype(np.float32)
    inv = (10000.0 ** (-np.arange(16, dtype=np.float32) / 16)).astype(np.float32)
    for d in range(128):
        dd = d % 64
        axis = dd // 32
        f = dd % 16
        half = (dd % 32) // 16
        ang = (rowp if axis == 0 else colp) * inv[f]
        c[d, 384:384 + SEQ] = np.cos(ang)
        c[d, 384 + SEQ:384 + 2 * SEQ] = np.sin(ang) * (-1.0 if half == 0 else 1.0)
    return c


def pcol(v, nch):
    return np.ascontiguousarray(np.asarray(v, np.float32).reshape(nch, 128).T)


def prep_shared(inp):
    sh = {}
    f = lambda a: np.ascontiguousarray(np.asarray(a, np.float32))
    sh["w_mod"] = f(inp["w_mod"])
    sh["b_modT"] = np.stack([pcol(inp["b_mod"][li], 48) for li in range(2)])
    sh["gmixT"] = np.stack([pcol(inp["norm_mix_g"][li], 8) for li in range(2)])
    sh["gffnT"] = np.stack([pcol(inp["norm_ffn_g"][li], 8) for li in range(2)])
    sh["w_in"] = f(inp["w_in"])
    sh["ml_gb"] = f(np.asarray(inp["ml_gate_b"]).reshape(2, 4, 4).transpose(0, 2, 1))
    sh["ml_ng"] = f(inp["ml_norm_g"])
    sh["df_lam"] = f(inp["df_lambda"])
    sh["df_ng"] = f(inp["df_norm_g"])
    sh["gl_wa"] = f(inp["gl_w_alpha"])
    sh["gl_baT"] = f(np.asarray(inp["gl_b_alpha"]).reshape(2, 2, 2, 128).transpose(0, 1, 3, 2))
    sh["gl_ng"] = f(inp["gl_norm_g"])
    sh["w_branch"] = f(inp["w_branch"])
    sh["w_out"] = f(inp["w_out"])
    sh["router_w"] = f(np.concatenate([inp["router_group_w"], inp["router_expert_w"]], axis=-1))
    sh["router_b"] = f(np.concatenate([inp["router_group_b"], inp["router_expert_b"]], axis=-1))
    sh["moe_wg"] = f(inp["moe_w_gate"])
    sh["moe_wu"] = f(inp["moe_w_up"])
    sh["moe_wd"] = f(inp["moe_w_down"])
    sh["fin_gT"] = pcol(inp["final_norm_g"], 8)
    sh["consts"] = make_consts()
    return sh


def prep_core(inp, core):
    b0 = 2 * core
    m = {}
    m["x2"] = np.ascontiguousarray(np.asarray(inp["x"][b0:b0 + 2], np.float32))
    m["ctx2"] = np.ascontiguousarray(np.asarray(inp["ctx"][b0:b0 + 2], np.float32))
    cs = np.stack([np.asarray(inp["c_ctx"], np.float32), np.asarray(inp["c"][b0], np.float32),
                   np.asarray(inp["c"][b0 + 1], np.float32)], axis=-1)
    m["cT"] = np.ascontiguousarray(cs.reshape(8, 128, 3).transpose(1, 0, 2))
    return m


_CACHE = {}
CORES_PER_LAUNCH = 8


def kernel(**inputs):
    if "nc" not in _CACHE:
        _CACHE["nc"] = build()[0]
    nc = _CACHE["nc"]
    sh = prep_shared(inputs)
    in_maps = []
    for core in range(8):
        m = dict(sh)
        m.update(prep_core(inputs, core))
        in_maps.append(m)
    outs = []
    for g0 in range(0, 8, CORES_PER_LAUNCH):
        res = run_bass_kernel_spmd(nc, in_maps[g0:g0 + CORES_PER_LAUNCH], core_ids=list(range(CORES_PER_LAUNCH)))
        outs.extend(np.asarray(r["y_out"], np.float32) for r in res.results)
    return np.concatenate(outs, axis=0)
```

```python
import contextlib
import math
import numpy as np
import concourse.bass as bass
import concourse.mybir as mybir
from concourse.bass_utils import run_bass_kernel_spmd

F32 = mybir.dt.float32
BF16 = mybir.dt.bfloat16
AF = mybir.ActivationFunctionType
ALU = mybir.AluOpType
AX = mybir.AxisListType

COMPUTE = ("pe", "act", "dve", "pool")


class _Op:
    __slots__ = ("eng", "fn", "deps", "marked", "val", "dma", "dsem", "dval", "qwait", "emitted", "seq")

    def __init__(self, eng, fn):
        self.eng = eng
        self.fn = fn
        self.deps = []
        self.marked = False
        self.val = None
        self.dma = False
        self.dsem = None
        self.dval = None
        self.qwait = None
        self.emitted = False


class MK:
    def __init__(self, nc, n_dma_sems=8):
        self.nc = nc
        self.stacks = [contextlib.ExitStack()]
        self.e = {"pe": nc.tensor, "act": nc.scalar, "dve": nc.vector, "pool": nc.gpsimd, "sp": nc.sync}
        self.ops = {k: [] for k in self.e}
        self.pstep = {}
        self.notrack = set()
        self.psum_names = set()
        self.recs = {}
        self.n_dma_sems = n_dma_sems
        self.dma_rr = {k: 0 for k in self.e}
        self.dma_last = {}
        self.sem = {k: nc.alloc_semaphore("s_" + k) for k in COMPUTE}
        self.dsems = {}
        for k in ("sp", "pool", "act"):
            for s in range(n_dma_sems):
                self.dsems[(k, s)] = nc.alloc_semaphore("d_%s_%d" % (k, s))
        self.cnt = {k: 0 for k in self.e}
        self.dcnt = {}
        self.seen = {k: {} for k in self.e}
        self.last_op = {k: None for k in self.e}
        self.n_inst = 0
        self.trace = {k: [] for k in self.e}

    def sbuf(self, name, shape, dtype):
        self.uid = getattr(self, "uid", 0) + 1
        name = "%s_%d" % (name, self.uid)
        t = self.stacks[-1].enter_context(self.nc.sbuf_tensor(name, list(shape), dtype))
        self.pstep[name] = int(np.prod(shape[1:]))
        self.recs.pop(name, None)
        return t

    def psum(self, name, shape, dtype=F32):
        t = self.stacks[-1].enter_context(self.nc.psum_tensor(name, list(shape), dtype))
        self.pstep[name] = int(np.prod(shape[1:]))
        self.psum_names.add(name)
        return t

    def dram(self, name, shape, dtype, kind="Internal", rowlen=None):
        t = self.nc.dram_tensor(name, list(shape), dtype, kind=kind)
        self.pstep[name] = int(rowlen if rowlen is not None else shape[-1])
        if kind == "ExternalInput":
            self.notrack.add(name)
        return t.ap()

    @contextlib.contextmanager
    def scope(self):
        self.stacks.append(contextlib.ExitStack())
        try:
            yield
            self.flush()
            self.barrier()
            self.flush()
        finally:
            self.stacks.pop().close()

    def _box(self, ap):
        name = ap.name
        ps = self.pstep[name]
        off = int(ap.offset)
        p0, f0 = divmod(off, ps)
        plo = phi = p0
        flo = fhi = f0
        for (step, cnt) in ap.ap:
            ext = step * (cnt - 1)
            if step != 0 and abs(step) >= ps and step % ps == 0:
                e = ext // ps
                if e < 0:
                    plo += e
                else:
                    phi += e
            else:
                if ext < 0:
                    flo += ext
                else:
                    fhi += ext
        return name, plo, phi, flo, fhi

    def _access(self, op, ap, is_write):
        if ap.name in self.notrack:
            return
        name, plo, phi, flo, fhi = self._box(ap)
        excl = name in self.psum_names
        if excl:
            flo, fhi = 0, self.pstep[name] - 1
            plo, phi = (plo // 32) * 32, (phi // 32) * 32 + 31
        lst = self.recs.get(name, [])
        keep = []
        for r in lst:
            ov = not (r[1] < plo or phi < r[0] or r[3] < flo or fhi < r[2])
            o = r[4]
            if ov and o is not op:
                same = (o.eng == op.eng) and not o.dma and not op.dma
                if same:
                    need = (op.eng != "pe") and r[5] and not is_write
                else:
                    need = excl or is_write or r[5]
                if need:
                    op.deps.append(o)
            contained = (plo <= r[0] and r[1] <= phi and flo <= r[2] and r[3] <= fhi)
            if contained and o is not op:
                same = (o.eng == op.eng) and not o.dma and not op.dma
                if excl or is_write or ((not r[5]) and same):
                    if not (excl and same and r[5] and not is_write and False):
                        continue
            keep.append(r)
        keep.append([plo, phi, flo, fhi, op, is_write])
        self.recs[name] = keep

    def _record(self, eng, fn, reads, writes, dma=False):
        op = _Op(eng, fn)
        op.dma = dma
        for ap in reads:
            if ap is not None and hasattr(ap, "ap"):
                self._access(op, ap, False)
        for ap in writes:
            if ap is not None and hasattr(ap, "ap"):
                self._access(op, ap, True)
        self.gseq = getattr(self, "gseq", 0) + 1
        op.seq = self.gseq
        if op.deps:
            best = {}
            keep = []
            for d_ in op.deps:
                if d_.dma:
                    keep.append(d_)
                else:
                    b_ = best.get(d_.eng)
                    if b_ is None or d_.seq > b_.seq:
                        best[d_.eng] = d_
            op.deps = keep + list(best.values())
        if dma:
            slot = self.dma_rr[eng]
            self.dma_rr[eng] = (slot + 1) % (2 if eng == "pool" else self.n_dma_sems)
            op.dsem = (eng, slot)
            op.qwait = self.dma_last.get((eng, slot))
            self.dma_last[(eng, slot)] = op
            self.dcnt[op.dsem] = self.dcnt.get(op.dsem, 0) + 16
            op.dval = self.dcnt[op.dsem]
        self.ops[eng].append(op)
        self.last_op[eng] = op
        return op

    def mm(self, out, lhsT, rhs, start=True, stop=True):
        return self._record("pe", lambda: self.nc.tensor.matmul(out, lhsT, rhs, start=start, stop=stop),
                            [lhsT, rhs], [out])

    def tr(self, out, in_, ident):
        return self._record("pe", lambda: self.nc.tensor.transpose(out, in_, ident), [in_, ident], [out])

    def act(self, out, in_, func, bias=None, scale=None, accum_out=None):
        kw = {}
        if bias is not None:
            kw["bias"] = bias
        if scale is not None:
            kw["scale"] = scale
        if accum_out is not None:
            kw["accum_out"] = accum_out
        return self._record("act", lambda: self.nc.scalar.activation(out, in_, func, **kw),
                            [in_, bias, scale], [out, accum_out])

    def tt(self, eng, out, in0, in1, op):
        return self._record(eng, lambda: self.e[eng].tensor_tensor(out, in0, in1, op), [in0, in1], [out])

    def ts(self, eng, out, in0, s1, s2=None, op0=ALU.mult, op1=None):
        kw = {}
        if op1 is not None:
            kw["op1"] = op1
        return self._record(eng, lambda: self.e[eng].tensor_scalar(out, in0, s1, s2, op0, **kw),
                            [in0, s1, s2], [out])

    def stt(self, eng, out, in0, scalar, in1, op0, op1):
        return self._record(eng, lambda: self.e[eng].scalar_tensor_tensor(out, in0, scalar, in1, op0, op1),
                            [in0, scalar, in1], [out])

    def copy(self, eng, out, in_):
        if eng == "act":
            return self._record("act", lambda: self.nc.scalar.copy(out, in_), [in_], [out])
        return self._record(eng, lambda: self.e[eng].tensor_copy(out, in_), [in_], [out])

    def memset(self, eng, ap, val):
        return self._record(eng, lambda: self.e[eng].memset(ap, val), [], [ap])

    def reduce(self, eng, out, in_, op, axis=AX.X):
        return self._record(eng, lambda: self.e[eng].tensor_reduce(out, in_, axis, op), [in_], [out])

    def scan(self, eng, out, d0, d1, init, op0, op1):
        return self._record(eng, lambda: self.e[eng].tensor_tensor_scan(out, d0, d1, init, op0, op1),
                            [d0, d1, init], [out])

    def recip(self, out, in_):
        return self._record("dve", lambda: self.nc.vector.reciprocal(out, in_), [in_], [out])

    def max8(self, out, in_):
        return self._record("dve", lambda: self.nc.vector.max(out, in_), [in_], [out])

    def dma(self, out, in_, q="sp", **kw):
        return self._record(q, lambda: self.e[q].dma_start(out=out, in_=in_, **kw), [in_], [out], dma=True)

    def barrier(self):
        lasts = [self.last_op[k] for k in COMPUTE if self.last_op[k] is not None]
        dlast = list(self.dma_last.values())
        for k in self.e:
            op = _Op(k, None)
            op.deps = [o for o in lasts if o.eng != k] + dlast
            self.ops[k].append(op)

    def flush(self):
        for k in self.ops:
            for op in self.ops[k]:
                for d in op.deps:
                    d.marked = True
        for k in self.ops:
            lst = self.ops[k]
            if not lst:
                continue
            if k in COMPUTE:
                real = [o for o in lst if o.fn is not None and not o.dma]
                if real:
                    real[-1].marked = True
                c = self.cnt[k]
                for op in lst:
                    if op.fn is not None and not op.dma and op.marked:
                        c += 1
                        op.val = c
                nxt = None
                for op in reversed(lst):
                    if op.fn is None or op.dma:
                        continue
                    if op.marked:
                        nxt = op.val
                    else:
                        op.val = nxt
        for k in self.ops:
            eng = self.e[k]
            seen = self.seen[k]
            for op in self.ops[k]:
                waits = {}
                for d in op.deps:
                    if d.dma:
                        key = ("d",) + d.dsem
                        s, v = self.dsems[d.dsem], d.dval
                    else:
                        if d.fn is None:
                            continue
                        key = ("c", d.eng)
                        s, v = self.sem[d.eng], d.val
                    if seen.get(key, 0) >= v:
                        continue
                    if key not in waits or waits[key][1] < v:
                        waits[key] = (s, v)
                if op.dma and op.qwait is not None:
                    key = ("d",) + op.dsem
                    v = op.qwait.dval
                    if seen.get(key, 0) < v and (key not in waits or waits[key][1] < v):
                        waits[key] = (self.dsems[op.dsem], v)
                for key, (s, v) in waits.items():
                    eng.wait_ge(s, v)
                    seen[key] = v
                    self.trace[k].append(("w", key, v))
                if op.fn is None:
                    continue
                ins = op.fn()
                self.n_inst += 1
                op.fn = True
                if op.dma:
                    ins.then_inc(self.dsems[op.dsem], 16)
                    self.trace[k].append(("i", ("d",) + op.dsem, 16))
                elif op.marked:
                    ins.then_inc(self.sem[k], 1)
                    self.cnt[k] = op.val
                    self.trace[k].append(("i", ("c", k), 1))
                else:
                    self.trace[k].append(("n", None, 0))
            self.ops[k] = []

    def simulate(self):
        sem = {}
        pos = {k: 0 for k in self.trace}
        progress = True
        while progress:
            progress = False
            for k, tr in self.trace.items():
                while pos[k] < len(tr):
                    typ, key, v = tr[pos[k]]
                    if typ == "w":
                        if sem.get(key, 0) < v:
                            break
                    elif typ == "i":
                        sem[key] = sem.get(key, 0) + v
                    pos[k] += 1
                    progress = True
        stuck = {k: (pos[k], len(tr), tr[pos[k]] if pos[k] < len(tr) else None) for k, tr in self.trace.items()}
        return all(pos[k] == len(tr) for k, tr in self.trace.items()), stuck, sem

    def finish(self):
        self.flush()
        for (k, slot), op in self.dma_last.items():
            self.e[k].wait_ge(self.dsems[(k, slot)], op.dval)
        self.stacks[0].close()


D = 1024
NTOK = 4608
NG = 9
SEQ = 2048
CTX = 256
NS = 2304
IN_W = 8240
EPS = 1e-6
C_MLQ, C_MLK, C_MLV, C_MLO, C_MLG = 0, 512, 1024, 1536, 2048
C_DFQ, C_DFK, C_DFV = 2064, 2576, 3088
C_GLQ, C_GLK, C_GLV, C_GLG, C_GLA = 3600, 3856, 4112, 4624, 5136
C_GATE = 5168
T_MLQ, T_MLK, T_DFQ, T_DFK, T_GLQ, T_GLK, T_GATE = 0, 512, 1024, 1536, 2048, 2304, 2560
T_ROWS = 2560 + 3072
K_MLK, K_MLV, K_MLO, K_DFV, K_GLK, K_GLV, K_GLG = 0, 512, 1024, 1536, 2048, 2304, 2816
K_COLS = 3328


def seq_ranges(s):
    return 256 * s, 512 + 2048 * s


def build(n_layers=2, stop_after=None, dbg=False, do_moe=True, skip=()):
    nc = bass.Bass("TRN2", target_bir_lowering=False)
    mk = MK(nc)
    IN = {}

    def din(name, shape, dtype=F32):
        IN[name] = mk.dram(name, shape, dtype, kind="ExternalInput")
        return IN[name]

    x2 = din("x2", [2, SEQ, D])
    ctx2 = din("ctx2", [2, CTX, D])
    cT = din("cT", [128, 8, 3])
    w_mod = din("w_mod", [2, D, 6 * D])
    b_modT = din("b_modT", [2, 128, 48])
    gmixT = din("gmixT", [2, 128, 8])
    gffnT = din("gffnT", [2, 128, 8])
    w_in = din("w_in", [2, D, IN_W])
    ml_gb = din("ml_gb", [2, 4, 4])
    ml_ng = din("ml_ng", [2, 128])
    df_lam = din("df_lam", [2, 4, 64])
    df_ng = din("df_ng", [2, 128])
    gl_wa = din("gl_wa", [2, 2, 16, 256])
    gl_baT = din("gl_baT", [2, 2, 128, 2])
    gl_ng = din("gl_ng", [2, 128])
    w_branch = din("w_branch", [2, 3, 512, D])
    w_out = din("w_out", [2, D, D])
    router_w = din("router_w", [2, D, 36])
    router_b = din("router_b", [2, 36])
    if do_moe:
        moe_wg = din("moe_wg", [2, 32, D, 512])
        moe_wu = din("moe_wu", [2, 32, D, 512])
        moe_wd = din("moe_wd", [2, 32, 512, D])
    fin_gT = din("fin_gT", [128, 8])
    consts = din("consts", [128, 128 + 128 + 128 + 2048 + 2048])

    okind = "ExternalOutput"
    y_out = mk.dram("y_out", [2, SEQ, D], F32, kind=okind)
    skind = "ExternalOutput" if dbg else "Internal"
    xT = mk.dram("xT", [D, NTOK], F32, kind=skind)
    PT = mk.dram("PT", [T_ROWS, NTOK], BF16, kind=skind)
    PK = mk.dram("PK", [NTOK, K_COLS], BF16, kind=skind)
    GT = mk.dram("GT", [48, NTOK], F32, kind=skind)
    BR = mk.dram("BR", [1536, NTOK], BF16, kind=skind)

    ident = mk.sbuf("ident", [128, 128], F32)
    identb = mk.sbuf("identb", [128, 128], BF16)
    onesb = mk.sbuf("onesb", [128, 128], BF16)
    onesf = mk.sbuf("onesf", [128, 128], F32)
    maskF = mk.sbuf("maskF", [128, 128], F32)
    maskB = mk.sbuf("maskB", [128, 128], F32)
    modv = mk.sbuf("modv", [128, 2, 6, 8, 3], F32)
    PS = [mk.psum("psb%d" % i, [128, 512], F32) for i in range(8)]

    mk.dma(ident[:], consts[:, 0:128])
    mk.dma(maskF[:], consts[:, 128:256])
    mk.dma(maskB[:], consts[:, 256:384])
    mk.copy("dve", identb[:], ident[:])
    mk.memset("pool", onesb[:], 1.0)
    mk.memset("pool", onesf[:], 1.0)

    rr = {"ev": 0}

    def evac_eng():
        rr["ev"] ^= 1
        return "act" if rr["ev"] else "dve"

    def phase_load_x():
        with mk.scope():
            xin = [mk.sbuf("xin%d" % i, [128, D], F32) for i in range(2)]
            xst = [mk.sbuf("xst%d" % i, [128, 8, 512], F32) for i in range(2)]
            for g in range(NG):
                st = xst[g % 2]
                for t4 in range(4):
                    tt = g * 4 + t4
                    if tt < 4:
                        src = ctx2[tt // 2, (tt % 2) * 128:(tt % 2) * 128 + 128, :]
                    else:
                        u = tt - 4
                        src = x2[u // 16, (u % 16) * 128:(u % 16) * 128 + 128, :]
                    xi = xin[tt % 2]
                    mk.dma(xi[:], src)
                    for half in range(2):
                        pb = PS[(tt * 2 + half) % 4]
                        for k4 in range(4):
                            kc = half * 4 + k4
                            mk.tr(pb[:, k4 * 128:(k4 + 1) * 128], xi[:, kc * 128:(kc + 1) * 128], ident[:])
                        mk.copy(evac_eng(), st[:, half * 4:(half + 1) * 4, t4 * 128:(t4 + 1) * 128],
                                pb[:].rearrange("p (k t) -> p k t", k=4))
                mk.dma(xT[:, g * 512:(g + 1) * 512].rearrange("(kc p) t -> p kc t", p=128), st[:], q="act")

    def phase_mod():
        with mk.scope():
            cs = mk.sbuf("cs", [128, 8, 3], F32)
            csg = mk.sbuf("csg", [128, 8, 3], F32)
            wm = [mk.sbuf("wm%d" % i, [128, 8, 512], F32) for i in range(2)]
            modT = mk.sbuf("modT", [128, 48, 3], F32)
            bm = mk.sbuf("bm", [128, 48], F32)
            gm = mk.sbuf("gm", [128, 8], F32)
            gf = mk.sbuf("gf", [128, 8], F32)
            mk.dma(cs[:], cT[:, :, :])
            mk.act(csg[:], cs[:], AF.Sigmoid)
            mk.tt("dve", cs[:], cs[:], csg[:], ALU.mult)
            for li in range(n_layers):
                mk.dma(bm[:], b_modT[li])
                mk.dma(gm[:], gmixT[li])
                mk.dma(gf[:], gffnT[li])
                for blk in range(12):
                    w = wm[blk % 2]
                    mk.dma(w[:], w_in_view(w_mod[li], blk * 512, 512), q=("sp" if blk % 2 else "act"))
                    for c4 in range(4):
                        fc = blk * 4 + c4
                        pb = PS[fc % 4]
                        for kc in range(8):
                            mk.mm(pb[:, 0:3], w[:, kc, c4 * 128:(c4 + 1) * 128], cs[:, kc, :],
                                  start=(kc == 0), stop=(kc == 7))
                        mk.ts("dve", modT[:, fc, :], pb[:, 0:3], bm[:, fc:fc + 1], None, op0=ALU.add)
                mv = modv[:, li]
                for j in range(3):
                    mk.ts("dve", mv[:, 0, :, j], modT[:, 8:16, j], 1.0, None, op0=ALU.add)
                    mk.tt("dve", mv[:, 0, :, j], mv[:, 0, :, j], gm[:], ALU.mult)
                    mk.copy("dve", mv[:, 1, :, j], modT[:, 0:8, j])
                    mk.copy("dve", mv[:, 2, :, j], modT[:, 16:24, j])
                    mk.ts("dve", mv[:, 3, :, j], modT[:, 32:40, j], 1.0, None, op0=ALU.add)
                    mk.tt("dve", mv[:, 3, :, j], mv[:, 3, :, j], gf[:], ALU.mult)
                    mk.copy("dve", mv[:, 4, :, j], modT[:, 24:32, j])
                    mk.copy("dve", mv[:, 5, :, j], modT[:, 40:48, j])

    def w_in_view(w2d, c0, n):
        return w2d[:, c0:c0 + n].rearrange("(kc p) n -> p kc n", p=128)

    def gset(g):
        return 0 if g == 0 else (1 if g <= 4 else 2)

    def norm_groups(li, which_gs, hT, scratch, h32_cb=None):
        xs_b, sq_b, rs_b = scratch
        for g in range(NG):
            j = gset(g)
            xs = xs_b[g % 2]
            sq = sq_b[g % 2]
            rs = rs_b[g % 2]
            mk.dma(xs[:], xT[:, g * 512:(g + 1) * 512].rearrange("(kc p) t -> p kc t", p=128),
                   q=("sp" if g % 2 else "act"))
            mk.tt("pool", sq[:], xs[:], xs[:], ALU.mult)
            pb = PS[4 + g % 2]
            for kc in range(8):
                mk.mm(pb[:], onesb[:], sq[:, kc, :], start=(kc == 0), stop=(kc == 7))
            mk.act(rs[:], pb[:], AF.Sqrt, bias=EPS, scale=1.0 / D)
            mk.recip(rs[:], rs[:])
            for kc in range(8):
                mk.stt("dve", xs[:, kc, :], xs[:, kc, :], modv[:, li, which_gs, kc, j:j + 1], rs[:],
                       ALU.mult, ALU.mult)
                mk.act(hT[:, kc, g * 512:(g + 1) * 512], xs[:, kc, :], AF.Identity,
                       bias=modv[:, li, which_gs + 1, kc, j:j + 1], scale=1.0)
                if h32_cb is not None:
                    mk.ts("pool", xs[:, kc, :], xs[:, kc, :], modv[:, li, which_gs + 1, kc, j:j + 1], None,
                          op0=ALU.add)
            if h32_cb is not None:
                h32_cb(g, xs)

    def phase_proj(li, hT):
        cosT = mk.sbuf("cosT", [128, SEQ], F32)
        sinT = mk.sbuf("sinT", [128, SEQ], F32)
        mk.dma(cosT[:], consts[:, 384:384 + SEQ])
        mk.dma(sinT[:], consts[:, 384 + SEQ:384 + 2 * SEQ])
        wb = [mk.sbuf("wblk%d" % i, [128, 8, 512], BF16) for i in range(2)]
        stT = [mk.sbuf("stT%d" % i, [128, NTOK], BF16) for i in range(2)]
        stK = [mk.sbuf("stK%d" % i, [128, 4, 512], BF16) for i in range(2)]
        stG = mk.sbuf("stG", [16, NTOK], F32)
        wrot = mk.sbuf("wrot", [128, 8, 512], BF16)
        stG2 = mk.sbuf("stG2", [32, NTOK], F32)
        t1 = [mk.sbuf("rp1_%d" % i, [128, 512], F32) for i in range(2)]
        t2 = [mk.sbuf("rp2_%d" % i, [128, 512], F32) for i in range(2)]
        cnt = {"blk": 0, "ps": 0, "st": 0}

        def load_blk(c0, n):
            w = wb[cnt["blk"] % 2]
            cnt["blk"] += 1
            mk.dma(w[:, :, 0:n], w_in_view(w_in[li], c0, n), q="pool")
            return w

        def nextps():
            cnt["ps"] += 1
            return PS[cnt["ps"] % 4]

        tblocks = [
            (C_MLQ, 512, T_MLQ, "copy", 1.0),
            (C_MLK, 512, T_MLK, "copy", 128.0 ** -0.5),
            (C_DFQ, 512, T_DFQ, "rope", 1.0),
            (C_DFK, 512, T_DFK, "rope", 1.0),
            (C_GLQ, 256, T_GLQ, "copy", 0.125),
            (C_GLK, 256, T_GLK, "copy", 1.0),
        ] + [(C_GATE + i * 512, 512, T_GATE + i * 512, "sigmoid", 1.0) for i in range(6)]
        for (c0, n, r0, mode, scale) in tblocks:
            w = load_blk(c0, n)
            if mode == "rope":
                wv = w[:].rearrange("p k (b h e) -> p (k b) h e", h=2, e=16)
                rv = wrot[:].rearrange("p k (b h e) -> p (k b) h e", h=2, e=16)
                mk.copy("pool", rv[:, :, 0, :], wv[:, :, 1, :])
                mk.copy("pool", rv[:, :, 1, :], wv[:, :, 0, :])
            for cc in range(n // 128):
                st = stT[cnt["st"] % 2]
                cnt["st"] += 1
                for g in range(NG):
                    pb = nextps()
                    for kc in range(8):
                        mk.mm(pb[:], w[:, kc, cc * 128:(cc + 1) * 128], hT[:, kc, g * 512:(g + 1) * 512],
                              start=(kc == 0), stop=(kc == 7))
                    dst = st[:, g * 512:(g + 1) * 512]
                    if mode == "sigmoid":
                        mk.act(dst, pb[:], AF.Sigmoid)
                    elif mode == "rope" and g >= 1:
                        pr = nextps()
                        for kc in range(8):
                            mk.mm(pr[:], wrot[:, kc, cc * 128:(cc + 1) * 128], hT[:, kc, g * 512:(g + 1) * 512],
                                  start=(kc == 0), stop=(kc == 7))
                        tok0 = ((g - 1) % 4) * 512
                        a = t1[g % 2]
                        b = t2[g % 2]
                        mk.tt("dve", a[:], pb[:], cosT[:, tok0:tok0 + 512], ALU.mult)
                        mk.tt("dve", b[:], pr[:], sinT[:, tok0:tok0 + 512], ALU.mult)
                        mk.tt("pool", dst, a[:], b[:], ALU.add)
                    else:
                        e = evac_eng()
                        if e == "act":
                            mk.act(dst, pb[:], AF.Copy, scale=scale)
                        else:
                            mk.ts("dve", dst, pb[:], scale, None, op0=ALU.mult)
                mk.dma(PT[r0 + cc * 128:r0 + (cc + 1) * 128, :], st[:], q="sp")
        for (c0, n, r0) in [(C_MLG, 16, 0), (C_GLA, 32, 16)]:
            w = load_blk(c0, n)
            for g in range(NG):
                pb = nextps()
                for kc in range(8):
                    mk.mm(pb[0:n, :], w[:, kc, 0:n], hT[:, kc, g * 512:(g + 1) * 512],
                          start=(kc == 0), stop=(kc == 7))
                if r0 == 0:
                    mk.copy("dve", stG[0:16, g * 512:(g + 1) * 512], pb[0:16, :])
                else:
                    mk.copy("dve", stG2[:, g * 512:(g + 1) * 512], pb[0:32, :])
        mk.dma(GT[0:16, :], stG[0:16, :], q="sp")
        mk.dma(GT[16:48, :], stG2[:, :], q="sp")
        kblocks = [
            (C_MLK, 512, K_MLK, "copy", 128.0 ** -0.5),
            (C_MLV, 512, K_MLV, "copy", 1.0),
            (C_MLO, 512, K_MLO, "sigmoid", 1.0),
            (C_DFV, 512, K_DFV, "copy", 1.0),
            (C_GLK, 256, K_GLK, "copy", 1.0),
            (C_GLV, 512, K_GLV, "copy", 1.0),
            (C_GLG, 512, K_GLG, "silu", 1.0),
        ]
        sg = [mk.sbuf("sgk%d" % i, [128, 512], F32) for i in range(2)]
        for (c0, n, k0, mode, scale) in kblocks:
            w = load_blk(c0, n)
            for t4 in range(NTOK // 512):
                st = stK[cnt["st"] % 2]
                cnt["st"] += 1
                for q4 in range(4):
                    tt_ = t4 * 4 + q4
                    pb = nextps()
                    for kc in range(8):
                        mk.mm(pb[:, 0:n], hT[:, kc, tt_ * 128:(tt_ + 1) * 128], w[:, kc, 0:n],
                              start=(kc == 0), stop=(kc == 7))
                    dst = st[:, q4, 0:n]
                    if mode == "sigmoid":
                        mk.act(dst, pb[:, 0:n], AF.Sigmoid)
                    elif mode == "silu":
                        s_ = sg[tt_ % 2]
                        mk.act(s_[:, 0:n], pb[:, 0:n], AF.Sigmoid)
                        mk.tt("dve", dst, pb[:, 0:n], s_[:, 0:n], ALU.mult)
                    else:
                        e = evac_eng()
                        if e == "act":
                            mk.act(dst, pb[:, 0:n], AF.Copy, scale=scale)
                        else:
                            mk.ts("dve", dst, pb[:, 0:n], scale, None, op0=ALU.mult)
                mk.dma(PK[t4 * 512:(t4 + 1) * 512, k0:k0 + n].rearrange("(a p) c -> p a c", p=128),
                       st[:, :, 0:n], q="sp")

    def load_seq_T(dst, src_rows, s, q="sp"):
        c0, l0 = seq_ranges(s)
        mk.dma(dst[:, 0:CTX], src_rows[:, c0:c0 + CTX], q=q)
        mk.dma(dst[:, CTX:NS], src_rows[:, l0:l0 + SEQ], q=q)

    def store_seq_T(dst_rows, src, s, q="sp"):
        c0, l0 = seq_ranges(s)
        mk.dma(dst_rows[:, c0:c0 + CTX], src[:, 0:CTX], q=q)
        mk.dma(dst_rows[:, l0:l0 + SEQ], src[:, CTX:NS], q=q)

    def load_seq_K(dst3, k0, ncol, s, q="sp"):
        c0, l0 = seq_ranges(s)
        mk.dma(dst3[:, 0:2], PK[c0:c0 + CTX, k0:k0 + ncol].rearrange("(a p) c -> p a c", p=128), q=q)
        mk.dma(dst3[:, 2:18], PK[l0:l0 + SEQ, k0:k0 + ncol].rearrange("(a p) c -> p a c", p=128), q=q)

    def bcast_row(dst, src_row):
        mk.dma(dst, src_row.to_broadcast([128, src_row.shape[-1]]))

    def tok_to_BR(src, r0, s, stg):
        for h in range(4):
            st = stg[h % 2]
            for t4 in range(5):
                n = min(4, 18 - t4 * 4)
                pb = PS[t4 % 4]
                for j in range(n):
                    t = t4 * 4 + j
                    mk.mm(pb[:, j * 128:(j + 1) * 128], src[:, t, h * 128:(h + 1) * 128], identb[:])
                mk.copy(evac_eng(), st[:, t4 * 512:t4 * 512 + n * 128], pb[:, 0:n * 128])
            store_seq_T(BR[r0 + h * 128:r0 + (h + 1) * 128, :], st, s, q="act")

    def phase_attn(li):
        lam_init = 0.8 - 0.6 * math.exp(-0.3 * li)
        with mk.scope():
            lam_t = mk.sbuf("lam_t", [1, 4, 64], F32)
            lam_p = mk.sbuf("lam_p", [1, 2, 64], F32)
            lam_s = mk.sbuf("lam_s", [1, 4], F32)
            nlam = mk.sbuf("nlam", [128, 1], F32)
            gv = mk.sbuf("dfgv", [128, 128], F32)
            mk.dma(lam_t[:], df_lam[li:li + 1, :, :])
            mk.tt("dve", lam_p[:, 0, :], lam_t[:, 0, :], lam_t[:, 1, :], ALU.mult)
            mk.tt("dve", lam_p[:, 1, :], lam_t[:, 2, :], lam_t[:, 3, :], ALU.mult)
            mk.reduce("dve", lam_s[:, 0:2], lam_p[:], ALU.add)
            mk.act(lam_s[:, 0:2], lam_s[:, 0:2], AF.Exp)
            mk.tt("dve", lam_s[:, 2:3], lam_s[:, 0:1], lam_s[:, 1:2], ALU.subtract)
            mk.ts("dve", lam_s[:, 2:3], lam_s[:, 2:3], lam_init, -1.0, op0=ALU.add, op1=ALU.mult)
            mk.mm(PS[6][:, 0:1], onesf[0:1, :], lam_s[0:1, 2:3])
            mk.copy("dve", nlam[:], PS[6][:, 0:1])
            bcast_row(gv[:], df_ng[li:li + 1, :])
            mk.ts("dve", gv[:], gv[:], 1.0 - lam_init, None, op0=ALU.mult)
            qT = mk.sbuf("aqT", [128, 4, NS], BF16)
            kT = mk.sbuf("akT", [128, 4, NS], BF16)
            sq = mk.sbuf("asq", [128, 4, NS], BF16)
            vA = mk.sbuf("avA", [128, 18, 4, 129], BF16)
            DF = mk.sbuf("aDF", [128, 18, 512], BF16)
            pt = [mk.sbuf("apt%d" % i, [128, 512], BF16) for i in range(4)]
            mxs = mk.sbuf("amx", [1, 64], F32)
            negM = mk.sbuf("anegM", [128, 1], F32)
            o1 = [mk.sbuf("ao1_%d" % i, [128, 128], F32) for i in range(2)]
            o2 = [mk.sbuf("ao2_%d" % i, [128, 128], F32) for i in range(2)]
            rc = [mk.sbuf("arc%d" % i, [128, 4], F32) for i in range(2)]
            stg = [mk.sbuf("astg%d" % i, [128, NS], BF16) for i in range(2)]
            mk.memset("pool", vA[:], 1.0)
            for s in range(2):
                for h in range(4):
                    load_seq_T(qT[:, h, :], PT[T_DFQ + h * 128:T_DFQ + (h + 1) * 128, :], s, q="sp")
                    load_seq_T(kT[:, h, :], PT[T_DFK + h * 128:T_DFK + (h + 1) * 128, :], s, q="act")
                c0, l0 = seq_ranges(s)
                for h in range(4):
                    mk.dma(vA[:, 0:2, h, 0:128],
                           PK[c0:c0 + CTX, K_DFV + h * 128:K_DFV + (h + 1) * 128].rearrange("(a p) c -> p a c", p=128))
                    mk.dma(vA[:, 2:18, h, 0:128],
                           PK[l0:l0 + SEQ, K_DFV + h * 128:K_DFV + (h + 1) * 128].rearrange("(a p) c -> p a c", p=128))
                mk.memset("dve", mxs[:], 0.0)
                for which, src in ((0, qT), (1, kT)):
                    mk.tt("pool", sq[:], src[:], src[:], ALU.mult)
                    idx = 0
                    for h in range(4):
                        for cst in range(0, NS, 512):
                            n = min(512, NS - cst)
                            pb = PS[3 + idx % 4]
                            mk.mm(pb[0:1, 0:n], onesb[:, 0:1], sq[:, h, cst:cst + n])
                            mk.reduce("dve", mxs[:, which * 32 + idx:which * 32 + idx + 1], pb[0:1, 0:n], ALU.max)
                            idx += 1
                mk.reduce("dve", mxs[:, 60:61], mxs[:, 0:32], ALU.max)
                mk.reduce("dve", mxs[:, 61:62], mxs[:, 32:60], ALU.max)
                mk.tt("dve", mxs[:, 62:63], mxs[:, 60:61], mxs[:, 61:62], ALU.mult)
                mk.act(mxs[:, 63:64], mxs[:, 62:63], AF.Sqrt, scale=1.0 / 64.0)
                mk.ts("dve", mxs[:, 63:64], mxs[:, 63:64], -1.0, None, op0=ALU.mult)
                mk.mm(PS[6][:, 0:1], onesf[0:1, :], mxs[0:1, 63:64])
                mk.copy("dve", negM[:], PS[6][:, 0:1])
                import os as _os
                if _os.environ.get('ATTN_STOP') == '1':
                    return
                qgroups = [(0, CTX, 0, 2)] + [(CTX + i * 512, 512, 0, 18) for i in range(4)]
                ci = 0
                it = 0
                for (q0, nq, tk0, tk1) in qgroups:
                    nsub = nq // 128
                    for h in range(4):
                        ob = 3 * (it % 2)
                        it += 1
                        O = [[PS[ob + (a * 4 + sb) // 3][:, ((a * 4 + sb) % 3) * 160:((a * 4 + sb) % 3) * 160 + 129]
                              for sb in range(4)] for a in range(2)]
                        steps = [(tk, a) for tk in range(tk0, tk1) for a in range(2)]
                        started = set()
                        bufs = {}

                        def emit_S(i):
                            tk, a = steps[i]
                            pb = PS[6 + (ci + i) % 2]
                            p_ = pt[(ci + i) % 4]
                            bufs[i] = p_
                            mk.mm(pb[:, 0:nq], kT[a * 64:(a + 1) * 64, h, tk * 128:(tk + 1) * 128],
                                  qT[a * 64:(a + 1) * 64, h, q0:q0 + nq])
                            mk.act(p_[:, 0:nq], pb[:, 0:nq], AF.Exp, bias=negM[:, 0:1], scale=0.125)

                        def emit_PV(i):
                            tk, a = steps[i]
                            p_ = bufs.pop(i)
                            for sb in range(nsub):
                                bank = (a * 4 + sb) // 3
                                st_ = (tk == tk0) and (bank not in started)
                                started.add(bank)
                                mk.mm(O[a][sb], p_[:, sb * 128:(sb + 1) * 128], vA[:, tk, h, :],
                                      start=st_, stop=(tk == tk1 - 1))

                        emit_S(0)
                        for i in range(len(steps)):
                            if i + 1 < len(steps):
                                emit_S(i + 1)
                            emit_PV(i)
                        ci += len(steps)
                        for sb in range(nsub):
                            tile = (q0 + sb * 128) // 128
                            r_ = rc[sb % 2]
                            a1 = o1[sb % 2]
                            a2 = o2[sb % 2]
                            mk.recip(r_[:, 0:1], O[0][sb][:, 128:129])
                            mk.recip(r_[:, 1:2], O[1][sb][:, 128:129])
                            mk.tt("dve", r_[:, 1:2], r_[:, 1:2], nlam[:, 0:1], ALU.mult)
                            mk.ts("dve", a1[:], O[0][sb][:, 0:128], r_[:, 0:1], None, op0=ALU.mult)
                            mk.stt("dve", a1[:], O[1][sb][:, 0:128], r_[:, 1:2], a1[:], ALU.mult, ALU.add)
                            mk.act(a2[:], a1[:], AF.Square, accum_out=r_[:, 2:3])
                            mk.act(r_[:, 3:4], r_[:, 2:3], AF.Sqrt, bias=EPS, scale=1.0 / 128.0)
                            mk.recip(r_[:, 3:4], r_[:, 3:4])
                            mk.stt("dve", DF[:, tile, h * 128:(h + 1) * 128], a1[:], r_[:, 3:4], gv[:],
                                   ALU.mult, ALU.mult)
                if _os.environ.get('ATTN_STOP') == '2':
                    return
                tok_to_BR(DF, 512, s, stg)
                if _os.environ.get('ATTN_STOP') == '3':
                    return

    def phase_merge(li):
        with mk.scope():
            wbr = mk.sbuf("wbr", [128, 12, D], BF16)
            wo = mk.sbuf("wo", [128, 8, D], BF16)
            for b in range(3):
                mk.dma(wbr[:, b * 4:(b + 1) * 4, :], w_branch[li, b].rearrange("(kc p) n -> p kc n", p=128), q="pool")
            mk.dma(wo[:], w_out[li].rearrange("(kc p) n -> p kc n", p=128), q="pool")
            brT = [mk.sbuf("mbr%d" % i, [128, 12, 512], BF16) for i in range(2)]
            gt = [mk.sbuf("mgt%d" % i, [128, 24, 512], BF16) for i in range(2)]
            xs_b = [mk.sbuf("mxs%d" % i, [128, 8, 512], F32) for i in range(2)]
            yT = [mk.sbuf("myT%d" % i, [128, 8, 512], BF16) for i in range(2)]
            ya = [mk.sbuf("mya%d" % i, [128, 512], F32) for i in range(2)]
            tmp = [mk.sbuf("mtm%d" % i, [128, 512], F32) for i in range(2)]
            ci = 0
            for g in range(NG):
                if li == n_layers - 1 and g == 0:
                    continue
                j = gset(g)
                b_ = brT[g % 2]
                g_ = gt[g % 2]
                xs = xs_b[g % 2]
                y_ = yT[g % 2]
                tsl = slice(g * 512, (g + 1) * 512)
                mk.dma(b_[:], BR[:, tsl].rearrange("(c p) t -> p c t", p=128), q="sp")
                mk.dma(g_[:], PT[T_GATE:T_GATE + 3072, tsl].rearrange("(c p) t -> p c t", p=128), q="act")
                mk.dma(xs[:], xT[:, tsl].rearrange("(kc p) t -> p kc t", p=128), q="sp")
                for oc in range(8):
                    acc = ya[oc % 2]
                    for br in range(3):
                        pb = PS[ci % 4]
                        ci += 1
                        for kc in range(4):
                            mk.mm(pb[:], wbr[:, br * 4 + kc, oc * 128:(oc + 1) * 128], b_[:, br * 4 + kc, :],
                                  start=(kc == 0), stop=(kc == 3))
                        if br == 0:
                            mk.tt("dve", acc[:], pb[:], g_[:, br * 8 + oc, :], ALU.mult)
                        else:
                            t_ = tmp[br % 2]
                            mk.tt("dve", t_[:], pb[:], g_[:, br * 8 + oc, :], ALU.mult)
                            mk.tt("pool", (acc[:] if br == 1 else y_[:, oc, :]), acc[:], t_[:], ALU.add)
                for oc in range(8):
                    pb = PS[4 + oc % 3]
                    for kc in range(8):
                        mk.mm(pb[:], wo[:, kc, oc * 128:(oc + 1) * 128], y_[:, kc, :], start=(kc == 0), stop=(kc == 7))
                    mk.stt("dve", xs[:, oc, :], pb[:], modv[:, li, 2, oc, j:j + 1], xs[:, oc, :], ALU.mult, ALU.add)
                mk.dma(xT[:, tsl].rearrange("(kc p) t -> p kc t", p=128), xs[:], q="act")

    def phase_moe(li):
        HALF = NTOK // 2
        with mk.scope():
            h2T = mk.sbuf("h2T", [128, 8, NTOK], BF16)
            comb = mk.sbuf("comb", [128, 36, 32], F32)
            with mk.scope():
                scratch = ([mk.sbuf("nxs%d" % i, [128, 8, 512], F32) for i in range(2)],
                           [mk.sbuf("nsq%d" % i, [128, 8, 512], BF16) for i in range(2)],
                           [mk.sbuf("nrs%d" % i, [128, 512], F32) for i in range(2)])
                wr = mk.sbuf("wr", [128, 8, 36], F32)
                rb = mk.sbuf("rb", [128, 36], F32)
                L = mk.sbuf("rL", [128, 36], F32)
                sm = mk.sbuf("rsm", [128, 16], F32)
                mg = mk.sbuf("rmg", [128, 4], F32)
                eg = mk.sbuf("reg", [128, 4], F32)
                ls = mk.sbuf("rls", [128, 8], F32)
                t8 = mk.sbuf("rt8", [128, 8], F32)
                s1 = mk.sbuf("rs1", [128, 8], F32)
                s2 = mk.sbuf("rs2", [128, 8], F32)
                mk.dma(wr[:], router_w[li].rearrange("(kc p) n -> p kc n", p=128))
                bcast_row(rb[:], router_b[li:li + 1, :])

                def route(g, xs):
                    for t4 in range(4):
                        tile = g * 4 + t4
                        pb = PS[t4 % 4]
                        for kc in range(8):
                            mk.mm(pb[:, 0:36], xs[:, kc, t4 * 128:(t4 + 1) * 128], wr[:, kc, :],
                                  start=(kc == 0), stop=(kc == 7))
                        mk.tt("dve", L[:], pb[:, 0:36], rb[:], ALU.add)
                        mk.reduce("dve", sm[:, 0:1], L[:, 0:4], ALU.max)
                        mk.ts("dve", mg[:], L[:, 0:4], sm[:, 0:1], None, op0=ALU.is_ge)
                        mk.ts("dve", sm[:, 1:2], sm[:, 0:1], -1.0, None, op0=ALU.mult)
                        mk.act(eg[:], L[:, 0:4], AF.Exp, bias=sm[:, 1:2], scale=1.0, accum_out=sm[:, 2:3])
                        mk.recip(sm[:, 3:4], sm[:, 2:3])
                        mk.ts("dve", ls[:], L[:, 4:12], mg[:, 0:1], None, op0=ALU.mult)
                        for gi in range(1, 4):
                            mk.stt("dve", ls[:], L[:, 4 + 8 * gi:12 + 8 * gi], mg[:, gi:gi + 1], ls[:],
                                   ALU.mult, ALU.add)
                        mk.max8(t8[:], ls[:])
                        mk.tt("dve", sm[:, 4:5], t8[:, 1:2], t8[:, 0:1], ALU.subtract)
                        mk.act(sm[:, 5:6], sm[:, 4:5], AF.Exp)
                        mk.ts("dve", sm[:, 6:7], sm[:, 5:6], 1.0, None, op0=ALU.add)
                        mk.recip(sm[:, 6:7], sm[:, 6:7])
                        mk.tt("dve", sm[:, 7:8], sm[:, 6:7], sm[:, 3:4], ALU.mult)
                        mk.tt("dve", sm[:, 8:9], sm[:, 7:8], sm[:, 5:6], ALU.mult)
                        mk.tt("dve", sm[:, 9:10], sm[:, 7:8], sm[:, 8:9], ALU.subtract)
                        mk.ts("dve", s1[:], ls[:], t8[:, 0:1], sm[:, 9:10], op0=ALU.is_ge, op1=ALU.mult)
                        mk.ts("dve", s2[:], ls[:], t8[:, 1:2], sm[:, 8:9], op0=ALU.is_ge, op1=ALU.mult)
                        mk.tt("dve", s1[:], s1[:], s2[:], ALU.add)
                        for gi in range(4):
                            mk.ts("dve", comb[:, tile, gi * 8:(gi + 1) * 8], s1[:], mg[:, gi:gi + 1], None,
                                  op0=ALU.mult)

                norm_groups(li, 3, h2T, scratch, h32_cb=route)
            if dbg:
                comb_dbg = mk.dram("comb_dbg%d" % li, [128, 36 * 32], F32, kind="ExternalOutput")
                mk.dma(comb_dbg[:, :], comb[:].rearrange("p a b -> p (a b)"))
            wg_b = [mk.sbuf("ewg%d" % i, [128, 8, 512], BF16) for i in range(2)]
            wu_b = [mk.sbuf("ewu%d" % i, [128, 8, 512], BF16) for i in range(2)]
            wd_b = [mk.sbuf("ewd%d" % i, [128, 4, D], BF16) for i in range(2)]
            hid_b = [mk.sbuf("ehid%d" % i, [128, 4, 512], BF16) for i in range(2)]
            sl_b = [mk.sbuf("esl%d" % i, [128, 512], F32) for i in range(2)]
            NPART = 3
            PT_TILES = 36 // NPART
            yacc = mk.sbuf("yacc", [128, PT_TILES, D], F32)
            xs_b = [mk.sbuf("exs%d" % i, [128, 8, 128], F32) for i in range(2)]
            ci = 0
            gi = 0
            pend = [None]
            dk = [0]

            def down(hid, wd, t0, nt, e, hf):
                for sb in range(nt // 128):
                    tile = (t0 + sb * 128) // 128
                    lt = tile - hf * PT_TILES
                    for hh in range(2):
                        py = PS[5 + dk[0] % 3]
                        dk[0] += 1
                        for fc in range(4):
                            mk.mm(py[:], hid[:, fc, sb * 128:(sb + 1) * 128], wd[:, fc, hh * 512:(hh + 1) * 512],
                                  start=(fc == 0), stop=(fc == 3))
                        mk.stt("dve", yacc[:, lt, hh * 512:(hh + 1) * 512], py[:], comb[:, tile, e:e + 1],
                               yacc[:, lt, hh * 512:(hh + 1) * 512], ALU.mult, ALU.add)

            for hf in range(NPART):
                mk.memset("pool", yacc[:], 0.0)
                grp = [(hf * PT_TILES * 128 + i * 512, 512) for i in range(PT_TILES // 4)]
                if li == n_layers - 1:
                    grp = [g_ for g_ in grp if g_[0] >= 512]
                for e in range(32):
                    wg, wu, wd = wg_b[e % 2], wu_b[e % 2], wd_b[e % 2]
                    mk.dma(wg[:], moe_wg[li, e].rearrange("(kc p) n -> p kc n", p=128), q="pool")
                    mk.dma(wu[:], moe_wu[li, e].rearrange("(kc p) n -> p kc n", p=128), q="pool")
                    mk.dma(wd[:], moe_wd[li, e].rearrange("(kc p) n -> p kc n", p=128), q="pool")
                    for (t0, nt) in grp:
                        hid = hid_b[gi % 2]
                        gi += 1
                        for fc in range(4):
                            pg = PS[ci % 3]
                            pu = PS[3 + ci % 2]
                            sl = sl_b[ci % 2]
                            ci += 1
                            for kc in range(8):
                                mk.mm(pg[:, 0:nt], wg[:, kc, fc * 128:(fc + 1) * 128], h2T[:, kc, t0:t0 + nt],
                                      start=(kc == 0), stop=(kc == 7))
                            for kc in range(8):
                                mk.mm(pu[:, 0:nt], wu[:, kc, fc * 128:(fc + 1) * 128], h2T[:, kc, t0:t0 + nt],
                                      start=(kc == 0), stop=(kc == 7))
                            mk.act(sl[:, 0:nt], pg[:, 0:nt], AF.Silu)
                            mk.tt("dve", hid[:, fc, 0:nt], pu[:, 0:nt], sl[:, 0:nt], ALU.mult)
                        if pend[0] is not None:
                            down(*pend[0])
                        pend[0] = (hid, wd, t0, nt, e, hf)
                if pend[0] is not None:
                    down(*pend[0])
                    pend[0] = None
                for lt in range(PT_TILES):
                    tile = hf * PT_TILES + lt
                    if li == n_layers - 1 and tile < 4:
                        continue
                    j = gset(tile // 4)
                    xs = xs_b[lt % 2]
                    tsl = slice(tile * 128, (tile + 1) * 128)
                    mk.dma(xs[:], xT[:, tsl].rearrange("(kc p) t -> p kc t", p=128), q="sp")
                    for half in range(2):
                        pb = PS[(lt * 2 + half) % 4]
                        for k4 in range(4):
                            kc = half * 4 + k4
                            mk.tr(pb[:, k4 * 128:(k4 + 1) * 128], yacc[:, lt, kc * 128:(kc + 1) * 128], ident[:])
                        for k4 in range(4):
                            kc = half * 4 + k4
                            mk.stt("dve", xs[:, kc, :], pb[:, k4 * 128:(k4 + 1) * 128], modv[:, li, 5, kc, j:j + 1],
                                   xs[:, kc, :], ALU.mult, ALU.add)
                    mk.dma(xT[:, tsl].rearrange("(kc p) t -> p kc t", p=128), xs[:], q="act")

    def phase_final():
        with mk.scope():
            fg = mk.sbuf("fg", [128, 8], F32)
            mk.dma(fg[:], fin_gT[:, :])
            xs_b = [mk.sbuf("fxs%d" % i, [128, 8, 512], F32) for i in range(2)]
            sq_b = [mk.sbuf("fsq%d" % i, [128, 8, 512], BF16) for i in range(2)]
            rs_b = [mk.sbuf("frs%d" % i, [128, 512], F32) for i in range(2)]
            ot = [mk.sbuf("fot%d" % i, [128, D], F32) for i in range(2)]
            for g in range(1, NG):
                xs, sq, rs = xs_b[g % 2], sq_b[g % 2], rs_b[g % 2]
                mk.dma(xs[:], xT[:, g * 512:(g + 1) * 512].rearrange("(kc p) t -> p kc t", p=128),
                       q=("sp" if g % 2 else "act"))
                mk.tt("pool", sq[:], xs[:], xs[:], ALU.mult)
                pb = PS[4 + g % 2]
                for kc in range(8):
                    mk.mm(pb[:], onesb[:], sq[:, kc, :], start=(kc == 0), stop=(kc == 7))
                mk.act(rs[:], pb[:], AF.Sqrt, bias=EPS, scale=1.0 / D)
                mk.recip(rs[:], rs[:])
                for kc in range(8):
                    mk.stt("dve", xs[:, kc, :], xs[:, kc, :], fg[:, kc:kc + 1], rs[:], ALU.mult, ALU.mult)
                for t4 in range(4):
                    u = (g - 1) * 4 + t4
                    o_ = ot[u % 2]
                    for half in range(2):
                        pq = PS[(u * 2 + half) % 4]
                        for k4 in range(4):
                            kc = half * 4 + k4
                            mk.tr(pq[:, k4 * 128:(k4 + 1) * 128], xs[:, kc, t4 * 128:(t4 + 1) * 128], ident[:])
                        mk.copy(evac_eng(), o_[:, half * 512:(half + 1) * 512], pq[:])
                    mk.dma(y_out[u // 16, (u % 16) * 128:(u % 16) * 128 + 128, :], o_[:], q="sp")

    def nat_chunk(d, cs):
        if d == 0:
            return cs
        return (3 - cs) if cs < 4 else (35 - (cs - 4))

    def rev_copy(eng, dst, src):
        mk.copy(eng, dst[:, 0:CTX], src[:, 0:CTX][:, ::-1])
        mk.copy(eng, dst[:, CTX:NS], src[:, CTX:NS][:, ::-1])

    def phase_mlstm(li):
        with mk.scope():
            HS = mk.sbuf("mHS", [128, 18, 512], F32)
            gb = mk.sbuf("mgb", [4, 4], F32)
            ngb = mk.sbuf("mngb", [4, 4], F32)
            gvn = mk.sbuf("mgvn", [128, 128], F32)
            ones4 = mk.sbuf("mones4", [4, NS], F32)
            mk.dma(gb[:], ml_gb[li])
            mk.ts("dve", ngb[:], gb[:], -1.0, None, op0=ALU.mult)
            bcast_row(gvn[:], ml_ng[li:li + 1, :])
            mk.memset("pool", ones4[:], 1.0)
            for s in range(2):
                with mk.scope():
                    qT = mk.sbuf("mqT", [128, 4, NS], BF16)
                    kT = mk.sbuf("mkT", [128, 4, NS], BF16)
                    kK = mk.sbuf("mkK", [128, 18, 512], BF16)
                    vA = mk.sbuf("mvA", [128, 18, 4, 129], BF16)
                    uV = mk.sbuf("muV", [128, 18, 4, 129], BF16)
                    R = [mk.sbuf("mR%d" % i, [4, NS], F32) for i in range(5)]
                    COL = mk.sbuf("mCOL", [128, 3, 18, 4], F32)
                    DECr = mk.sbuf("mDECr", [4, 36, 4], F32)
                    DECb = mk.sbuf("mDECb", [128, 36, 4], F32)
                    CT = mk.sbuf("mCT", [128, 4, 129], F32)
                    tmpC = mk.sbuf("mtmpC", [128, 4, 129], F32)
                    CTb = [mk.sbuf("mCTb%d" % i, [128, 4, 129], BF16) for i in range(2)]
                    smb = [mk.sbuf("msm%d" % i, [128, 4, 128], BF16) for i in range(2)]
                    t4b = [mk.sbuf("mt4%d" % i, [128, 4], F32) for i in range(2)]
                    tmo = [mk.sbuf("mtmo%d" % i, [128, 4, 128], F32) for i in range(2)]
                    c0, l0 = seq_ranges(s)
                    for h in range(4):
                        load_seq_T(qT[:, h, :], PT[T_MLQ + h * 128:T_MLQ + (h + 1) * 128, :], s, q="sp")
                        load_seq_T(kT[:, h, :], PT[T_MLK + h * 128:T_MLK + (h + 1) * 128, :], s, q="act")
                    load_seq_K(kK, K_MLK, 512, s)
                    mk.memset("pool", vA[:], 1.0)
                    for h in range(4):
                        mk.dma(vA[:, 0:2, h, 0:128],
                               PK[c0:c0 + CTX, K_MLV + h * 128:K_MLV + (h + 1) * 128].rearrange("(a p) c -> p a c", p=128))
                        mk.dma(vA[:, 2:18, h, 0:128],
                               PK[l0:l0 + SEQ, K_MLV + h * 128:K_MLV + (h + 1) * 128].rearrange("(a p) c -> p a c", p=128))
                    for d in range(2):
                        irow = GT[d * 8:d * 8 + 4, :]
                        frow = GT[d * 8 + 4:d * 8 + 8, :]
                        if d == 0:
                            load_seq_T(R[1], irow, s)
                            load_seq_T(R[0], frow, s)
                        else:
                            load_seq_T(R[3], irow, s)
                            load_seq_T(R[4], frow, s)
                            rev_copy("dve", R[1], R[3])
                            rev_copy("pool", R[0], R[4])
                        R0, R1, R2, R3, R4 = R
                        v3 = lambda t: t[:].rearrange("p (c j) -> p c j", j=64)
                        mk.act(R0[:], R0[:], AF.Exp, bias=ngb[:, 2 * d + 1:2 * d + 2], scale=-1.0)
                        mk.act(R0[:], R0[:], AF.Ln, bias=1.0, scale=1.0)
                        mk.ts("dve", R0[:], R0[:], -1.0, None, op0=ALU.mult)
                        mk.scan("dve", R2[:], ones4[:], R0[:], 0.0, ALU.mult, ALU.add)
                        mk.stt("dve", R1[:], R1[:], gb[:, 2 * d:2 * d + 1], R2[:], ALU.add, ALU.subtract)
                        mk.scan("dve", R0[:], ones4[:], R1[:], 0.0, ALU.mult, ALU.max)
                        mprev = v3(R0)[:, 0:35, 63:64].to_broadcast([4, 35, 64])
                        mk.copy("dve", R3[:, 0:64], R1[:, 0:64])
                        mk.tt("dve", v3(R3)[:, 1:36, :], v3(R1)[:, 1:36, :], mprev, ALU.subtract)
                        mk.act(R3[:], R3[:], AF.Exp)
                        mk.ts("dve", R4[:, 0:64], R0[:, 0:64], -1.0, None, op0=ALU.mult)
                        mk.tt("dve", v3(R4)[:, 1:36, :], mprev, v3(R0)[:, 1:36, :], ALU.subtract)
                        mk.act(R4[:], R4[:], AF.Exp)
                        mk.tt("dve", R2[:], R2[:], R0[:], ALU.add)
                        mk.act(R2[:], R2[:], AF.Exp, scale=-1.0)
                        mk.tt("dve", DECr[:], v3(R4)[:, :, 63:64].to_broadcast([4, 36, 4]),
                              ident[0:4, 0:4].unsqueeze(1).to_broadcast([4, 36, 4]), ALU.mult)
                        mk.mm(PS[6][:, 0:144], onesf[0:4, :], DECr[:].rearrange("p c h -> p (c h)"))
                        mk.copy("dve", DECb[:].rearrange("p c h -> p (c h)"), PS[6][:, 0:144])
                        if d == 0:
                            NAT = [R3, R4, R2]
                        else:
                            rev_copy("dve", R0, R3)
                            rev_copy("pool", R1, R4)
                            rev_copy("dve", R3, R2)
                            NAT = [R0, R1, R3]
                        for qi, arr in enumerate(NAT):
                            for t in range(18):
                                o_ = (qi * 18 + t) * 4
                                mk.tr(PS[5][:, o_:o_ + 4], arr[:, t * 128:(t + 1) * 128], ident[0:4, 0:4])
                        mk.copy("dve", COL[:].rearrange("p a t h -> p (a t h)"), PS[5][:, 0:216])
                        mk.tt("pool", uV[:], vA[:], COL[:, 0].unsqueeze(3).to_broadcast([128, 18, 4, 129]), ALU.mult)
                        mk.memset("dve", CT[:], 0.0)
                        mk.memset("pool", CTb[0][:], 0.0)
                        mask = maskF if d == 0 else maskB
                        cur_tile = -1
                        for cs in range(36):
                            c = nat_chunk(d, cs)
                            tile, par = c // 2, c % 2
                            rows = slice(par * 64, par * 64 + 64)
                            ctb_cur, ctb_nxt = CTb[cs % 2], CTb[(cs + 1) % 2]
                            if tile != cur_tile:
                                cur_tile = tile
                                sm = smb[tile % 2]
                                for h in range(4):
                                    mk.mm(PS[4][:, h * 128:(h + 1) * 128], kT[:, h, tile * 128:(tile + 1) * 128],
                                          qT[:, h, tile * 128:(tile + 1) * 128])
                                mk.tt("dve", sm[:], PS[4][:].rearrange("p (h j) -> p h j", h=4),
                                      mask[:].unsqueeze(1).to_broadcast([128, 4, 128]), ALU.mult)
                            for h in range(4):
                                o_ = PS[h // 2][rows, (h % 2) * 256:(h % 2) * 256 + 129]
                                mk.mm(o_, sm[:, h, par * 64:par * 64 + 64], uV[:, tile, h, :], start=True, stop=False)
                                mk.mm(o_, qT[:, h, c * 64:(c + 1) * 64], ctb_cur[:, h, :], start=False, stop=True)
                            for h in range(4):
                                mk.mm(PS[2 + h // 2][:, (h % 2) * 256:(h % 2) * 256 + 129],
                                      kK[rows, tile, h * 128:(h + 1) * 128], uV[rows, tile, h, :])
                            for b in range(2):
                                mk.tt("dve", tmpC[:, 2 * b:2 * b + 2, :],
                                      PS[2 + b][:, 0:512].rearrange("p (h v) -> p h v", h=2)[:, :, 0:129], CT[:, 2 * b:2 * b + 2, :], ALU.add)
                            mk.tt("pool", CT[:], tmpC[:], DECb[:, cs, :].unsqueeze(2).to_broadcast([128, 4, 129]), ALU.mult)
                            mk.copy("act", ctb_nxt[:], CT[:])
                            t4 = t4b[cs % 2]
                            for b in range(2):
                                mk.tt("dve", t4[rows, 2 * b:2 * b + 2], PS[b][rows, 128:512:256],
                                      COL[rows, 1, tile, 2 * b:2 * b + 2], ALU.mult)
                            mk.stt("dve", t4[rows, :], t4[rows, :], -1.0, t4[rows, :], ALU.mult, ALU.max)
                            mk.tt("dve", t4[rows, :], t4[rows, :], COL[rows, 2, tile, :], ALU.max)
                            mk.recip(t4[rows, :], t4[rows, :])
                            mk.tt("dve", t4[rows, :], t4[rows, :], COL[rows, 1, tile, :], ALU.mult)
                            for b in range(2):
                                src = PS[b][rows, 0:512].rearrange("p (h v) -> p h v", h=2)[:, :, 0:128]
                                sc = t4[rows, 2 * b:2 * b + 2].unsqueeze(2).to_broadcast([64, 2, 128])
                                hs = HS[rows, tile, 2 * b * 128:(2 * b + 2) * 128].rearrange("p (h v) -> p h v", h=2)
                                if d == 0:
                                    mk.tt("dve", hs, src, sc, ALU.mult)
                                else:
                                    to = tmo[cs % 2][rows, 2 * b:2 * b + 2, :]
                                    mk.tt("dve", to, src, sc, ALU.mult)
                                    mk.tt("pool", hs, hs, to, ALU.add)
                with mk.scope():
                    sgo = mk.sbuf("msgo", [128, 18, 512], BF16)
                    sqh = mk.sbuf("msqh", [128, 18, 512], F32)
                    ML = mk.sbuf("mML", [128, 18, 512], BF16)
                    ss = mk.sbuf("mss", [128, 72], F32)
                    stg = [mk.sbuf("mstg%d" % i, [128, NS], BF16) for i in range(2)]
                    load_seq_K(sgo, K_MLO, 512, s)
                    finish_branch(HS, sgo, gvn, sqh, ss, ML)
                    tok_to_BR(ML, 0, s, stg)

    def finish_branch(HS, gate, gvn, sqh, ss, OUT):
        h3 = lambda t: t[:].rearrange("p t (h v) -> p (t h) v", v=128)
        mk.tt("pool", sqh[:], HS[:], HS[:], ALU.mult)
        mk.reduce("dve", ss[:], h3(sqh), ALU.add)
        mk.act(ss[:], ss[:], AF.Sqrt, bias=EPS, scale=1.0 / 128.0)
        mk.recip(ss[:], ss[:])
        mk.tt("dve", h3(sqh), h3(HS), ss[:].unsqueeze(2).to_broadcast([128, 72, 128]), ALU.mult)
        mk.tt("pool", h3(sqh), h3(sqh), gvn[:].unsqueeze(1).to_broadcast([128, 72, 128]), ALU.mult)
        mk.tt("dve", OUT[:], sqh[:], gate[:], ALU.mult)

    def phase_gla(li):
        with mk.scope():
            OS = mk.sbuf("gOS", [128, 18, 512], F32)
            gvn = mk.sbuf("ggvn", [128, 128], F32)
            wa = mk.sbuf("gwa", [16, 2, 256], F32)
            nba = mk.sbuf("gnba", [128, 2, 2], F32)
            RST = mk.sbuf("gRST", [128, NS], F32)
            bcast_row(gvn[:], gl_ng[li:li + 1, :])
            mk.dma(wa[:], gl_wa[li].rearrange("d r n -> r d n"))
            mk.dma(nba[:], gl_baT[li].rearrange("d p h -> p d h"))
            mk.ts("dve", nba[:], nba[:], -1.0, None, op0=ALU.mult)
            mk.memset("pool", RST[:], 1.0)
            mk.memset("pool", RST[:].rearrange("p (c j) -> p c j", j=64)[:, :, 0:1], 0.0)
            for s in range(2):
                with mk.scope():
                    qT = mk.sbuf("gqT", [128, 2, NS], BF16)
                    kT = mk.sbuf("gkT", [128, 2, NS], BF16)
                    vK = mk.sbuf("gvK", [128, 18, 512], BF16)
                    aT = mk.sbuf("gaT", [16, 2, NS], F32)
                    LA = mk.sbuf("gLA", [128, 2, NS], F32)
                    Pc = mk.sbuf("gPc", [128, 2, NS], F32)
                    qg = mk.sbuf("gqg", [128, 2, NS], BF16)
                    kg = mk.sbuf("gkg", [128, 2, NS], BF16)
                    kg2 = mk.sbuf("gkg2", [128, 2, NS], BF16)
                    kgK = mk.sbuf("gkgK", [128, 18, 256], BF16)
                    Tc = mk.sbuf("gTc", [128, 2, 36], F32)
                    eb = mk.sbuf("geb", [128, 2, 36], F32)
                    S = mk.sbuf("gS", [128, 2, 128], F32)
                    Sb = [mk.sbuf("gSb%d" % i, [128, 4, 128], BF16) for i in range(2)]
                    amb = [mk.sbuf("gam%d" % i, [128, 4, 128], BF16) for i in range(2)]
                    for hh in range(2):
                        load_seq_T(qT[:, hh, :], PT[T_GLQ + hh * 128:T_GLQ + (hh + 1) * 128, :], s, q="sp")
                        load_seq_T(kT[:, hh, :], PT[T_GLK + hh * 128:T_GLK + (hh + 1) * 128, :], s, q="act")
                    load_seq_K(vK, K_GLV, 512, s)
                    for d in range(2):
                        load_seq_T(aT[:, d, :], GT[16 + d * 16:32 + d * 16, :], s)
                    v4 = lambda t: t[:].rearrange("p h (c j) -> p h c j", j=64)
                    for d in range(2):
                        ci = 0
                        for hh in range(2):
                            for t0 in range(0, NS, 512):
                                n = min(512, NS - t0)
                                pb = PS[5 + ci % 2]
                                ci += 1
                                mk.mm(pb[:, 0:n], wa[:, d, hh * 128:(hh + 1) * 128], aT[:, d, t0:t0 + n])
                                mk.act(LA[:, hh, t0:t0 + n], pb[:, 0:n], AF.Exp, bias=nba[:, d, hh:hh + 1], scale=-1.0)
                        mk.act(LA[:], LA[:], AF.Ln, bias=1.0, scale=1.0)
                        mk.ts("dve", LA[:], LA[:], -1.0 / 16.0, None, op0=ALU.mult)
                        for hh in range(2):
                            mk.scan("dve", Pc[:, hh, :], RST[:], LA[:, hh, :], 0.0, ALU.mult, ALU.add)
                        mk.copy("dve", Tc[:], v4(Pc)[:, :, :, 63])
                        mk.act(eb[:], Tc[:], AF.Exp)
                        if d == 1:
                            mk.tt("dve", v4(Pc), Tc[:].unsqueeze(3).to_broadcast([128, 2, 36, 64]), v4(Pc), ALU.subtract)
                            mk.tt("pool", Pc[:], Pc[:], LA[:], ALU.add)
                        mk.act(LA[:], Pc[:], AF.Exp)
                        mk.tt("dve", qg[:], qT[:], LA[:], ALU.mult)
                        mk.act(LA[:], Pc[:], AF.Exp, scale=-1.0)
                        mk.tt("dve", kg[:], kT[:], LA[:], ALU.mult)
                        mk.tt("pool", v4(kg2), v4(kg), eb[:].unsqueeze(3).to_broadcast([128, 2, 36, 64]), ALU.mult)
                        import os as _osg
                        _gs = _osg.environ.get("GLA_STOP", "")
                        if _gs == "1":
                            return
                        for hh in range(2):
                            for t in range(18):
                                pv = PS[5 + t % 2][:, (t % 4) * 128:(t % 4) * 128 + 128]
                                mk.mm(pv, kg2[:, hh, t * 128:(t + 1) * 128], identb[:])
                                mk.copy(evac_eng(), kgK[:, t, hh * 128:(hh + 1) * 128], pv)
                        if _gs == "2":
                            return
                        mk.memset("dve", S[:], 0.0)
                        mk.memset("pool", Sb[0][:], 0.0)
                        mk.memset("pool", Sb[1][:], 0.0)
                        mask = maskF if d == 0 else maskB
                        cur_tile = -1
                        for cs in range(36):
                            c = nat_chunk(d, cs)
                            tile, par = c // 2, c % 2
                            rows = slice(par * 64, par * 64 + 64)
                            sb_cur, sb_nxt = Sb[cs % 2], Sb[(cs + 1) % 2]
                            if tile != cur_tile:
                                cur_tile = tile
                                am = amb[tile % 2]
                                for h in range(4):
                                    hr = slice((h % 2) * 64, (h % 2) * 64 + 64)
                                    mk.mm(PS[4 + h % 2][:, (h // 2) * 128:(h // 2 + 1) * 128],
                                          kg[hr, h // 2, tile * 128:(tile + 1) * 128],
                                          qg[hr, h // 2, tile * 128:(tile + 1) * 128])
                                for wh in range(2):
                                    mk.tt("dve", am[:, wh::2, :], PS[4 + wh][:, 0:256].rearrange("p (h j) -> p h j", h=2),
                                          mask[:].unsqueeze(1).to_broadcast([128, 2, 128]), ALU.mult)
                            _lv = int(_gs) if _gs else 9
                            if _lv >= 4:
                                for h in range(4):
                                    o_ = PS[h // 2][rows, (h % 2) * 128:(h % 2) * 128 + 128]
                                    mk.mm(o_, am[:, h, par * 64:par * 64 + 64], vK[:, tile, h * 128:(h + 1) * 128],
                                          start=True, stop=False)
                                    mk.mm(o_, qg[:, h // 2, c * 64:(c + 1) * 64], sb_cur[:, h, :], start=False, stop=True)
                            if _lv >= 5:
                                for h in range(4):
                                    hh, wh = h // 2, h % 2
                                    mk.mm(PS[2 + hh][:, wh * 128:(wh + 1) * 128], kgK[rows, tile, hh * 128:(hh + 1) * 128],
                                          vK[rows, tile, h * 128:(h + 1) * 128])
                            if _lv >= 6:
                                for h in range(4):
                                    hh, wh = h // 2, h % 2
                                    hr = slice(wh * 64, wh * 64 + 64)
                                    mk.stt("dve", S[hr, hh, :], S[hr, hh, :], eb[hr, hh, c:c + 1],
                                           PS[2 + hh][hr, wh * 128:(wh + 1) * 128], ALU.mult, ALU.add)
                            if _lv >= 7:
                                for wh in range(2):
                                    hr = slice(wh * 64, wh * 64 + 64)
                                    mk.copy("act", sb_nxt[hr, wh::2, :], S[hr, :, :])
                            if _lv >= 4:
                                for b in range(2):
                                    dst = OS[rows, tile, b * 256:(b + 1) * 256]
                                    if d == 0:
                                        mk.copy("dve", dst, PS[b][rows, 0:256])
                                    else:
                                        mk.tt("dve", dst, dst, PS[b][rows, 0:256], ALU.add)
                with mk.scope():
                    sg = mk.sbuf("gsg", [128, 18, 512], BF16)
                    sqh = mk.sbuf("gsqh", [128, 18, 512], F32)
                    GLo = mk.sbuf("gGLo", [128, 18, 512], BF16)
                    ss = mk.sbuf("gss", [128, 72], F32)
                    stg = [mk.sbuf("gstg%d" % i, [128, NS], BF16) for i in range(2)]
                    load_seq_K(sg, K_GLG, 512, s)
                    finish_branch(OS, sg, gvn, sqh, ss, GLo)
                    tok_to_BR(GLo, 1024, s, stg)

    phase_load_x()
    phase_mod()
    order = ["norm1", "proj", "mlstm", "attn", "gla", "merge", "moe"]
    lim = order.index(stop_after) if stop_after else len(order)
    for li in range(n_layers):
        with mk.scope():
            hT = mk.sbuf("hT", [128, 8, NTOK], BF16)
            with mk.scope():
                scratch = ([mk.sbuf("nxs%d" % i, [128, 8, 512], F32) for i in range(2)],
                           [mk.sbuf("nsq%d" % i, [128, 8, 512], BF16) for i in range(2)],
                           [mk.sbuf("nrs%d" % i, [128, 512], F32) for i in range(2)])
                norm_groups(li, 0, hT, scratch)
            if dbg:
                hT_dbg = mk.dram("hT_dbg%d" % li, [D, NTOK], BF16, kind="ExternalOutput")
                mk.dma(hT_dbg[:, :].rearrange("(kc p) t -> p kc t", p=128), hT[:])
            if lim >= 1:
                with mk.scope():
                    phase_proj(li, hT)
        if lim >= 2 and "mlstm" not in skip:
            phase_mlstm(li)
        if lim >= 3 and "attn" not in skip:
            phase_attn(li)
        if lim >= 4 and "gla" not in skip:
            phase_gla(li)
        if lim >= 5:
            phase_merge(li)
        if lim >= 6 and do_moe:
            phase_moe(li)
    if lim >= 6:
        phase_final()
    if dbg:
        modv_dbg = mk.dram("modv_dbg", [128, 2 * 6 * 8 * 3], F32, kind="ExternalOutput")
        mk.dma(modv_dbg[:, :], modv[:].rearrange("p a b c d -> p (a b c d)"))
    mk.finish()
    return nc, mk


def make_consts():
    c = np.zeros((128, 384 + 2 * SEQ), np.float32)
    c[:, 0:128] = np.eye(128, dtype=np.float32)
    i = np.arange(128)[:, None]
    j = np.arange(128)[None, :]
    same = (i // 64) == (j // 64)
    c[:, 128:256] = (same & (i <= j)).astype(np.float32)
    c[:, 256:384] = (same & (i >= j)).astype(np.float32)
    t = np.arange(SEQ)
    rowp = (t // 64).astype(np.float32)
    colp = (t % 64).astype(np.float32)
    inv = (10000.0 ** (-np.arange(16, dtype=np.float32) / 16)).astype(np.float32)
    for d in range(128):
        dd = d % 64
        axis = dd // 32
        f = dd % 16
        half = (dd % 32) // 16
        ang = (rowp if axis == 0 else colp) * inv[f]
        c[d, 384:384 + SEQ] = np.cos(ang)
        c[d, 384 + SEQ:384 + 2 * SEQ] = np.sin(ang) * (-1.0 if half == 0 else 1.0)
    return c


def pcol(v, nch):
    return np.ascontiguousarray(np.asarray(v, np.float32).reshape(nch, 128).T)


def prep_shared(inp):
    sh = {}
    f = lambda a: np.ascontiguousarray(np.asarray(a, np.float32))
    sh["w_mod"] = f(inp["w_mod"])
    sh["b_modT"] = np.stack([pcol(inp["b_mod"][li], 48) for li in range(2)])
    sh["gmixT"] = np.stack([pcol(inp["norm_mix_g"][li], 8) for li in range(2)])
    sh["gffnT"] = np.stack([pcol(inp["norm_ffn_g"][li], 8) for li in range(2)])
    sh["w_in"] = f(inp["w_in"])
    sh["ml_gb"] = f(np.asarray(inp["ml_gate_b"]).reshape(2, 4, 4).transpose(0, 2, 1))
    sh["ml_ng"] = f(inp["ml_norm_g"])
    sh["df_lam"] = f(inp["df_lambda"])
    sh["df_ng"] = f(inp["df_norm_g"])
    sh["gl_wa"] = f(inp["gl_w_alpha"])
    sh["gl_baT"] = f(np.asarray(inp["gl_b_alpha"]).reshape(2, 2, 2, 128).transpose(0, 1, 3, 2))
    sh["gl_ng"] = f(inp["gl_norm_g"])
    sh["w_branch"] = f(inp["w_branch"])
    sh["w_out"] = f(inp["w_out"])
    sh["router_w"] = f(np.concatenate([inp["router_group_w"], inp["router_expert_w"]], axis=-1))
    sh["router_b"] = f(np.concatenate([inp["router_group_b"], inp["router_expert_b"]], axis=-1))
    sh["moe_wg"] = f(inp["moe_w_gate"])
    sh["moe_wu"] = f(inp["moe_w_up"])
    sh["moe_wd"] = f(inp["moe_w_down"])
    sh["fin_gT"] = pcol(inp["final_norm_g"], 8)
    sh["consts"] = make_consts()
    return sh


def prep_core(inp, core):
    b0 = 2 * core
    m = {}
    m["x2"] = np.ascontiguousarray(np.asarray(inp["x"][b0:b0 + 2], np.float32))
    m["ctx2"] = np.ascontiguousarray(np.asarray(inp["ctx"][b0:b0 + 2], np.float32))
    cs = np.stack([np.asarray(inp["c_ctx"], np.float32), np.asarray(inp["c"][b0], np.float32),
                   np.asarray(inp["c"][b0 + 1], np.float32)], axis=-1)
    m["cT"] = np.ascontiguousarray(cs.reshape(8, 128, 3).transpose(1, 0, 2))
    return m


_CACHE = {}
CORES_PER_LAUNCH = 8


def kernel(**inputs):
    if "nc" not in _CACHE:
        _CACHE["nc"] = build()[0]
    nc = _CACHE["nc"]
    sh = prep_shared(inputs)
    in_maps = []
    for core in range(8):
        m = dict(sh)
        m.update(prep_core(inputs, core))
        in_maps.append(m)
    outs = []
    for g0 in range(0, 8, CORES_PER_LAUNCH):
        res = run_bass_kernel_spmd(nc, in_maps[g0:g0 + CORES_PER_LAUNCH], core_ids=list(range(CORES_PER_LAUNCH)))
        outs.extend(np.asarray(r["y_out"], np.float32) for r in res.results)
    return np.concatenate(outs, axis=0)
```

```python
import contextlib
import math
import numpy as np
import concourse.bass as bass
import concourse.mybir as mybir
from concourse.bass_utils import run_bass_kernel_spmd

F32 = mybir.dt.float32
BF16 = mybir.dt.bfloat16
AF = mybir.ActivationFunctionType
ALU = mybir.AluOpType
AX = mybir.AxisListType

COMPUTE = ("pe", "act", "dve", "pool")


class _Op:
    __slots__ = ("eng", "fn", "deps", "marked", "val", "dma", "dsem", "dval", "qwait", "emitted", "seq")

    def __init__(self, eng, fn):
        self.eng = eng
        self.fn = fn
        self.deps = []
        self.marked = False
        self.val = None
        self.dma = False
        self.dsem = None
        self.dval = None
        self.qwait = None
        self.emitted = False


class MK:
    def __init__(self, nc, n_dma_sems=8):
        self.nc = nc
        self.stacks = [contextlib.ExitStack()]
        self.e = {"pe": nc.tensor, "act": nc.scalar, "dve": nc.vector, "pool": nc.gpsimd, "sp": nc.sync}
        self.ops = {k: [] for k in self.e}
        self.pstep = {}
        self.notrack = set()
        self.psum_names = set()
        self.recs = {}
        self.n_dma_sems = n_dma_sems
        self.dma_rr = {k: 0 for k in self.e}
        self.dma_last = {}
        self.sem = {k: nc.alloc_semaphore("s_" + k) for k in COMPUTE}
        self.dsems = {}
        for k in ("sp", "pool", "act"):
            for s in range(n_dma_sems):
                self.dsems[(k, s)] = nc.alloc_semaphore("d_%s_%d" % (k, s))
        self.cnt = {k: 0 for k in self.e}
        self.dcnt = {}
        self.seen = {k: {} for k in self.e}
        self.last_op = {k: None for k in self.e}
        self.n_inst = 0
        self.trace = {k: [] for k in self.e}

    def sbuf(self, name, shape, dtype):
        self.uid = getattr(self, "uid", 0) + 1
        name = "%s_%d" % (name, self.uid)
        t = self.stacks[-1].enter_context(self.nc.sbuf_tensor(name, list(shape), dtype))
        self.pstep[name] = int(np.prod(shape[1:]))
        self.recs.pop(name, None)
        return t

    def psum(self, name, shape, dtype=F32):
        t = self.stacks[-1].enter_context(self.nc.psum_tensor(name, list(shape), dtype))
        self.pstep[name] = int(np.prod(shape[1:]))
        self.psum_names.add(name)
        return t

    def dram(self, name, shape, dtype, kind="Internal", rowlen=None):
        t = self.nc.dram_tensor(name, list(shape), dtype, kind=kind)
        self.pstep[name] = int(rowlen if rowlen is not None else shape[-1])
        if kind == "ExternalInput":
            self.notrack.add(name)
        return t.ap()

    @contextlib.contextmanager
    def scope(self):
        self.stacks.append(contextlib.ExitStack())
        try:
            yield
            self.flush()
            self.barrier()
            self.flush()
        finally:
            self.stacks.pop().close()

    def _box(self, ap):
        name = ap.name
        ps = self.pstep[name]
        off = int(ap.offset)
        p0, f0 = divmod(off, ps)
        plo = phi = p0
        flo = fhi = f0
        for (step, cnt) in ap.ap:
            ext = step * (cnt - 1)
            if step != 0 and abs(step) >= ps and step % ps == 0:
                e = ext // ps
                if e < 0:
                    plo += e
                else:
                    phi += e
            else:
                if ext < 0:
                    flo += ext
                else:
                    fhi += ext
        return name, plo, phi, flo, fhi

    def _access(self, op, ap, is_write):
        if ap.name in self.notrack:
            return
        name, plo, phi, flo, fhi = self._box(ap)
        excl = name in self.psum_names
        if excl:
            flo, fhi = 0, self.pstep[name] - 1
            plo, phi = (plo // 32) * 32, (phi // 32) * 32 + 31
        lst = self.recs.get(name, [])
        keep = []
        for r in lst:
            ov = not (r[1] < plo or phi < r[0] or r[3] < flo or fhi < r[2])
            o = r[4]
            if ov and o is not op:
                same = (o.eng == op.eng) and not o.dma and not op.dma
                if same:
                    need = (op.eng != "pe") and r[5] and not is_write
                else:
                    need = excl or is_write or r[5]
                if need:
                    op.deps.append(o)
            contained = (plo <= r[0] and r[1] <= phi and flo <= r[2] and r[3] <= fhi)
            if contained and o is not op:
                same = (o.eng == op.eng) and not o.dma and not op.dma
                if excl or is_write or ((not r[5]) and same):
                    if not (excl and same and r[5] and not is_write and False):
                        continue
            keep.append(r)
        keep.append([plo, phi, flo, fhi, op, is_write])
        self.recs[name] = keep

    def _record(self, eng, fn, reads, writes, dma=False):
        op = _Op(eng, fn)
        op.dma = dma
        for ap in reads:
            if ap is not None and hasattr(ap, "ap"):
                self._access(op, ap, False)
        for ap in writes:
            if ap is not None and hasattr(ap, "ap"):
                self._access(op, ap, True)
        self.gseq = getattr(self, "gseq", 0) + 1
        op.seq = self.gseq
        if op.deps:
            best = {}
            keep = []
            for d_ in op.deps:
                if d_.dma:
                    keep.append(d_)
                else:
                    b_ = best.get(d_.eng)
                    if b_ is None or d_.seq > b_.seq:
                        best[d_.eng] = d_
            op.deps = keep + list(best.values())
        if dma:
            slot = self.dma_rr[eng]
            self.dma_rr[eng] = (slot + 1) % (2 if eng == "pool" else self.n_dma_sems)
            op.dsem = (eng, slot)
            op.qwait = self.dma_last.get((eng, slot))
            self.dma_last[(eng, slot)] = op
            self.dcnt[op.dsem] = self.dcnt.get(op.dsem, 0) + 16
            op.dval = self.dcnt[op.dsem]
        self.ops[eng].append(op)
        self.last_op[eng] = op
        return op

    def mm(self, out, lhsT, rhs, start=True, stop=True):
        return self._record("pe", lambda: self.nc.tensor.matmul(out, lhsT, rhs, start=start, stop=stop),
                            [lhsT, rhs], [out])

    def tr(self, out, in_, ident):
        return self._record("pe", lambda: self.nc.tensor.transpose(out, in_, ident), [in_, ident], [out])

    def act(self, out, in_, func, bias=None, scale=None, accum_out=None):
        kw = {}
        if bias is not None:
            kw["bias"] = bias
        if scale is not None:
            kw["scale"] = scale
        if accum_out is not None:
            kw["accum_out"] = accum_out
        return self._record("act", lambda: self.nc.scalar.activation(out, in_, func, **kw),
                            [in_, bias, scale], [out, accum_out])

    def tt(self, eng, out, in0, in1, op):
        return self._record(eng, lambda: self.e[eng].tensor_tensor(out, in0, in1, op), [in0, in1], [out])

    def ts(self, eng, out, in0, s1, s2=None, op0=ALU.mult, op1=None):
        kw = {}
        if op1 is not None:
            kw["op1"] = op1
        return self._record(eng, lambda: self.e[eng].tensor_scalar(out, in0, s1, s2, op0, **kw),
                            [in0, s1, s2], [out])

    def stt(self, eng, out, in0, scalar, in1, op0, op1):
        return self._record(eng, lambda: self.e[eng].scalar_tensor_tensor(out, in0, scalar, in1, op0, op1),
                            [in0, scalar, in1], [out])

    def copy(self, eng, out, in_):
        if eng == "act":
            return self._record("act", lambda: self.nc.scalar.copy(out, in_), [in_], [out])
        return self._record(eng, lambda: self.e[eng].tensor_copy(out, in_), [in_], [out])

    def memset(self, eng, ap, val):
        return self._record(eng, lambda: self.e[eng].memset(ap, val), [], [ap])

    def reduce(self, eng, out, in_, op, axis=AX.X):
        return self._record(eng, lambda: self.e[eng].tensor_reduce(out, in_, axis, op), [in_], [out])

    def scan(self, eng, out, d0, d1, init, op0, op1):
        return self._record(eng, lambda: self.e[eng].tensor_tensor_scan(out, d0, d1, init, op0, op1),
                            [d0, d1, init], [out])

    def recip(self, out, in_):
        return self._record("dve", lambda: self.nc.vector.reciprocal(out, in_), [in_], [out])

    def max8(self, out, in_):
        return self._record("dve", lambda: self.nc.vector.max(out, in_), [in_], [out])

    def dma(self, out, in_, q="sp", **kw):
        return self._record(q, lambda: self.e[q].dma_start(out=out, in_=in_, **kw), [in_], [out], dma=True)

    def barrier(self):
        lasts = [self.last_op[k] for k in COMPUTE if self.last_op[k] is not None]
        dlast = list(self.dma_last.values())
        for k in self.e:
            op = _Op(k, None)
            op.deps = [o for o in lasts if o.eng != k] + dlast
            self.ops[k].append(op)

    def flush(self):
        for k in self.ops:
            for op in self.ops[k]:
                for d in op.deps:
                    d.marked = True
        for k in self.ops:
            lst = self.ops[k]
            if not lst:
                continue
            if k in COMPUTE:
                real = [o for o in lst if o.fn is not None and not o.dma]
                if real:
                    real[-1].marked = True
                c = self.cnt[k]
                for op in lst:
                    if op.fn is not None and not op.dma and op.marked:
                        c += 1
                        op.val = c
                nxt = None
                for op in reversed(lst):
                    if op.fn is None or op.dma:
                        continue
                    if op.marked:
                        nxt = op.val
                    else:
                        op.val = nxt
        for k in self.ops:
            eng = self.e[k]
            seen = self.seen[k]
            for op in self.ops[k]:
                waits = {}
                for d in op.deps:
                    if d.dma:
                        key = ("d",) + d.dsem
                        s, v = self.dsems[d.dsem], d.dval
                    else:
                        if d.fn is None:
                            continue
                        key = ("c", d.eng)
                        s, v = self.sem[d.eng], d.val
                    if seen.get(key, 0) >= v:
                        continue
                    if key not in waits or waits[key][1] < v:
                        waits[key] = (s, v)
                if op.dma and op.qwait is not None:
                    key = ("d",) + op.dsem
                    v = op.qwait.dval
                    if seen.get(key, 0) < v and (key not in waits or waits[key][1] < v):
                        waits[key] = (self.dsems[op.dsem], v)
                for key, (s, v) in waits.items():
                    eng.wait_ge(s, v)
                    seen[key] = v
                    self.trace[k].append(("w", key, v))
                if op.fn is None:
                    continue
                ins = op.fn()
                self.n_inst += 1
                op.fn = True
                if op.dma:
                    ins.then_inc(self.dsems[op.dsem], 16)
                    self.trace[k].append(("i", ("d",) + op.dsem, 16))
                elif op.marked:
                    ins.then_inc(self.sem[k], 1)
                    self.cnt[k] = op.val
                    self.trace[k].append(("i", ("c", k), 1))
                else:
                    self.trace[k].append(("n", None, 0))
            self.ops[k] = []

    def simulate(self):
        sem = {}
        pos = {k: 0 for k in self.trace}
        progress = True
        while progress:
            progress = False
            for k, tr in self.trace.items():
                while pos[k] < len(tr):
                    typ, key, v = tr[pos[k]]
                    if typ == "w":
                        if sem.get(key, 0) < v:
                            break
                    elif typ == "i":
                        sem[key] = sem.get(key, 0) + v
                    pos[k] += 1
                    progress = True
        stuck = {k: (pos[k], len(tr), tr[pos[k]] if pos[k] < len(tr) else None) for k, tr in self.trace.items()}
        return all(pos[k] == len(tr) for k, tr in self.trace.items()), stuck, sem

    def finish(self):
        self.flush()
        for (k, slot), op in self.dma_last.items():
            self.e[k].wait_ge(self.dsems[(k, slot)], op.dval)
        self.stacks[0].close()


D = 1024
NTOK = 4608
NG = 9
SEQ = 2048
CTX = 256
NS = 2304
IN_W = 8240
EPS = 1e-6
C_MLQ, C_MLK, C_MLV, C_MLO, C_MLG = 0, 512, 1024, 1536, 2048
C_DFQ, C_DFK, C_DFV = 2064, 2576, 3088
C_GLQ, C_GLK, C_GLV, C_GLG, C_GLA = 3600, 3856, 4112, 4624, 5136
C_GATE = 5168
T_MLQ, T_MLK, T_DFQ, T_DFK, T_GLQ, T_GLK, T_GATE = 0, 512, 1024, 1536, 2048, 2304, 2560
T_ROWS = 2560 + 3072
K_MLK, K_MLV, K_MLO, K_DFV, K_GLK, K_GLV, K_GLG = 0, 512, 1024, 1536, 2048, 2304, 2816
K_COLS = 3328


def seq_ranges(s):
    return 256 * s, 512 + 2048 * s


def build(n_layers=2, stop_after=None, dbg=False, do_moe=True, skip=()):
    nc = bass.Bass("TRN2", target_bir_lowering=False)
    mk = MK(nc)
    IN = {}

    def din(name, shape, dtype=F32):
        IN[name] = mk.dram(name, shape, dtype, kind="ExternalInput")
        return IN[name]

    x2 = din("x2", [2, SEQ, D])
    ctx2 = din("ctx2", [2, CTX, D])
    cT = din("cT", [128, 8, 3])
    w_mod = din("w_mod", [2, D, 6 * D])
    b_modT = din("b_modT", [2, 128, 48])
    gmixT = din("gmixT", [2, 128, 8])
    gffnT = din("gffnT", [2, 128, 8])
    w_in = din("w_in", [2, D, IN_W])
    ml_gb = din("ml_gb", [2, 4, 4])
    ml_ng = din("ml_ng", [2, 128])
    df_lam = din("df_lam", [2, 4, 64])
    df_ng = din("df_ng", [2, 128])
    gl_wa = din("gl_wa", [2, 2, 16, 256])
    gl_baT = din("gl_baT", [2, 2, 128, 2])
    gl_ng = din("gl_ng", [2, 128])
    w_branch = din("w_branch", [2, 3, 512, D])
    w_out = din("w_out", [2, D, D])
    router_w = din("router_w", [2, D, 36])
    router_b = din("router_b", [2, 36])
    if do_moe:
        moe_wg = din("moe_wg", [2, 32, D, 512])
        moe_wu = din("moe_wu", [2, 32, D, 512])
        moe_wd = din("moe_wd", [2, 32, 512, D])
    fin_gT = din("fin_gT", [128, 8])
    consts = din("consts", [128, 128 + 128 + 128 + 2048 + 2048])

    okind = "ExternalOutput"
    y_out = mk.dram("y_out", [2, SEQ, D], F32, kind=okind)
    skind = "ExternalOutput" if dbg else "Internal"
    xT = mk.dram("xT", [D, NTOK], F32, kind=skind)
    PT = mk.dram("PT", [T_ROWS, NTOK], BF16, kind=skind)
    PK = mk.dram("PK", [NTOK, K_COLS], BF16, kind=skind)
    GT = mk.dram("GT", [48, NTOK], F32, kind=skind)
    BR = mk.dram("BR", [1536, NTOK], BF16, kind=skind)

    ident = mk.sbuf("ident", [128, 128], F32)
    identb = mk.sbuf("identb", [128, 128], BF16)
    onesb = mk.sbuf("onesb", [128, 128], BF16)
    onesf = mk.sbuf("onesf", [128, 128], F32)
    maskF = mk.sbuf("maskF", [128, 128], F32)
    maskB = mk.sbuf("maskB", [128, 128], F32)
    modv = mk.sbuf("modv", [128, 2, 6, 8, 3], F32)
    PS = [mk.psum("psb%d" % i, [128, 512], F32) for i in range(8)]

    mk.dma(ident[:], consts[:, 0:128])
    mk.dma(maskF[:], consts[:, 128:256])
    mk.dma(maskB[:], consts[:, 256:384])
    mk.copy("dve", identb[:], ident[:])
    mk.memset("pool", onesb[:], 1.0)
    mk.memset("pool", onesf[:], 1.0)

    rr = {"ev": 0}

    def evac_eng():
        rr["ev"] ^= 1
        return "act" if rr["ev"] else "dve"

    def phase_load_x():
        with mk.scope():
            xin = [mk.sbuf("xin%d" % i, [128, D], F32) for i in range(2)]
            xst = [mk.sbuf("xst%d" % i, [128, 8, 512], F32) for i in range(2)]
            for g in range(NG):
                st = xst[g % 2]
                for t4 in range(4):
                    tt = g * 4 + t4
                    if tt < 4:
                        src = ctx2[tt // 2, (tt % 2) * 128:(tt % 2) * 128 + 128, :]
                    else:
                        u = tt - 4
                        src = x2[u // 16, (u % 16) * 128:(u % 16) * 128 + 128, :]
                    xi = xin[tt % 2]
                    mk.dma(xi[:], src)
                    for half in range(2):
                        pb = PS[(tt * 2 + half) % 4]
                        for k4 in range(4):
                            kc = half * 4 + k4
                            mk.tr(pb[:, k4 * 128:(k4 + 1) * 128], xi[:, kc * 128:(kc + 1) * 128], ident[:])
                        mk.copy(evac_eng(), st[:, half * 4:(half + 1) * 4, t4 * 128:(t4 + 1) * 128],
                                pb[:].rearrange("p (k t) -> p k t", k=4))
                mk.dma(xT[:, g * 512:(g + 1) * 512].rearrange("(kc p) t -> p kc t", p=128), st[:], q="act")

    def phase_mod():
        with mk.scope():
            cs = mk.sbuf("cs", [128, 8, 3], F32)
            csg = mk.sbuf("csg", [128, 8, 3], F32)
            wm = [mk.sbuf("wm%d" % i, [128, 8, 512], F32) for i in range(2)]
            modT = mk.sbuf("modT", [128, 48, 3], F32)
            bm = mk.sbuf("bm", [128, 48], F32)
            gm = mk.sbuf("gm", [128, 8], F32)
            gf = mk.sbuf("gf", [128, 8], F32)
            mk.dma(cs[:], cT[:, :, :])
            mk.act(csg[:], cs[:], AF.Sigmoid)
            mk.tt("dve", cs[:], cs[:], csg[:], ALU.mult)
            for li in range(n_layers):
                mk.dma(bm[:], b_modT[li])
                mk.dma(gm[:], gmixT[li])
                mk.dma(gf[:], gffnT[li])
                for blk in range(12):
                    w = wm[blk % 2]
                    mk.dma(w[:], w_in_view(w_mod[li], blk * 512, 512), q=("sp" if blk % 2 else "act"))
                    for c4 in range(4):
                        fc = blk * 4 + c4
                        pb = PS[fc % 4]
                        for kc in range(8):
                            mk.mm(pb[:, 0:3], w[:, kc, c4 * 128:(c4 + 1) * 128], cs[:, kc, :],
                                  start=(kc == 0), stop=(kc == 7))
                        mk.ts("dve", modT[:, fc, :], pb[:, 0:3], bm[:, fc:fc + 1], None, op0=ALU.add)
                mv = modv[:, li]
                for j in range(3):
                    mk.ts("dve", mv[:, 0, :, j], modT[:, 8:16, j], 1.0, None, op0=ALU.add)
                    mk.tt("dve", mv[:, 0, :, j], mv[:, 0, :, j], gm[:], ALU.mult)
                    mk.copy("dve", mv[:, 1, :, j], modT[:, 0:8, j])
                    mk.copy("dve", mv[:, 2, :, j], modT[:, 16:24, j])
                    mk.ts("dve", mv[:, 3, :, j], modT[:, 32:40, j], 1.0, None, op0=ALU.add)
                    mk.tt("dve", mv[:, 3, :, j], mv[:, 3, :, j], gf[:], ALU.mult)
                    mk.copy("dve", mv[:, 4, :, j], modT[:, 24:32, j])
                    mk.copy("dve", mv[:, 5, :, j], modT[:, 40:48, j])

    def w_in_view(w2d, c0, n):
        return w2d[:, c0:c0 + n].rearrange("(kc p) n -> p kc n", p=128)

    def gset(g):
        return 0 if g == 0 else (1 if g <= 4 else 2)

    def norm_groups(li, which_gs, hT, scratch, h32_cb=None):
        xs_b, sq_b, rs_b = scratch
        for g in range(NG):
            j = gset(g)
            xs = xs_b[g % 2]
            sq = sq_b[g % 2]
            rs = rs_b[g % 2]
            mk.dma(xs[:], xT[:, g * 512:(g + 1) * 512].rearrange("(kc p) t -> p kc t", p=128),
                   q=("sp" if g % 2 else "act"))
            mk.tt("pool", sq[:], xs[:], xs[:], ALU.mult)
            pb = PS[4 + g % 2]
            for kc in range(8):
                mk.mm(pb[:], onesb[:], sq[:, kc, :], start=(kc == 0), stop=(kc == 7))
            mk.act(rs[:], pb[:], AF.Sqrt, bias=EPS, scale=1.0 / D)
            mk.recip(rs[:], rs[:])
            for kc in range(8):
                mk.stt("dve", xs[:, kc, :], xs[:, kc, :], modv[:, li, which_gs, kc, j:j + 1], rs[:],
                       ALU.mult, ALU.mult)
                mk.act(hT[:, kc, g * 512:(g + 1) * 512], xs[:, kc, :], AF.Identity,
                       bias=modv[:, li, which_gs + 1, kc, j:j + 1], scale=1.0)
                if h32_cb is not None:
                    mk.ts("pool", xs[:, kc, :], xs[:, kc, :], modv[:, li, which_gs + 1, kc, j:j + 1], None,
                          op0=ALU.add)
            if h32_cb is not None:
                h32_cb(g, xs)

    def phase_proj(li, hT):
        cosT = mk.sbuf("cosT", [128, SEQ], F32)
        sinT = mk.sbuf("sinT", [128, SEQ], F32)
        mk.dma(cosT[:], consts[:, 384:384 + SEQ])
        mk.dma(sinT[:], consts[:, 384 + SEQ:384 + 2 * SEQ])
        wb = [mk.sbuf("wblk%d" % i, [128, 8, 512], BF16) for i in range(2)]
        stT = [mk.sbuf("stT%d" % i, [128, NTOK], BF16) for i in range(2)]
        stK = [mk.sbuf("stK%d" % i, [128, 4, 512], BF16) for i in range(2)]
        stG = mk.sbuf("stG", [16, NTOK], F32)
        wrot = mk.sbuf("wrot", [128, 8, 512], BF16)
        stG2 = mk.sbuf("stG2", [32, NTOK], F32)
        t1 = [mk.sbuf("rp1_%d" % i, [128, 512], F32) for i in range(2)]
        t2 = [mk.sbuf("rp2_%d" % i, [128, 512], F32) for i in range(2)]
        cnt = {"blk": 0, "ps": 0, "st": 0}

        def load_blk(c0, n):
            w = wb[cnt["blk"] % 2]
            cnt["blk"] += 1
            mk.dma(w[:, :, 0:n], w_in_view(w_in[li], c0, n), q="pool")
            return w

        def nextps():
            cnt["ps"] += 1
            return PS[cnt["ps"] % 4]

        tblocks = [
            (C_MLQ, 512, T_MLQ, "copy", 1.0),
            (C_MLK, 512, T_MLK, "copy", 128.0 ** -0.5),
            (C_DFQ, 512, T_DFQ, "rope", 1.0),
            (C_DFK, 512, T_DFK, "rope", 1.0),
            (C_GLQ, 256, T_GLQ, "copy", 0.125),
            (C_GLK, 256, T_GLK, "copy", 1.0),
        ] + [(C_GATE + i * 512, 512, T_GATE + i * 512, "sigmoid", 1.0) for i in range(6)]
        for (c0, n, r0, mode, scale) in tblocks:
            w = load_blk(c0, n)
            if mode == "rope":
                wv = w[:].rearrange("p k (b h e) -> p (k b) h e", h=2, e=16)
                rv = wrot[:].rearrange("p k (b h e) -> p (k b) h e", h=2, e=16)
                mk.copy("pool", rv[:, :, 0, :], wv[:, :, 1, :])
                mk.copy("pool", rv[:, :, 1, :], wv[:, :, 0, :])
            for cc in range(n // 128):
                st = stT[cnt["st"] % 2]
                cnt["st"] += 1
                for g in range(NG):
                    pb = nextps()
                    for kc in range(8):
                        mk.mm(pb[:], w[:, kc, cc * 128:(cc + 1) * 128], hT[:, kc, g * 512:(g + 1) * 512],
                              start=(kc == 0), stop=(kc == 7))
                    dst = st[:, g * 512:(g + 1) * 512]
                    if mode == "sigmoid":
                        mk.act(dst, pb[:], AF.Sigmoid)
                    elif mode == "rope" and g >= 1:
                        pr = nextps()
                        for kc in range(8):
                            mk.mm(pr[:], wrot[:, kc, cc * 128:(cc + 1) * 128], hT[:, kc, g * 512:(g + 1) * 512],
                                  start=(kc == 0), stop=(kc == 7))
                        tok0 = ((g - 1) % 4) * 512
                        a = t1[g % 2]
                        b = t2[g % 2]
                        mk.tt("dve", a[:], pb[:], cosT[:, tok0:tok0 + 512], ALU.mult)
                        mk.tt("dve", b[:], pr[:], sinT[:, tok0:tok0 + 512], ALU.mult)
                        mk.tt("pool", dst, a[:], b[:], ALU.add)
                    else:
                        e = evac_eng()
                        if e == "act":
                            mk.act(dst, pb[:], AF.Copy, scale=scale)
                        else:
                            mk.ts("dve", dst, pb[:], scale, None, op0=ALU.mult)
                mk.dma(PT[r0 + cc * 128:r0 + (cc + 1) * 128, :], st[:], q="sp")
        for (c0, n, r0) in [(C_MLG, 16, 0), (C_GLA, 32, 16)]:
            w = load_blk(c0, n)
            for g in range(NG):
                pb = nextps()
                for kc in range(8):
                    mk.mm(pb[0:n, :], w[:, kc, 0:n], hT[:, kc, g * 512:(g + 1) * 512],
                          start=(kc == 0), stop=(kc == 7))
                if r0 == 0:
                    mk.copy("dve", stG[0:16, g * 512:(g + 1) * 512], pb[0:16, :])
                else:
                    mk.copy("dve", stG2[:, g * 512:(g + 1) * 512], pb[0:32, :])
        mk.dma(GT[0:16, :], stG[0:16, :], q="sp")
        mk.dma(GT[16:48, :], stG2[:, :], q="sp")
        kblocks = [
            (C_MLK, 512, K_MLK, "copy", 128.0 ** -0.5),
            (C_MLV, 512, K_MLV, "copy", 1.0),
            (C_MLO, 512, K_MLO, "sigmoid", 1.0),
            (C_DFV, 512, K_DFV, "copy", 1.0),
            (C_GLK, 256, K_GLK, "copy", 1.0),
            (C_GLV, 512, K_GLV, "copy", 1.0),
            (C_GLG, 512, K_GLG, "silu", 1.0),
        ]
        sg = [mk.sbuf("sgk%d" % i, [128, 512], F32) for i in range(2)]
        for (c0, n, k0, mode, scale) in kblocks:
            w = load_blk(c0, n)
            for t4 in range(NTOK // 512):
                st = stK[cnt["st"] % 2]
                cnt["st"] += 1
                for q4 in range(4):
                    tt_ = t4 * 4 + q4
                    pb = nextps()
                    for kc in range(8):
                        mk.mm(pb[:, 0:n], hT[:, kc, tt_ * 128:(tt_ + 1) * 128], w[:, kc, 0:n],
                              start=(kc == 0), stop=(kc == 7))
                    dst = st[:, q4, 0:n]
                    if mode == "sigmoid":
                        mk.act(dst, pb[:, 0:n], AF.Sigmoid)
                    elif mode == "silu":
                        s_ = sg[tt_ % 2]
                        mk.act(s_[:, 0:n], pb[:, 0:n], AF.Sigmoid)
                        mk.tt("dve", dst, pb[:, 0:n], s_[:, 0:n], ALU.mult)
                    else:
                        e = evac_eng()
                        if e == "act":
                            mk.act(dst, pb[:, 0:n], AF.Copy, scale=scale)
                        else:
                            mk.ts("dve", dst, pb[:, 0:n], scale, None, op0=ALU.mult)
                mk.dma(PK[t4 * 512:(t4 + 1) * 512, k0:k0 + n].rearrange("(a p) c -> p a c", p=128),
                       st[:, :, 0:n], q="sp")

    def load_seq_T(dst, src_rows, s, q="sp"):
        c0, l0 = seq_ranges(s)
        mk.dma(dst[:, 0:CTX], src_rows[:, c0:c0 + CTX], q=q)
        mk.dma(dst[:, CTX:NS], src_rows[:, l0:l0 + SEQ], q=q)

    def store_seq_T(dst_rows, src, s, q="sp"):
        c0, l0 = seq_ranges(s)
        mk.dma(dst_rows[:, c0:c0 + CTX], src[:, 0:CTX], q=q)
        mk.dma(dst_rows[:, l0:l0 + SEQ], src[:, CTX:NS], q=q)

    def load_seq_K(dst3, k0, ncol, s, q="sp"):
        c0, l0 = seq_ranges(s)
        mk.dma(dst3[:, 0:2], PK[c0:c0 + CTX, k0:k0 + ncol].rearrange("(a p) c -> p a c", p=128), q=q)
        mk.dma(dst3[:, 2:18], PK[l0:l0 + SEQ, k0:k0 + ncol].rearrange("(a p) c -> p a c", p=128), q=q)

    def bcast_row(dst, src_row):
        mk.dma(dst, src_row.to_broadcast([128, src_row.shape[-1]]))

    def tok_to_BR(src, r0, s, stg):
        for h in range(4):
            st = stg[h % 2]
            for t4 in range(5):
                n = min(4, 18 - t4 * 4)
                pb = PS[t4 % 4]
                for j in range(n):
                    t = t4 * 4 + j
                    mk.mm(pb[:, j * 128:(j + 1) * 128], src[:, t, h * 128:(h + 1) * 128], identb[:])
                mk.copy(evac_eng(), st[:, t4 * 512:t4 * 512 + n * 128], pb[:, 0:n * 128])
            store_seq_T(BR[r0 + h * 128:r0 + (h + 1) * 128, :], st, s, q="act")

    def phase_attn(li):
        lam_init = 0.8 - 0.6 * math.exp(-0.3 * li)
        with mk.scope():
            lam_t = mk.sbuf("lam_t", [1, 4, 64], F32)
            lam_p = mk.sbuf("lam_p", [1, 2, 64], F32)
            lam_s = mk.sbuf("lam_s", [1, 4], F32)
            nlam = mk.sbuf("nlam", [128, 1], F32)
            gv = mk.sbuf("dfgv", [128, 128], F32)
            mk.dma(lam_t[:], df_lam[li:li + 1, :, :])
            mk.tt("dve", lam_p[:, 0, :], lam_t[:, 0, :], lam_t[:, 1, :], ALU.mult)
            mk.tt("dve", lam_p[:, 1, :], lam_t[:, 2, :], lam_t[:, 3, :], ALU.mult)
            mk.reduce("dve", lam_s[:, 0:2], lam_p[:], ALU.add)
            mk.act(lam_s[:, 0:2], lam_s[:, 0:2], AF.Exp)
            mk.tt("dve", lam_s[:, 2:3], lam_s[:, 0:1], lam_s[:, 1:2], ALU.subtract)
            mk.ts("dve", lam_s[:, 2:3], lam_s[:, 2:3], lam_init, -1.0, op0=ALU.add, op1=ALU.mult)
            mk.mm(PS[6][:, 0:1], onesf[0:1, :], lam_s[0:1, 2:3])
            mk.copy("dve", nlam[:], PS[6][:, 0:1])
            bcast_row(gv[:], df_ng[li:li + 1, :])
            mk.ts("dve", gv[:], gv[:], 1.0 - lam_init, None, op0=ALU.mult)
            qT = mk.sbuf("aqT", [128, 4, NS], BF16)
            kT = mk.sbuf("akT", [128, 4, NS], BF16)
            sq = mk.sbuf("asq", [128, 4, NS], BF16)
            vA = mk.sbuf("avA", [128, 18, 4, 129], BF16)
            DF = mk.sbuf("aDF", [128, 18, 512], BF16)
            DFo = mk.sbuf("aDFo", [128, 18, 512], F32)
            sqh = mk.sbuf("asqh", [128, 18, 512], F32)
            ssq = mk.sbuf("assq", [128, 72], F32)
            pt = [mk.sbuf("apt%d" % i, [128, 512], BF16) for i in range(4)]
            mxs = mk.sbuf("amx", [1, 64], F32)
            negM = mk.sbuf("anegM", [128, 1], F32)
            o1 = [mk.sbuf("ao1_%d" % i, [128, 128], F32) for i in range(2)]
            o2 = [mk.sbuf("ao2_%d" % i, [128, 128], F32) for i in range(2)]
            rc = [mk.sbuf("arc%d" % i, [128, 4], F32) for i in range(2)]
            stg = [mk.sbuf("astg%d" % i, [128, NS], BF16) for i in range(2)]
            mk.memset("pool", vA[:], 1.0)
            for s in range(2):
                for h in range(4):
                    load_seq_T(qT[:, h, :], PT[T_DFQ + h * 128:T_DFQ + (h + 1) * 128, :], s, q="sp")
                    load_seq_T(kT[:, h, :], PT[T_DFK + h * 128:T_DFK + (h + 1) * 128, :], s, q="act")
                c0, l0 = seq_ranges(s)
                for h in range(4):
                    mk.dma(vA[:, 0:2, h, 0:128],
                           PK[c0:c0 + CTX, K_DFV + h * 128:K_DFV + (h + 1) * 128].rearrange("(a p) c -> p a c", p=128))
                    mk.dma(vA[:, 2:18, h, 0:128],
                           PK[l0:l0 + SEQ, K_DFV + h * 128:K_DFV + (h + 1) * 128].rearrange("(a p) c -> p a c", p=128))
                mk.memset("dve", mxs[:], 0.0)
                for which, src in ((0, qT), (1, kT)):
                    mk.tt("pool", sq[:], src[:], src[:], ALU.mult)
                    idx = 0
                    for h in range(4):
                        for cst in range(0, NS, 512):
                            n = min(512, NS - cst)
                            pb = PS[3 + idx % 4]
                            mk.mm(pb[0:1, 0:n], onesb[:, 0:1], sq[:, h, cst:cst + n])
                            mk.reduce("dve", mxs[:, which * 32 + idx:which * 32 + idx + 1], pb[0:1, 0:n], ALU.max)
                            idx += 1
                mk.reduce("dve", mxs[:, 60:61], mxs[:, 0:32], ALU.max)
                mk.reduce("dve", mxs[:, 61:62], mxs[:, 32:60], ALU.max)
                mk.tt("dve", mxs[:, 62:63], mxs[:, 60:61], mxs[:, 61:62], ALU.mult)
                mk.act(mxs[:, 63:64], mxs[:, 62:63], AF.Sqrt, scale=1.0 / 64.0)
                mk.ts("dve", mxs[:, 63:64], mxs[:, 63:64], -1.0, None, op0=ALU.mult)
                mk.mm(PS[6][:, 0:1], onesf[0:1, :], mxs[0:1, 63:64])
                mk.copy("dve", negM[:], PS[6][:, 0:1])
                import os as _os
                if _os.environ.get('ATTN_STOP') == '1':
                    return
                qgroups = [(0, CTX, 0, 2)] + [(CTX + i * 512, 512, 0, 18) for i in range(4)]
                ci = 0
                it = 0
                for (q0, nq, tk0, tk1) in qgroups:
                    nsub = nq // 128
                    for h in range(4):
                        ob = 3 * (it % 2)
                        it += 1
                        O = [[PS[ob + (a * 4 + sb) // 3][:, ((a * 4 + sb) % 3) * 160:((a * 4 + sb) % 3) * 160 + 129]
                              for sb in range(4)] for a in range(2)]
                        steps = [(tk, a) for tk in range(tk0, tk1) for a in range(2)]
                        started = set()
                        bufs = {}

                        def emit_S(i):
                            tk, a = steps[i]
                            pb = PS[6 + (ci + i) % 2]
                            p_ = pt[(ci + i) % 4]
                            bufs[i] = p_
                            mk.mm(pb[:, 0:nq], kT[a * 64:(a + 1) * 64, h, tk * 128:(tk + 1) * 128],
                                  qT[a * 64:(a + 1) * 64, h, q0:q0 + nq])
                            mk.act(p_[:, 0:nq], pb[:, 0:nq], AF.Exp, bias=negM[:, 0:1], scale=0.125)

                        def emit_PV(i):
                            tk, a = steps[i]
                            p_ = bufs.pop(i)
                            for sb in range(nsub):
                                bank = (a * 4 + sb) // 3
                                st_ = (tk == tk0) and (bank not in started)
                                started.add(bank)
                                mk.mm(O[a][sb], p_[:, sb * 128:(sb + 1) * 128], vA[:, tk, h, :],
                                      start=st_, stop=(tk == tk1 - 1))

                        emit_S(0)
                        for i in range(len(steps)):
                            if i + 1 < len(steps):
                                emit_S(i + 1)
                            emit_PV(i)
                        ci += len(steps)
                        for sb in range(nsub):
                            tile = (q0 + sb * 128) // 128
                            r_ = rc[sb % 2]
                            a1 = o1[sb % 2]
                            a2 = o2[sb % 2]
                            mk.recip(r_[:, 0:1], O[0][sb][:, 128:129])
                            mk.recip(r_[:, 1:2], O[1][sb][:, 128:129])
                            mk.tt("dve", r_[:, 1:2], r_[:, 1:2], nlam[:, 0:1], ALU.mult)
                            dst_ = DFo[:, tile, h * 128:(h + 1) * 128]
                            mk.ts("dve", a1[:], O[0][sb][:, 0:128], r_[:, 0:1], None, op0=ALU.mult)
                            mk.stt("dve", dst_, O[1][sb][:, 0:128], r_[:, 1:2], a1[:], ALU.mult, ALU.add)
                finish_branch(DFo, None, gv, sqh, ssq, DF)
                tok_to_BR(DF, 512, s, stg)
                if _os.environ.get('ATTN_STOP') == '3':
                    return

    def phase_merge(li):
        with mk.scope():
            wbr = mk.sbuf("wbr", [128, 12, D], BF16)
            wo = mk.sbuf("wo", [128, 8, D], BF16)
            for b in range(3):
                mk.dma(wbr[:, b * 4:(b + 1) * 4, :], w_branch[li, b].rearrange("(kc p) n -> p kc n", p=128), q="pool")
            mk.dma(wo[:], w_out[li].rearrange("(kc p) n -> p kc n", p=128), q="pool")
            brT = [mk.sbuf("mbr%d" % i, [128, 12, 512], BF16) for i in range(2)]
            gt = [mk.sbuf("mgt%d" % i, [128, 24, 512], BF16) for i in range(2)]
            xs_b = [mk.sbuf("mxs%d" % i, [128, 8, 512], F32) for i in range(2)]
            yT = [mk.sbuf("myT%d" % i, [128, 8, 512], BF16) for i in range(2)]
            ya = [mk.sbuf("mya%d" % i, [128, 512], F32) for i in range(2)]
            tmp = [mk.sbuf("mtm%d" % i, [128, 512], F32) for i in range(2)]
            ci = 0
            for g in range(NG):
                if li == n_layers - 1 and g == 0:
                    continue
                j = gset(g)
                b_ = brT[g % 2]
                g_ = gt[g % 2]
                xs = xs_b[g % 2]
                y_ = yT[g % 2]
                tsl = slice(g * 512, (g + 1) * 512)
                mk.dma(b_[:], BR[:, tsl].rearrange("(c p) t -> p c t", p=128), q="sp")
                mk.dma(g_[:], PT[T_GATE:T_GATE + 3072, tsl].rearrange("(c p) t -> p c t", p=128), q="act")
                mk.dma(xs[:], xT[:, tsl].rearrange("(kc p) t -> p kc t", p=128), q="sp")
                for oc in range(8):
                    acc = ya[oc % 2]
                    for br in range(3):
                        pb = PS[ci % 4]
                        ci += 1
                        for kc in range(4):
                            mk.mm(pb[:], wbr[:, br * 4 + kc, oc * 128:(oc + 1) * 128], b_[:, br * 4 + kc, :],
                                  start=(kc == 0), stop=(kc == 3))
                        if br == 0:
                            mk.tt("dve", acc[:], pb[:], g_[:, br * 8 + oc, :], ALU.mult)
                        else:
                            t_ = tmp[br % 2]
                            mk.tt("dve", t_[:], pb[:], g_[:, br * 8 + oc, :], ALU.mult)
                            mk.tt("pool", (acc[:] if br == 1 else y_[:, oc, :]), acc[:], t_[:], ALU.add)
                for oc in range(8):
                    pb = PS[4 + oc % 3]
                    for kc in range(8):
                        mk.mm(pb[:], wo[:, kc, oc * 128:(oc + 1) * 128], y_[:, kc, :], start=(kc == 0), stop=(kc == 7))
                    mk.stt("dve", xs[:, oc, :], pb[:], modv[:, li, 2, oc, j:j + 1], xs[:, oc, :], ALU.mult, ALU.add)
                mk.dma(xT[:, tsl].rearrange("(kc p) t -> p kc t", p=128), xs[:], q="act")

    def phase_moe(li):
        HALF = NTOK // 2
        with mk.scope():
            h2T = mk.sbuf("h2T", [128, 8, NTOK], BF16)
            comb = mk.sbuf("comb", [128, 36, 32], F32)
            with mk.scope():
                scratch = ([mk.sbuf("nxs%d" % i, [128, 8, 512], F32) for i in range(2)],
                           [mk.sbuf("nsq%d" % i, [128, 8, 512], BF16) for i in range(2)],
                           [mk.sbuf("nrs%d" % i, [128, 512], F32) for i in range(2)])
                wr = mk.sbuf("wr", [128, 8, 36], F32)
                rb = mk.sbuf("rb", [128, 36], F32)
                L = mk.sbuf("rL", [128, 36], F32)
                sm = mk.sbuf("rsm", [128, 16], F32)
                mg = mk.sbuf("rmg", [128, 4], F32)
                eg = mk.sbuf("reg", [128, 4], F32)
                ls = mk.sbuf("rls", [128, 8], F32)
                t8 = mk.sbuf("rt8", [128, 8], F32)
                s1 = mk.sbuf("rs1", [128, 8], F32)
                s2 = mk.sbuf("rs2", [128, 8], F32)
                mk.dma(wr[:], router_w[li].rearrange("(kc p) n -> p kc n", p=128))
                bcast_row(rb[:], router_b[li:li + 1, :])

                def route(g, xs):
                    for t4 in range(4):
                        tile = g * 4 + t4
                        pb = PS[t4 % 4]
                        for kc in range(8):
                            mk.mm(pb[:, 0:36], xs[:, kc, t4 * 128:(t4 + 1) * 128], wr[:, kc, :],
                                  start=(kc == 0), stop=(kc == 7))
                        mk.tt("dve", L[:], pb[:, 0:36], rb[:], ALU.add)
                        mk.reduce("dve", sm[:, 0:1], L[:, 0:4], ALU.max)
                        mk.ts("dve", mg[:], L[:, 0:4], sm[:, 0:1], None, op0=ALU.is_ge)
                        mk.ts("dve", sm[:, 1:2], sm[:, 0:1], -1.0, None, op0=ALU.mult)
                        mk.act(eg[:], L[:, 0:4], AF.Exp, bias=sm[:, 1:2], scale=1.0, accum_out=sm[:, 2:3])
                        mk.recip(sm[:, 3:4], sm[:, 2:3])
                        mk.ts("dve", ls[:], L[:, 4:12], mg[:, 0:1], None, op0=ALU.mult)
                        for gi in range(1, 4):
                            mk.stt("dve", ls[:], L[:, 4 + 8 * gi:12 + 8 * gi], mg[:, gi:gi + 1], ls[:],
                                   ALU.mult, ALU.add)
                        mk.max8(t8[:], ls[:])
                        mk.tt("dve", sm[:, 4:5], t8[:, 1:2], t8[:, 0:1], ALU.subtract)
                        mk.act(sm[:, 5:6], sm[:, 4:5], AF.Exp)
                        mk.ts("dve", sm[:, 6:7], sm[:, 5:6], 1.0, None, op0=ALU.add)
                        mk.recip(sm[:, 6:7], sm[:, 6:7])
                        mk.tt("dve", sm[:, 7:8], sm[:, 6:7], sm[:, 3:4], ALU.mult)
                        mk.tt("dve", sm[:, 8:9], sm[:, 7:8], sm[:, 5:6], ALU.mult)
                        mk.tt("dve", sm[:, 9:10], sm[:, 7:8], sm[:, 8:9], ALU.subtract)
                        mk.ts("dve", s1[:], ls[:], t8[:, 0:1], sm[:, 9:10], op0=ALU.is_ge, op1=ALU.mult)
                        mk.ts("dve", s2[:], ls[:], t8[:, 1:2], sm[:, 8:9], op0=ALU.is_ge, op1=ALU.mult)
                        mk.tt("dve", s1[:], s1[:], s2[:], ALU.add)
                        for gi in range(4):
                            mk.ts("dve", comb[:, tile, gi * 8:(gi + 1) * 8], s1[:], mg[:, gi:gi + 1], None,
                                  op0=ALU.mult)

                norm_groups(li, 3, h2T, scratch, h32_cb=route)
            if dbg:
                comb_dbg = mk.dram("comb_dbg%d" % li, [128, 36 * 32], F32, kind="ExternalOutput")
                mk.dma(comb_dbg[:, :], comb[:].rearrange("p a b -> p (a b)"))
            wg_b = [mk.sbuf("ewg%d" % i, [128, 8, 512], BF16) for i in range(2)]
            wu_b = [mk.sbuf("ewu%d" % i, [128, 8, 512], BF16) for i in range(2)]
            wd_b = [mk.sbuf("ewd%d" % i, [128, 4, D], BF16) for i in range(2)]
            hid_b = [mk.sbuf("ehid%d" % i, [128, 4, 512], BF16) for i in range(2)]
            sl_b = [mk.sbuf("esl%d" % i, [128, 512], F32) for i in range(2)]
            NPART = 3
            PT_TILES = 36 // NPART
            yacc = mk.sbuf("yacc", [128, PT_TILES, D], F32)
            xs_b = [mk.sbuf("exs%d" % i, [128, 8, 128], F32) for i in range(2)]
            ci = 0
            gi = 0
            pend = [None]
            dk = [0]

            def down(hid, wd, t0, nt, e, hf):
                for sb in range(nt // 128):
                    tile = (t0 + sb * 128) // 128
                    lt = tile - hf * PT_TILES
                    for hh in range(2):
                        py = PS[5 + dk[0] % 3]
                        dk[0] += 1
                        for fc in range(4):
                            mk.mm(py[:], hid[:, fc, sb * 128:(sb + 1) * 128], wd[:, fc, hh * 512:(hh + 1) * 512],
                                  start=(fc == 0), stop=(fc == 3))
                        mk.stt("dve", yacc[:, lt, hh * 512:(hh + 1) * 512], py[:], comb[:, tile, e:e + 1],
                               yacc[:, lt, hh * 512:(hh + 1) * 512], ALU.mult, ALU.add)

            for hf in range(NPART):
                mk.memset("pool", yacc[:], 0.0)
                grp = [(hf * PT_TILES * 128 + i * 512, 512) for i in range(PT_TILES // 4)]
                if li == n_layers - 1:
                    grp = [g_ for g_ in grp if g_[0] >= 512]
                for e in range(32):
                    wg, wu, wd = wg_b[e % 2], wu_b[e % 2], wd_b[e % 2]
                    mk.dma(wg[:], moe_wg[li, e].rearrange("(kc p) n -> p kc n", p=128), q="pool")
                    mk.dma(wu[:], moe_wu[li, e].rearrange("(kc p) n -> p kc n", p=128), q="pool")
                    mk.dma(wd[:], moe_wd[li, e].rearrange("(kc p) n -> p kc n", p=128), q="pool")
                    for (t0, nt) in grp:
                        hid = hid_b[gi % 2]
                        gi += 1
                        for fc in range(4):
                            pg = PS[ci % 3]
                            pu = PS[3 + ci % 2]
                            sl = sl_b[ci % 2]
                            ci += 1
                            for kc in range(8):
                                mk.mm(pg[:, 0:nt], wg[:, kc, fc * 128:(fc + 1) * 128], h2T[:, kc, t0:t0 + nt],
                                      start=(kc == 0), stop=(kc == 7))
                            for kc in range(8):
                                mk.mm(pu[:, 0:nt], wu[:, kc, fc * 128:(fc + 1) * 128], h2T[:, kc, t0:t0 + nt],
                                      start=(kc == 0), stop=(kc == 7))
                            mk.act(sl[:, 0:nt], pg[:, 0:nt], AF.Silu)
                            mk.tt("dve", hid[:, fc, 0:nt], pu[:, 0:nt], sl[:, 0:nt], ALU.mult)
                        if pend[0] is not None:
                            down(*pend[0])
                        pend[0] = (hid, wd, t0, nt, e, hf)
                if pend[0] is not None:
                    down(*pend[0])
                    pend[0] = None
                for lt in range(PT_TILES):
                    tile = hf * PT_TILES + lt
                    if li == n_layers - 1 and tile < 4:
                        continue
                    j = gset(tile // 4)
                    xs = xs_b[lt % 2]
                    tsl = slice(tile * 128, (tile + 1) * 128)
                    mk.dma(xs[:], xT[:, tsl].rearrange("(kc p) t -> p kc t", p=128), q="sp")
                    for half in range(2):
                        pb = PS[(lt * 2 + half) % 4]
                        for k4 in range(4):
                            kc = half * 4 + k4
                            mk.tr(pb[:, k4 * 128:(k4 + 1) * 128], yacc[:, lt, kc * 128:(kc + 1) * 128], ident[:])
                        for k4 in range(4):
                            kc = half * 4 + k4
                            mk.stt("dve", xs[:, kc, :], pb[:, k4 * 128:(k4 + 1) * 128], modv[:, li, 5, kc, j:j + 1],
                                   xs[:, kc, :], ALU.mult, ALU.add)
                    mk.dma(xT[:, tsl].rearrange("(kc p) t -> p kc t", p=128), xs[:], q="act")

    def phase_final():
        with mk.scope():
            fg = mk.sbuf("fg", [128, 8], F32)
            mk.dma(fg[:], fin_gT[:, :])
            xs_b = [mk.sbuf("fxs%d" % i, [128, 8, 512], F32) for i in range(2)]
            sq_b = [mk.sbuf("fsq%d" % i, [128, 8, 512], BF16) for i in range(2)]
            rs_b = [mk.sbuf("frs%d" % i, [128, 512], F32) for i in range(2)]
            ot = [mk.sbuf("fot%d" % i, [128, D], F32) for i in range(2)]
            for g in range(1, NG):
                xs, sq, rs = xs_b[g % 2], sq_b[g % 2], rs_b[g % 2]
                mk.dma(xs[:], xT[:, g * 512:(g + 1) * 512].rearrange("(kc p) t -> p kc t", p=128),
                       q=("sp" if g % 2 else "act"))
                mk.tt("pool", sq[:], xs[:], xs[:], ALU.mult)
                pb = PS[4 + g % 2]
                for kc in range(8):
                    mk.mm(pb[:], onesb[:], sq[:, kc, :], start=(kc == 0), stop=(kc == 7))
                mk.act(rs[:], pb[:], AF.Sqrt, bias=EPS, scale=1.0 / D)
                mk.recip(rs[:], rs[:])
                for kc in range(8):
                    mk.stt("dve", xs[:, kc, :], xs[:, kc, :], fg[:, kc:kc + 1], rs[:], ALU.mult, ALU.mult)
                for t4 in range(4):
                    u = (g - 1) * 4 + t4
                    o_ = ot[u % 2]
                    for half in range(2):
                        pq = PS[(u * 2 + half) % 4]
                        for k4 in range(4):
                            kc = half * 4 + k4
                            mk.tr(pq[:, k4 * 128:(k4 + 1) * 128], xs[:, kc, t4 * 128:(t4 + 1) * 128], ident[:])
                        mk.copy(evac_eng(), o_[:, half * 512:(half + 1) * 512], pq[:])
                    mk.dma(y_out[u // 16, (u % 16) * 128:(u % 16) * 128 + 128, :], o_[:], q="sp")

    def nat_chunk(d, cs):
        if d == 0:
            return cs
        return (3 - cs) if cs < 4 else (35 - (cs - 4))

    def rev_copy(eng, dst, src):
        mk.copy(eng, dst[:, 0:CTX], src[:, 0:CTX][:, ::-1])
        mk.copy(eng, dst[:, CTX:NS], src[:, CTX:NS][:, ::-1])

    def phase_mlstm(li):
        with mk.scope():
            HS = mk.sbuf("mHS", [128, 18, 512], F32)
            gb = mk.sbuf("mgb", [4, 4], F32)
            ngb = mk.sbuf("mngb", [4, 4], F32)
            gvn = mk.sbuf("mgvn", [128, 128], F32)
            ones4 = mk.sbuf("mones4", [4, NS], F32)
            mk.dma(gb[:], ml_gb[li])
            mk.ts("dve", ngb[:], gb[:], -1.0, None, op0=ALU.mult)
            bcast_row(gvn[:], ml_ng[li:li + 1, :])
            mk.memset("pool", ones4[:], 1.0)
            for s in range(2):
                with mk.scope():
                    qT = mk.sbuf("mqT", [128, 4, NS], BF16)
                    kT = mk.sbuf("mkT", [128, 4, NS], BF16)
                    kK = mk.sbuf("mkK", [128, 18, 512], BF16)
                    vA = mk.sbuf("mvA", [128, 18, 4, 129], BF16)
                    uV = mk.sbuf("muV", [128, 18, 4, 129], BF16)
                    R = [mk.sbuf("mR%d" % i, [4, NS], F32) for i in range(5)]
                    COL = mk.sbuf("mCOL", [128, 3, 18, 4], F32)
                    DECr = mk.sbuf("mDECr", [4, 36, 4], F32)
                    DECb = mk.sbuf("mDECb", [128, 36, 4], F32)
                    CT = mk.sbuf("mCT", [128, 4, 129], F32)
                    tmpC = mk.sbuf("mtmpC", [128, 4, 129], F32)
                    CTb = [mk.sbuf("mCTb%d" % i, [128, 4, 129], BF16) for i in range(2)]
                    smb = [mk.sbuf("msm%d" % i, [128, 4, 128], BF16) for i in range(2)]
                    t4b = [mk.sbuf("mt4%d" % i, [128, 4], F32) for i in range(2)]
                    tmo = [mk.sbuf("mtmo%d" % i, [128, 4, 128], F32) for i in range(2)]
                    c0, l0 = seq_ranges(s)
                    for h in range(4):
                        load_seq_T(qT[:, h, :], PT[T_MLQ + h * 128:T_MLQ + (h + 1) * 128, :], s, q="sp")
                        load_seq_T(kT[:, h, :], PT[T_MLK + h * 128:T_MLK + (h + 1) * 128, :], s, q="act")
                    load_seq_K(kK, K_MLK, 512, s)
                    mk.memset("pool", vA[:], 1.0)
                    for h in range(4):
                        mk.dma(vA[:, 0:2, h, 0:128],
                               PK[c0:c0 + CTX, K_MLV + h * 128:K_MLV + (h + 1) * 128].rearrange("(a p) c -> p a c", p=128))
                        mk.dma(vA[:, 2:18, h, 0:128],
                               PK[l0:l0 + SEQ, K_MLV + h * 128:K_MLV + (h + 1) * 128].rearrange("(a p) c -> p a c", p=128))
                    for d in range(2):
                        irow = GT[d * 8:d * 8 + 4, :]
                        frow = GT[d * 8 + 4:d * 8 + 8, :]
                        if d == 0:
                            load_seq_T(R[1], irow, s)
                            load_seq_T(R[0], frow, s)
                        else:
                            load_seq_T(R[3], irow, s)
                            load_seq_T(R[4], frow, s)
                            rev_copy("dve", R[1], R[3])
                            rev_copy("pool", R[0], R[4])
                        R0, R1, R2, R3, R4 = R
                        v3 = lambda t: t[:].rearrange("p (c j) -> p c j", j=64)
                        mk.act(R0[:], R0[:], AF.Exp, bias=ngb[:, 2 * d + 1:2 * d + 2], scale=-1.0)
                        mk.act(R0[:], R0[:], AF.Ln, bias=1.0, scale=1.0)
                        mk.ts("dve", R0[:], R0[:], -1.0, None, op0=ALU.mult)
                        mk.scan("dve", R2[:], ones4[:], R0[:], 0.0, ALU.mult, ALU.add)
                        mk.stt("dve", R1[:], R1[:], gb[:, 2 * d:2 * d + 1], R2[:], ALU.add, ALU.subtract)
                        mk.scan("dve", R0[:], ones4[:], R1[:], 0.0, ALU.mult, ALU.max)
                        mprev = v3(R0)[:, 0:35, 63:64].to_broadcast([4, 35, 64])
                        mk.copy("dve", R3[:, 0:64], R1[:, 0:64])
                        mk.tt("dve", v3(R3)[:, 1:36, :], v3(R1)[:, 1:36, :], mprev, ALU.subtract)
                        mk.act(R3[:], R3[:], AF.Exp)
                        mk.ts("dve", R4[:, 0:64], R0[:, 0:64], -1.0, None, op0=ALU.mult)
                        mk.tt("dve", v3(R4)[:, 1:36, :], mprev, v3(R0)[:, 1:36, :], ALU.subtract)
                        mk.act(R4[:], R4[:], AF.Exp)
                        mk.tt("dve", R2[:], R2[:], R0[:], ALU.add)
                        mk.act(R2[:], R2[:], AF.Exp, scale=-1.0)
                        mk.tt("dve", DECr[:], v3(R4)[:, :, 63:64].to_broadcast([4, 36, 4]),
                              ident[0:4, 0:4].unsqueeze(1).to_broadcast([4, 36, 4]), ALU.mult)
                        mk.mm(PS[6][:, 0:144], onesf[0:4, :], DECr[:].rearrange("p c h -> p (c h)"))
                        mk.copy("dve", DECb[:].rearrange("p c h -> p (c h)"), PS[6][:, 0:144])
                        if d == 0:
                            NAT = [R3, R4, R2]
                        else:
                            rev_copy("dve", R0, R3)
                            rev_copy("pool", R1, R4)
                            rev_copy("dve", R3, R2)
                            NAT = [R0, R1, R3]
                        for qi, arr in enumerate(NAT):
                            for t in range(18):
                                o_ = (qi * 18 + t) * 4
                                mk.tr(PS[5][:, o_:o_ + 4], arr[:, t * 128:(t + 1) * 128], ident[0:4, 0:4])
                        mk.copy("dve", COL[:].rearrange("p a t h -> p (a t h)"), PS[5][:, 0:216])
                        mk.tt("pool", uV[:], vA[:], COL[:, 0].unsqueeze(3).to_broadcast([128, 18, 4, 129]), ALU.mult)
                        mk.memset("dve", CT[:], 0.0)
                        mk.memset("pool", CTb[0][:], 0.0)
                        mask = maskF if d == 0 else maskB
                        cur_tile = -1
                        for cs in range(36):
                            c = nat_chunk(d, cs)
                            tile, par = c // 2, c % 2
                            rows = slice(par * 64, par * 64 + 64)
                            ctb_cur, ctb_nxt = CTb[cs % 2], CTb[(cs + 1) % 2]
                            if tile != cur_tile:
                                cur_tile = tile
                                sm = smb[tile % 2]
                                for h in range(4):
                                    mk.mm(PS[4][:, h * 128:(h + 1) * 128], kT[:, h, tile * 128:(tile + 1) * 128],
                                          qT[:, h, tile * 128:(tile + 1) * 128])
                                mk.tt("dve", sm[:], PS[4][:].rearrange("p (h j) -> p h j", h=4),
                                      mask[:].unsqueeze(1).to_broadcast([128, 4, 128]), ALU.mult)
                            for h in range(4):
                                o_ = PS[h // 2][rows, (h % 2) * 256:(h % 2) * 256 + 129]
                                mk.mm(o_, sm[:, h, par * 64:par * 64 + 64], uV[:, tile, h, :], start=True, stop=False)
                                mk.mm(o_, qT[:, h, c * 64:(c + 1) * 64], ctb_cur[:, h, :], start=False, stop=True)
                            for h in range(4):
                                mk.mm(PS[2 + h // 2][:, (h % 2) * 256:(h % 2) * 256 + 129],
                                      kK[rows, tile, h * 128:(h + 1) * 128], uV[rows, tile, h, :])
                            for b in range(2):
                                mk.tt("dve", tmpC[:, 2 * b:2 * b + 2, :],
                                      PS[2 + b][:, 0:512].rearrange("p (h v) -> p h v", h=2)[:, :, 0:129], CT[:, 2 * b:2 * b + 2, :], ALU.add)
                            mk.tt("pool", CT[:], tmpC[:], DECb[:, cs, :].unsqueeze(2).to_broadcast([128, 4, 129]), ALU.mult)
                            mk.copy("act", ctb_nxt[:], CT[:])
                            t4 = t4b[cs % 2]
                            for b in range(2):
                                mk.tt("dve", t4[rows, 2 * b:2 * b + 2], PS[b][rows, 128:512:256],
                                      COL[rows, 1, tile, 2 * b:2 * b + 2], ALU.mult)
                            mk.stt("dve", t4[rows, :], t4[rows, :], -1.0, t4[rows, :], ALU.mult, ALU.max)
                            mk.tt("dve", t4[rows, :], t4[rows, :], COL[rows, 2, tile, :], ALU.max)
                            mk.recip(t4[rows, :], t4[rows, :])
                            mk.tt("dve", t4[rows, :], t4[rows, :], COL[rows, 1, tile, :], ALU.mult)
                            for b in range(2):
                                src = PS[b][rows, 0:512].rearrange("p (h v) -> p h v", h=2)[:, :, 0:128]
                                sc = t4[rows, 2 * b:2 * b + 2].unsqueeze(2).to_broadcast([64, 2, 128])
                                hs = HS[rows, tile, 2 * b * 128:(2 * b + 2) * 128].rearrange("p (h v) -> p h v", h=2)
                                if d == 0:
                                    mk.tt("dve", hs, src, sc, ALU.mult)
                                else:
                                    to = tmo[cs % 2][rows, 2 * b:2 * b + 2, :]
                                    mk.tt("dve", to, src, sc, ALU.mult)
                                    mk.tt("pool", hs, hs, to, ALU.add)
                with mk.scope():
                    sgo = mk.sbuf("msgo", [128, 18, 512], BF16)
                    sqh = mk.sbuf("msqh", [128, 18, 512], F32)
                    ML = mk.sbuf("mML", [128, 18, 512], BF16)
                    ss = mk.sbuf("mss", [128, 72], F32)
                    stg = [mk.sbuf("mstg%d" % i, [128, NS], BF16) for i in range(2)]
                    load_seq_K(sgo, K_MLO, 512, s)
                    finish_branch(HS, sgo, gvn, sqh, ss, ML)
                    tok_to_BR(ML, 0, s, stg)

    def finish_branch(HS, gate, gvn, sqh, ss, OUT):
        h3 = lambda t: t[:].rearrange("p t (h v) -> p (t h) v", v=128)
        mk.tt("pool", sqh[:], HS[:], HS[:], ALU.mult)
        mk.reduce("dve", ss[:], h3(sqh), ALU.add)
        mk.act(ss[:], ss[:], AF.Sqrt, bias=EPS, scale=1.0 / 128.0)
        mk.recip(ss[:], ss[:])
        mk.tt("dve", h3(sqh), h3(HS), ss[:].unsqueeze(2).to_broadcast([128, 72, 128]), ALU.mult)
        if gate is None:
            mk.tt("pool", h3(OUT), h3(sqh), gvn[:].unsqueeze(1).to_broadcast([128, 72, 128]), ALU.mult)
        else:
            mk.tt("pool", h3(sqh), h3(sqh), gvn[:].unsqueeze(1).to_broadcast([128, 72, 128]), ALU.mult)
            mk.tt("dve", OUT[:], sqh[:], gate[:], ALU.mult)

    def phase_gla(li):
        with mk.scope():
            OS = mk.sbuf("gOS", [128, 18, 512], F32)
            gvn = mk.sbuf("ggvn", [128, 128], F32)
            wa = mk.sbuf("gwa", [16, 2, 256], F32)
            nba = mk.sbuf("gnba", [128, 2, 2], F32)
            RST = mk.sbuf("gRST", [128, NS], F32)
            bcast_row(gvn[:], gl_ng[li:li + 1, :])
            mk.dma(wa[:], gl_wa[li].rearrange("d r n -> r d n"))
            mk.dma(nba[:], gl_baT[li].rearrange("d p h -> p d h"))
            mk.ts("dve", nba[:], nba[:], -1.0, None, op0=ALU.mult)
            mk.memset("pool", RST[:], 1.0)
            mk.memset("pool", RST[:].rearrange("p (c j) -> p c j", j=64)[:, :, 0:1], 0.0)
            for s in range(2):
                with mk.scope():
                    qT = mk.sbuf("gqT", [128, 2, NS], BF16)
                    kT = mk.sbuf("gkT", [128, 2, NS], BF16)
                    vK = mk.sbuf("gvK", [128, 18, 512], BF16)
                    aT = mk.sbuf("gaT", [16, 2, NS], F32)
                    LA = mk.sbuf("gLA", [128, 2, NS], F32)
                    Pc = mk.sbuf("gPc", [128, 2, NS], F32)
                    qg = mk.sbuf("gqg", [128, 2, NS], BF16)
                    kg = mk.sbuf("gkg", [128, 2, NS], BF16)
                    kg2 = mk.sbuf("gkg2", [128, 2, NS], BF16)
                    kgK = mk.sbuf("gkgK", [128, 18, 256], BF16)
                    Tc = mk.sbuf("gTc", [128, 2, 36], F32)
                    eb = mk.sbuf("geb", [128, 2, 36], F32)
                    S = mk.sbuf("gS", [128, 2, 128], F32)
                    Sb = [mk.sbuf("gSb%d" % i, [128, 4, 128], BF16) for i in range(2)]
                    amb = [mk.sbuf("gam%d" % i, [128, 4, 128], BF16) for i in range(2)]
                    for hh in range(2):
                        load_seq_T(qT[:, hh, :], PT[T_GLQ + hh * 128:T_GLQ + (hh + 1) * 128, :], s, q="sp")
                        load_seq_T(kT[:, hh, :], PT[T_GLK + hh * 128:T_GLK + (hh + 1) * 128, :], s, q="act")
                    load_seq_K(vK, K_GLV, 512, s)
                    for d in range(2):
                        load_seq_T(aT[:, d, :], GT[16 + d * 16:32 + d * 16, :], s)
                    v4 = lambda t: t[:].rearrange("p h (c j) -> p h c j", j=64)
                    for d in range(2):
                        ci = 0
                        for hh in range(2):
                            for t0 in range(0, NS, 512):
                                n = min(512, NS - t0)
                                pb = PS[5 + ci % 2]
                                ci += 1
                                mk.mm(pb[:, 0:n], wa[:, d, hh * 128:(hh + 1) * 128], aT[:, d, t0:t0 + n])
                                mk.act(LA[:, hh, t0:t0 + n], pb[:, 0:n], AF.Exp, bias=nba[:, d, hh:hh + 1], scale=-1.0)
                        mk.act(LA[:], LA[:], AF.Ln, bias=1.0, scale=1.0)
                        mk.ts("dve", LA[:], LA[:], -1.0 / 16.0, None, op0=ALU.mult)
                        for hh in range(2):
                            mk.scan("dve", Pc[:, hh, :], RST[:], LA[:, hh, :], 0.0, ALU.mult, ALU.add)
                        mk.copy("dve", Tc[:], v4(Pc)[:, :, :, 63])
                        mk.act(eb[:], Tc[:], AF.Exp)
                        if d == 1:
                            mk.tt("dve", v4(Pc), Tc[:].unsqueeze(3).to_broadcast([128, 2, 36, 64]), v4(Pc), ALU.subtract)
                            mk.tt("pool", Pc[:], Pc[:], LA[:], ALU.add)
                        mk.act(LA[:], Pc[:], AF.Exp)
                        mk.tt("dve", qg[:], qT[:], LA[:], ALU.mult)
                        mk.act(LA[:], Pc[:], AF.Exp, scale=-1.0)
                        mk.tt("dve", kg[:], kT[:], LA[:], ALU.mult)
                        mk.tt("pool", v4(kg2), v4(kg), eb[:].unsqueeze(3).to_broadcast([128, 2, 36, 64]), ALU.mult)
                        import os as _osg
                        _gs = _osg.environ.get("GLA_STOP", "")
                        if _gs == "1":
                            return
                        for hh in range(2):
                            for t in range(18):
                                pv = PS[5 + t % 2][:, (t % 4) * 128:(t % 4) * 128 + 128]
                                mk.mm(pv, kg2[:, hh, t * 128:(t + 1) * 128], identb[:])
                                mk.copy(evac_eng(), kgK[:, t, hh * 128:(hh + 1) * 128], pv)
                        if _gs == "2":
                            return
                        mk.memset("dve", S[:], 0.0)
                        mk.memset("pool", Sb[0][:], 0.0)
                        mk.memset("pool", Sb[1][:], 0.0)
                        mask = maskF if d == 0 else maskB
                        cur_tile = -1
                        for cs in range(36):
                            c = nat_chunk(d, cs)
                            tile, par = c // 2, c % 2
                            rows = slice(par * 64, par * 64 + 64)
                            sb_cur, sb_nxt = Sb[cs % 2], Sb[(cs + 1) % 2]
                            if tile != cur_tile:
                                cur_tile = tile
                                am = amb[tile % 2]
                                for h in range(4):
                                    hr = slice((h % 2) * 64, (h % 2) * 64 + 64)
                                    mk.mm(PS[4 + h % 2][:, (h // 2) * 128:(h // 2 + 1) * 128],
                                          kg[hr, h // 2, tile * 128:(tile + 1) * 128],
                                          qg[hr, h // 2, tile * 128:(tile + 1) * 128])
                                for wh in range(2):
                                    mk.tt("dve", am[:, wh::2, :], PS[4 + wh][:, 0:256].rearrange("p (h j) -> p h j", h=2),
                                          mask[:].unsqueeze(1).to_broadcast([128, 2, 128]), ALU.mult)
                            _lv = int(_gs) if _gs else 9
                            if _lv >= 4:
                                for h in range(4):
                                    o_ = PS[h // 2][rows, (h % 2) * 128:(h % 2) * 128 + 128]
                                    mk.mm(o_, am[:, h, par * 64:par * 64 + 64], vK[:, tile, h * 128:(h + 1) * 128],
                                          start=True, stop=False)
                                    mk.mm(o_, qg[:, h // 2, c * 64:(c + 1) * 64], sb_cur[:, h, :], start=False, stop=True)
                            if _lv >= 5:
                                for h in range(4):
                                    hh, wh = h // 2, h % 2
                                    mk.mm(PS[2 + hh][:, wh * 128:(wh + 1) * 128], kgK[rows, tile, hh * 128:(hh + 1) * 128],
                                          vK[rows, tile, h * 128:(h + 1) * 128])
                            if _lv >= 6:
                                for h in range(4):
                                    hh, wh = h // 2, h % 2
                                    hr = slice(wh * 64, wh * 64 + 64)
                                    mk.stt("dve", S[hr, hh, :], S[hr, hh, :], eb[hr, hh, c:c + 1],
                                           PS[2 + hh][hr, wh * 128:(wh + 1) * 128], ALU.mult, ALU.add)
                            if _lv >= 7:
                                for wh in range(2):
                                    hr = slice(wh * 64, wh * 64 + 64)
                                    mk.copy("act", sb_nxt[hr, wh::2, :], S[hr, :, :])
                            if _lv >= 4:
                                for b in range(2):
                                    dst = OS[rows, tile, b * 256:(b + 1) * 256]
                                    if d == 0:
                                        mk.copy("dve", dst, PS[b][rows, 0:256])
                                    else:
                                        mk.tt("dve", dst, dst, PS[b][rows, 0:256], ALU.add)
                with mk.scope():
                    sg = mk.sbuf("gsg", [128, 18, 512], BF16)
                    sqh = mk.sbuf("gsqh", [128, 18, 512], F32)
                    GLo = mk.sbuf("gGLo", [128, 18, 512], BF16)
                    ss = mk.sbuf("gss", [128, 72], F32)
                    stg = [mk.sbuf("gstg%d" % i, [128, NS], BF16) for i in range(2)]
                    load_seq_K(sg, K_GLG, 512, s)
                    finish_branch(OS, sg, gvn, sqh, ss, GLo)
                    tok_to_BR(GLo, 1024, s, stg)

    phase_load_x()
    phase_mod()
    order = ["norm1", "proj", "mlstm", "attn", "gla", "merge", "moe"]
    lim = order.index(stop_after) if stop_after else len(order)
    for li in range(n_layers):
        with mk.scope():
            hT = mk.sbuf("hT", [128, 8, NTOK], BF16)
            with mk.scope():
                scratch = ([mk.sbuf("nxs%d" % i, [128, 8, 512], F32) for i in range(2)],
                           [mk.sbuf("nsq%d" % i, [128, 8, 512], BF16) for i in range(2)],
                           [mk.sbuf("nrs%d" % i, [128, 512], F32) for i in range(2)])
                norm_groups(li, 0, hT, scratch)
            if dbg:
                hT_dbg = mk.dram("hT_dbg%d" % li, [D, NTOK], BF16, kind="ExternalOutput")
                mk.dma(hT_dbg[:, :].rearrange("(kc p) t -> p kc t", p=128), hT[:])
            if lim >= 1:
                with mk.scope():
                    phase_proj(li, hT)
        if lim >= 2 and "mlstm" not in skip:
            phase_mlstm(li)
        if lim >= 3 and "attn" not in skip:
            phase_attn(li)
        if lim >= 4 and "gla" not in skip:
            phase_gla(li)
        if lim >= 5:
            phase_merge(li)
        if lim >= 6 and do_moe:
            phase_moe(li)
    if lim >= 6:
        phase_final()
    if dbg:
        modv_dbg = mk.dram("modv_dbg", [128, 2 * 6 * 8 * 3], F32, kind="ExternalOutput")
        mk.dma(modv_dbg[:, :], modv[:].rearrange("p a b c d -> p (a b c d)"))
    mk.finish()
    return nc, mk


def make_consts():
    c = np.zeros((128, 384 + 2 * SEQ), np.float32)
    c[:, 0:128] = np.eye(128, dtype=np.float32)
    i = np.arange(128)[:, None]
    j = np.arange(128)[None, :]
    same = (i // 64) == (j // 64)
    c[:, 128:256] = (same & (i <= j)).astype(np.float32)
    c[:, 256:384] = (same & (i >= j)).astype(np.float32)
    t = np.arange(SEQ)
    rowp = (t // 64).astype(np.float32)
    colp = (t % 64).astype(np.float32)
    inv = (10000.0 ** (-np.arange(16, dtype=np.float32) / 16)).astype(np.float32)
    for d in range(128):
        dd = d % 64
        axis = dd // 32
        f = dd % 16
        half = (dd % 32) // 16
        ang = (rowp if axis == 0 else colp) * inv[f]
        c[d, 384:384 + SEQ] = np.cos(ang)
        c[d, 384 + SEQ:384 + 2 * SEQ] = np.sin(ang) * (-1.0 if half == 0 else 1.0)
    return c


def pcol(v, nch):
    return np.ascontiguousarray(np.asarray(v, np.float32).reshape(nch, 128).T)


def prep_shared(inp):
    sh = {}
    f = lambda a: np.ascontiguousarray(np.asarray(a, np.float32))
    sh["w_mod"] = f(inp["w_mod"])
    sh["b_modT"] = np.stack([pcol(inp["b_mod"][li], 48) for li in range(2)])
    sh["gmixT"] = np.stack([pcol(inp["norm_mix_g"][li], 8) for li in range(2)])
    sh["gffnT"] = np.stack([pcol(inp["norm_ffn_g"][li], 8) for li in range(2)])
    sh["w_in"] = f(inp["w_in"])
    sh["ml_gb"] = f(np.asarray(inp["ml_gate_b"]).reshape(2, 4, 4).transpose(0, 2, 1))
    sh["ml_ng"] = f(inp["ml_norm_g"])
    sh["df_lam"] = f(inp["df_lambda"])
    sh["df_ng"] = f(inp["df_norm_g"])
    sh["gl_wa"] = f(inp["gl_w_alpha"])
    sh["gl_baT"] = f(np.asarray(inp["gl_b_alpha"]).reshape(2, 2, 2, 128).transpose(0, 1, 3, 2))
    sh["gl_ng"] = f(inp["gl_norm_g"])
    sh["w_branch"] = f(inp["w_branch"])
    sh["w_out"] = f(inp["w_out"])
    sh["router_w"] = f(np.concatenate([inp["router_group_w"], inp["router_expert_w"]], axis=-1))
    sh["router_b"] = f(np.concatenate([inp["router_group_b"], inp["router_expert_b"]], axis=-1))
    sh["moe_wg"] = f(inp["moe_w_gate"])
    sh["moe_wu"] = f(inp["moe_w_up"])
    sh["moe_wd"] = f(inp["moe_w_down"])
    sh["fin_gT"] = pcol(inp["final_norm_g"], 8)
    sh["consts"] = make_consts()
    return sh


def prep_core(inp, core):
    b0 = 2 * core
    m = {}
    m["x2"] = np.ascontiguousarray(np.asarray(inp["x"][b0:b0 + 2], np.float32))
    m["ctx2"] = np.ascontiguousarray(np.asarray(inp["ctx"][b0:b0 + 2], np.float32))
    cs = np.stack([np.asarray(inp["c_ctx"], np.float32), np.asarray(inp["c"][b0], np.float32),
                   np.asarray(inp["c"][b0 + 1], np.float32)], axis=-1)
    m["cT"] = np.ascontiguousarray(cs.reshape(8, 128, 3).transpose(1, 0, 2))
    return m


_CACHE = {}
CORES_PER_LAUNCH = 8


def kernel(**inputs):
    if "nc" not in _CACHE:
        _CACHE["nc"] = build()[0]
    nc = _CACHE["nc"]
    sh = prep_shared(inputs)
    in_maps = []
    for core in range(8):
        m = dict(sh)
        m.update(prep_core(inputs, core))
        in_maps.append(m)
    outs = []
    for g0 in range(0, 8, CORES_PER_LAUNCH):
        res = run_bass_kernel_spmd(nc, in_maps[g0:g0 + CORES_PER_LAUNCH], core_ids=list(range(CORES_PER_LAUNCH)))
        outs.extend(np.asarray(r["y_out"], np.float32) for r in res.results)
    return np.concatenate(outs, axis=0)
```
